# Optimizing a Trainium2 kernel written in Bass

```python
import jax, jax.numpy as jnp
from jax import lax
import numpy as np

D_MODEL = 1024
BATCH = 4
SEQ = 4096
DEPTH = 1

CHUNK = 64
Q_BLOCK = 128
D_MIX = D_MODEL
A_HEADS = 8
A_HEAD_DIM = 64
A_WIDTH = A_HEADS * A_HEAD_DIM
IDX_HEADS = 4
IDX_DIM = 64
TOPK_MAX = 256
B_HEADS = 4
B_KEY_DIM = 64
B_VAL_DIM = 128
B_WIDTH = B_HEADS * B_VAL_DIM
ROPE_THETA = 500000.0
ROT_DIM = A_HEAD_DIM // 4
N_EXPERTS = 32
TOP_K_EXPERTS = 4
D_EXPERT = D_MODEL
SWIGLU_LIMIT = 7.0
SWIGLU_ALPHA = 1.702
EXPERT_BLOCK = 256
RMS_EPS = 1e-6

SPLITS = (A_WIDTH, A_WIDTH, A_WIDTH,
          IDX_HEADS * IDX_DIM, IDX_DIM, IDX_HEADS,
          B_HEADS * B_KEY_DIM, B_HEADS * B_KEY_DIM,
          B_WIDTH, B_WIDTH)
D_IN = sum(SPLITS)
SPLIT_POINTS = tuple(int(v) for v in np.cumsum(SPLITS)[:-1])

kernel_name = "hybrid_dsa_hgrn2_moe_adaln_block"

F32 = jnp.float32


def rmsnorm(x, g):
    xf = x.astype(F32)
    y = xf * lax.rsqrt(jnp.mean(xf * xf, axis=-1, keepdims=True) + RMS_EPS)
    return (y * g.astype(F32)).astype(x.dtype)


def rope_tables(positions):
    inv_freq = ROPE_THETA ** (-(jnp.arange(0, ROT_DIM, 2, dtype=F32) / ROT_DIM))
    ang = positions.astype(F32)[..., None] * inv_freq
    return jnp.cos(ang)[:, :, None, :], jnp.sin(ang)[:, :, None, :]


def apply_partial_rope(x, cos, sin):
    r = cos.shape[-1]
    x1, x2, rest = x[..., :r], x[..., r:2 * r], x[..., 2 * r:]
    xf1, xf2 = x1.astype(F32), x2.astype(F32)
    rot = jnp.concatenate([xf1 * cos - xf2 * sin, xf2 * cos + xf1 * sin], axis=-1)
    return jnp.concatenate([rot.astype(x.dtype), rest], axis=-1)


def dsa_attention(q, k, v, qi, ki, wi):
    B, S = q.shape[0], q.shape[1]
    topk = min(TOPK_MAX, S // 4)
    nqb = S // Q_BLOCK
    key_chunk = jnp.arange(S) // CHUNK
    scale = A_HEAD_DIM ** -0.5

    def blocks(a):
        return jnp.swapaxes(a.reshape((B, nqb, Q_BLOCK) + a.shape[2:]), 0, 1)

    def one_block(args):
        n, qb, qib, wib = args
        q_chunk = (n * Q_BLOCK + jnp.arange(Q_BLOCK)) // CHUNK
        logits = jnp.einsum('bqhd,bsd->bqhs', qib, ki) * (IDX_DIM ** -0.5)
        score = jnp.einsum('bqh,bqhs->bqs', wib, jax.nn.relu(logits)).astype(F32)
        admissible = key_chunk[None, :] <= q_chunk[:, None]
        score = jnp.where(admissible[None], score, -jnp.inf)
        _, idx = lax.top_k(score, topk)
        valid = (idx // CHUNK) <= q_chunk[None, :, None]
        k_sel = jax.vmap(lambda kb, ib: kb[ib])(k, idx)
        v_sel = jax.vmap(lambda vb, ib: vb[ib])(v, idx)
        s = jnp.einsum('bqhd,bqkhd->bqhk', qb, k_sel).astype(F32) * scale
        s = jnp.where(valid[:, :, None, :], s, -jnp.inf)
        p = jax.nn.softmax(s, axis=-1).astype(v.dtype)
        return jnp.einsum('bqhk,bqkhd->bqhd', p, v_sel)

    out = lax.map(one_block, (jnp.arange(nqb), blocks(q), blocks(qi), blocks(wi)))
    return jnp.swapaxes(out, 0, 1).reshape(B, S, -1)


def hgrn2_chunkwise(q, log_f, k, v):
    B, S, H, dk = q.shape
    dv = v.shape[-1]
    nc = S // CHUNK

    def to_chunks(a):
        return a.astype(F32).reshape(B, nc, CHUNK, H, a.shape[-1]).transpose(1, 0, 3, 2, 4)

    tri = jnp.tril(jnp.ones((CHUNK, CHUNK), bool))[:, :, None]

    def step(state, inp):
        qc, lfc, kc, vc = inp
        b = jnp.cumsum(lfc, axis=2)
        o_inter = jnp.einsum('bhtd,bhde->bhte', qc * jnp.exp(b), state)
        diff = b[:, :, :, None, :] - b[:, :, None, :, :]
        decay = jnp.exp(jnp.where(tri, diff, -jnp.inf))
        attn = jnp.einsum('bhtd,bhsd,bhtsd->bhts', qc, kc, decay)
        o_intra = jnp.einsum('bhts,bhse->bhte', attn, vc)
        b_last = b[:, :, -1:, :]
        new_state = (jnp.exp(b_last[:, :, 0, :])[..., None] * state
                     + jnp.einsum('bhsd,bhse->bhde', kc * jnp.exp(b_last - b), vc))
        return new_state, o_inter + o_intra

    s0 = jnp.zeros((B, H, dk, dv), F32)
    _, o = lax.scan(step, s0, (to_chunks(q), to_chunks(log_f), to_chunks(k), to_chunks(v)))
    return o.transpose(1, 0, 3, 2, 4).reshape(B, S, H, dv)


def moe_ffn(h, router_w, router_b, w1, b1, w2, b2):
    B, S, D = h.shape
    T = B * S
    xt = h.reshape(T, D)
    logits = (xt @ router_w + router_b).astype(F32)
    top_vals, top_idx = lax.top_k(logits, TOP_K_EXPERTS)
    gates = jax.nn.softmax(top_vals, axis=-1)

    n_assign = T * TOP_K_EXPERTS
    e_flat = top_idx.reshape(-1).astype(jnp.int32)
    tok_flat = jnp.arange(n_assign, dtype=jnp.int32) // TOP_K_EXPERTS
    order = jnp.argsort(e_flat)
    e_sorted = e_flat[order]
    tok_sorted = tok_flat[order]
    counts = jnp.bincount(e_flat, length=N_EXPERTS).astype(jnp.int32)
    padded = (counts + EXPERT_BLOCK - 1) // EXPERT_BLOCK * EXPERT_BLOCK
    start = jnp.cumsum(counts) - counts
    pad_end = jnp.cumsum(padded)
    pad_start = pad_end - padded
    row = pad_start[e_sorted] + jnp.arange(n_assign, dtype=jnp.int32) - start[e_sorted]
    n_blocks = -(-n_assign // EXPERT_BLOCK) + N_EXPERTS
    n_rows = n_blocks * EXPERT_BLOCK
    row_tok = jnp.full((n_rows,), T, jnp.int32).at[row].set(tok_sorted)
    x_rows = jnp.concatenate([xt, jnp.zeros((1, D), xt.dtype)], axis=0)[row_tok]
    x_rows = x_rows.reshape(n_blocks, EXPERT_BLOCK, D)
    block_start = jnp.arange(n_blocks, dtype=jnp.int32) * EXPERT_BLOCK
    block_expert = jnp.minimum(jnp.searchsorted(pad_end, block_start, side='right'),
                               N_EXPERTS - 1).astype(jnp.int32)

    def expert_block(args):
        xb, e = args
        hg = xb @ w1[e] + b1[e]
        glu, lin = jnp.split(hg, 2, axis=-1)
        glu = jnp.minimum(glu, SWIGLU_LIMIT)
        lin = jnp.clip(lin, -SWIGLU_LIMIT, SWIGLU_LIMIT)
        act = glu * jax.nn.sigmoid(SWIGLU_ALPHA * glu) * (lin + 1)
        return act @ w2[e] + b2[e]

    y_rows = lax.map(expert_block, (x_rows, block_expert)).reshape(n_rows, D)
    row_orig = jnp.zeros((n_assign,), jnp.int32).at[order].set(row)
    y = y_rows[row_orig].reshape(T, TOP_K_EXPERTS, D)
    out = jnp.einsum('tk,tkd->td', gates.astype(y.dtype), y)
    return out.reshape(B, S, D)


def setup_inputs(seed: int = 0) -> dict:
    key = jax.random.key(seed)
    ks = jax.random.split(key, 20)
    D, F = D_MODEL, D_EXPERT
    nrm = jax.random.normal
    x = nrm(ks[0], (BATCH, SEQ, D), F32)
    c = nrm(ks[1], (BATCH, D), F32)
    offset = jax.random.randint(ks[2], (BATCH, 1), 0, 65536, dtype=jnp.int32)
    positions = (offset + jnp.arange(SEQ, dtype=jnp.int32)[None, :]).astype(jnp.int32)
    ada_w = nrm(ks[3], (DEPTH, D, 6 * D), F32) * (0.5 * D ** -0.5)
    ada_b = nrm(ks[4], (DEPTH, 6 * D), F32) * 0.02
    norm1_g = 1.0 + 0.02 * nrm(ks[5], (DEPTH, D), F32)
    w_in = nrm(ks[6], (DEPTH, D, D_IN), F32) * D ** -0.5
    hg_norm_g = 1.0 + 0.02 * nrm(ks[7], (DEPTH, B_VAL_DIM), F32)
    lb_logits = 0.5 * nrm(ks[8], (DEPTH + 1, B_HEADS * B_KEY_DIM), F32)
    w_out = nrm(ks[9], (DEPTH, D_MIX, D), F32) * D_MIX ** -0.5
    norm2_g = 1.0 + 0.02 * nrm(ks[10], (DEPTH, D), F32)
    router_w = nrm(ks[11], (DEPTH, D, N_EXPERTS), F32) * D ** -0.5
    router_b = 0.01 * nrm(ks[12], (DEPTH, N_EXPERTS), F32)
    moe_w1 = nrm(ks[13], (DEPTH, N_EXPERTS, D, 2 * F), F32) * D ** -0.5
    moe_b1 = 0.01 * nrm(ks[14], (DEPTH, N_EXPERTS, 2 * F), F32)
    moe_w2 = nrm(ks[15], (DEPTH, N_EXPERTS, F, D), F32) * F ** -0.5
    moe_b2 = 0.01 * nrm(ks[16], (DEPTH, N_EXPERTS, D), F32)
    final_g = 1.0 + 0.02 * nrm(ks[17], (D,), F32)
    return {"x": x, "c": c, "positions": positions, "ada_w": ada_w, "ada_b": ada_b,
            "norm1_g": norm1_g, "w_in": w_in, "hg_norm_g": hg_norm_g, "lb_logits": lb_logits,
            "w_out": w_out, "norm2_g": norm2_g, "router_w": router_w, "router_b": router_b,
            "moe_w1": moe_w1, "moe_b1": moe_b1, "moe_w2": moe_w2, "moe_b2": moe_b2,
            "final_g": final_g}


def reference(x, c, positions, ada_w, ada_b, norm1_g, w_in, hg_norm_g, lb_logits,
              w_out, norm2_g, router_w, router_b, moe_w1, moe_b1, moe_w2, moe_b2, final_g):
    B, S, _ = x.shape
    cos, sin = rope_tables(positions)
    lower_bounds = jnp.cumsum(jax.nn.softmax(lb_logits.astype(F32), axis=0), axis=0)

    for layer in range(DEPTH):
        mod = jax.nn.silu(c) @ ada_w[layer] + ada_b[layer]
        shift1, scale1, gate1, shift2, scale2, gate2 = [m[:, None, :] for m in jnp.split(mod, 6, axis=-1)]

        h = rmsnorm(x, norm1_g[layer]) * (1 + scale1) + shift1
        proj = h @ w_in[layer]
        qa, ka, va, qi, ki, wi, qb, fb, ib, gb = jnp.split(proj, SPLIT_POINTS, axis=-1)

        qa = apply_partial_rope(qa.reshape(B, S, A_HEADS, A_HEAD_DIM), cos, sin)
        ka = apply_partial_rope(ka.reshape(B, S, A_HEADS, A_HEAD_DIM), cos, sin)
        va = va.reshape(B, S, A_HEADS, A_HEAD_DIM)
        qi = apply_partial_rope(qi.reshape(B, S, IDX_HEADS, IDX_DIM), cos, sin)
        ki = apply_partial_rope(ki[:, :, None, :], cos, sin)[:, :, 0, :]
        wi = wi * (IDX_HEADS ** -0.5)
        out_a = dsa_attention(qa, ka, va, qi, ki, wi)

        lb = lower_bounds[layer]
        f = lb + (1.0 - lb) * jax.nn.sigmoid(fb.astype(F32))
        log_f = jnp.log(f).reshape(B, S, B_HEADS, B_KEY_DIM)
        k_in = (1.0 - f).reshape(B, S, B_HEADS, B_KEY_DIM)
        o_b = hgrn2_chunkwise(qb.reshape(B, S, B_HEADS, B_KEY_DIM), log_f, k_in,
                              ib.reshape(B, S, B_HEADS, B_VAL_DIM))
        o_b = rmsnorm(o_b, hg_norm_g[layer]).astype(x.dtype)
        out_b = (o_b * jax.nn.silu(gb.reshape(B, S, B_HEADS, B_VAL_DIM))).reshape(B, S, B_WIDTH)

        mix = jnp.concatenate([out_a.astype(x.dtype), out_b], axis=-1)
        x = x + gate1 * (mix @ w_out[layer])

        h2 = rmsnorm(x, norm2_g[layer]) * (1 + scale2) + shift2
        x = x + gate2 * moe_ffn(h2, router_w[layer], router_b[layer], moe_w1[layer],
                                moe_b1[layer], moe_w2[layer], moe_b2[layer])

    return rmsnorm(x, final_g)
```

```python
from contextlib import ExitStack
import numpy as np
import ml_dtypes
import concourse.bass as bass
import concourse.mybir as mybir
from concourse.bass_utils import run_bass_kernel_spmd

F32 = mybir.dt.float32
BF16 = mybir.dt.bfloat16
I32 = mybir.dt.int32
U8 = mybir.dt.uint8
AF = mybir.ActivationFunctionType
ALU = mybir.AluOpType
AX = mybir.AxisListType
DSZ = {F32: 4, BF16: 2, I32: 4, U8: 1}

ENGS = ["tensor", "vector", "scalar", "gpsimd", "sync"]
SEM_LIMIT = 30000
EPS = 1e-6
PI = float(np.pi)
NT_OWN = 16
NT_ALL = 32
RNG = 32.0
NBIS = 13
NEG = -1.0e30


class Buf:
    __slots__ = ("name", "w", "r", "dsem", "dcount")

    def __init__(self, name):
        self.name = name
        self.w = {}
        self.r = {}
        self.dsem = None
        self.dcount = 0


class T:
    __slots__ = ("ap", "buf")

    def __init__(self, ap, buf):
        self.ap = ap
        self.buf = buf

    def __getitem__(self, key):
        return T(self.ap[key], self.buf)

    def bitcast(self, dt):
        return T(self.ap.bitcast(dt), self.buf)

    def re(self, pat, **kw):
        return T(self.ap.rearrange(pat, **kw), self.buf)

    def bc(self, shape):
        return T(self.ap.to_broadcast(list(shape)), self.buf)

    def unsq(self, ax):
        return T(self.ap.unsqueeze(ax), self.buf)


def _bufs(ts):
    out = []
    for t in ts:
        if t is None:
            continue
        b = t.buf if isinstance(t, T) else t
        if b is not None and b not in out:
            out.append(b)
    return out


class Rot:
    def __init__(self, items):
        self.items = list(items)
        self.i = 0

    def next(self):
        t = self.items[self.i % len(self.items)]
        self.i += 1
        return t


class K:
    def __init__(self, nc, stack):
        self.nc = nc
        self.stack = stack
        self.streams = {e: [] for e in ENGS}
        self.esem = {}
        self.ecount = {e: 0 for e in ENGS}
        self.known = {e: {} for e in ENGS}
        self.nsem = 0
        for e in ENGS:
            self.esem[e] = self.new_sem("e_" + e)
        self.dma_bufs = []
        self.n_ops = 0
        self.rstack = []

    def new_sem(self, name):
        self.nsem += 1
        return self.stack.enter_context(self.nc.semaphore(f"{name}_{self.nsem}"))

    def _need(self, eng, tokens):
        waits = []
        kn = self.known[eng]
        for sem, val in tokens.items():
            if kn.get(sem, 0) < val:
                kn[sem] = val
                waits.append((sem, val))
        return waits

    def _deps(self, eng, rd, wr, skip_waw=None):
        tokens = {}
        mysem = self.esem[eng]

        def add(d, is_raw):
            for sem, val in d.items():
                if sem is mysem and eng == "tensor":
                    continue
                if (not is_raw) and skip_waw is not None and sem is skip_waw:
                    continue
                if tokens.get(sem, 0) < val:
                    tokens[sem] = val
        for b in rd:
            add(b.w, True)
        for b in wr:
            add(b.w, False)
            add(b.r, False)
        self._note_region(eng, tokens)
        return self._need(eng, tokens)

    def _note_region(self, eng, tokens):
        for rg in self.rstack:
            ext = rg["ext"][eng]
            for sem, val in tokens.items():
                if val <= rg["start"].get(sem, 0):
                    if ext.get(sem, 0) < val:
                        ext[sem] = val

    def _note_inc(self, eng, sem, inc):
        for rg in self.rstack:
            d = rg["incs"][eng]
            d[sem] = d.get(sem, 0) + inc

    def val_load(self, t):
        self.nvals = getattr(self, "nvals", 0) + 1
        key = self.nvals
        for e in ENGS:
            waits = self._deps(e, _bufs([t]), [])
            self.streams[e].append(("load", waits, key, t.ap))
        return key

    def cond_begin(self, key, thr):
        if not self.rstack:
            for e in ENGS:
                if self.ecount[e] >= SEM_LIMIT - 6000:
                    self.esem[e] = self.new_sem("e_" + e)
                    self.ecount[e] = 0
        start = {}
        for e in ENGS:
            start[self.esem[e]] = self.ecount[e]
        for b in self.dma_bufs:
            start[b.dsem] = b.dcount
        rg = {"key": key, "thr": thr, "start": start,
              "known0": {e: dict(self.known[e]) for e in ENGS},
              "ext": {e: {} for e in ENGS}, "incs": {e: {} for e in ENGS}}
        self.rstack.append(rg)
        for e in ENGS:
            self.streams[e].append(("begin", rg))

    def cond_end(self):
        rg = self.rstack.pop()
        for e in ENGS:
            kn = dict(rg["known0"][e])
            ew = []
            for sem, val in rg["ext"][e].items():
                if kn.get(sem, 0) < val:
                    kn[sem] = val
                    ew.append((sem, val))
            rg["ext"][e] = ew
            self.known[e] = kn
            self.streams[e].append(("end", rg))

    def op(self, eng, fn, rd=(), wr=()):
        rd = _bufs(rd)
        wr = _bufs(wr)
        waits = self._deps(eng, rd, wr)
        if self.ecount[eng] >= SEM_LIMIT and not self.rstack:
            self.esem[eng] = self.new_sem("e_" + eng)
            self.ecount[eng] = 0
        self.ecount[eng] += 1
        sem = self.esem[eng]
        val = self.ecount[eng]
        self.streams[eng].append((waits, fn, sem, 1))
        self._note_inc(eng, sem, 1)
        for b in rd:
            if b.r.get(sem, 0) < val:
                b.r[sem] = val
        for b in wr:
            b.w = {sem: val}
            b.r = {}
        self.n_ops += 1

    def dma(self, eng, out, in_, chan=None, **kw):
        rd = _bufs([in_])
        wr = _bufs([out])
        if chan is None:
            chan = out.buf
        if chan.dsem is None:
            chan.dsem = self.new_sem("d_" + chan.name)
            self.dma_bufs.append(chan)
        waits = self._deps(eng, rd, wr, skip_waw=chan.dsem)
        chan.dcount += 16
        sem, val = chan.dsem, chan.dcount
        oap, iap = out.ap, in_.ap
        self.streams[eng].append((waits, lambda e: e.dma_start(out=oap, in_=iap, **kw), sem, 16))
        self._note_inc(eng, sem, 16)
        for b in rd:
            if b.r.get(sem, 0) < val:
                b.r[sem] = val
        for b in wr:
            b.w = {sem: val}
            b.r = {}
        self.n_ops += 1

    def idma(self, out, in_, idx, scatter, chan):
        rd = _bufs([in_, idx])
        wr = _bufs([out])
        if chan.dsem is None:
            chan.dsem = self.new_sem("d_" + chan.name)
            self.dma_bufs.append(chan)
        waits = self._deps("gpsimd", rd, wr, skip_waw=chan.dsem)
        chan.dcount += 16
        sem, val = chan.dsem, chan.dcount
        oap, iap, xap = out.ap, in_.ap, idx.ap
        if scatter:
            fn = lambda e: e.indirect_dma_start(out=oap, out_offset=bass.IndirectOffsetOnAxis(xap, 0), in_=iap, in_offset=None)
        else:
            fn = lambda e: e.indirect_dma_start(out=oap, out_offset=None, in_=iap, in_offset=bass.IndirectOffsetOnAxis(xap, 0))
        self.streams["gpsimd"].append((waits, fn, sem, 16))
        self._note_inc("gpsimd", sem, 16)
        for b in rd:
            if b.r.get(sem, 0) < val:
                b.r[sem] = val
        for b in wr:
            b.w = {sem: val}
            b.r = {}
        self.n_ops += 1

    def _all_tokens(self):
        tokens = {}
        for e in ENGS:
            if self.ecount[e] > 0:
                tokens[self.esem[e]] = self.ecount[e]
        for b in self.dma_bufs:
            tokens[b.dsem] = b.dcount
        return tokens

    def barrier(self):
        tokens = self._all_tokens()
        for e in ENGS:
            waits = self._need(e, dict(tokens))
            if waits:
                self.streams[e].append((waits, None, None, 0))

    def final_wait(self, eng="sync"):
        waits = self._need(eng, self._all_tokens())
        self.streams[eng].append((waits, None, None, 0))

    def emit(self):
        nc = self.nc
        with nc.Block() as block:
            def run(name):
                def body(e):
                    items = self.streams[name]
                    vals = {}

                    def run_items(lst):
                        i = 0
                        while i < len(lst):
                            it = lst[i]
                            if it[0] == "load":
                                for s, v in it[1]:
                                    e.wait_ge(s, v)
                                if "reg" not in vals:
                                    vals["reg"] = e.alloc_register("cnd_" + name)
                                e.load(vals["reg"], it[3])
                                vals["key"] = it[2]
                                i += 1
                            elif it[0] == "begin":
                                rg = it[1]
                                assert vals["key"] == rg["key"]
                                j = i + 1
                                while not (lst[j][0] == "end" and lst[j][1] is rg):
                                    j += 1
                                bodyl = lst[i + 1:j]
                                with e.If_cmp(vals["reg"], rg["thr"], "IS_LE"):
                                    for s, v in rg["ext"][name]:
                                        e.wait_ge(s, v)
                                    for s, tot in rg["incs"][name].items():
                                        if rg["start"].get(s, 0) > 0:
                                            e.wait_ge(s, rg["start"][s])
                                        e.nop().then_inc(s, tot)
                                with e.Else():
                                    run_items(bodyl)
                                i = j + 1
                            else:
                                waits, fn, sem, inc = it
                                for s, v in waits:
                                    e.wait_ge(s, v)
                                if fn is not None:
                                    fn(e).then_inc(sem, inc)
                                i += 1
                    run_items(items)
                return body
            block.tensor(run("tensor"))
            block.vector(run("vector"))
            block.scalar(run("scalar"))
            block.gpsimd(run("gpsimd"))
            block.sync(run("sync"))

    def ps(self, name, shape, dtype):
        t = self.stack.enter_context(self.nc.psum_tensor(name, list(shape), dtype))
        return T(t[:], Buf(name))

    def dram(self, name, shape, dtype, kind):
        t = self.nc.dram_tensor(name, list(shape), dtype, kind=kind)
        return T(t.ap(), Buf(name))

    def mm(self, out, lhsT, rhs, start=True, stop=True, **kw):
        rd = [lhsT, rhs] + ([] if start else [out])
        self.op("tensor", lambda e: e.matmul(out.ap, lhsT.ap, rhs.ap, start=start, stop=stop, **kw),
                rd=rd, wr=[out])

    def tr(self, out, in_, ident):
        self.op("tensor", lambda e: e.transpose(out.ap, in_.ap, ident.ap), rd=[in_, ident], wr=[out])

    def act(self, out, in_, func, bias=None, scale=None, accum=None):
        kw = {}
        rd = [in_]
        if bias is not None:
            if isinstance(bias, T):
                kw["bias"] = bias.ap
                rd.append(bias)
            else:
                kw["bias"] = bias
        if scale is not None:
            if isinstance(scale, T):
                kw["scale"] = scale.ap
                rd.append(scale)
            else:
                kw["scale"] = scale
        wr = [out]
        if accum is not None:
            kw["accum_out"] = accum.ap
            wr.append(accum)
        self.op("scalar", lambda e: e.activation(out.ap, in_.ap, func, **kw), rd=rd, wr=wr)

    def ts(self, eng, out, in0, s1, s2, op0, op1=None, accum=None):
        rd = [in0]
        a1, a2 = s1, s2
        if isinstance(s1, T):
            rd.append(s1)
            a1 = s1.ap
        if isinstance(s2, T):
            rd.append(s2)
            a2 = s2.ap
        kw = {}
        wr = [out]
        if op1 is not None:
            kw["op1"] = op1
        if accum is not None:
            kw["accum_out"] = accum.ap
            wr.append(accum)
        self.op(eng, lambda e: e.tensor_scalar(out.ap, in0.ap, a1, a2, op0, **kw), rd=rd, wr=wr)

    def tt(self, eng, out, in0, in1, op):
        self.op(eng, lambda e: e.tensor_tensor(out.ap, in0.ap, in1.ap, op), rd=[in0, in1], wr=[out])

    def stt(self, out, in0, scalar, in1, op0, op1):
        rd = [in0, in1]
        a = scalar
        if isinstance(scalar, T):
            rd.append(scalar)
            a = scalar.ap
        self.op("vector", lambda e: e.scalar_tensor_tensor(out.ap, in0.ap, a, in1.ap, op0, op1),
                rd=rd, wr=[out])

    def copy(self, eng, out, in_):
        if eng == "scalar":
            self.op(eng, lambda e: e.copy(out.ap, in_.ap), rd=[in_], wr=[out])
        else:
            self.op(eng, lambda e: e.tensor_copy(out.ap, in_.ap), rd=[in_], wr=[out])

    def memset(self, eng, out, val):
        self.op(eng, lambda e: e.memset(out.ap, val), rd=[], wr=[out])

    def recip(self, out, in_):
        self.op("vector", lambda e: e.reciprocal(out.ap, in_.ap), rd=[in_], wr=[out])

    def reduce(self, out, in_, op):
        self.op("vector", lambda e: e.tensor_reduce(out.ap, in_.ap, AX.X, op), rd=[in_], wr=[out])

    def scan_add(self, out, ones, data, initial):
        rd = [ones, data]
        a = initial
        if isinstance(initial, T):
            rd.append(initial)
            a = initial.ap
        self.op("vector", lambda e: e.tensor_tensor_scan(out.ap, ones.ap, data.ap, a, ALU.mult, ALU.add),
                rd=rd, wr=[out])

    def vmax8(self, out, in_):
        self.op("vector", lambda e: e.max(out.ap, in_.ap), rd=[in_], wr=[out])


class Arena:
    def __init__(self, k, nbytes):
        self.k = k
        self.nbytes = nbytes
        t = k.stack.enter_context(k.nc.sbuf_tensor("arena", [128, nbytes], U8))
        self.ap = t[:]
        self.off = 0
        self.top = nbytes
        self.n = 0

    def alloc(self, name, shape, dtype, top=False):
        free = int(np.prod(shape[1:]))
        nb = free * DSZ[dtype]
        if top:
            off = (self.top - nb) // 64 * 64
            assert off >= self.off, f"arena overflow (top) at {name}"
            self.top = off
        else:
            off = (self.off + 63) // 64 * 64
            assert off + nb <= self.top, f"arena overflow at {name}: {off}+{nb} > {self.top}"
            self.off = off + nb
        ap = self.ap[0:shape[0], off:off + nb].bitcast(dtype)
        if len(shape) == 3:
            ap = ap.rearrange("p (a b) -> p a b", a=shape[1])
        elif len(shape) == 4:
            ap = ap.rearrange("p (a b c) -> p a b c", a=shape[1], b=shape[2])
        self.n += 1
        return T(ap, Buf(f"{name}_{self.n}"))

    def mark(self):
        return self.off

    def reset(self, m):
        self.off = m


def build_nc(debug=False):
    nc = bass.Bass("TRN2", target_bir_lowering=False)
    st = ExitStack()
    k = K(nc, st)
    A = Arena(k, 207 * 1024)

    def DI(name, shape, dt=F32):
        return k.dram(name, shape, dt, "ExternalInput")

    xo = DI("xo", [2048, 1024]); xp = DI("xp", [2048, 1024])
    cvec = DI("cvec", [1024]); posr = DI("posr", [4096], I32)
    ada_w = DI("ada_w", [1024, 6144]); ada_b = DI("ada_b", [6144])
    n1g = DI("norm1_g", [1024]); n2g = DI("norm2_g", [1024]); fing = DI("final_g", [1024])
    hgg = DI("hg_norm_g", [128]); lbl = DI("lb_logits", [2, 256])
    w_ka = DI("w_ka", [1024, 512]); w_kas = DI("w_kas", [1024, 512])
    w_ki = DI("w_ki", [1024, 128]); w_kis = DI("w_kis", [1024, 128])
    w_va = DI("w_va", [1024, 512])
    w_qa = DI("w_qa", [1024, 512]); w_qas = DI("w_qas", [1024, 512])
    w_qi = DI("w_qi", [1024, 256]); w_qis = DI("w_qis", [1024, 256])
    w_wi = DI("w_wi", [1024, 4])
    w_qb = DI("w_qb", [1024, 256]); w_fb = DI("w_fb", [1024, 256])
    w_ib = DI("w_ib", [1024, 512]); w_gb = DI("w_gb", [1024, 512])
    w_out = DI("w_out", [1024, 1024])
    rw = DI("router_w", [1024, 32]); rb = DI("router_b", [32])
    w1 = DI("moe_w1", [32, 1024, 2048]); b1 = DI("moe_b1", [32, 2048])
    w2 = DI("moe_w2", [32, 1024, 1024]); b2 = DI("moe_b2", [32, 1024])
    c_identb = DI("c_identb", [128, 128], BF16); c_identf = DI("c_identf", [128, 128])
    c_negI = DI("c_negI", [128, 128], BF16); c_tri2 = DI("c_tri2", [128, 128])
    c_diag = DI("c_diag", [128, 128]); c_cols = DI("c_cols", [128, 4])
    yout = k.dram("y", [2048, 1024], F32, "ExternalOutput")
    x1s = k.dram("x1s", [2048, 1024], F32, "Internal")
    park = k.dram("park", [4, 128, 1024], F32, "Internal")
    XG = k.dram("XG", [32 * 2048, 1024], BF16, "Internal")
    YG = k.dram("YG", [32 * 2048, 1024], F32, "Internal")
    c_ltb = DI("c_ltb", [128, 128], BF16); c_erow = DI("c_erow", [128, 32])
    dbg = {}
    if debug:
        dbg["mixT"] = k.dram("dbg_mixT", [8, 128, 2048], F32, "ExternalOutput")
        dbg["x1"] = k.dram("dbg_x1", [2048, 1024], F32, "ExternalOutput")
        dbg["G"] = k.dram("dbg_G", [128, 16 * 32], F32, "ExternalOutput")

    banks = [k.ps(f"bank{i}", [128, 512], F32) for i in range(8)]

    def wload(dst, src, rows=8, eng="gpsimd"):
        k.dma(eng, dst, src.re("(kc p) n -> p kc n", p=128))

    identb = A.alloc("identb", [128, 128], BF16); k.dma("sync", identb, c_identb)
    identf = A.alloc("identf", [128, 128], F32); k.dma("sync", identf, c_identf)
    negI = A.alloc("negI", [128, 128], BF16); k.dma("sync", negI, c_negI)
    tri2 = A.alloc("tri2", [128, 128], F32); k.dma("sync", tri2, c_tri2)
    diagm = A.alloc("diagm", [128, 128], F32); k.dma("sync", diagm, c_diag)
    ccols = A.alloc("ccols", [128, 4], F32); k.dma("sync", ccols, c_cols)
    invf = ccols[:, 0:1]; sgnc = ccols[:, 1:2]; negb = ccols[:, 2:3]; sflag = ccols[:, 3:4]
    zerob = A.alloc("zerob", [128, 512], BF16); k.memset("vector", zerob, 0.0)
    onesf = A.alloc("onesf", [128, 512], F32); k.memset("vector", onesf, 1.0)
    hgbc = A.alloc("hgbc", [128, 128], F32)
    k.dma("sync", hgbc, T(hgg.ap.partition_broadcast(128), hgg.buf))
    rbbc = A.alloc("rbbc", [128, 32], F32)
    k.dma("sync", rbbc, T(rb.ap.partition_broadcast(128), rb.buf))
    mcols = A.alloc("mcols", [128, 32], F32)
    s1col = A.alloc("s1col", [128, 8], F32); s2col = A.alloc("s2col", [128, 8], F32)
    lbcol = A.alloc("lbcol", [128, 2], F32); omlb = A.alloc("omlb", [128, 2], F32)
    Gall = A.alloc("Gall", [128, 16, 32], F32)
    smalls = Rot([A.alloc(f"sm{i}", [128, 1], F32) for i in range(12)])
    epsc = A.alloc("epsc", [128, 1], F32); k.memset("vector", epsc, EPS)
    small_mark = A.mark()
    mixA = A.alloc("mixA", [128, 4, 2048], BF16)
    persist_mark = A.mark()

    g1bc = A.alloc("g1bc", [128, 1024], F32)
    g2bc = A.alloc("g2bc", [128, 1024], F32)
    s2row = A.alloc("s2row", [128, 1024], F32)
    sh2row = A.alloc("sh2row", [128, 1024], F32)
    cT = A.alloc("cT", [128, 8], F32)
    k.dma("sync", cT, cvec.re("(j p) -> p j", p=128), allow_slow_non_contiguous=True)
    siluc = A.alloc("siluc", [128, 8], BF16)
    k.act(siluc, cT, AF.Silu)
    modrow = A.alloc("modrow", [1, 6144], F32)
    adab = A.alloc("adab", [1, 6144], F32)
    k.dma("sync", adab, ada_b.re("(o n) -> o n", o=1))
    ones1 = A.alloc("ones1", [1, 128], F32); k.memset("vector", ones1, 1.0)
    aslots = Rot([A.alloc(f"adaw{i}", [128, 8, 512], BF16) for i in range(2)])
    prot = Rot(banks[0:4])
    adaw_v = ada_w.re("(kc p) n -> p kc n", p=128)
    for nb in range(12):
        sl = aslots.next()
        k.dma("gpsimd", sl, adaw_v[:, :, nb * 512:(nb + 1) * 512])
        pb = prot.next()
        for kc in range(8):
            k.mm(pb[0:1, :], siluc[:, kc:kc + 1], sl[:, kc, :], start=(kc == 0), stop=(kc == 7))
        k.tt("vector", modrow[0:1, nb * 512:(nb + 1) * 512], pb[0:1, :], adab[0:1, nb * 512:(nb + 1) * 512], ALU.add)
    for (dst, off) in ((g1bc, 2048), (g2bc, 5120), (sh2row, 3072), (s2row, 4096)):
        for nb in range(2):
            pb = prot.next()
            k.mm(pb, ones1[0:1, :], modrow[0:1, off + nb * 512: off + (nb + 1) * 512])
            k.copy("vector", dst[:, nb * 512:(nb + 1) * 512], pb)
    pb = prot.next()
    for i, off in enumerate((0, 1024, 3072, 4096)):
        for j in range(8):
            k.mm(pb[:, i * 8 + j: i * 8 + j + 1], modrow[0:1, off + j * 128: off + (j + 1) * 128], ones1[0:1, 0:1])
    k.copy("vector", mcols, pb[:, 0:32])
    n2bc = A.alloc("n2bc", [128, 1024], F32)
    k.dma("sync", n2bc, T(n2g.ap.partition_broadcast(128), n2g.buf))
    k.ts("vector", s2row, s2row, 1.0, None, ALU.add)
    k.tt("vector", s2row, s2row, n2bc, ALU.mult)
    gcol = A.alloc("gcol", [128, 16], F32)
    k.dma("sync", gcol[:, 0:8], n1g.re("(j p) -> p j", p=128), allow_slow_non_contiguous=True)
    k.dma("sync", gcol[:, 8:16], n2g.re("(j p) -> p j", p=128), allow_slow_non_contiguous=True)
    tmp8 = A.alloc("tmp8", [128, 8], F32)
    k.ts("vector", tmp8, mcols[:, 8:16], 1.0, None, ALU.add)
    k.tt("vector", s1col, tmp8, gcol[:, 0:8], ALU.mult)
    tmp8b = A.alloc("tmp8b", [128, 8], F32)
    k.ts("vector", tmp8b, mcols[:, 24:32], 1.0, None, ALU.add)
    k.tt("vector", s2col, tmp8b, gcol[:, 8:16], ALU.mult)
    sh1col = mcols[:, 0:8]; sh2col = mcols[:, 16:24]
    lbl_sb = A.alloc("lbl_sb", [128, 2, 2], F32)
    for l in range(2):
        k.dma("sync", lbl_sb[:, l, :], lbl[l, :].re("(c p) -> p c", p=128), allow_slow_non_contiguous=True)
    dl = A.alloc("dl", [128, 2], F32)
    k.tt("vector", dl, lbl_sb[:, 0, :], lbl_sb[:, 1, :], ALU.subtract)
    k.act(lbcol, dl, AF.Sigmoid)
    k.ts("vector", omlb, lbcol, -1.0, 1.0, ALU.mult, ALU.add)
    for pi_, t_ in enumerate((g1bc, g2bc, s2row, sh2row)):
        k.dma("sync", park[pi_], t_, chan=t_.buf)
    k.barrier()
    A.reset(persist_mark)

    def make_hT(src, tile0, ntiles, scol, shcol, hT, xslots, tmps, prot):
        sqj, xn, tmpf = tmps
        sbc = scol.unsq(2).bc([128, 8, 128])
        shbc = shcol.unsq(2).bc([128, 8, 128])
        for i in range(ntiles):
            xt = xslots.next()
            k.dma("sync", xt, src[(tile0 + i) * 128:(tile0 + i + 1) * 128, :])
            ss = smalls.next(); rs = smalls.next(); rstd = smalls.next()
            k.act(sqj, xt, AF.Square, accum=ss)
            k.act(rs, ss, AF.Sqrt, scale=1.0 / 1024, bias=epsc)
            k.recip(rstd, rs)
            k.act(xn, xt, AF.Identity, scale=rstd)
            pb = prot.next()
            pv = pb.bitcast(BF16).re("p (j t) -> p j t", j=8)
            for j in range(8):
                k.tr(pv[:, j, :], xn[:, j * 128:(j + 1) * 128], identb)
            k.tt("vector", tmpf, pv, sbc, ALU.mult)
            k.tt("vector", hT[:, :, i * 128:(i + 1) * 128], tmpf, shbc, ALU.add)

    def rope_tables(slot0, n, cosg, sing, tmps):
        posi, pf, ang, kf = tmps
        ki32 = posi
        k.dma("sync", posi[:, 0:n], T(posr.ap[slot0:slot0 + n].partition_broadcast(128), posr.buf))
        k.copy("vector", pf[:, 0:n], posi[:, 0:n])
        k.ts("vector", ang[:, 0:n], pf[:, 0:n], invf, None, ALU.mult)
        k.ts("vector", kf[:, 0:n], ang[:, 0:n], 1.0 / (2 * PI), None, ALU.mult)
        k.copy("vector", ki32[:, 0:n], kf[:, 0:n])
        k.copy("vector", kf[:, 0:n], ki32[:, 0:n])
        C1 = 6.28125
        C2 = 2 * PI - C1
        k.stt(ang[:, 0:n], kf[:, 0:n], -C1, ang[:, 0:n], ALU.mult, ALU.add)
        k.stt(ang[:, 0:n], kf[:, 0:n], -C2, ang[:, 0:n], ALU.mult, ALU.add)
        k.ts("vector", ang[:, 0:n], ang[:, 0:n], -PI, PI, ALU.max, ALU.min)
        k.act(sing[:, 0:n], ang[:, 0:n], AF.Sin, scale=sgnc)
        k.ts("vector", pf[:, 0:n], ang[:, 0:n], PI / 2, None, ALU.add)
        k.ts("vector", kf[:, 0:n], pf[:, 0:n], PI, -2 * PI, ALU.is_gt, ALU.mult)
        k.tt("vector", pf[:, 0:n], pf[:, 0:n], kf[:, 0:n], ALU.add)
        k.ts("vector", pf[:, 0:n], pf[:, 0:n], -PI, PI, ALU.max, ALU.min)
        k.act(cosg[:, 0:n], pf[:, 0:n], AF.Sin)

    def proj_fm(dst_fn, wsb, wsw, nchunks, hT, n, cosg, sing, prot, rtmps):
        t1, t2 = rtmps
        for c in range(nchunks):
            pa = prot.next()
            for kc in range(8):
                k.mm(pa[:, 0:n], wsb[:, kc, c * 128:(c + 1) * 128], hT[:, kc, 0:n], start=(kc == 0), stop=(kc == 7))
            if wsw is None:
                k.copy("scalar", dst_fn(c), pa[:, 0:n])
                continue
            pb2 = prot.next()
            for kc in range(8):
                k.mm(pb2[:, 0:n], wsw[:, kc, c * 128:(c + 1) * 128], hT[:, kc, 0:n], start=(kc == 0), stop=(kc == 7))
            k.tt("vector", t1[:, 0:n], pa[:, 0:n], cosg[:, 0:n], ALU.mult)
            k.tt("vector", t2[:, 0:n], pb2[:, 0:n], sing[:, 0:n], ALU.mult)
            dst_fn(c, t1[:, 0:n], t2[:, 0:n])

    kaT = A.alloc("kaT", [128, 4, 4096], BF16)
    kiT = A.alloc("kiT", [128, 4096], BF16)
    va = A.alloc("va", [128, NT_ALL, 8, 65], BF16)
    k.memset("gpsimd", va[:, :, :, 64:65], 1.0)
    kside_mark = A.mark()
    wka = A.alloc("wka", [128, 8, 512], BF16); wload(wka, w_ka)
    wkas = A.alloc("wkas", [128, 8, 512], BF16); wload(wkas, w_kas)
    wki = A.alloc("wki", [128, 8, 128], BF16); wload(wki, w_ki)
    wkis = A.alloc("wkis", [128, 8, 128], BF16); wload(wkis, w_kis)
    wva = A.alloc("wva", [128, 8, 512], BF16); wload(wva, w_va)
    hT = A.alloc("hT", [128, 8, 512], BF16)
    xslots = Rot([A.alloc(f"xs{i}", [128, 1024], F32) for i in range(2)])
    ntmps = (A.alloc("sqj", [128, 1024], BF16), A.alloc("xn", [128, 1024], BF16), A.alloc("tmpf", [128, 8, 128], F32))
    cosg = A.alloc("cosg", [128, 512], F32); sing = A.alloc("sing", [128, 512], F32)
    rtm = (A.alloc("posi", [128, 512], I32), A.alloc("pf", [128, 512], F32), A.alloc("ang", [128, 512], F32),
           A.alloc("kf", [128, 512], F32))
    rt12 = (rtm[1], rtm[3])
    prot = Rot(banks)
    for g in range(8):
        src = xp if g < 4 else xo
        make_hT(src, (g % 4) * 4, 4, s1col, sh1col, hT, xslots, ntmps, prot)
        rope_tables(g * 512, 512, cosg, sing, rtm)

        def dst_ka(c, a=None, b=None, g=g):
            k.tt("gpsimd", kaT[:, c, g * 512:(g + 1) * 512], a, b, ALU.add)
        proj_fm(dst_ka, wka, wkas, 4, hT, 512, cosg, sing, prot, rt12)

        def dst_ki(c, a=None, b=None, g=g):
            k.tt("gpsimd", kiT[:, g * 512:(g + 1) * 512], a, b, ALU.add)
        proj_fm(dst_ki, wki, wkis, 1, hT, 512, cosg, sing, prot, rt12)
        for i in range(4):
            pa = prot.next()
            for kc in range(8):
                k.mm(pa, hT[:, kc, i * 128:(i + 1) * 128], wva[:, kc, :], start=(kc == 0), stop=(kc == 7))
            k.copy("scalar", va[:, g * 4 + i, :, 0:64], pa.re("p (h d) -> p h d", h=8))
    k.barrier()
    A.reset(kside_mark)

    qaT = A.alloc("qaT", [128, 4, 2048], BF16)
    qiT = A.alloc("qiT", [128, 2, 2048], BF16)
    wis = A.alloc("wis", [128, 16, 4], F32)
    qside_mark = A.mark()
    wqa = A.alloc("wqa", [128, 8, 512], BF16); wload(wqa, w_qa)
    wqas = A.alloc("wqas", [128, 8, 512], BF16); wload(wqas, w_qas)
    wqi = A.alloc("wqi", [128, 8, 256], BF16); wload(wqi, w_qi)
    wqis = A.alloc("wqis", [128, 8, 256], BF16); wload(wqis, w_qis)
    wwi = A.alloc("wwi", [128, 8, 4], BF16); wload(wwi, w_wi)
    hT = A.alloc("hT", [128, 8, 512], BF16)
    xslots = Rot([A.alloc(f"xs{i}", [128, 1024], F32) for i in range(2)])
    ntmps = (A.alloc("sqj", [128, 1024], BF16), A.alloc("xn", [128, 1024], BF16), A.alloc("tmpf", [128, 8, 128], F32))
    cosg = A.alloc("cosg", [128, 512], F32); sing = A.alloc("sing", [128, 512], F32)
    rtm = (A.alloc("posi", [128, 512], I32), A.alloc("pf", [128, 512], F32), A.alloc("ang", [128, 512], F32),
           A.alloc("kf", [128, 512], F32))
    rt12 = (rtm[1], rtm[3])
    for g in range(4):
        make_hT(xo, g * 4, 4, s1col, sh1col, hT, xslots, ntmps, prot)
        rope_tables(2048 + g * 512, 512, cosg, sing, rtm)

        def dst_qa(c, a=None, b=None, g=g):
            k.tt("gpsimd", qaT[:, c, g * 512:(g + 1) * 512], a, b, ALU.add)
        proj_fm(dst_qa, wqa, wqas, 4, hT, 512, cosg, sing, prot, rt12)

        def dst_qi(c, a=None, b=None, g=g):
            k.tt("gpsimd", qiT[:, c, g * 512:(g + 1) * 512], a, b, ALU.add)
        proj_fm(dst_qi, wqi, wqis, 2, hT, 512, cosg, sing, prot, rt12)
        for i in range(4):
            pa = prot.next()
            for kc in range(8):
                k.mm(pa[:, 0:4], hT[:, kc, i * 128:(i + 1) * 128], wwi[:, kc, :], start=(kc == 0), stop=(kc == 7))
            k.ts("vector", wis[:, g * 4 + i, :], pa[:, 0:4], 0.5 * 0.125, None, ALU.mult)
    k.barrier()
    A.reset(qside_mark)

    scs = Rot([A.alloc(f"sc{i}", [128, 4096], F32) for i in range(2)])
    mbars = Rot([A.alloc(f"mbar{i}", [128, 4096], BF16) for i in range(2)])
    rts = Rot([A.alloc(f"rt{i}", [128, 512], F32) for i in range(4)])
    pTs = Rot([A.alloc(f"pT{i}", [128, 512], BF16) for i in range(3)])
    absw = A.alloc("absw", [128, 4], F32); sgnw = A.alloc("sgnw", [128, 4], F32)
    lo = A.alloc("lo", [128, 1], F32); mid = A.alloc("mid", [128, 1], F32)
    cnt = A.alloc("cnt", [128, 1], F32); dlt = A.alloc("dlt", [128, 1], F32)
    rmax = A.alloc("rmax", [128, 1], F32)
    sga = A.alloc("sga", [128, 1], F32); tcomb = A.alloc("tcomb", [128, 1], F32)
    rden = A.alloc("rden", [128, 8], F32)
    oa = A.alloc("oa", [128, 8, 64], BF16)
    zqs = Rot([A.alloc(f"zq{i}", [128, 4, 2, 128], BF16) for i in range(2)])
    zis = Rot([A.alloc(f"zi{i}", [128, 2, 2, 128], BF16) for i in range(2)])
    for z in zqs.items + zis.items:
        k.memset("gpsimd", z, 0.0)
    sc_rot = Rot(banks[0:2])
    s_rot = Rot(banks[2:4])
    po_rot = Rot([(banks[4], banks[5])])
    acc_rot = Rot(banks[6:8])
    dsgs = Rot([A.alloc(f"dsg{i}", [128, 4, 128], F32) for i in range(2)])

    def stage_a(n):
        nkt = 16 + n + 1
        nk = nkt * 128
        qs = slice(n * 128, (n + 1) * 128)
        zq = zqs.next(); zi = zis.next(); mbar = mbars.next(); sc = scs.next()
        for hp in range(2):
            ph = slice(hp * 64, (hp + 1) * 64)
            k.copy("gpsimd", zq[ph, :, hp, :], qaT[ph, :, qs])
            k.copy("gpsimd", zi[ph, :, hp, :], qiT[ph, :, qs])
        k.ts("vector", sgnw, wis[:, n, :], -1.0, None, ALU.mult)
        k.tt("vector", absw, wis[:, n, :], sgnw, ALU.max)
        k.ts("vector", sgnw, wis[:, n, :], 0.0, 2.0, ALU.is_ge, ALU.mult)
        k.ts("vector", sgnw, sgnw, -1.0, None, ALU.add)
        dsg = dsgs.next()
        for h in range(4):
            k.ts("vector", dsg[:, h, :], identf, sgnw[:, h:h + 1], None, ALU.mult)
        units = []
        for kb in range((nkt + 3) // 4):
            ncols = min(512, nk - kb * 512)
            for h in range(4):
                units.append((kb, h, ncols))
        st_ = {"pend": None, "pacc": None}

        def acc_unit(kb, h, ncols, rt):
            if h == 0:
                st_["pacc"] = acc_rot.next()
            pacc = st_["pacc"]
            k.mm(pacc[:, 0:ncols], dsg[:, h, :], rt[:, 0:ncols], start=(h == 0), stop=(h == 3))
            if h == 3:
                cs = slice(kb * 512, kb * 512 + ncols)
                if kb < 4:
                    k.act(sc[:, cs], pacc[:, 0:ncols], AF.Identity, bias=negb)
                else:
                    k.copy("scalar", sc[:, cs], pacc[:, 0:ncols])

        for (kb, h, ncols) in units:
            c, hp = h // 2, h % 2
            cs = slice(kb * 512, kb * 512 + ncols)
            pa = sc_rot.next()
            k.mm(pa[:, 0:ncols], zi[:, c, hp, :], kiT[:, cs])
            rt = rts.next()
            k.act(rt[:, 0:ncols], pa[:, 0:ncols], AF.Relu, scale=absw[:, h:h + 1])
            if st_["pend"] is not None:
                acc_unit(*st_["pend"])
            st_["pend"] = (kb, h, ncols, rt)
        acc_unit(*st_["pend"])
        ds_ = slice((nkt - 1) * 128, nkt * 128)
        k.tt("vector", sc[:, ds_], sc[:, ds_], diagm, ALU.add)
        k.reduce(rmax, sc[:, 0:nk], ALU.max)
        k.ts("vector", mid, rmax, -RNG + RNG / 2, None, ALU.add)
        for it in range(NBIS):
            wn = RNG / (2 ** (it + 2))
            k.ts("vector", mbar[:, 0:nk], sc[:, 0:nk], mid, None, ALU.is_gt, op1=ALU.add, accum=cnt)
            k.ts("vector", dlt, cnt, 255.5, 2.0 * wn, ALU.is_ge, ALU.mult)
            k.stt(mid, dlt, -wn, mid, ALU.add, ALU.add)
        k.ts("vector", lo, mid, -RNG / (2 ** (NBIS + 1)), None, ALU.add)
        k.ts("vector", mbar[:, 0:nk], sc[:, 0:nk], lo, None, ALU.is_le)
        return (n, nkt, qs, zq, mbar)

    def stage_b(ctx):
        n, nkt, qs, zq, mbar = ctx
        poA, poB = po_rot.next()
        k.mm(poA[:, 0:260], zerob[:, 0:128], zerob[:, 0:260], start=True, stop=False, skip_group_check=True)
        k.mm(poB[:, 0:260], zerob[:, 0:128], zerob[:, 0:260], start=True, stop=False, skip_group_check=True)
        groups = [(j, gq) for j in range(nkt) for gq in range(2)]

        def logits(j, gq):
            ks_ = slice(j * 128, (j + 1) * 128)
            pS = s_rot.next()
            for hh in range(4):
                h = gq * 4 + hh
                c, hp = h // 2, h % 2
                k.mm(pS[:, hh * 128:(hh + 1) * 128], kaT[:, c, ks_], zq[:, c, hp, :], start=True, stop=False)
                k.mm(pS[:, hh * 128:(hh + 1) * 128], mbar[:, ks_], negI, start=False, stop=True)
            pT = pTs.next()
            k.act(pT, pS, AF.Exp, scale=0.125)
            return pT

        def pv_mm(j, gq, pT):
            po = poA if gq == 0 else poB
            for hh in range(4):
                h = gq * 4 + hh
                k.mm(po[:, hh * 65:(hh + 1) * 65], pT[:, hh * 128:(hh + 1) * 128], va[:, j, h, :],
                     start=False, stop=(j == nkt - 1), skip_group_check=True)

        pend = None
        for (j, gq) in groups:
            pT = logits(j, gq)
            if pend is not None:
                pv_mm(*pend)
            pend = (j, gq, pT)
        pv_mm(*pend)
        for gq, po in enumerate((poA, poB)):
            pv = po[:, 0:260].re("p (h d) -> p h d", h=4)
            k.recip(rden[:, gq * 4:(gq + 1) * 4], pv[:, :, 64])
            k.tt("vector", oa[:, gq * 4:(gq + 1) * 4, :], pv[:, :, 0:64],
                 rden[:, gq * 4:(gq + 1) * 4].unsq(2).bc([128, 4, 64]), ALU.mult)
        pa = s_rot.next()
        pv = pa.bitcast(BF16).re("p (j t) -> p j t", j=8)
        oaf = oa.re("p h d -> p (h d)")
        for c in range(4):
            k.tr(pv[:, c, :], oaf[:, c * 128:(c + 1) * 128], identb)
        k.copy("scalar", mixA[:, :, qs], pv[:, 0:4, :])

    ctx = stage_a(0)
    for n in range(NT_OWN):
        nxt = stage_a(n + 1) if n + 1 < NT_OWN else None
        stage_b(ctx)
        ctx = nxt
    k.barrier()
    A.reset(persist_mark)

    mixB = A.alloc("mixB", [128, 4, 2048], BF16)
    hg_mark = A.mark()
    wqb = A.alloc("wqb", [128, 8, 256], BF16); wload(wqb, w_qb)
    wfb = A.alloc("wfb", [128, 8, 256], BF16); wload(wfb, w_fb)
    wib = A.alloc("wib", [128, 8, 512], BF16); wload(wib, w_ib)
    wgb = A.alloc("wgb", [128, 8, 512], BF16); wload(wgb, w_gb)
    hT = A.alloc("hT", [128, 8, 512], BF16)
    xslots = Rot([A.alloc(f"xs{i}", [128, 1024], F32) for i in range(2)])
    ntmps = (A.alloc("sqj", [128, 1024], BF16), A.alloc("xn", [128, 1024], BF16), A.alloc("tmpf", [128, 8, 128], F32))
    vtok_s = [A.alloc(f"vtok{i}", [128, 4, 512], BF16) for i in range(2)]
    sgt_s = [A.alloc(f"sgt{i}", [128, 4, 512], BF16) for i in range(2)]
    sgf = A.alloc("sgf", [128, 512], F32)
    S = A.alloc("S", [128, 2, 128], F32); k.memset("vector", S, 0.0)
    Sbf = [A.alloc(f"Sbf{i}", [128, 2, 128], BF16) for i in range(8)]
    Bext = [A.alloc(f"Bext{c2}", [128, 513], F32) for c2 in range(2)]
    for c2 in range(2):
        k.memset("vector", Bext[c2][:, 0:1], 0.0)
    sig = A.alloc("sig", [128, 512], F32); fT = A.alloc("fT", [128, 512], F32)
    logf = A.alloc("logf", [128, 512], F32); omf = A.alloc("omf", [128, 512], F32)
    qbf = A.alloc("qbf", [128, 512], F32)
    D1 = A.alloc("D1", [128, 8, 64], F32); D3 = A.alloc("D3", [128, 8, 64], F32); D4 = A.alloc("D4", [128, 8, 64], F32)
    E1 = A.alloc("E1", [128, 512], F32); E2 = A.alloc("E2", [128, 512], F32)
    E3 = A.alloc("E3", [128, 512], F32); E4 = A.alloc("E4", [128, 512], F32)
    dd = A.alloc("dd", [128, 8], F32)
    gsets = []
    for si in range(2):
        dec_ = [A.alloc(f"dec{si}_{c2}", [128, 8], F32) for c2 in range(2)]
        qtZ_ = [A.alloc(f"qtZ{si}_{c2}", [128, 2, 512], BF16) for c2 in range(2)]
        qeZ_ = [A.alloc(f"qeZ{si}_{c2}", [128, 2, 512], BF16) for c2 in range(2)]
        ktT_ = [A.alloc(f"ktT{si}_{c2}", [128, 512], BF16) for c2 in range(2)]
        kdT_ = [A.alloc(f"kdT{si}_{c2}", [128, 512], BF16) for c2 in range(2)]
        for c2 in range(2):
            k.memset("gpsimd", qtZ_[c2], 0.0)
            k.memset("gpsimd", qeZ_[c2], 0.0)
        gsets.append((vtok_s[si], sgt_s[si], dec_, qtZ_, qeZ_, ktT_, kdT_))
    kdZ = [[A.alloc(f"kdZ{c2}_{i}", [128, 2, 128], BF16) for i in range(2)] for c2 in range(2)]
    for c2 in range(2):
        for i in range(2):
            k.memset("gpsimd", kdZ[c2][i], 0.0)
    attS = [A.alloc(f"attS{i}", [128, 4, 128], BF16) for i in range(2)]
    for i in range(2):
        k.memset("gpsimd", attS[i], 0.0)
    ssq = A.alloc("ssq", [128, 4], F32); rs4 = A.alloc("rs4", [128, 4], F32); rstd4 = A.alloc("rstd4", [128, 4], F32)
    sqj2 = A.alloc("sqj2", [128, 128], BF16)
    ob1 = A.alloc("ob1", [128, 4, 128], F32); ob2 = A.alloc("ob2", [128, 4, 128], F32)
    obb = A.alloc("obb", [128, 512], BF16)
    prot = Rot(banks[0:4])
    kv_rot = Rot(banks[4:6])
    at_rot = Rot([(banks[6], banks[7])])
    tri2b = tri2.unsq(1).bc([128, 2, 128])
    def hg_front(g):
        own = g >= 4
        vtok, sgt, dec, qtZ, qeZ, ktT, kdT = gsets[g % 2]
        src = xo if own else xp
        make_hT(src, (g % 4) * 4, 4, s1col, sh1col, hT, xslots, ntmps, prot)
        for i in range(4):
            pa = prot.next()
            for kc in range(8):
                k.mm(pa, hT[:, kc, i * 128:(i + 1) * 128], wib[:, kc, :], start=(kc == 0), stop=(kc == 7))
            k.copy("scalar", vtok[:, i, :], pa)
            if own:
                pa = prot.next()
                for kc in range(8):
                    k.mm(pa, hT[:, kc, i * 128:(i + 1) * 128], wgb[:, kc, :], start=(kc == 0), stop=(kc == 7))
                k.act(sgf, pa, AF.Sigmoid)
                k.tt("vector", sgt[:, i, :], sgf, pa, ALU.mult)
        for c2 in range(2):
            pa = prot.next()
            for kc in range(8):
                k.mm(pa, wfb[:, kc, c2 * 128:(c2 + 1) * 128], hT[:, kc, :], start=(kc == 0), stop=(kc == 7))
            k.act(sig, pa, AF.Sigmoid)
            k.ts("vector", fT, sig, omlb[:, c2:c2 + 1], lbcol[:, c2:c2 + 1], ALU.mult, ALU.add)
            k.act(logf, fT, AF.Ln)
            k.ts("vector", omf, fT, -1.0, 1.0, ALU.mult, ALU.add)
            Bx = Bext[c2]
            k.scan_add(Bx[:, 1:513], onesf, logf, Bx[:, 0:1])
            Bg = Bx[:, 1:513].re("p (c t) -> p c t", c=8)
            Bprev = Bx[:, 0:512].re("p (c t) -> p c t", c=8)[:, :, 0:1]
            Blast = Bg[:, :, 63:64]
            Bmid = Bg[:, :, 31:32]
            k.tt("vector", D4, Blast.bc([128, 8, 64]), Bg, ALU.subtract)
            k.act(E4, D4.re("p c t -> p (c t)"), AF.Exp)
            k.tt("vector", kdT[c2], omf, E4, ALU.mult)
            k.tt("vector", dd, Blast.re("p c o -> p (c o)"), Bprev.re("p c o -> p (c o)"), ALU.subtract)
            k.act(dec[c2], dd, AF.Exp)
            if own:
                pq = prot.next()
                for kc in range(8):
                    k.mm(pq, wqb[:, kc, c2 * 128:(c2 + 1) * 128], hT[:, kc, :], start=(kc == 0), stop=(kc == 7))
                k.copy("scalar", qbf, pq)
                k.tt("vector", D1, Bg, Bmid.bc([128, 8, 64]), ALU.subtract)
                k.act(E1, D1.re("p c t -> p (c t)"), AF.Exp)
                k.act(E2, D1.re("p c t -> p (c t)"), AF.Exp, scale=-1.0)
                k.tt("vector", D3, Bg, Bprev.bc([128, 8, 64]), ALU.subtract)
                k.act(E3, D3.re("p c t -> p (c t)"), AF.Exp)
                k.tt("vector", ktT[c2], omf, E2, ALU.mult)
                for hp in range(2):
                    ph = slice(hp * 64, (hp + 1) * 64)
                    k.tt("vector", qtZ[c2][ph, hp, :], qbf[ph, :], E1[ph, :], ALU.mult)
                    k.tt("vector", qeZ[c2][ph, hp, :], qbf[ph, :], E3[ph, :], ALU.mult)
            k.copy("vector", Bx[:, 0:1], Bx[:, 512:513])

    def hg_back(g):
        own = g >= 4
        vtok, sgt, dec, qtZ, qeZ, ktT, kdT = gsets[g % 2]
        for i in range(4):
            ts_ = slice(i * 128, (i + 1) * 128)
            kz = []
            for c2 in range(2):
                pa = prot.next()
                pv = pa.bitcast(BF16)
                k.tr(pv[:, 0:128], kdT[c2][:, ts_], identb)
                kzz = kdZ[c2][i % 2]
                for cp in range(2):
                    k.copy("scalar", kzz[cp * 64:(cp + 1) * 64, cp, :], pv[cp * 64:(cp + 1) * 64, 0:128])
                kz.append(kzz)
            if own:
                pA0, pA1 = at_rot.next()
                for cp in range(2):
                    cs_ = slice(i * 128 + cp * 64, i * 128 + (cp + 1) * 64)
                    for h in range(4):
                        c2, hp = h // 2, h % 2
                        pbk = pA0 if hp == 0 else pA1
                        k.mm(pbk[cp * 64:(cp + 1) * 64, c2 * 64:(c2 + 1) * 64], ktT[c2][:, cs_], qtZ[c2][:, hp, cs_])
                aS = attS[i % 2]
                for hp, pbk in enumerate((pA0, pA1)):
                    for c2 in range(2):
                        h = 2 * c2 + hp
                        for cp in range(2):
                            ph = slice(cp * 64, (cp + 1) * 64)
                            k.tt("vector", aS[ph, h, cp * 64:(cp + 1) * 64], pbk[ph, c2 * 64:(c2 + 1) * 64],
                                 tri2[ph, cp * 64:(cp + 1) * 64], ALU.mult)
            for cp in range(2):
                cidx = i * 2 + cp
                if own:
                    k.copy("scalar", Sbf[cidx], S)
                pkv = kv_rot.next()
                for h in range(4):
                    c2, hp = h // 2, h % 2
                    k.mm(pkv[hp * 64:(hp + 1) * 64, c2 * 128:(c2 + 1) * 128], kz[c2][:, cp, hp * 64:(hp + 1) * 64],
                         vtok[:, i, h * 128:(h + 1) * 128])
                for c2 in range(2):
                    k.stt(S[:, c2, :], S[:, c2, :], dec[c2][:, cidx:cidx + 1], pkv[:, c2 * 128:(c2 + 1) * 128], ALU.mult, ALU.add)
                if g == 3 and cidx == 7:
                    k.ts("vector", S.re("p a b -> p (a b)"), S.re("p a b -> p (a b)"), sflag, None, ALU.mult)
            if own:
                po = prot.next()
                for h in range(4):
                    c2, hp = h // 2, h % 2
                    k.mm(po[:, h * 128:(h + 1) * 128], aS[:, h, :], vtok[:, i, h * 128:(h + 1) * 128], start=True, stop=False)
                    for cp in range(2):
                        cidx = i * 2 + cp
                        cs_ = slice(i * 128 + cp * 64, i * 128 + (cp + 1) * 64)
                        k.mm(po[cp * 64:(cp + 1) * 64, h * 128:(h + 1) * 128], qeZ[c2][:, hp, cs_], Sbf[cidx][:, c2, :],
                             start=False, stop=True)
                pov = po.re("p (h e) -> p h e", h=4)
                for h in range(4):
                    k.act(sqj2, po[:, h * 128:(h + 1) * 128], AF.Square, accum=ssq[:, h:h + 1])
                k.act(rs4, ssq, AF.Sqrt, scale=1.0 / 128, bias=epsc)
                k.recip(rstd4, rs4)
                k.tt("vector", ob1, pov, rstd4.unsq(2).bc([128, 4, 128]), ALU.mult)
                k.tt("vector", ob2, ob1, hgbc.unsq(1).bc([128, 4, 128]), ALU.mult)
                k.tt("vector", obb, ob2.re("p h e -> p (h e)"), sgt[:, i, :], ALU.mult)
                pa = prot.next()
                pv = pa.bitcast(BF16).re("p (j t) -> p j t", j=8)
                for c in range(4):
                    k.tr(pv[:, c, :], obb[:, c * 128:(c + 1) * 128], identb)
                tt0 = (g - 4) * 512 + i * 128
                k.copy("scalar", mixB[:, :, tt0:tt0 + 128], pv[:, 0:4, :])

    hg_front(0)
    for g in range(8):
        if g + 1 < 8:
            hg_front(g + 1)
        hg_back(g)
    k.barrier()
    A.reset(hg_mark)

    idxall = A.alloc("idxall", [128, 16, 4], I32, top=True)
    gkall = A.alloc("gkall", [128, 16, 4], F32, top=True)
    nblk_i = A.alloc("nblk_i", [128, 32], I32, top=True)
    p5_mark = A.mark()
    g1bc = A.alloc("g1bc", [128, 1024], F32); k.dma("sync", g1bc, park[0])
    s2row = A.alloc("s2row", [128, 1024], F32); k.dma("sync", s2row, park[2])
    sh2row = A.alloc("sh2row", [128, 1024], F32); k.dma("sync", sh2row, park[3])
    wo = A.alloc("wo", [128, 8, 1024], BF16); wload(wo, w_out)
    rwf = A.alloc("rwf", [128, 8, 32], F32)
    k.dma("sync", rwf, rw.re("(kc p) e -> p kc e", p=128))
    LTb = A.alloc("LTb", [128, 128], BF16); k.dma("sync", LTb, c_ltb)
    onesb = A.alloc("onesb", [128, 128], BF16); k.memset("vector", onesb, 1.0)
    erow = A.alloc("erow", [128, 32], F32); k.dma("sync", erow, c_erow)
    carry = A.alloc("carry", [128, 32], F32); k.memset("vector", carry, 0.0)
    xslots = Rot([A.alloc(f"xs{i}", [128, 1024], F32) for i in range(2)])
    x1r = Rot([A.alloc(f"x1_{i}", [128, 1024], F32) for i in range(2)])
    h2ts = Rot([A.alloc(f"h2tok{i}", [128, 1024], BF16) for i in range(2)])
    ytmp = A.alloc("ytmp", [128, 1024], F32)
    sqj = A.alloc("sqj", [128, 1024], BF16)
    xnf = A.alloc("xnf", [128, 1024], F32)
    h2f = A.alloc("h2f", [128, 8, 128], F32); h2ft = A.alloc("h2ft", [128, 8, 128], F32)
    lg = A.alloc("lg", [128, 32], F32); m8 = A.alloc("m8", [128, 8], F32)
    msk = A.alloc("msk", [128, 32], F32); ex = A.alloc("ex", [128, 32], F32)
    mskb = A.alloc("mskb", [128, 32], BF16)
    rank = A.alloc("rank", [128, 32], F32); keyt = A.alloc("keyt", [128, 32], F32)
    ek8 = A.alloc("ek8", [128, 8], F32); oh = A.alloc("oh", [128, 32], F32); ohr = A.alloc("ohr", [128, 32], F32)
    rk = A.alloc("rk", [128, 1], F32); slf = A.alloc("slf", [128, 1], F32)
    nmax = A.alloc("nmax", [128, 1], F32); zs = A.alloc("zs", [128, 1], F32); rz = A.alloc("rz", [128, 1], F32)
    y_rot = Rot([(banks[0], banks[1]), (banks[2], banks[3])])
    t_rot = Rot([(banks[4], banks[5]), (banks[6], banks[7])])
    s2bc = s2col.unsq(2).bc([128, 8, 128]); sh2bc = sh2col.unsq(2).bc([128, 8, 128])
    def p5_front(i):
        lg = lgs.next()
        ts_ = slice(i * 128, (i + 1) * 128)
        xt = xslots.next()
        k.dma("sync", xt, xo[ts_, :])
        py = y_rot.next()
        for nb in range(2):
            for kc in range(8):
                mixsrc = mixA if kc < 4 else mixB
                k.mm(py[nb], mixsrc[:, kc % 4, ts_], wo[:, kc, nb * 512:(nb + 1) * 512], start=(kc == 0), stop=(kc == 7))
        x1 = x1r.next()
        for nb in range(2):
            ns_ = slice(nb * 512, (nb + 1) * 512)
            k.tt("vector", ytmp[:, ns_], py[nb], g1bc[:, ns_], ALU.mult)
            k.tt("vector", x1[:, ns_], ytmp[:, ns_], xt[:, ns_], ALU.add)
        k.dma("sync", x1s[ts_, :], x1, chan=x1.buf)
        if debug:
            k.dma("sync", dbg["x1"][ts_, :], x1, chan=x1.buf)
        ss = smalls.next(); rs = smalls.next(); rstd = smalls.next()
        k.act(sqj, x1, AF.Square, accum=ss)
        k.act(rs, ss, AF.Sqrt, scale=1.0 / 1024, bias=epsc)
        k.recip(rstd, rs)
        k.act(xnf, x1, AF.Identity, scale=rstd)
        h2tok = h2ts.next()
        k.tt("vector", ytmp, xnf, s2row, ALU.mult)
        k.tt("vector", h2tok, ytmp, sh2row, ALU.add)
        pt = t_rot.next()
        for j in range(8):
            k.tr(pt[j // 4][:, (j % 4) * 128:(j % 4 + 1) * 128], xnf[:, j * 128:(j + 1) * 128], identf)
        for hb in range(2):
            pv = pt[hb].re("p (j t) -> p j t", j=4)
            k.tt("vector", h2ft[:, hb * 4:(hb + 1) * 4, :], pv, s2bc[:, hb * 4:(hb + 1) * 4, :], ALU.mult)
        k.tt("vector", h2f, h2ft, sh2bc, ALU.add)
        pl = y_rot.next()[0]
        for kc in range(8):
            k.mm(pl[:, 0:32], h2f[:, kc, :], rwf[:, kc, :], start=(kc == 0), stop=(kc == 7))
        k.tt("vector", lg, pl[:, 0:32], rbbc, ALU.add)
        return (i, lg, h2tok)

    def p5_back(ctx):
        i, lg, h2tok = ctx
        k.vmax8(m8, lg)
        k.ts("vector", msk, lg, m8[:, 3:4], None, ALU.is_ge)
        k.ts("vector", nmax, m8[:, 0:1], -1.0, None, ALU.mult)
        k.act(ex, lg, AF.Exp, bias=nmax)
        k.tt("vector", ex, ex, msk, ALU.mult)
        k.reduce(zs, ex, ALU.add)
        k.recip(rz, zs)
        k.ts("vector", Gall[:, i, :], ex, rz, None, ALU.mult)
        k.copy("vector", mskb, msk)
        pr = y_rot.next()[1]
        k.mm(pr[:, 0:32], LTb, mskb)
        k.mm(pr[:, 32:64], onesb, mskb)
        k.tt("vector", rank, pr[:, 0:32], carry, ALU.add)
        k.tt("vector", carry, carry, pr[:, 32:64], ALU.add)
        k.tt("vector", keyt, msk, erow, ALU.mult)
        k.vmax8(ek8, keyt)
        for kk in range(4):
            k.ts("vector", oh, erow, ek8[:, kk:kk + 1], None, ALU.is_equal)
            k.tt("vector", ohr, oh, rank, ALU.mult)
            k.reduce(rk, ohr, ALU.add)
            k.tt("vector", ohr, oh, Gall[:, i, :], ALU.mult)
            k.reduce(gkall[:, i, kk:kk + 1], ohr, ALU.add)
            k.ts("vector", slf, ek8[:, kk:kk + 1], -1.0, 2048.0, ALU.add, ALU.mult)
            k.tt("vector", slf, slf, rk, ALU.add)
            ix = T(idxall.ap[:, i, kk:kk + 1], Buf(f"idx_{i}_{kk}"))
            idxT[(i, kk)] = ix
            k.copy("vector", ix, slf)
            k.idma(XG, h2tok, ix, scatter=True, chan=h2tok.buf)

    idxT = {}
    lgs = Rot([A.alloc(f"lg{i}", [128, 32], F32) for i in range(2)])
    ctx5 = p5_front(0)
    for i in range(NT_OWN):
        nxt5 = p5_front(i + 1) if i + 1 < NT_OWN else None
        p5_back(ctx5)
        ctx5 = nxt5
    k.ts("vector", rank, carry, 63.5, 1.0 / 128, ALU.add, ALU.mult)
    k.copy("vector", nblk_i, rank)
    if debug:
        k.dma("sync", dbg["G"], Gall.re("p a b -> p (a b)"), chan=Gall.buf)
    k.barrier()
    A.reset(small_mark)

    b1T = A.alloc("b1T", [128, 16, 32], F32)
    b1_mark = A.mark()
    b1sb = A.alloc("b1sb", [32, 2048], F32); k.dma("sync", b1sb, b1)
    pb = banks[0]
    for c in range(16):
        k.tr(pb[:, c * 32:(c + 1) * 32], b1sb[0:32, c * 128:(c + 1) * 128], identf[0:32, 0:32])
    k.copy("vector", b1T, pb.re("p (c e) -> p c e", c=16))
    k.barrier()
    A.reset(b1_mark)
    p6_mark = A.mark()
    w1b = A.alloc("w1b", [128, 8, 2048], BF16)
    w2b = A.alloc("w2b", [128, 8, 1024], BF16)
    stg1 = [A.alloc(f"stg1_{p}", [128, 2048], F32) for p in range(8)]
    stg2 = [A.alloc(f"stg2_{j}", [128, 2, 1024], F32) for j in range(4)]
    xbs = Rot([A.alloc(f"xb{i}", [128, 1024], BF16) for i in range(3)])
    xets = Rot([A.alloc(f"xet{i}", [128, 8, 128], BF16) for i in range(2)])
    actTs = Rot([A.alloc(f"actT{i}", [128, 8, 128], BF16) for i in range(2)])
    ysbs = Rot([A.alloc(f"ysb{i}", [128, 1024], F32) for i in range(2)])
    gts = Rot([A.alloc(f"gt{i}", [128, 4, 128], F32) for i in range(1)])
    lts = Rot([A.alloc(f"lt{i}", [128, 4, 128], F32) for i in range(1)])
    sts = Rot([A.alloc(f"st{i}", [128, 4, 128], F32) for i in range(1)])
    gss = Rot([A.alloc(f"gs{i}", [128, 4, 128], F32) for i in range(1)])
    ptr_bank = banks[0]
    mm1_banks = banks[1:5]
    y_banks = (banks[5], banks[6])

    def piece_dma(e, p):
        if p < 8:
            k.dma("sync", stg1[p], w1[e][p * 128:(p + 1) * 128, :])
        else:
            j = p - 8
            k.dma("sync", stg2[j], w2[e][j * 256:(j + 1) * 256, :].re("(a p) n -> p a n", p=128))

    def piece_cast(p):
        dst, src = (w1b[:, p, :], stg1[p]) if p < 8 else (w2b[:, 2 * (p - 8):2 * (p - 8) + 2, :], stg2[p - 8])
        k.copy("vector", dst, src)

    for p in range(12):
        piece_dma(0, p)
    for p in range(12):
        piece_cast(p)
        piece_dma(1, p)
    key_next = k.val_load(nblk_i[0:1, 0:1])
    xb_first = xbs.next()
    k.dma("gpsimd", xb_first, XG[0:128, :])
    for e in range(32):
        key = key_next
        xb_next = xb_first
        for blk in range(16):
            k.cond_begin(key, blk)
            r0 = e * 2048 + blk * 128
            xb = xb_next
            if blk + 1 < 16:
                xb_next = xbs.next()
                k.dma("gpsimd", xb_next, XG[r0 + 128:r0 + 256, :])
            pv = ptr_bank.bitcast(BF16).re("p (j t) -> p j t", j=8)
            for j in range(8):
                k.tr(pv[:, j, :], xb[:, j * 128:(j + 1) * 128], identb)
            xet = xets.next()
            k.copy("scalar", xet, pv)
            for gp in range(2):
                for g4 in (gp, 2 + gp):
                    pbk = mm1_banks[g4]
                    for q in range(4):
                        fc = g4 * 4 + q
                        for kc in range(8):
                            k.mm(pbk[:, q * 128:(q + 1) * 128], w1b[:, kc, fc * 128:(fc + 1) * 128], xet[:, kc, :],
                                 start=(kc == 0), stop=(kc == 7))
            actT = actTs.next()
            ysb = ysbs.next()
            for gp in range(2):
                pg = mm1_banks[gp].re("p (q c) -> p q c", q=4)
                pl_ = mm1_banks[2 + gp].re("p (q c) -> p q c", q=4)
                bg = b1T[:, gp * 4:(gp + 1) * 4, e:e + 1].bc([128, 4, 128])
                bl = b1T[:, 8 + gp * 4:8 + (gp + 1) * 4, e:e + 1].bc([128, 4, 128])
                gt_ = gts.next(); lt_ = lts.next(); st_ = sts.next(); gs_ = gss.next()
                k.tt("vector", gt_, pg, bg, ALU.add)
                k.ts("vector", gt_, gt_, 7.0, None, ALU.min)
                k.act(st_, gt_, AF.Sigmoid, scale=1.702)
                k.tt("vector", lt_, pl_, bl, ALU.add)
                k.ts("vector", lt_, lt_, 7.0, -7.0, ALU.min, ALU.max)
                k.tt("vector", gs_, gt_, st_, ALU.mult)
                k.stt(actT[:, gp * 4:(gp + 1) * 4, :], lt_, 1.0, gs_, ALU.add, ALU.mult)
                for nb in range(2):
                    for f in range(gp * 4, gp * 4 + 4):
                        k.mm(y_banks[nb], actT[:, f, :], w2b[:, f, nb * 512:(nb + 1) * 512], start=(f == 0), stop=(f == 7))
            k.copy("scalar", ysb[:, 0:512], y_banks[0])
            k.copy("vector", ysb[:, 512:1024], y_banks[1])
            k.dma("gpsimd", YG[r0:r0 + 128, :], ysb, chan=ysb.buf)
        for blk in range(16):
            k.cond_end()
        if e + 1 < 32:
            key_next = k.val_load(nblk_i[0:1, e + 1:e + 2])
            xb_first = xbs.next()
            k.dma("gpsimd", xb_first, XG[(e + 1) * 2048:(e + 1) * 2048 + 128, :])
            for p in range(12):
                piece_cast(p)
                if e + 2 < 32:
                    piece_dma(e + 2, p)
    k.barrier()
    A.reset(p6_mark)

    GT = A.alloc("GT", [32, 128], F32)
    g2bc = A.alloc("g2bc", [128, 1024], F32); k.dma("sync", g2bc, park[1])
    fgbc = A.alloc("fgbc", [128, 1024], F32)
    k.dma("sync", fgbc, T(fing.ap.partition_broadcast(128), fing.buf))
    b2sb = A.alloc("b2sb", [32, 1024], F32); k.dma("sync", b2sb, b2)
    accs = Rot([A.alloc(f"acc{i}", [128, 1024], F32) for i in range(2)])
    ygs = Rot([A.alloc(f"yg{i}", [128, 1024], F32) for i in range(8)])
    xslots = Rot([A.alloc(f"xs{i}", [128, 1024], F32) for i in range(2)])
    fo = Rot([A.alloc(f"fo{i}", [128, 1024], F32) for i in range(2)])
    sqj = A.alloc("sqj", [128, 1024], BF16)
    pg_rot = Rot(banks[0:2])
    y_rot = Rot([(banks[4], banks[5]), (banks[6], banks[7])])
    for ti in range(NT_OWN):
        ts_ = slice(ti * 128, (ti + 1) * 128)
        acc = accs.next()
        pgt = pg_rot.next()
        k.tr(pgt[0:32, 0:128], Gall[:, ti, :], identf)
        k.copy("vector", GT, pgt[0:32, 0:128])
        py = y_rot.next()
        for nb in range(2):
            k.mm(py[nb], GT[0:32, :], b2sb[0:32, nb * 512:(nb + 1) * 512])
            k.copy("scalar", acc[:, nb * 512:(nb + 1) * 512], py[nb])
        for kk in range(4):
            yg = ygs.next()
            k.idma(yg, YG, idxT[(ti, kk)], scatter=False, chan=yg.buf)
            k.stt(acc, yg, gkall[:, ti, kk:kk + 1], acc, ALU.mult, ALU.add)
        xt = xslots.next()
        k.dma("sync", xt, x1s[ts_, :])
        k.tt("vector", acc, acc, g2bc, ALU.mult)
        k.tt("vector", xt, xt, acc, ALU.add)
        ss = smalls.next(); rs = smalls.next(); rstd = smalls.next()
        k.act(sqj, xt, AF.Square, accum=ss)
        k.act(rs, ss, AF.Sqrt, scale=1.0 / 1024, bias=epsc)
        k.recip(rstd, rs)
        ot = fo.next()
        k.stt(ot, xt, rstd, fgbc, ALU.mult, ALU.mult)
        k.dma("sync", yout[ts_, :], ot, chan=ot.buf)
    k.final_wait("sync")
    k.emit()
    st.close()
    return nc, k


SPL = np.cumsum([0, 512, 512, 512, 256, 64, 4, 256, 256, 512, 512])


def _swap_perm(ncols):
    p = np.arange(ncols)
    for h0 in range(0, ncols, 64):
        p[h0:h0 + 8] = np.arange(h0 + 8, h0 + 16)
        p[h0 + 8:h0 + 16] = np.arange(h0, h0 + 8)
    return p


def _consts(half):
    identf = np.eye(128, dtype=np.float32)
    identb = identf.astype(ml_dtypes.bfloat16)
    negI = (-1024.0 * identf).astype(ml_dtypes.bfloat16)
    s = np.arange(128)[:, None]
    t = np.arange(128)[None, :]
    tri2 = (((s // 64) == (t // 64)) & ((s % 64) <= (t % 64))).astype(np.float32)
    diag = np.where((t // 64) <= (s // 64), 0.0, NEG).astype(np.float32)
    cols = np.zeros((128, 4), np.float32)
    inv_freq = (500000.0 ** (-(np.arange(0, 16, 2, dtype=np.float32) / 16))).astype(np.float32)
    for p in range(128):
        d = p % 64
        if d < 16:
            cols[p, 0] = inv_freq[d % 8]
            cols[p, 1] = -1.0 if d < 8 else 1.0
    cols[:, 2] = 0.0 if half == 1 else NEG
    cols[:, 3] = float(half)
    ltb = (s < t).astype(np.float32).astype(ml_dtypes.bfloat16)
    erow = np.broadcast_to(np.arange(1, 33, dtype=np.float32)[None, :], (128, 32)).copy()
    return {"c_ltb": ltb, "c_erow": erow, "c_identb": identb, "c_identf": identf, "c_negI": negI, "c_tri2": tri2, "c_diag": diag, "c_cols": cols}


_CACHE = {}


def kernel(x, c, positions, ada_w, ada_b, norm1_g, w_in, hg_norm_g, lb_logits, w_out, norm2_g,
           router_w, router_b, moe_w1, moe_b1, moe_w2, moe_b2, final_g, _debug=False):
    f = lambda a: np.ascontiguousarray(np.asarray(a, dtype=np.float32))
    x = f(x); c = f(c); positions = np.ascontiguousarray(np.asarray(positions, dtype=np.int32))
    w_in0 = f(w_in)[0]
    parts = [w_in0[:, SPL[i]:SPL[i + 1]] for i in range(10)]
    qa, ka, va, qi, ki, wi, qb, fb, ib, gb = parts
    ki2 = np.concatenate([ki, ki], axis=1)
    shared = {
        "ada_w": f(ada_w)[0], "ada_b": f(ada_b)[0], "norm1_g": f(norm1_g)[0], "norm2_g": f(norm2_g)[0],
        "final_g": f(final_g), "hg_norm_g": f(hg_norm_g)[0], "lb_logits": f(lb_logits),
        "w_ka": f(ka), "w_kas": f(ka[:, _swap_perm(512)]), "w_ki": f(ki2), "w_kis": f(ki2[:, _swap_perm(128)]),
        "w_va": f(va), "w_qa": f(qa), "w_qas": f(qa[:, _swap_perm(512)]),
        "w_qi": f(qi), "w_qis": f(qi[:, _swap_perm(256)]), "w_wi": f(wi),
        "w_qb": f(qb), "w_fb": f(fb), "w_ib": f(ib), "w_gb": f(gb),
        "w_out": f(w_out)[0], "router_w": f(router_w)[0], "router_b": f(router_b)[0],
        "moe_w1": f(moe_w1)[0], "moe_b1": f(moe_b1)[0], "moe_w2": f(moe_w2)[0], "moe_b2": f(moe_b2)[0],
    }
    in_maps = []
    for j in range(8):
        b, half = j // 2, j % 2
        m = dict(shared)
        m["xo"] = np.ascontiguousarray(x[b, half * 2048:(half + 1) * 2048])
        m["xp"] = np.ascontiguousarray(x[b, 0:2048])
        m["cvec"] = np.ascontiguousarray(c[b])
        m["posr"] = np.ascontiguousarray(np.concatenate([positions[b, 0:2048], positions[b, half * 2048:(half + 1) * 2048]]))
        m.update(_consts(half))
        in_maps.append(m)
    key = bool(_debug)
    if key not in _CACHE:
        _CACHE[key] = build_nc(debug=key)[0]
    nc = _CACHE[key]
    res = run_bass_kernel_spmd(nc, in_maps, core_ids=list(range(8)))
    out = np.empty((4, 4096, 1024), np.float32)
    for j in range(8):
        b, half = j // 2, j % 2
        out[b, half * 2048:(half + 1) * 2048] = res.results[j]["y"]
    if _debug:
        return out, res.results
    return out
```

```python
from contextlib import ExitStack
import numpy as np
import ml_dtypes
import concourse.bass as bass
import concourse.mybir as mybir
from concourse.bass_utils import run_bass_kernel_spmd

F32 = mybir.dt.float32
BF16 = mybir.dt.bfloat16
I32 = mybir.dt.int32
U8 = mybir.dt.uint8
AF = mybir.ActivationFunctionType
ALU = mybir.AluOpType
AX = mybir.AxisListType
DSZ = {F32: 4, BF16: 2, I32: 4, U8: 1}

ENGS = ["tensor", "vector", "scalar", "gpsimd", "sync"]
SEM_LIMIT = 30000
EPS = 1e-6
PI = float(np.pi)
NT_OWN = 16
NT_ALL = 32
RNG = 32.0
NBIS = 13
NEG = -1.0e30


class Buf:
    __slots__ = ("name", "w", "r", "dsem", "dcount")

    def __init__(self, name):
        self.name = name
        self.w = {}
        self.r = {}
        self.dsem = None
        self.dcount = 0


class T:
    __slots__ = ("ap", "buf")

    def __init__(self, ap, buf):
        self.ap = ap
        self.buf = buf

    def __getitem__(self, key):
        return T(self.ap[key], self.buf)

    def bitcast(self, dt):
        return T(self.ap.bitcast(dt), self.buf)

    def re(self, pat, **kw):
        return T(self.ap.rearrange(pat, **kw), self.buf)

    def bc(self, shape):
        return T(self.ap.to_broadcast(list(shape)), self.buf)

    def unsq(self, ax):
        return T(self.ap.unsqueeze(ax), self.buf)


def _bufs(ts):
    out = []
    for t in ts:
        if t is None:
            continue
        b = t.buf if isinstance(t, T) else t
        if b is not None and b not in out:
            out.append(b)
    return out


class Rot:
    def __init__(self, items):
        self.items = list(items)
        self.i = 0

    def next(self):
        t = self.items[self.i % len(self.items)]
        self.i += 1
        return t


class K:
    def __init__(self, nc, stack):
        self.nc = nc
        self.stack = stack
        self.streams = {e: [] for e in ENGS}
        self.esem = {}
        self.ecount = {e: 0 for e in ENGS}
        self.known = {e: {} for e in ENGS}
        self.nsem = 0
        for e in ENGS:
            self.esem[e] = self.new_sem("e_" + e)
        self.dma_bufs = []
        self.n_ops = 0
        self.rstack = []

    def new_sem(self, name):
        self.nsem += 1
        return self.stack.enter_context(self.nc.semaphore(f"{name}_{self.nsem}"))

    def _need(self, eng, tokens):
        waits = []
        kn = self.known[eng]
        for sem, val in tokens.items():
            if kn.get(sem, 0) < val:
                kn[sem] = val
                waits.append((sem, val))
        return waits

    def _deps(self, eng, rd, wr, skip_waw=None):
        tokens = {}
        mysem = self.esem[eng]

        def add(d, is_raw):
            for sem, val in d.items():
                if sem is mysem and eng == "tensor":
                    continue
                if (not is_raw) and skip_waw is not None and sem is skip_waw:
                    continue
                if tokens.get(sem, 0) < val:
                    tokens[sem] = val
        for b in rd:
            add(b.w, True)
        for b in wr:
            add(b.w, False)
            add(b.r, False)
        self._note_region(eng, tokens)
        return self._need(eng, tokens)

    def _note_region(self, eng, tokens):
        for rg in self.rstack:
            ext = rg["ext"][eng]
            for sem, val in tokens.items():
                if val <= rg["start"].get(sem, 0):
                    if ext.get(sem, 0) < val:
                        ext[sem] = val

    def _note_inc(self, eng, sem, inc):
        for rg in self.rstack:
            d = rg["incs"][eng]
            d[sem] = d.get(sem, 0) + inc

    def val_load(self, t):
        self.nvals = getattr(self, "nvals", 0) + 1
        key = self.nvals
        for e in ENGS:
            waits = self._deps(e, _bufs([t]), [])
            self.streams[e].append(("load", waits, key, t.ap))
        return key

    def cond_begin(self, key, thr):
        if not self.rstack:
            for e in ENGS:
                if self.ecount[e] >= SEM_LIMIT - 6000:
                    self.esem[e] = self.new_sem("e_" + e)
                    self.ecount[e] = 0
        start = {}
        for e in ENGS:
            start[self.esem[e]] = self.ecount[e]
        for b in self.dma_bufs:
            start[b.dsem] = b.dcount
        rg = {"key": key, "thr": thr, "start": start,
              "known0": {e: dict(self.known[e]) for e in ENGS},
              "ext": {e: {} for e in ENGS}, "incs": {e: {} for e in ENGS}}
        self.rstack.append(rg)
        for e in ENGS:
            self.streams[e].append(("begin", rg))

    def cond_end(self):
        rg = self.rstack.pop()
        for e in ENGS:
            kn = dict(rg["known0"][e])
            ew = []
            for sem, val in rg["ext"][e].items():
                if kn.get(sem, 0) < val:
                    kn[sem] = val
                    ew.append((sem, val))
            rg["ext"][e] = ew
            self.known[e] = kn
            self.streams[e].append(("end", rg))

    def op(self, eng, fn, rd=(), wr=()):
        rd = _bufs(rd)
        wr = _bufs(wr)
        waits = self._deps(eng, rd, wr)
        if self.ecount[eng] >= SEM_LIMIT and not self.rstack:
            self.esem[eng] = self.new_sem("e_" + eng)
            self.ecount[eng] = 0
        self.ecount[eng] += 1
        sem = self.esem[eng]
        val = self.ecount[eng]
        self.streams[eng].append((waits, fn, sem, 1))
        self._note_inc(eng, sem, 1)
        for b in rd:
            if b.r.get(sem, 0) < val:
                b.r[sem] = val
        for b in wr:
            b.w = {sem: val}
            b.r = {}
        self.n_ops += 1

    def dma(self, eng, out, in_, chan=None, **kw):
        rd = _bufs([in_])
        wr = _bufs([out])
        if chan is None:
            chan = out.buf
        if chan.dsem is None:
            chan.dsem = self.new_sem("d_" + chan.name)
            self.dma_bufs.append(chan)
        waits = self._deps(eng, rd, wr, skip_waw=chan.dsem)
        chan.dcount += 16
        sem, val = chan.dsem, chan.dcount
        oap, iap = out.ap, in_.ap
        self.streams[eng].append((waits, lambda e: e.dma_start(out=oap, in_=iap, **kw), sem, 16))
        self._note_inc(eng, sem, 16)
        for b in rd:
            if b.r.get(sem, 0) < val:
                b.r[sem] = val
        for b in wr:
            b.w = {sem: val}
            b.r = {}
        self.n_ops += 1

    def idma(self, out, in_, idx, scatter, chan):
        rd = _bufs([in_, idx])
        wr = _bufs([out])
        if chan.dsem is None:
            chan.dsem = self.new_sem("d_" + chan.name)
            self.dma_bufs.append(chan)
        waits = self._deps("gpsimd", rd, wr, skip_waw=chan.dsem)
        chan.dcount += 16
        sem, val = chan.dsem, chan.dcount
        oap, iap, xap = out.ap, in_.ap, idx.ap
        if scatter:
            fn = lambda e: e.indirect_dma_start(out=oap, out_offset=bass.IndirectOffsetOnAxis(xap, 0), in_=iap, in_offset=None)
        else:
            fn = lambda e: e.indirect_dma_start(out=oap, out_offset=None, in_=iap, in_offset=bass.IndirectOffsetOnAxis(xap, 0))
        self.streams["gpsimd"].append((waits, fn, sem, 16))
        self._note_inc("gpsimd", sem, 16)
        for b in rd:
            if b.r.get(sem, 0) < val:
                b.r[sem] = val
        for b in wr:
            b.w = {sem: val}
            b.r = {}
        self.n_ops += 1

    def _all_tokens(self):
        tokens = {}
        for e in ENGS:
            if self.ecount[e] > 0:
                tokens[self.esem[e]] = self.ecount[e]
        for b in self.dma_bufs:
            tokens[b.dsem] = b.dcount
        return tokens

    def barrier(self):
        tokens = self._all_tokens()
        for e in ENGS:
            waits = self._need(e, dict(tokens))
            if waits:
                self.streams[e].append((waits, None, None, 0))

    def final_wait(self, eng="sync"):
        waits = self._need(eng, self._all_tokens())
        self.streams[eng].append((waits, None, None, 0))

    def emit(self):
        nc = self.nc
        with nc.Block() as block:
            def run(name):
                def body(e):
                    items = self.streams[name]
                    vals = {}

                    def run_items(lst):
                        i = 0
                        while i < len(lst):
                            it = lst[i]
                            if it[0] == "load":
                                for s, v in it[1]:
                                    e.wait_ge(s, v)
                                if "reg" not in vals:
                                    vals["reg"] = e.alloc_register("cnd_" + name)
                                e.load(vals["reg"], it[3])
                                vals["key"] = it[2]
                                i += 1
                            elif it[0] == "begin":
                                rg = it[1]
                                assert vals["key"] == rg["key"]
                                j = i + 1
                                while not (lst[j][0] == "end" and lst[j][1] is rg):
                                    j += 1
                                bodyl = lst[i + 1:j]
                                with e.If_cmp(vals["reg"], rg["thr"], "IS_LE"):
                                    for s, v in rg["ext"][name]:
                                        e.wait_ge(s, v)
                                    for s, tot in rg["incs"][name].items():
                                        if rg["start"].get(s, 0) > 0:
                                            e.wait_ge(s, rg["start"][s])
                                        e.nop().then_inc(s, tot)
                                with e.Else():
                                    run_items(bodyl)
                                i = j + 1
                            else:
                                waits, fn, sem, inc = it
                                for s, v in waits:
                                    e.wait_ge(s, v)
                                if fn is not None:
                                    fn(e).then_inc(sem, inc)
                                i += 1
                    run_items(items)
                return body
            block.tensor(run("tensor"))
            block.vector(run("vector"))
            block.scalar(run("scalar"))
            block.gpsimd(run("gpsimd"))
            block.sync(run("sync"))

    def ps(self, name, shape, dtype):
        t = self.stack.enter_context(self.nc.psum_tensor(name, list(shape), dtype))
        return T(t[:], Buf(name))

    def dram(self, name, shape, dtype, kind):
        t = self.nc.dram_tensor(name, list(shape), dtype, kind=kind)
        return T(t.ap(), Buf(name))

    def mm(self, out, lhsT, rhs, start=True, stop=True, **kw):
        rd = [lhsT, rhs] + ([] if start else [out])
        self.op("tensor", lambda e: e.matmul(out.ap, lhsT.ap, rhs.ap, start=start, stop=stop, **kw),
                rd=rd, wr=[out])

    def tr(self, out, in_, ident):
        self.op("tensor", lambda e: e.transpose(out.ap, in_.ap, ident.ap), rd=[in_, ident], wr=[out])

    def act(self, out, in_, func, bias=None, scale=None, accum=None):
        kw = {}
        rd = [in_]
        if bias is not None:
            if isinstance(bias, T):
                kw["bias"] = bias.ap
                rd.append(bias)
            else:
                kw["bias"] = bias
        if scale is not None:
            if isinstance(scale, T):
                kw["scale"] = scale.ap
                rd.append(scale)
            else:
                kw["scale"] = scale
        wr = [out]
        if accum is not None:
            kw["accum_out"] = accum.ap
            wr.append(accum)
        self.op("scalar", lambda e: e.activation(out.ap, in_.ap, func, **kw), rd=rd, wr=wr)

    def ts(self, eng, out, in0, s1, s2, op0, op1=None, accum=None):
        rd = [in0]
        a1, a2 = s1, s2
        if isinstance(s1, T):
            rd.append(s1)
            a1 = s1.ap
        if isinstance(s2, T):
            rd.append(s2)
            a2 = s2.ap
        kw = {}
        wr = [out]
        if op1 is not None:
            kw["op1"] = op1
        if accum is not None:
            kw["accum_out"] = accum.ap
            wr.append(accum)
        self.op(eng, lambda e: e.tensor_scalar(out.ap, in0.ap, a1, a2, op0, **kw), rd=rd, wr=wr)

    def tt(self, eng, out, in0, in1, op):
        self.op(eng, lambda e: e.tensor_tensor(out.ap, in0.ap, in1.ap, op), rd=[in0, in1], wr=[out])

    def stt(self, out, in0, scalar, in1, op0, op1):
        rd = [in0, in1]
        a = scalar
        if isinstance(scalar, T):
            rd.append(scalar)
            a = scalar.ap
        self.op("vector", lambda e: e.scalar_tensor_tensor(out.ap, in0.ap, a, in1.ap, op0, op1),
                rd=rd, wr=[out])

    def copy(self, eng, out, in_):
        if eng == "scalar":
            self.op(eng, lambda e: e.copy(out.ap, in_.ap), rd=[in_], wr=[out])
        else:
            self.op(eng, lambda e: e.tensor_copy(out.ap, in_.ap), rd=[in_], wr=[out])

    def memset(self, eng, out, val):
        self.op(eng, lambda e: e.memset(out.ap, val), rd=[], wr=[out])

    def recip(self, out, in_):
        self.op("vector", lambda e: e.reciprocal(out.ap, in_.ap), rd=[in_], wr=[out])

    def reduce(self, out, in_, op):
        self.op("vector", lambda e: e.tensor_reduce(out.ap, in_.ap, AX.X, op), rd=[in_], wr=[out])

    def scan_add(self, out, ones, data, initial):
        rd = [ones, data]
        a = initial
        if isinstance(initial, T):
            rd.append(initial)
            a = initial.ap
        self.op("vector", lambda e: e.tensor_tensor_scan(out.ap, ones.ap, data.ap, a, ALU.mult, ALU.add),
                rd=rd, wr=[out])

    def vmax8(self, out, in_):
        self.op("vector", lambda e: e.max(out.ap, in_.ap), rd=[in_], wr=[out])


class Arena:
    def __init__(self, k, nbytes):
        self.k = k
        self.nbytes = nbytes
        t = k.stack.enter_context(k.nc.sbuf_tensor("arena", [128, nbytes], U8))
        self.ap = t[:]
        self.off = 0
        self.top = nbytes
        self.n = 0

    def alloc(self, name, shape, dtype, top=False):
        free = int(np.prod(shape[1:]))
        nb = free * DSZ[dtype]
        if top:
            off = (self.top - nb) // 64 * 64
            assert off >= self.off, f"arena overflow (top) at {name}"
            self.top = off
        else:
            off = (self.off + 63) // 64 * 64
            assert off + nb <= self.top, f"arena overflow at {name}: {off}+{nb} > {self.top}"
            self.off = off + nb
        ap = self.ap[0:shape[0], off:off + nb].bitcast(dtype)
        if len(shape) == 3:
            ap = ap.rearrange("p (a b) -> p a b", a=shape[1])
        elif len(shape) == 4:
            ap = ap.rearrange("p (a b c) -> p a b c", a=shape[1], b=shape[2])
        self.n += 1
        return T(ap, Buf(f"{name}_{self.n}"))

    def mark(self):
        return self.off

    def reset(self, m):
        self.off = m


def build_nc(debug=False):
    nc = bass.Bass("TRN2", target_bir_lowering=False)
    st = ExitStack()
    k = K(nc, st)
    A = Arena(k, 207 * 1024)

    def DI(name, shape, dt=F32):
        return k.dram(name, shape, dt, "ExternalInput")

    xo = DI("xo", [2048, 1024]); xp = DI("xp", [2048, 1024])
    cvec = DI("cvec", [1024]); posr = DI("posr", [4096], I32)
    ada_w = DI("ada_w", [1024, 6144]); ada_b = DI("ada_b", [6144])
    n1g = DI("norm1_g", [1024]); n2g = DI("norm2_g", [1024]); fing = DI("final_g", [1024])
    hgg = DI("hg_norm_g", [128]); lbl = DI("lb_logits", [2, 256])
    w_ka = DI("w_ka", [1024, 512]); w_kas = DI("w_kas", [1024, 512])
    w_ki = DI("w_ki", [1024, 128]); w_kis = DI("w_kis", [1024, 128])
    w_va = DI("w_va", [1024, 512])
    w_qa = DI("w_qa", [1024, 512]); w_qas = DI("w_qas", [1024, 512])
    w_qi = DI("w_qi", [1024, 256]); w_qis = DI("w_qis", [1024, 256])
    w_wi = DI("w_wi", [1024, 4])
    w_qb = DI("w_qb", [1024, 256]); w_fb = DI("w_fb", [1024, 256])
    w_ib = DI("w_ib", [1024, 512]); w_gb = DI("w_gb", [1024, 512])
    w_out = DI("w_out", [1024, 1024])
    rw = DI("router_w", [1024, 32]); rb = DI("router_b", [32])
    w1 = DI("moe_w1", [32, 1024, 2048]); b1 = DI("moe_b1", [32, 2048])
    w2 = DI("moe_w2", [32, 1024, 1024]); b2 = DI("moe_b2", [32, 1024])
    c_identb = DI("c_identb", [128, 128], BF16); c_identf = DI("c_identf", [128, 128])
    c_negI = DI("c_negI", [128, 128], BF16); c_tri2 = DI("c_tri2", [128, 128])
    c_diag = DI("c_diag", [128, 128]); c_cols = DI("c_cols", [128, 4])
    yout = k.dram("y", [2048, 1024], F32, "ExternalOutput")
    x1s = k.dram("x1s", [2048, 1024], F32, "Internal")
    XG = k.dram("XG", [32 * 2048, 1024], BF16, "Internal")
    YG = k.dram("YG", [32 * 2048, 1024], F32, "Internal")
    c_ltb = DI("c_ltb", [128, 128], BF16); c_erow = DI("c_erow", [128, 32])
    dbg = {}
    if debug:
        dbg["mixT"] = k.dram("dbg_mixT", [8, 128, 2048], F32, "ExternalOutput")
        dbg["x1"] = k.dram("dbg_x1", [2048, 1024], F32, "ExternalOutput")
        dbg["G"] = k.dram("dbg_G", [128, 16 * 32], F32, "ExternalOutput")

    banks = [k.ps(f"bank{i}", [128, 512], F32) for i in range(8)]

    def wload(dst, src, rows=8, eng="gpsimd"):
        k.dma(eng, dst, src.re("(kc p) n -> p kc n", p=128))

    identb = A.alloc("identb", [128, 128], BF16); k.dma("sync", identb, c_identb)
    identf = A.alloc("identf", [128, 128], F32); k.dma("sync", identf, c_identf)
    negI = A.alloc("negI", [128, 128], BF16); k.dma("sync", negI, c_negI)
    tri2 = A.alloc("tri2", [128, 128], F32); k.dma("sync", tri2, c_tri2)
    diagm = A.alloc("diagm", [128, 128], F32); k.dma("sync", diagm, c_diag)
    ccols = A.alloc("ccols", [128, 4], F32); k.dma("sync", ccols, c_cols)
    invf = ccols[:, 0:1]; sgnc = ccols[:, 1:2]; negb = ccols[:, 2:3]; sflag = ccols[:, 3:4]
    zerob = A.alloc("zerob", [128, 512], BF16); k.memset("vector", zerob, 0.0)
    onesf = A.alloc("onesf", [128, 512], F32); k.memset("vector", onesf, 1.0)
    g1bc = A.alloc("g1bc", [128, 1024], F32)
    g2bc = A.alloc("g2bc", [128, 1024], F32)
    fgbc = A.alloc("fgbc", [128, 1024], F32)
    s2row = A.alloc("s2row", [128, 1024], F32)
    sh2row = A.alloc("sh2row", [128, 1024], F32)
    k.dma("sync", fgbc, T(fing.ap.partition_broadcast(128), fing.buf))
    hgbc = A.alloc("hgbc", [128, 128], F32)
    k.dma("sync", hgbc, T(hgg.ap.partition_broadcast(128), hgg.buf))
    rbbc = A.alloc("rbbc", [128, 32], F32)
    k.dma("sync", rbbc, T(rb.ap.partition_broadcast(128), rb.buf))
    mcols = A.alloc("mcols", [128, 32], F32)
    s1col = A.alloc("s1col", [128, 8], F32); s2col = A.alloc("s2col", [128, 8], F32)
    lbcol = A.alloc("lbcol", [128, 2], F32); omlb = A.alloc("omlb", [128, 2], F32)
    Gall = A.alloc("Gall", [128, 16, 32], F32)
    smalls = Rot([A.alloc(f"sm{i}", [128, 1], F32) for i in range(12)])
    epsc = A.alloc("epsc", [128, 1], F32); k.memset("vector", epsc, EPS)
    small_mark = A.mark()
    mixA = A.alloc("mixA", [128, 4, 2048], BF16)
    persist_mark = A.mark()

    cT = A.alloc("cT", [128, 8], F32)
    k.dma("sync", cT, cvec.re("(j p) -> p j", p=128), allow_slow_non_contiguous=True)
    siluc = A.alloc("siluc", [128, 8], BF16)
    k.act(siluc, cT, AF.Silu)
    modrow = A.alloc("modrow", [1, 6144], F32)
    adab = A.alloc("adab", [1, 6144], F32)
    k.dma("sync", adab, ada_b.re("(o n) -> o n", o=1))
    ones1 = A.alloc("ones1", [1, 128], F32); k.memset("vector", ones1, 1.0)
    aslots = Rot([A.alloc(f"adaw{i}", [128, 8, 512], BF16) for i in range(2)])
    prot = Rot(banks[0:4])
    adaw_v = ada_w.re("(kc p) n -> p kc n", p=128)
    for nb in range(12):
        sl = aslots.next()
        k.dma("gpsimd", sl, adaw_v[:, :, nb * 512:(nb + 1) * 512])
        pb = prot.next()
        for kc in range(8):
            k.mm(pb[0:1, :], siluc[:, kc:kc + 1], sl[:, kc, :], start=(kc == 0), stop=(kc == 7))
        k.tt("vector", modrow[0:1, nb * 512:(nb + 1) * 512], pb[0:1, :], adab[0:1, nb * 512:(nb + 1) * 512], ALU.add)
    for (dst, off) in ((g1bc, 2048), (g2bc, 5120), (sh2row, 3072), (s2row, 4096)):
        for nb in range(2):
            pb = prot.next()
            k.mm(pb, ones1[0:1, :], modrow[0:1, off + nb * 512: off + (nb + 1) * 512])
            k.copy("vector", dst[:, nb * 512:(nb + 1) * 512], pb)
    pb = prot.next()
    for i, off in enumerate((0, 1024, 3072, 4096)):
        for j in range(8):
            k.mm(pb[:, i * 8 + j: i * 8 + j + 1], modrow[0:1, off + j * 128: off + (j + 1) * 128], ones1[0:1, 0:1])
    k.copy("vector", mcols, pb[:, 0:32])
    n2bc = A.alloc("n2bc", [128, 1024], F32)
    k.dma("sync", n2bc, T(n2g.ap.partition_broadcast(128), n2g.buf))
    k.ts("vector", s2row, s2row, 1.0, None, ALU.add)
    k.tt("vector", s2row, s2row, n2bc, ALU.mult)
    gcol = A.alloc("gcol", [128, 16], F32)
    k.dma("sync", gcol[:, 0:8], n1g.re("(j p) -> p j", p=128), allow_slow_non_contiguous=True)
    k.dma("sync", gcol[:, 8:16], n2g.re("(j p) -> p j", p=128), allow_slow_non_contiguous=True)
    tmp8 = A.alloc("tmp8", [128, 8], F32)
    k.ts("vector", tmp8, mcols[:, 8:16], 1.0, None, ALU.add)
    k.tt("vector", s1col, tmp8, gcol[:, 0:8], ALU.mult)
    tmp8b = A.alloc("tmp8b", [128, 8], F32)
    k.ts("vector", tmp8b, mcols[:, 24:32], 1.0, None, ALU.add)
    k.tt("vector", s2col, tmp8b, gcol[:, 8:16], ALU.mult)
    sh1col = mcols[:, 0:8]; sh2col = mcols[:, 16:24]
    lbl_sb = A.alloc("lbl_sb", [128, 2, 2], F32)
    for l in range(2):
        k.dma("sync", lbl_sb[:, l, :], lbl[l, :].re("(c p) -> p c", p=128), allow_slow_non_contiguous=True)
    dl = A.alloc("dl", [128, 2], F32)
    k.tt("vector", dl, lbl_sb[:, 0, :], lbl_sb[:, 1, :], ALU.subtract)
    k.act(lbcol, dl, AF.Sigmoid)
    k.ts("vector", omlb, lbcol, -1.0, 1.0, ALU.mult, ALU.add)
    k.barrier()
    A.reset(persist_mark)

    def make_hT(src, tile0, ntiles, scol, shcol, hT, xslots, tmps, prot):
        sqj, xns, tmpf = tmps
        sbc = scol.unsq(2).bc([128, 8, 128])
        shbc = shcol.unsq(2).bc([128, 8, 128])

        def norm(i):
            xt = xslots.next()
            k.dma("sync", xt, src[(tile0 + i) * 128:(tile0 + i + 1) * 128, :])
            ss = smalls.next(); rs = smalls.next(); rstd = smalls.next()
            k.act(sqj, xt, AF.Square, accum=ss)
            k.act(rs, ss, AF.Sqrt, scale=1.0 / 1024, bias=epsc)
            k.recip(rstd, rs)
            xn = xns[i % 2]
            k.act(xn, xt, AF.Identity, scale=rstd)
            return xn

        def evac(i, xn):
            pb = prot.next()
            pv = pb.bitcast(BF16).re("p (j t) -> p j t", j=8)
            for j in range(8):
                k.tr(pv[:, j, :], xn[:, j * 128:(j + 1) * 128], identb)
            k.tt("vector", tmpf, pv, sbc, ALU.mult)
            k.tt("vector", hT[:, :, i * 128:(i + 1) * 128], tmpf, shbc, ALU.add)

        pend = norm(0)
        for i in range(ntiles):
            nxt = norm(i + 1) if i + 1 < ntiles else None
            evac(i, pend)
            pend = nxt

    def rope_tables(slot0, n, cosg, sing, tmps):
        posi, pf, ang, kf = tmps
        ki32 = posi
        k.dma("sync", posi[:, 0:n], T(posr.ap[slot0:slot0 + n].partition_broadcast(128), posr.buf))
        k.copy("vector", pf[:, 0:n], posi[:, 0:n])
        k.ts("vector", ang[:, 0:n], pf[:, 0:n], invf, None, ALU.mult)
        k.ts("vector", kf[:, 0:n], ang[:, 0:n], 1.0 / (2 * PI), None, ALU.mult)
        k.copy("vector", ki32[:, 0:n], kf[:, 0:n])
        k.copy("vector", kf[:, 0:n], ki32[:, 0:n])
        C1 = 6.28125
        C2 = 2 * PI - C1
        k.stt(ang[:, 0:n], kf[:, 0:n], -C1, ang[:, 0:n], ALU.mult, ALU.add)
        k.stt(ang[:, 0:n], kf[:, 0:n], -C2, ang[:, 0:n], ALU.mult, ALU.add)
        k.ts("vector", ang[:, 0:n], ang[:, 0:n], -PI, PI, ALU.max, ALU.min)
        k.act(sing[:, 0:n], ang[:, 0:n], AF.Sin, scale=sgnc)
        k.ts("vector", pf[:, 0:n], ang[:, 0:n], PI / 2, None, ALU.add)
        k.ts("vector", kf[:, 0:n], pf[:, 0:n], PI, -2 * PI, ALU.is_gt, ALU.mult)
        k.tt("vector", pf[:, 0:n], pf[:, 0:n], kf[:, 0:n], ALU.add)
        k.ts("vector", pf[:, 0:n], pf[:, 0:n], -PI, PI, ALU.max, ALU.min)
        k.act(cosg[:, 0:n], pf[:, 0:n], AF.Sin)

    def proj_fm(dst_fn, wsb, wsw, nchunks, hT, n, cosg, sing, prot, rtmps):
        t1, t2 = rtmps
        for c in range(nchunks):
            pa = prot.next()
            for kc in range(8):
                k.mm(pa[:, 0:n], wsb[:, kc, c * 128:(c + 1) * 128], hT[:, kc, 0:n], start=(kc == 0), stop=(kc == 7))
            if wsw is None:
                k.copy("scalar", dst_fn(c), pa[:, 0:n])
                continue
            pb2 = prot.next()
            for kc in range(8):
                k.mm(pb2[:, 0:n], wsw[:, kc, c * 128:(c + 1) * 128], hT[:, kc, 0:n], start=(kc == 0), stop=(kc == 7))
            k.tt("vector", t1[:, 0:n], pa[:, 0:n], cosg[:, 0:n], ALU.mult)
            k.tt("vector", t2[:, 0:n], pb2[:, 0:n], sing[:, 0:n], ALU.mult)
            dst_fn(c, t1[:, 0:n], t2[:, 0:n])

    kaT = A.alloc("kaT", [128, 4, 4096], BF16)
    kiT = A.alloc("kiT", [128, 4096], BF16)
    va = A.alloc("va", [128, NT_ALL, 8, 65], BF16)
    k.memset("gpsimd", va[:, :, :, 64:65], 1.0)
    kside_mark = A.mark()
    wka = A.alloc("wka", [128, 8, 512], BF16); wload(wka, w_ka)
    wkas = A.alloc("wkas", [128, 8, 512], BF16); wload(wkas, w_kas)
    wki = A.alloc("wki", [128, 8, 128], BF16); wload(wki, w_ki)
    wkis = A.alloc("wkis", [128, 8, 128], BF16); wload(wkis, w_kis)
    wva = A.alloc("wva", [128, 8, 512], BF16); wload(wva, w_va)
    hT = A.alloc("hT", [128, 8, 512], BF16)
    xslots = Rot([A.alloc(f"xs{i}", [128, 1024], F32) for i in range(2)])
    ntmps = (A.alloc("sqj", [128, 1024], BF16), [A.alloc(f"xn{i_}", [128, 1024], BF16) for i_ in range(2)], A.alloc("tmpf", [128, 8, 128], F32))
    cosg = A.alloc("cosg", [128, 512], F32); sing = A.alloc("sing", [128, 512], F32)
    rtm = (A.alloc("posi", [128, 512], I32), A.alloc("pf", [128, 512], F32), A.alloc("ang", [128, 512], F32),
           A.alloc("kf", [128, 512], F32))
    rt12 = (rtm[1], rtm[3])
    prot = Rot(banks)
    for g in range(8):
        src = xp if g < 4 else xo
        rope_tables(g * 512, 512, cosg, sing, rtm)
        make_hT(src, (g % 4) * 4, 4, s1col, sh1col, hT, xslots, ntmps, prot)

        def dst_ka(c, a=None, b=None, g=g):
            k.tt("gpsimd", kaT[:, c, g * 512:(g + 1) * 512], a, b, ALU.add)
        proj_fm(dst_ka, wka, wkas, 4, hT, 512, cosg, sing, prot, rt12)

        def dst_ki(c, a=None, b=None, g=g):
            k.tt("gpsimd", kiT[:, g * 512:(g + 1) * 512], a, b, ALU.add)
        proj_fm(dst_ki, wki, wkis, 1, hT, 512, cosg, sing, prot, rt12)
        for i in range(4):
            pa = prot.next()
            for kc in range(8):
                k.mm(pa, hT[:, kc, i * 128:(i + 1) * 128], wva[:, kc, :], start=(kc == 0), stop=(kc == 7))
            k.copy("scalar", va[:, g * 4 + i, :, 0:64], pa.re("p (h d) -> p h d", h=8))
    k.barrier()
    A.reset(kside_mark)

    qaT = A.alloc("qaT", [128, 4, 2048], BF16)
    qiT = A.alloc("qiT", [128, 2, 2048], BF16)
    wis = A.alloc("wis", [128, 16, 4], F32)
    qside_mark = A.mark()
    wqa = A.alloc("wqa", [128, 8, 512], BF16); wload(wqa, w_qa)
    wqas = A.alloc("wqas", [128, 8, 512], BF16); wload(wqas, w_qas)
    wqi = A.alloc("wqi", [128, 8, 256], BF16); wload(wqi, w_qi)
    wqis = A.alloc("wqis", [128, 8, 256], BF16); wload(wqis, w_qis)
    wwi = A.alloc("wwi", [128, 8, 4], BF16); wload(wwi, w_wi)
    hT = A.alloc("hT", [128, 8, 512], BF16)
    xslots = Rot([A.alloc(f"xs{i}", [128, 1024], F32) for i in range(2)])
    ntmps = (A.alloc("sqj", [128, 1024], BF16), [A.alloc(f"xn{i_}", [128, 1024], BF16) for i_ in range(2)], A.alloc("tmpf", [128, 8, 128], F32))
    cosg = A.alloc("cosg", [128, 512], F32); sing = A.alloc("sing", [128, 512], F32)
    rtm = (A.alloc("posi", [128, 512], I32), A.alloc("pf", [128, 512], F32), A.alloc("ang", [128, 512], F32),
           A.alloc("kf", [128, 512], F32))
    rt12 = (rtm[1], rtm[3])
    for g in range(4):
        rope_tables(2048 + g * 512, 512, cosg, sing, rtm)
        make_hT(xo, g * 4, 4, s1col, sh1col, hT, xslots, ntmps, prot)

        def dst_qa(c, a=None, b=None, g=g):
            k.tt("gpsimd", qaT[:, c, g * 512:(g + 1) * 512], a, b, ALU.add)
        proj_fm(dst_qa, wqa, wqas, 4, hT, 512, cosg, sing, prot, rt12)

        def dst_qi(c, a=None, b=None, g=g):
            k.tt("gpsimd", qiT[:, c, g * 512:(g + 1) * 512], a, b, ALU.add)
        proj_fm(dst_qi, wqi, wqis, 2, hT, 512, cosg, sing, prot, rt12)
        for i in range(4):
            pa = prot.next()
            for kc in range(8):
                k.mm(pa[:, 0:4], hT[:, kc, i * 128:(i + 1) * 128], wwi[:, kc, :], start=(kc == 0), stop=(kc == 7))
            k.ts("vector", wis[:, g * 4 + i, :], pa[:, 0:4], 0.5 * 0.125, None, ALU.mult)
    k.barrier()
    A.reset(qside_mark)

    sc = A.alloc("sc", [128, 4096], F32)
    mbars = Rot([A.alloc(f"mbar{i}", [128, 4096], BF16) for i in range(2)])
    rts = Rot([A.alloc(f"rt{i}", [128, 512], F32) for i in range(4)])
    pTs = Rot([A.alloc(f"pT{i}", [128, 512], BF16) for i in range(3)])
    absw = A.alloc("absw", [128, 4], F32); sgnw = A.alloc("sgnw", [128, 4], F32)
    lo = A.alloc("lo", [128, 1], F32); mid = A.alloc("mid", [128, 1], F32)
    cnt = A.alloc("cnt", [128, 1], F32); dlt = A.alloc("dlt", [128, 1], F32)
    rmax = A.alloc("rmax", [128, 1], F32)
    sga = A.alloc("sga", [128, 1], F32); tcomb = A.alloc("tcomb", [128, 1], F32)
    rden = A.alloc("rden", [128, 8], F32)
    oa = A.alloc("oa", [128, 8, 64], BF16)
    zqs = Rot([A.alloc(f"zq{i}", [128, 4, 2, 128], BF16) for i in range(2)])
    zis = Rot([A.alloc(f"zi{i}", [128, 2, 2, 128], BF16) for i in range(2)])
    for z in zqs.items + zis.items:
        k.memset("gpsimd", z, 0.0)
    sc_rot = Rot(banks[0:2])
    s_rot = Rot(banks[2:4])
    po_rot = Rot([(banks[4], banks[5]), (banks[6], banks[7])])

    def stage_a(n):
        nkt = 16 + n + 1
        nk = nkt * 128
        qs = slice(n * 128, (n + 1) * 128)
        zq = zqs.next(); zi = zis.next(); mbar = mbars.next()
        for hp in range(2):
            ph = slice(hp * 64, (hp + 1) * 64)
            k.copy("gpsimd", zq[ph, :, hp, :], qaT[ph, :, qs])
            k.copy("gpsimd", zi[ph, :, hp, :], qiT[ph, :, qs])
        k.ts("vector", sgnw, wis[:, n, :], -1.0, None, ALU.mult)
        k.tt("vector", absw, wis[:, n, :], sgnw, ALU.max)
        k.ts("vector", sgnw, wis[:, n, :], 0.0, 2.0, ALU.is_ge, ALU.mult)
        k.ts("vector", sgnw, sgnw, -1.0, None, ALU.add)
        for kb in range((nkt + 3) // 4):
            ncols = min(512, nk - kb * 512)
            cs = slice(kb * 512, kb * 512 + ncols)
            for h in range(4):
                c, hp = h // 2, h % 2
                pa = sc_rot.next()
                k.mm(pa[:, 0:ncols], zi[:, c, hp, :], kiT[:, cs])
                rt = rts.next()
                k.act(rt[:, 0:ncols], pa[:, 0:ncols], AF.Relu, scale=absw[:, h:h + 1])
                if h == 0:
                    k.ts("vector", sc[:, cs], rt[:, 0:ncols], sgnw[:, 0:1], None, ALU.mult)
                else:
                    k.stt(sc[:, cs], rt[:, 0:ncols], sgnw[:, h:h + 1], sc[:, cs], ALU.mult, ALU.add)
        k.ts("vector", sc[:, 0:2048], sc[:, 0:2048], negb, None, ALU.add)
        ds_ = slice((nkt - 1) * 128, nkt * 128)
        k.tt("vector", sc[:, ds_], sc[:, ds_], diagm, ALU.add)
        k.reduce(rmax, sc[:, 0:nk], ALU.max)
        k.ts("vector", mid, rmax, -RNG + RNG / 2, None, ALU.add)
        for it in range(NBIS):
            wn = RNG / (2 ** (it + 2))
            k.ts("vector", mbar[:, 0:nk], sc[:, 0:nk], mid, None, ALU.is_gt, op1=ALU.add, accum=cnt)
            k.ts("vector", dlt, cnt, 255.5, 2.0 * wn, ALU.is_ge, ALU.mult)
            k.stt(mid, dlt, -wn, mid, ALU.add, ALU.add)
        k.ts("vector", lo, mid, -RNG / (2 ** (NBIS + 1)), None, ALU.add)
        k.ts("vector", mbar[:, 0:nk], sc[:, 0:nk], lo, None, ALU.is_le)
        return (n, nkt, qs, zq, mbar)

    def stage_b(ctx):
        n, nkt, qs, zq, mbar = ctx
        poA, poB = po_rot.next()
        k.mm(poA[:, 0:260], zerob[:, 0:128], zerob[:, 0:260], start=True, stop=False, skip_group_check=True)
        k.mm(poB[:, 0:260], zerob[:, 0:128], zerob[:, 0:260], start=True, stop=False, skip_group_check=True)
        groups = [(j, gq) for j in range(nkt) for gq in range(2)]

        def logits(j, gq):
            ks_ = slice(j * 128, (j + 1) * 128)
            pS = s_rot.next()
            for hh in range(4):
                h = gq * 4 + hh
                c, hp = h // 2, h % 2
                k.mm(pS[:, hh * 128:(hh + 1) * 128], kaT[:, c, ks_], zq[:, c, hp, :], start=True, stop=False)
                k.mm(pS[:, hh * 128:(hh + 1) * 128], mbar[:, ks_], negI, start=False, stop=True)
            pT = pTs.next()
            k.act(pT, pS, AF.Exp, scale=0.125)
            return pT

        def pv_mm(j, gq, pT):
            po = poA if gq == 0 else poB
            for hh in range(4):
                h = gq * 4 + hh
                k.mm(po[:, hh * 65:(hh + 1) * 65], pT[:, hh * 128:(hh + 1) * 128], va[:, j, h, :],
                     start=False, stop=(j == nkt - 1), skip_group_check=True)

        pend = None
        for (j, gq) in groups:
            pT = logits(j, gq)
            if pend is not None:
                pv_mm(*pend)
            pend = (j, gq, pT)
        pv_mm(*pend)
        for gq, po in enumerate((poA, poB)):
            pv = po[:, 0:260].re("p (h d) -> p h d", h=4)
            k.recip(rden[:, gq * 4:(gq + 1) * 4], pv[:, :, 64])
            k.tt("vector", oa[:, gq * 4:(gq + 1) * 4, :], pv[:, :, 0:64],
                 rden[:, gq * 4:(gq + 1) * 4].unsq(2).bc([128, 4, 64]), ALU.mult)
        pa = s_rot.next()
        pv = pa.bitcast(BF16).re("p (j t) -> p j t", j=8)
        oaf = oa.re("p h d -> p (h d)")
        for c in range(4):
            k.tr(pv[:, c, :], oaf[:, c * 128:(c + 1) * 128], identb)
        k.copy("scalar", mixA[:, :, qs], pv[:, 0:4, :])

    ctx = stage_a(0)
    for n in range(NT_OWN):
        nxt = stage_a(n + 1) if n + 1 < NT_OWN else None
        stage_b(ctx)
        ctx = nxt
    k.barrier()
    A.reset(persist_mark)

    mixB = A.alloc("mixB", [128, 4, 2048], BF16)
    hg_mark = A.mark()
    wqb = A.alloc("wqb", [128, 8, 256], BF16); wload(wqb, w_qb)
    wfb = A.alloc("wfb", [128, 8, 256], BF16); wload(wfb, w_fb)
    wib = A.alloc("wib", [128, 8, 512], BF16); wload(wib, w_ib)
    wgb = A.alloc("wgb", [128, 8, 512], BF16); wload(wgb, w_gb)
    hT = A.alloc("hT", [128, 8, 512], BF16)
    xslots = Rot([A.alloc(f"xs{i}", [128, 1024], F32) for i in range(2)])
    ntmps = (A.alloc("sqj", [128, 1024], BF16), [A.alloc(f"xn{i_}", [128, 1024], BF16) for i_ in range(2)], A.alloc("tmpf", [128, 8, 128], F32))
    vtok_s = [A.alloc(f"vtok{i}", [128, 4, 512], BF16) for i in range(2)]
    sgt_s = [A.alloc(f"sgt{i}", [128, 4, 512], BF16) for i in range(2)]
    sgf = A.alloc("sgf", [128, 512], F32)
    S = A.alloc("S", [128, 2, 128], F32); k.memset("vector", S, 0.0)
    Sbf = [A.alloc(f"Sbf{i}", [128, 2, 128], BF16) for i in range(8)]
    Bext = [A.alloc(f"Bext{c2}", [128, 513], F32) for c2 in range(2)]
    for c2 in range(2):
        k.memset("vector", Bext[c2][:, 0:1], 0.0)
    sig = A.alloc("sig", [128, 512], F32); fT = A.alloc("fT", [128, 512], F32)
    logf = A.alloc("logf", [128, 512], F32); omf = A.alloc("omf", [128, 512], F32)
    qbf = A.alloc("qbf", [128, 512], F32)
    D1 = A.alloc("D1", [128, 8, 64], F32); D3 = A.alloc("D3", [128, 8, 64], F32); D4 = A.alloc("D4", [128, 8, 64], F32)
    E1 = A.alloc("E1", [128, 512], F32); E2 = A.alloc("E2", [128, 512], F32)
    E3 = A.alloc("E3", [128, 512], F32); E4 = A.alloc("E4", [128, 512], F32)
    dd = A.alloc("dd", [128, 8], F32)
    gsets = []
    for si in range(2):
        dec_ = [A.alloc(f"dec{si}_{c2}", [128, 8], F32) for c2 in range(2)]
        qtZ_ = [A.alloc(f"qtZ{si}_{c2}", [128, 2, 512], BF16) for c2 in range(2)]
        qeZ_ = [A.alloc(f"qeZ{si}_{c2}", [128, 2, 512], BF16) for c2 in range(2)]
        ktT_ = [A.alloc(f"ktT{si}_{c2}", [128, 512], BF16) for c2 in range(2)]
        kdT_ = [A.alloc(f"kdT{si}_{c2}", [128, 512], BF16) for c2 in range(2)]
        for c2 in range(2):
            k.memset("gpsimd", qtZ_[c2], 0.0)
            k.memset("gpsimd", qeZ_[c2], 0.0)
        gsets.append((vtok_s[si], sgt_s[si], dec_, qtZ_, qeZ_, ktT_, kdT_))
    kdZ = [[A.alloc(f"kdZ{c2}_{i}", [128, 2, 128], BF16) for i in range(2)] for c2 in range(2)]
    for c2 in range(2):
        for i in range(2):
            k.memset("gpsimd", kdZ[c2][i], 0.0)
    attS = [A.alloc(f"attS{i}", [128, 4, 128], BF16) for i in range(2)]
    for i in range(2):
        k.memset("gpsimd", attS[i], 0.0)
    ssq = A.alloc("ssq", [128, 4], F32); rs4 = A.alloc("rs4", [128, 4], F32); rstd4 = A.alloc("rstd4", [128, 4], F32)
    sqj2 = A.alloc("sqj2", [128, 128], BF16)
    ob1 = A.alloc("ob1", [128, 4, 128], F32); ob2 = A.alloc("ob2", [128, 4, 128], F32)
    obb = A.alloc("obb", [128, 512], BF16)
    prot = Rot(banks[0:4])
    kv_rot = Rot(banks[4:6])
    at_rot = Rot([(banks[6], banks[7])])
    tri2b = tri2.unsq(1).bc([128, 2, 128])
    def hg_front(g):
        own = g >= 4
        vtok, sgt, dec, qtZ, qeZ, ktT, kdT = gsets[g % 2]
        src = xo if own else xp
        make_hT(src, (g % 4) * 4, 4, s1col, sh1col, hT, xslots, ntmps, prot)
        for i in range(4):
            pa = prot.next()
            for kc in range(8):
                k.mm(pa, hT[:, kc, i * 128:(i + 1) * 128], wib[:, kc, :], start=(kc == 0), stop=(kc == 7))
            k.copy("scalar", vtok[:, i, :], pa)
            if own:
                pa = prot.next()
                for kc in range(8):
                    k.mm(pa, hT[:, kc, i * 128:(i + 1) * 128], wgb[:, kc, :], start=(kc == 0), stop=(kc == 7))
                k.act(sgf, pa, AF.Sigmoid)
                k.tt("vector", sgt[:, i, :], sgf, pa, ALU.mult)
        for c2 in range(2):
            pa = prot.next()
            for kc in range(8):
                k.mm(pa, wfb[:, kc, c2 * 128:(c2 + 1) * 128], hT[:, kc, :], start=(kc == 0), stop=(kc == 7))
            k.act(sig, pa, AF.Sigmoid)
            k.ts("vector", fT, sig, omlb[:, c2:c2 + 1], lbcol[:, c2:c2 + 1], ALU.mult, ALU.add)
            k.act(logf, fT, AF.Ln)
            k.ts("vector", omf, fT, -1.0, 1.0, ALU.mult, ALU.add)
            Bx = Bext[c2]
            k.scan_add(Bx[:, 1:513], onesf, logf, Bx[:, 0:1])
            Bg = Bx[:, 1:513].re("p (c t) -> p c t", c=8)
            Bprev = Bx[:, 0:512].re("p (c t) -> p c t", c=8)[:, :, 0:1]
            Blast = Bg[:, :, 63:64]
            Bmid = Bg[:, :, 31:32]
            k.tt("vector", D4, Blast.bc([128, 8, 64]), Bg, ALU.subtract)
            k.act(E4, D4.re("p c t -> p (c t)"), AF.Exp)
            k.tt("vector", kdT[c2], omf, E4, ALU.mult)
            k.tt("vector", dd, Blast.re("p c o -> p (c o)"), Bprev.re("p c o -> p (c o)"), ALU.subtract)
            k.act(dec[c2], dd, AF.Exp)
            if own:
                pq = prot.next()
                for kc in range(8):
                    k.mm(pq, wqb[:, kc, c2 * 128:(c2 + 1) * 128], hT[:, kc, :], start=(kc == 0), stop=(kc == 7))
                k.copy("scalar", qbf, pq)
                k.tt("vector", D1, Bg, Bmid.bc([128, 8, 64]), ALU.subtract)
                k.act(E1, D1.re("p c t -> p (c t)"), AF.Exp)
                k.act(E2, D1.re("p c t -> p (c t)"), AF.Exp, scale=-1.0)
                k.tt("vector", D3, Bg, Bprev.bc([128, 8, 64]), ALU.subtract)
                k.act(E3, D3.re("p c t -> p (c t)"), AF.Exp)
                k.tt("vector", ktT[c2], omf, E2, ALU.mult)
                for hp in range(2):
                    ph = slice(hp * 64, (hp + 1) * 64)
                    k.tt("vector", qtZ[c2][ph, hp, :], qbf[ph, :], E1[ph, :], ALU.mult)
                    k.tt("vector", qeZ[c2][ph, hp, :], qbf[ph, :], E3[ph, :], ALU.mult)
            k.copy("vector", Bx[:, 0:1], Bx[:, 512:513])

    def hg_back(g):
        own = g >= 4
        vtok, sgt, dec, qtZ, qeZ, ktT, kdT = gsets[g % 2]
        for i in range(4):
            ts_ = slice(i * 128, (i + 1) * 128)
            kz = []
            for c2 in range(2):
                pa = prot.next()
                pv = pa.bitcast(BF16)
                k.tr(pv[:, 0:128], kdT[c2][:, ts_], identb)
                kzz = kdZ[c2][i % 2]
                for cp in range(2):
                    k.copy("scalar", kzz[cp * 64:(cp + 1) * 64, cp, :], pv[cp * 64:(cp + 1) * 64, 0:128])
                kz.append(kzz)
            if own:
                pA0, pA1 = at_rot.next()
                for cp in range(2):
                    cs_ = slice(i * 128 + cp * 64, i * 128 + (cp + 1) * 64)
                    for h in range(4):
                        c2, hp = h // 2, h % 2
                        pbk = pA0 if hp == 0 else pA1
                        k.mm(pbk[cp * 64:(cp + 1) * 64, c2 * 64:(c2 + 1) * 64], ktT[c2][:, cs_], qtZ[c2][:, hp, cs_])
                aS = attS[i % 2]
                for hp, pbk in enumerate((pA0, pA1)):
                    for c2 in range(2):
                        h = 2 * c2 + hp
                        for cp in range(2):
                            ph = slice(cp * 64, (cp + 1) * 64)
                            k.tt("vector", aS[ph, h, cp * 64:(cp + 1) * 64], pbk[ph, c2 * 64:(c2 + 1) * 64],
                                 tri2[ph, cp * 64:(cp + 1) * 64], ALU.mult)
            for cp in range(2):
                cidx = i * 2 + cp
                if own:
                    k.copy("scalar", Sbf[cidx], S)
                pkv = kv_rot.next()
                for h in range(4):
                    c2, hp = h // 2, h % 2
                    k.mm(pkv[hp * 64:(hp + 1) * 64, c2 * 128:(c2 + 1) * 128], kz[c2][:, cp, hp * 64:(hp + 1) * 64],
                         vtok[:, i, h * 128:(h + 1) * 128])
                for c2 in range(2):
                    k.stt(S[:, c2, :], S[:, c2, :], dec[c2][:, cidx:cidx + 1], pkv[:, c2 * 128:(c2 + 1) * 128], ALU.mult, ALU.add)
                if g == 3 and cidx == 7:
                    k.ts("vector", S.re("p a b -> p (a b)"), S.re("p a b -> p (a b)"), sflag, None, ALU.mult)
            if own:
                po = prot.next()
                for h in range(4):
                    c2, hp = h // 2, h % 2
                    k.mm(po[:, h * 128:(h + 1) * 128], aS[:, h, :], vtok[:, i, h * 128:(h + 1) * 128], start=True, stop=False)
                    for cp in range(2):
                        cidx = i * 2 + cp
                        cs_ = slice(i * 128 + cp * 64, i * 128 + (cp + 1) * 64)
                        k.mm(po[cp * 64:(cp + 1) * 64, h * 128:(h + 1) * 128], qeZ[c2][:, hp, cs_], Sbf[cidx][:, c2, :],
                             start=False, stop=True)
                pov = po.re("p (h e) -> p h e", h=4)
                for h in range(4):
                    k.act(sqj2, po[:, h * 128:(h + 1) * 128], AF.Square, accum=ssq[:, h:h + 1])
                k.act(rs4, ssq, AF.Sqrt, scale=1.0 / 128, bias=epsc)
                k.recip(rstd4, rs4)
                k.tt("vector", ob1, pov, rstd4.unsq(2).bc([128, 4, 128]), ALU.mult)
                k.tt("vector", ob2, ob1, hgbc.unsq(1).bc([128, 4, 128]), ALU.mult)
                k.tt("vector", obb, ob2.re("p h e -> p (h e)"), sgt[:, i, :], ALU.mult)
                pa = prot.next()
                pv = pa.bitcast(BF16).re("p (j t) -> p j t", j=8)
                for c in range(4):
                    k.tr(pv[:, c, :], obb[:, c * 128:(c + 1) * 128], identb)
                tt0 = (g - 4) * 512 + i * 128
                k.copy("scalar", mixB[:, :, tt0:tt0 + 128], pv[:, 0:4, :])

    hg_front(0)
    for g in range(8):
        if g + 1 < 8:
            hg_front(g + 1)
        hg_back(g)
    k.barrier()
    A.reset(hg_mark)

    idxall = A.alloc("idxall", [128, 16, 4], I32, top=True)
    gkall = A.alloc("gkall", [128, 16, 4], F32, top=True)
    nblk_i = A.alloc("nblk_i", [128, 32], I32, top=True)
    p5_mark = A.mark()
    wo = A.alloc("wo", [128, 8, 1024], BF16); wload(wo, w_out)
    rwf = A.alloc("rwf", [128, 8, 32], F32)
    k.dma("sync", rwf, rw.re("(kc p) e -> p kc e", p=128))
    LTb = A.alloc("LTb", [128, 128], BF16); k.dma("sync", LTb, c_ltb)
    onesb = A.alloc("onesb", [128, 128], BF16); k.memset("vector", onesb, 1.0)
    erow = A.alloc("erow", [128, 32], F32); k.dma("sync", erow, c_erow)
    carry = A.alloc("carry", [128, 32], F32); k.memset("vector", carry, 0.0)
    xslots = Rot([A.alloc(f"xs{i}", [128, 1024], F32) for i in range(2)])
    x1r = Rot([A.alloc(f"x1_{i}", [128, 1024], F32) for i in range(2)])
    h2ts = Rot([A.alloc(f"h2tok{i}", [128, 1024], BF16) for i in range(2)])
    ytmp = A.alloc("ytmp", [128, 1024], F32)
    sqj = A.alloc("sqj", [128, 1024], BF16)
    xnf = A.alloc("xnf", [128, 1024], F32)
    h2f = A.alloc("h2f", [128, 8, 128], F32); h2ft = A.alloc("h2ft", [128, 8, 128], F32)
    lg = A.alloc("lg", [128, 32], F32); m8 = A.alloc("m8", [128, 8], F32)
    msk = A.alloc("msk", [128, 32], F32); ex = A.alloc("ex", [128, 32], F32)
    mskb = A.alloc("mskb", [128, 32], BF16)
    rank = A.alloc("rank", [128, 32], F32); keyt = A.alloc("keyt", [128, 32], F32)
    ek8 = A.alloc("ek8", [128, 8], F32); oh = A.alloc("oh", [128, 32], F32); ohr = A.alloc("ohr", [128, 32], F32)
    rk = A.alloc("rk", [128, 1], F32); slf = A.alloc("slf", [128, 1], F32)
    nmax = A.alloc("nmax", [128, 1], F32); zs = A.alloc("zs", [128, 1], F32); rz = A.alloc("rz", [128, 1], F32)
    y_rot = Rot([(banks[0], banks[1]), (banks[2], banks[3])])
    t_rot = Rot([(banks[4], banks[5]), (banks[6], banks[7])])
    s2bc = s2col.unsq(2).bc([128, 8, 128]); sh2bc = sh2col.unsq(2).bc([128, 8, 128])
    def p5_front(i):
        lg = lgs.next()
        ts_ = slice(i * 128, (i + 1) * 128)
        xt = xslots.next()
        k.dma("sync", xt, xo[ts_, :])
        py = y_rot.next()
        for nb in range(2):
            for kc in range(8):
                mixsrc = mixA if kc < 4 else mixB
                k.mm(py[nb], mixsrc[:, kc % 4, ts_], wo[:, kc, nb * 512:(nb + 1) * 512], start=(kc == 0), stop=(kc == 7))
        x1 = x1r.next()
        for nb in range(2):
            ns_ = slice(nb * 512, (nb + 1) * 512)
            k.tt("vector", ytmp[:, ns_], py[nb], g1bc[:, ns_], ALU.mult)
            k.tt("vector", x1[:, ns_], ytmp[:, ns_], xt[:, ns_], ALU.add)
        k.dma("sync", x1s[ts_, :], x1, chan=x1.buf)
        if debug:
            k.dma("sync", dbg["x1"][ts_, :], x1, chan=x1.buf)
        ss = smalls.next(); rs = smalls.next(); rstd = smalls.next()
        k.act(sqj, x1, AF.Square, accum=ss)
        k.act(rs, ss, AF.Sqrt, scale=1.0 / 1024, bias=epsc)
        k.recip(rstd, rs)
        k.act(xnf, x1, AF.Identity, scale=rstd)
        h2tok = h2ts.next()
        k.tt("vector", ytmp, xnf, s2row, ALU.mult)
        k.tt("vector", h2tok, ytmp, sh2row, ALU.add)
        pt = t_rot.next()
        for j in range(8):
            k.tr(pt[j // 4][:, (j % 4) * 128:(j % 4 + 1) * 128], xnf[:, j * 128:(j + 1) * 128], identf)
        for hb in range(2):
            pv = pt[hb].re("p (j t) -> p j t", j=4)
            k.tt("vector", h2ft[:, hb * 4:(hb + 1) * 4, :], pv, s2bc[:, hb * 4:(hb + 1) * 4, :], ALU.mult)
        k.tt("vector", h2f, h2ft, sh2bc, ALU.add)
        pl = y_rot.next()[0]
        for kc in range(8):
            k.mm(pl[:, 0:32], h2f[:, kc, :], rwf[:, kc, :], start=(kc == 0), stop=(kc == 7))
        k.tt("vector", lg, pl[:, 0:32], rbbc, ALU.add)
        return (i, lg, h2tok)

    def p5_back(ctx):
        i, lg, h2tok = ctx
        k.vmax8(m8, lg)
        k.ts("vector", msk, lg, m8[:, 3:4], None, ALU.is_ge)
        k.ts("vector", nmax, m8[:, 0:1], -1.0, None, ALU.mult)
        k.act(ex, lg, AF.Exp, bias=nmax)
        k.tt("vector", ex, ex, msk, ALU.mult)
        k.reduce(zs, ex, ALU.add)
        k.recip(rz, zs)
        k.ts("vector", Gall[:, i, :], ex, rz, None, ALU.mult)
        k.copy("vector", mskb, msk)
        pr = y_rot.next()[1]
        k.mm(pr[:, 0:32], LTb, mskb)
        k.mm(pr[:, 32:64], onesb, mskb)
        k.tt("vector", rank, pr[:, 0:32], carry, ALU.add)
        k.tt("vector", carry, carry, pr[:, 32:64], ALU.add)
        k.tt("vector", keyt, msk, erow, ALU.mult)
        k.vmax8(ek8, keyt)
        for kk in range(4):
            k.ts("vector", oh, erow, ek8[:, kk:kk + 1], None, ALU.is_equal)
            k.tt("vector", ohr, oh, rank, ALU.mult)
            k.reduce(rk, ohr, ALU.add)
            k.tt("vector", ohr, oh, Gall[:, i, :], ALU.mult)
            k.reduce(gkall[:, i, kk:kk + 1], ohr, ALU.add)
            k.ts("vector", slf, ek8[:, kk:kk + 1], -1.0, 2048.0, ALU.add, ALU.mult)
            k.tt("vector", slf, slf, rk, ALU.add)
            ix = T(idxall.ap[:, i, kk:kk + 1], Buf(f"idx_{i}_{kk}"))
            idxT[(i, kk)] = ix
            k.copy("vector", ix, slf)
            k.idma(XG, h2tok, ix, scatter=True, chan=h2tok.buf)

    idxT = {}
    lgs = Rot([A.alloc(f"lg{i}", [128, 32], F32) for i in range(2)])
    ctx5 = p5_front(0)
    for i in range(NT_OWN):
        nxt5 = p5_front(i + 1) if i + 1 < NT_OWN else None
        p5_back(ctx5)
        ctx5 = nxt5
    k.ts("vector", rank, carry, 63.5, 1.0 / 128, ALU.add, ALU.mult)
    k.copy("vector", nblk_i, rank)
    if debug:
        k.dma("sync", dbg["G"], Gall.re("p a b -> p (a b)"), chan=Gall.buf)
    k.barrier()
    A.reset(small_mark)

    b1T = A.alloc("b1T", [128, 16, 32], F32)
    b1_mark = A.mark()
    b1sb = A.alloc("b1sb", [32, 2048], F32); k.dma("sync", b1sb, b1)
    pb = banks[0]
    for c in range(16):
        k.tr(pb[:, c * 32:(c + 1) * 32], b1sb[0:32, c * 128:(c + 1) * 128], identf[0:32, 0:32])
    k.copy("vector", b1T, pb.re("p (c e) -> p c e", c=16))
    k.barrier()
    A.reset(b1_mark)
    p6_mark = A.mark()
    w1b = A.alloc("w1b", [128, 8, 2048], BF16)
    w2b = A.alloc("w2b", [128, 8, 1024], BF16)
    stg1 = [A.alloc(f"stg1_{p}", [128, 2048], F32) for p in range(8)]
    stg2 = [A.alloc(f"stg2_{j}", [128, 2, 1024], F32) for j in range(4)]
    xbs = Rot([A.alloc(f"xb{i}", [128, 1024], BF16) for i in range(3)])
    xets = Rot([A.alloc(f"xet{i}", [128, 8, 128], BF16) for i in range(2)])
    actTs = Rot([A.alloc(f"actT{i}", [128, 8, 128], BF16) for i in range(2)])
    ysbs = Rot([A.alloc(f"ysb{i}", [128, 1024], F32) for i in range(2)])
    gts = Rot([A.alloc(f"gt{i}", [128, 4, 128], F32) for i in range(1)])
    lts = Rot([A.alloc(f"lt{i}", [128, 4, 128], F32) for i in range(1)])
    sts = Rot([A.alloc(f"st{i}", [128, 4, 128], F32) for i in range(1)])
    gss = Rot([A.alloc(f"gs{i}", [128, 4, 128], F32) for i in range(1)])
    ptr_bank = banks[0]
    mm1_banks = banks[1:5]
    y_banks = (banks[5], banks[6])

    def piece_dma(e, p):
        if p < 8:
            k.dma("sync", stg1[p], w1[e][p * 128:(p + 1) * 128, :])
        else:
            j = p - 8
            k.dma("sync", stg2[j], w2[e][j * 256:(j + 1) * 256, :].re("(a p) n -> p a n", p=128))

    def piece_cast(p):
        dst, src = (w1b[:, p, :], stg1[p]) if p < 8 else (w2b[:, 2 * (p - 8):2 * (p - 8) + 2, :], stg2[p - 8])
        k.copy("vector", dst, src)

    for p in range(12):
        piece_dma(0, p)
    for p in range(12):
        piece_cast(p)
        piece_dma(1, p)
    key_next = k.val_load(nblk_i[0:1, 0:1])
    xb_first = xbs.next()
    k.dma("gpsimd", xb_first, XG[0:128, :])
    for e in range(32):
        key = key_next
        xb_next = xb_first
        for blk in range(16):
            k.cond_begin(key, blk)
            r0 = e * 2048 + blk * 128
            xb = xb_next
            if blk + 1 < 16:
                xb_next = xbs.next()
                k.dma("gpsimd", xb_next, XG[r0 + 128:r0 + 256, :])
            pv = ptr_bank.bitcast(BF16).re("p (j t) -> p j t", j=8)
            for j in range(8):
                k.tr(pv[:, j, :], xb[:, j * 128:(j + 1) * 128], identb)
            xet = xets.next()
            k.copy("scalar", xet, pv)
            for gp in range(2):
                for g4 in (gp, 2 + gp):
                    pbk = mm1_banks[g4]
                    for q in range(4):
                        fc = g4 * 4 + q
                        for kc in range(8):
                            k.mm(pbk[:, q * 128:(q + 1) * 128], w1b[:, kc, fc * 128:(fc + 1) * 128], xet[:, kc, :],
                                 start=(kc == 0), stop=(kc == 7))
            actT = actTs.next()
            ysb = ysbs.next()
            for gp in range(2):
                pg = mm1_banks[gp].re("p (q c) -> p q c", q=4)
                pl_ = mm1_banks[2 + gp].re("p (q c) -> p q c", q=4)
                bg = b1T[:, gp * 4:(gp + 1) * 4, e:e + 1].bc([128, 4, 128])
                bl = b1T[:, 8 + gp * 4:8 + (gp + 1) * 4, e:e + 1].bc([128, 4, 128])
                gt_ = gts.next(); lt_ = lts.next(); st_ = sts.next(); gs_ = gss.next()
                k.tt("vector", gt_, pg, bg, ALU.add)
                k.ts("vector", gt_, gt_, 7.0, None, ALU.min)
                k.act(st_, gt_, AF.Sigmoid, scale=1.702)
                k.tt("vector", lt_, pl_, bl, ALU.add)
                k.ts("vector", lt_, lt_, 7.0, -7.0, ALU.min, ALU.max)
                k.tt("vector", gs_, gt_, st_, ALU.mult)
                k.stt(actT[:, gp * 4:(gp + 1) * 4, :], lt_, 1.0, gs_, ALU.add, ALU.mult)
                for nb in range(2):
                    for f in range(gp * 4, gp * 4 + 4):
                        k.mm(y_banks[nb], actT[:, f, :], w2b[:, f, nb * 512:(nb + 1) * 512], start=(f == 0), stop=(f == 7))
            k.copy("scalar", ysb[:, 0:512], y_banks[0])
            k.copy("vector", ysb[:, 512:1024], y_banks[1])
            k.dma("gpsimd", YG[r0:r0 + 128, :], ysb, chan=ysb.buf)
        for blk in range(16):
            k.cond_end()
        if e + 1 < 32:
            key_next = k.val_load(nblk_i[0:1, e + 1:e + 2])
            xb_first = xbs.next()
            k.dma("gpsimd", xb_first, XG[(e + 1) * 2048:(e + 1) * 2048 + 128, :])
            for p in range(12):
                piece_cast(p)
                if e + 2 < 32:
                    piece_dma(e + 2, p)
    k.barrier()
    A.reset(p6_mark)

    GT = A.alloc("GT", [32, 128], F32)
    b2sb = A.alloc("b2sb", [32, 1024], F32); k.dma("sync", b2sb, b2)
    accs = Rot([A.alloc(f"acc{i}", [128, 1024], F32) for i in range(2)])
    ygs = Rot([A.alloc(f"yg{i}", [128, 1024], F32) for i in range(8)])
    xslots = Rot([A.alloc(f"xs{i}", [128, 1024], F32) for i in range(2)])
    fo = Rot([A.alloc(f"fo{i}", [128, 1024], F32) for i in range(2)])
    sqj = A.alloc("sqj", [128, 1024], BF16)
    pg_rot = Rot(banks[0:2])
    y_rot = Rot([(banks[4], banks[5]), (banks[6], banks[7])])
    for ti in range(NT_OWN):
        ts_ = slice(ti * 128, (ti + 1) * 128)
        acc = accs.next()
        pgt = pg_rot.next()
        k.tr(pgt[0:32, 0:128], Gall[:, ti, :], identf)
        k.copy("vector", GT, pgt[0:32, 0:128])
        py = y_rot.next()
        for nb in range(2):
            k.mm(py[nb], GT[0:32, :], b2sb[0:32, nb * 512:(nb + 1) * 512])
            k.copy("scalar", acc[:, nb * 512:(nb + 1) * 512], py[nb])
        for kk in range(4):
            yg = ygs.next()
            k.idma(yg, YG, idxT[(ti, kk)], scatter=False, chan=yg.buf)
            k.stt(acc, yg, gkall[:, ti, kk:kk + 1], acc, ALU.mult, ALU.add)
        xt = xslots.next()
        k.dma("sync", xt, x1s[ts_, :])
        k.tt("vector", acc, acc, g2bc, ALU.mult)
        k.tt("vector", xt, xt, acc, ALU.add)
        ss = smalls.next(); rs = smalls.next(); rstd = smalls.next()
        k.act(sqj, xt, AF.Square, accum=ss)
        k.act(rs, ss, AF.Sqrt, scale=1.0 / 1024, bias=epsc)
        k.recip(rstd, rs)
        ot = fo.next()
        k.stt(ot, xt, rstd, fgbc, ALU.mult, ALU.mult)
        k.dma("sync", yout[ts_, :], ot, chan=ot.buf)
    k.final_wait("sync")
    k.emit()
    st.close()
    return nc, k


SPL = np.cumsum([0, 512, 512, 512, 256, 64, 4, 256, 256, 512, 512])


def _swap_perm(ncols):
    p = np.arange(ncols)
    for h0 in range(0, ncols, 64):
        p[h0:h0 + 8] = np.arange(h0 + 8, h0 + 16)
        p[h0 + 8:h0 + 16] = np.arange(h0, h0 + 8)
    return p


def _consts(half):
    identf = np.eye(128, dtype=np.float32)
    identb = identf.astype(ml_dtypes.bfloat16)
    negI = (-1024.0 * identf).astype(ml_dtypes.bfloat16)
    s = np.arange(128)[:, None]
    t = np.arange(128)[None, :]
    tri2 = (((s // 64) == (t // 64)) & ((s % 64) <= (t % 64))).astype(np.float32)
    diag = np.where((t // 64) <= (s // 64), 0.0, NEG).astype(np.float32)
    cols = np.zeros((128, 4), np.float32)
    inv_freq = (500000.0 ** (-(np.arange(0, 16, 2, dtype=np.float32) / 16))).astype(np.float32)
    for p in range(128):
        d = p % 64
        if d < 16:
            cols[p, 0] = inv_freq[d % 8]
            cols[p, 1] = -1.0 if d < 8 else 1.0
    cols[:, 2] = 0.0 if half == 1 else NEG
    cols[:, 3] = float(half)
    ltb = (s < t).astype(np.float32).astype(ml_dtypes.bfloat16)
    erow = np.broadcast_to(np.arange(1, 33, dtype=np.float32)[None, :], (128, 32)).copy()
    return {"c_ltb": ltb, "c_erow": erow, "c_identb": identb, "c_identf": identf, "c_negI": negI, "c_tri2": tri2, "c_diag": diag, "c_cols": cols}


_CACHE = {}


def kernel(x, c, positions, ada_w, ada_b, norm1_g, w_in, hg_norm_g, lb_logits, w_out, norm2_g,
           router_w, router_b, moe_w1, moe_b1, moe_w2, moe_b2, final_g, _debug=False):
    f = lambda a: np.ascontiguousarray(np.asarray(a, dtype=np.float32))
    x = f(x); c = f(c); positions = np.ascontiguousarray(np.asarray(positions, dtype=np.int32))
    w_in0 = f(w_in)[0]
    parts = [w_in0[:, SPL[i]:SPL[i + 1]] for i in range(10)]
    qa, ka, va, qi, ki, wi, qb, fb, ib, gb = parts
    ki2 = np.concatenate([ki, ki], axis=1)
    shared = {
        "ada_w": f(ada_w)[0], "ada_b": f(ada_b)[0], "norm1_g": f(norm1_g)[0], "norm2_g": f(norm2_g)[0],
        "final_g": f(final_g), "hg_norm_g": f(hg_norm_g)[0], "lb_logits": f(lb_logits),
        "w_ka": f(ka), "w_kas": f(ka[:, _swap_perm(512)]), "w_ki": f(ki2), "w_kis": f(ki2[:, _swap_perm(128)]),
        "w_va": f(va), "w_qa": f(qa), "w_qas": f(qa[:, _swap_perm(512)]),
        "w_qi": f(qi), "w_qis": f(qi[:, _swap_perm(256)]), "w_wi": f(wi),
        "w_qb": f(qb), "w_fb": f(fb), "w_ib": f(ib), "w_gb": f(gb),
        "w_out": f(w_out)[0], "router_w": f(router_w)[0], "router_b": f(router_b)[0],
        "moe_w1": f(moe_w1)[0], "moe_b1": f(moe_b1)[0], "moe_w2": f(moe_w2)[0], "moe_b2": f(moe_b2)[0],
    }
    in_maps = []
    for j in range(8):
        b, half = j // 2, j % 2
        m = dict(shared)
        m["xo"] = np.ascontiguousarray(x[b, half * 2048:(half + 1) * 2048])
        m["xp"] = np.ascontiguousarray(x[b, 0:2048])
        m["cvec"] = np.ascontiguousarray(c[b])
        m["posr"] = np.ascontiguousarray(np.concatenate([positions[b, 0:2048], positions[b, half * 2048:(half + 1) * 2048]]))
        m.update(_consts(half))
        in_maps.append(m)
    key = bool(_debug)
    if key not in _CACHE:
        _CACHE[key] = build_nc(debug=key)[0]
    nc = _CACHE[key]
    res = run_bass_kernel_spmd(nc, in_maps, core_ids=list(range(8)))
    out = np.empty((4, 4096, 1024), np.float32)
    for j in range(8):
        b, half = j // 2, j % 2
        out[b, half * 2048:(half + 1) * 2048] = res.results[j]["y"]
    if _debug:
        return out, res.results
    return out
```

```python
from contextlib import ExitStack
import numpy as np
import ml_dtypes
import concourse.bass as bass
import concourse.mybir as mybir
from concourse.bass_utils import run_bass_kernel_spmd

F32 = mybir.dt.float32
BF16 = mybir.dt.bfloat16
I32 = mybir.dt.int32
U8 = mybir.dt.uint8
AF = mybir.ActivationFunctionType
ALU = mybir.AluOpType
AX = mybir.AxisListType
DSZ = {F32: 4, BF16: 2, I32: 4, U8: 1}

ENGS = ["tensor", "vector", "scalar", "gpsimd", "sync"]
SEM_LIMIT = 30000
EPS = 1e-6
PI = float(np.pi)
NT_OWN = 16
NT_ALL = 32
RNG = 32.0
NBIS = 13
NEG = -1.0e30


class Buf:
    __slots__ = ("name", "w", "r", "dsem", "dcount")

    def __init__(self, name):
        self.name = name
        self.w = {}
        self.r = {}
        self.dsem = None
        self.dcount = 0


class T:
    __slots__ = ("ap", "buf")

    def __init__(self, ap, buf):
        self.ap = ap
        self.buf = buf

    def __getitem__(self, key):
        return T(self.ap[key], self.buf)

    def bitcast(self, dt):
        return T(self.ap.bitcast(dt), self.buf)

    def re(self, pat, **kw):
        return T(self.ap.rearrange(pat, **kw), self.buf)

    def bc(self, shape):
        return T(self.ap.to_broadcast(list(shape)), self.buf)

    def unsq(self, ax):
        return T(self.ap.unsqueeze(ax), self.buf)


def _bufs(ts):
    out = []
    for t in ts:
        if t is None:
            continue
        b = t.buf if isinstance(t, T) else t
        if b is not None and b not in out:
            out.append(b)
    return out


class Rot:
    def __init__(self, items):
        self.items = list(items)
        self.i = 0

    def next(self):
        t = self.items[self.i % len(self.items)]
        self.i += 1
        return t


class K:
    def __init__(self, nc, stack):
        self.nc = nc
        self.stack = stack
        self.streams = {e: [] for e in ENGS}
        self.esem = {}
        self.ecount = {e: 0 for e in ENGS}
        self.known = {e: {} for e in ENGS}
        self.nsem = 0
        for e in ENGS:
            self.esem[e] = self.new_sem("e_" + e)
        self.dma_bufs = []
        self.n_ops = 0
        self.rstack = []

    def new_sem(self, name):
        self.nsem += 1
        return self.stack.enter_context(self.nc.semaphore(f"{name}_{self.nsem}"))

    def _need(self, eng, tokens):
        waits = []
        kn = self.known[eng]
        for sem, val in tokens.items():
            if kn.get(sem, 0) < val:
                kn[sem] = val
                waits.append((sem, val))
        return waits

    def _deps(self, eng, rd, wr, skip_waw=None):
        tokens = {}
        mysem = self.esem[eng]

        def add(d, is_raw):
            for sem, val in d.items():
                if sem is mysem and eng == "tensor":
                    continue
                if (not is_raw) and skip_waw is not None and sem is skip_waw:
                    continue
                if tokens.get(sem, 0) < val:
                    tokens[sem] = val
        for b in rd:
            add(b.w, True)
        for b in wr:
            add(b.w, False)
            add(b.r, False)
        self._note_region(eng, tokens)
        return self._need(eng, tokens)

    def _note_region(self, eng, tokens):
        for rg in self.rstack:
            ext = rg["ext"][eng]
            for sem, val in tokens.items():
                if val <= rg["start"].get(sem, 0):
                    if ext.get(sem, 0) < val:
                        ext[sem] = val

    def _note_inc(self, eng, sem, inc):
        for rg in self.rstack:
            d = rg["incs"][eng]
            d[sem] = d.get(sem, 0) + inc

    def val_load(self, t):
        self.nvals = getattr(self, "nvals", 0) + 1
        key = self.nvals
        for e in ENGS:
            waits = self._deps(e, _bufs([t]), [])
            self.streams[e].append(("load", waits, key, t.ap))
        return key

    def cond_begin(self, key, thr):
        if not self.rstack:
            for e in ENGS:
                if self.ecount[e] >= SEM_LIMIT - 6000:
                    self.esem[e] = self.new_sem("e_" + e)
                    self.ecount[e] = 0
        start = {}
        for e in ENGS:
            start[self.esem[e]] = self.ecount[e]
        for b in self.dma_bufs:
            start[b.dsem] = b.dcount
        rg = {"key": key, "thr": thr, "start": start,
              "known0": {e: dict(self.known[e]) for e in ENGS},
              "ext": {e: {} for e in ENGS}, "incs": {e: {} for e in ENGS}}
        self.rstack.append(rg)
        for e in ENGS:
            self.streams[e].append(("begin", rg))

    def cond_end(self):
        rg = self.rstack.pop()
        for e in ENGS:
            kn = dict(rg["known0"][e])
            ew = []
            for sem, val in rg["ext"][e].items():
                if kn.get(sem, 0) < val:
                    kn[sem] = val
                    ew.append((sem, val))
            rg["ext"][e] = ew
            self.known[e] = kn
            self.streams[e].append(("end", rg))

    def op(self, eng, fn, rd=(), wr=()):
        rd = _bufs(rd)
        wr = _bufs(wr)
        waits = self._deps(eng, rd, wr)
        if self.ecount[eng] >= SEM_LIMIT and not self.rstack:
            self.esem[eng] = self.new_sem("e_" + eng)
            self.ecount[eng] = 0
        self.ecount[eng] += 1
        sem = self.esem[eng]
        val = self.ecount[eng]
        self.streams[eng].append((waits, fn, sem, 1))
        self._note_inc(eng, sem, 1)
        for b in rd:
            if b.r.get(sem, 0) < val:
                b.r[sem] = val
        for b in wr:
            b.w = {sem: val}
            b.r = {}
        self.n_ops += 1

    def dma(self, eng, out, in_, chan=None, **kw):
        rd = _bufs([in_])
        wr = _bufs([out])
        if chan is None:
            chan = out.buf
        if chan.dsem is None:
            chan.dsem = self.new_sem("d_" + chan.name)
            self.dma_bufs.append(chan)
        waits = self._deps(eng, rd, wr, skip_waw=chan.dsem)
        chan.dcount += 16
        sem, val = chan.dsem, chan.dcount
        oap, iap = out.ap, in_.ap
        self.streams[eng].append((waits, lambda e: e.dma_start(out=oap, in_=iap, **kw), sem, 16))
        self._note_inc(eng, sem, 16)
        for b in rd:
            if b.r.get(sem, 0) < val:
                b.r[sem] = val
        for b in wr:
            b.w = {sem: val}
            b.r = {}
        self.n_ops += 1

    def idma(self, out, in_, idx, scatter, chan):
        rd = _bufs([in_, idx])
        wr = _bufs([out])
        if chan.dsem is None:
            chan.dsem = self.new_sem("d_" + chan.name)
            self.dma_bufs.append(chan)
        waits = self._deps("gpsimd", rd, wr, skip_waw=chan.dsem)
        chan.dcount += 16
        sem, val = chan.dsem, chan.dcount
        oap, iap, xap = out.ap, in_.ap, idx.ap
        if scatter:
            fn = lambda e: e.indirect_dma_start(out=oap, out_offset=bass.IndirectOffsetOnAxis(xap, 0), in_=iap, in_offset=None)
        else:
            fn = lambda e: e.indirect_dma_start(out=oap, out_offset=None, in_=iap, in_offset=bass.IndirectOffsetOnAxis(xap, 0))
        self.streams["gpsimd"].append((waits, fn, sem, 16))
        self._note_inc("gpsimd", sem, 16)
        for b in rd:
            if b.r.get(sem, 0) < val:
                b.r[sem] = val
        for b in wr:
            b.w = {sem: val}
            b.r = {}
        self.n_ops += 1

    def _all_tokens(self):
        tokens = {}
        for e in ENGS:
            if self.ecount[e] > 0:
                tokens[self.esem[e]] = self.ecount[e]
        for b in self.dma_bufs:
            tokens[b.dsem] = b.dcount
        return tokens

    def barrier(self):
        tokens = self._all_tokens()
        for e in ENGS:
            waits = self._need(e, dict(tokens))
            if waits:
                self.streams[e].append((waits, None, None, 0))

    def final_wait(self, eng="sync"):
        waits = self._need(eng, self._all_tokens())
        self.streams[eng].append((waits, None, None, 0))

    def emit(self):
        nc = self.nc
        with nc.Block() as block:
            def run(name):
                def body(e):
                    items = self.streams[name]
                    vals = {}

                    def run_items(lst):
                        i = 0
                        while i < len(lst):
                            it = lst[i]
                            if it[0] == "load":
                                for s, v in it[1]:
                                    e.wait_ge(s, v)
                                if "reg" not in vals:
                                    vals["reg"] = e.alloc_register("cnd_" + name)
                                e.load(vals["reg"], it[3])
                                vals["key"] = it[2]
                                i += 1
                            elif it[0] == "begin":
                                rg = it[1]
                                assert vals["key"] == rg["key"]
                                j = i + 1
                                while not (lst[j][0] == "end" and lst[j][1] is rg):
                                    j += 1
                                bodyl = lst[i + 1:j]
                                with e.If_cmp(vals["reg"], rg["thr"], "IS_LE"):
                                    for s, v in rg["ext"][name]:
                                        e.wait_ge(s, v)
                                    for s, tot in rg["incs"][name].items():
                                        if rg["start"].get(s, 0) > 0:
                                            e.wait_ge(s, rg["start"][s])
                                        e.nop().then_inc(s, tot)
                                with e.Else():
                                    run_items(bodyl)
                                i = j + 1
                            else:
                                waits, fn, sem, inc = it
                                for s, v in waits:
                                    e.wait_ge(s, v)
                                if fn is not None:
                                    fn(e).then_inc(sem, inc)
                                i += 1
                    run_items(items)
                return body
            block.tensor(run("tensor"))
            block.vector(run("vector"))
            block.scalar(run("scalar"))
            block.gpsimd(run("gpsimd"))
            block.sync(run("sync"))

    def ps(self, name, shape, dtype):
        t = self.stack.enter_context(self.nc.psum_tensor(name, list(shape), dtype))
        return T(t[:], Buf(name))

    def dram(self, name, shape, dtype, kind):
        t = self.nc.dram_tensor(name, list(shape), dtype, kind=kind)
        return T(t.ap(), Buf(name))

    def mm(self, out, lhsT, rhs, start=True, stop=True, **kw):
        rd = [lhsT, rhs] + ([] if start else [out])
        self.op("tensor", lambda e: e.matmul(out.ap, lhsT.ap, rhs.ap, start=start, stop=stop, **kw),
                rd=rd, wr=[out])

    def tr(self, out, in_, ident):
        self.op("tensor", lambda e: e.transpose(out.ap, in_.ap, ident.ap), rd=[in_, ident], wr=[out])

    def act(self, out, in_, func, bias=None, scale=None, accum=None):
        kw = {}
        rd = [in_]
        if bias is not None:
            if isinstance(bias, T):
                kw["bias"] = bias.ap
                rd.append(bias)
            else:
                kw["bias"] = bias
        if scale is not None:
            if isinstance(scale, T):
                kw["scale"] = scale.ap
                rd.append(scale)
            else:
                kw["scale"] = scale
        wr = [out]
        if accum is not None:
            kw["accum_out"] = accum.ap
            wr.append(accum)
        self.op("scalar", lambda e: e.activation(out.ap, in_.ap, func, **kw), rd=rd, wr=wr)

    def ts(self, eng, out, in0, s1, s2, op0, op1=None, accum=None):
        rd = [in0]
        a1, a2 = s1, s2
        if isinstance(s1, T):
            rd.append(s1)
            a1 = s1.ap
        if isinstance(s2, T):
            rd.append(s2)
            a2 = s2.ap
        kw = {}
        wr = [out]
        if op1 is not None:
            kw["op1"] = op1
        if accum is not None:
            kw["accum_out"] = accum.ap
            wr.append(accum)
        self.op(eng, lambda e: e.tensor_scalar(out.ap, in0.ap, a1, a2, op0, **kw), rd=rd, wr=wr)

    def tt(self, eng, out, in0, in1, op):
        self.op(eng, lambda e: e.tensor_tensor(out.ap, in0.ap, in1.ap, op), rd=[in0, in1], wr=[out])

    def stt(self, out, in0, scalar, in1, op0, op1):
        rd = [in0, in1]
        a = scalar
        if isinstance(scalar, T):
            rd.append(scalar)
            a = scalar.ap
        self.op("vector", lambda e: e.scalar_tensor_tensor(out.ap, in0.ap, a, in1.ap, op0, op1),
                rd=rd, wr=[out])

    def copy(self, eng, out, in_):
        if eng == "scalar":
            self.op(eng, lambda e: e.copy(out.ap, in_.ap), rd=[in_], wr=[out])
        else:
            self.op(eng, lambda e: e.tensor_copy(out.ap, in_.ap), rd=[in_], wr=[out])

    def memset(self, eng, out, val):
        self.op(eng, lambda e: e.memset(out.ap, val), rd=[], wr=[out])

    def recip(self, out, in_):
        self.op("vector", lambda e: e.reciprocal(out.ap, in_.ap), rd=[in_], wr=[out])

    def reduce(self, out, in_, op):
        self.op("vector", lambda e: e.tensor_reduce(out.ap, in_.ap, AX.X, op), rd=[in_], wr=[out])

    def scan_add(self, out, ones, data, initial):
        rd = [ones, data]
        a = initial
        if isinstance(initial, T):
            rd.append(initial)
            a = initial.ap
        self.op("vector", lambda e: e.tensor_tensor_scan(out.ap, ones.ap, data.ap, a, ALU.mult, ALU.add),
                rd=rd, wr=[out])

    def vmax8(self, out, in_):
        self.op("vector", lambda e: e.max(out.ap, in_.ap), rd=[in_], wr=[out])


class Arena:
    def __init__(self, k, nbytes):
        self.k = k
        self.nbytes = nbytes
        t = k.stack.enter_context(k.nc.sbuf_tensor("arena", [128, nbytes], U8))
        self.ap = t[:]
        self.off = 0
        self.top = nbytes
        self.n = 0

    def alloc(self, name, shape, dtype, top=False):
        free = int(np.prod(shape[1:]))
        nb = free * DSZ[dtype]
        if top:
            off = (self.top - nb) // 64 * 64
            assert off >= self.off, f"arena overflow (top) at {name}"
            self.top = off
        else:
            off = (self.off + 63) // 64 * 64
            assert off + nb <= self.top, f"arena overflow at {name}: {off}+{nb} > {self.top}"
            self.off = off + nb
        ap = self.ap[0:shape[0], off:off + nb].bitcast(dtype)
        if len(shape) == 3:
            ap = ap.rearrange("p (a b) -> p a b", a=shape[1])
        elif len(shape) == 4:
            ap = ap.rearrange("p (a b c) -> p a b c", a=shape[1], b=shape[2])
        self.n += 1
        return T(ap, Buf(f"{name}_{self.n}"))

    def mark(self):
        return self.off

    def reset(self, m):
        self.off = m


def build_nc(debug=False):
    nc = bass.Bass("TRN2", target_bir_lowering=False)
    st = ExitStack()
    k = K(nc, st)
    A = Arena(k, 207 * 1024)

    def DI(name, shape, dt=F32):
        return k.dram(name, shape, dt, "ExternalInput")

    xo = DI("xo", [2048, 1024]); xp = DI("xp", [2048, 1024])
    cvec = DI("cvec", [1024]); posr = DI("posr", [4096], I32)
    ada_w = DI("ada_w", [1024, 6144]); ada_b = DI("ada_b", [6144])
    n1g = DI("norm1_g", [1024]); n2g = DI("norm2_g", [1024]); fing = DI("final_g", [1024])
    hgg = DI("hg_norm_g", [128]); lbl = DI("lb_logits", [2, 256])
    w_ka = DI("w_ka", [1024, 512]); w_kas = DI("w_kas", [1024, 512])
    w_ki = DI("w_ki", [1024, 128]); w_kis = DI("w_kis", [1024, 128])
    w_va = DI("w_va", [1024, 512])
    w_qa = DI("w_qa", [1024, 512]); w_qas = DI("w_qas", [1024, 512])
    w_qi = DI("w_qi", [1024, 256]); w_qis = DI("w_qis", [1024, 256])
    w_wi = DI("w_wi", [1024, 4])
    w_qb = DI("w_qb", [1024, 256]); w_fb = DI("w_fb", [1024, 256])
    w_ib = DI("w_ib", [1024, 512]); w_gb = DI("w_gb", [1024, 512])
    w_out = DI("w_out", [1024, 1024])
    rw = DI("router_w", [1024, 32]); rb = DI("router_b", [32])
    w1 = DI("moe_w1", [32, 1024, 2048]); b1 = DI("moe_b1", [32, 2048])
    w2 = DI("moe_w2", [32, 1024, 1024]); b2 = DI("moe_b2", [32, 1024])
    c_identb = DI("c_identb", [128, 128], BF16); c_identf = DI("c_identf", [128, 128])
    c_negI = DI("c_negI", [128, 128], BF16); c_tri2 = DI("c_tri2", [128, 128])
    c_diag = DI("c_diag", [128, 128]); c_cols = DI("c_cols", [128, 4])
    yout = k.dram("y", [2048, 1024], F32, "ExternalOutput")
    x1s = k.dram("x1s", [2048, 1024], F32, "Internal")
    XG = k.dram("XG", [32 * 2048, 1024], BF16, "Internal")
    YG = k.dram("YG", [32 * 2048, 1024], F32, "Internal")
    c_ltb = DI("c_ltb", [128, 128], BF16); c_erow = DI("c_erow", [128, 32])
    dbg = {}
    if debug:
        dbg["mixT"] = k.dram("dbg_mixT", [8, 128, 2048], F32, "ExternalOutput")
        dbg["x1"] = k.dram("dbg_x1", [2048, 1024], F32, "ExternalOutput")
        dbg["G"] = k.dram("dbg_G", [128, 16 * 32], F32, "ExternalOutput")

    banks = [k.ps(f"bank{i}", [128, 512], F32) for i in range(8)]

    def wload(dst, src, rows=8, eng="gpsimd"):
        k.dma(eng, dst, src.re("(kc p) n -> p kc n", p=128))

    identb = A.alloc("identb", [128, 128], BF16); k.dma("sync", identb, c_identb)
    identf = A.alloc("identf", [128, 128], F32); k.dma("sync", identf, c_identf)
    negI = A.alloc("negI", [128, 128], BF16); k.dma("sync", negI, c_negI)
    tri2 = A.alloc("tri2", [128, 128], F32); k.dma("sync", tri2, c_tri2)
    diagm = A.alloc("diagm", [128, 128], F32); k.dma("sync", diagm, c_diag)
    ccols = A.alloc("ccols", [128, 4], F32); k.dma("sync", ccols, c_cols)
    invf = ccols[:, 0:1]; sgnc = ccols[:, 1:2]; negb = ccols[:, 2:3]; sflag = ccols[:, 3:4]
    zerob = A.alloc("zerob", [128, 512], BF16); k.memset("vector", zerob, 0.0)
    onesf = A.alloc("onesf", [128, 512], F32); k.memset("vector", onesf, 1.0)
    g1bc = A.alloc("g1bc", [128, 1024], F32)
    g2bc = A.alloc("g2bc", [128, 1024], F32)
    fgbc = A.alloc("fgbc", [128, 1024], F32)
    s2row = A.alloc("s2row", [128, 1024], F32)
    sh2row = A.alloc("sh2row", [128, 1024], F32)
    k.dma("sync", fgbc, T(fing.ap.partition_broadcast(128), fing.buf))
    hgbc = A.alloc("hgbc", [128, 128], F32)
    k.dma("sync", hgbc, T(hgg.ap.partition_broadcast(128), hgg.buf))
    rbbc = A.alloc("rbbc", [128, 32], F32)
    k.dma("sync", rbbc, T(rb.ap.partition_broadcast(128), rb.buf))
    mcols = A.alloc("mcols", [128, 32], F32)
    s1col = A.alloc("s1col", [128, 8], F32); s2col = A.alloc("s2col", [128, 8], F32)
    lbcol = A.alloc("lbcol", [128, 2], F32); omlb = A.alloc("omlb", [128, 2], F32)
    Gall = A.alloc("Gall", [128, 16, 32], F32)
    smalls = Rot([A.alloc(f"sm{i}", [128, 1], F32) for i in range(12)])
    epsc = A.alloc("epsc", [128, 1], F32); k.memset("vector", epsc, EPS)
    small_mark = A.mark()
    mixA = A.alloc("mixA", [128, 4, 2048], BF16)
    persist_mark = A.mark()

    cT = A.alloc("cT", [128, 8], F32)
    k.dma("sync", cT, cvec.re("(j p) -> p j", p=128), allow_slow_non_contiguous=True)
    siluc = A.alloc("siluc", [128, 8], BF16)
    k.act(siluc, cT, AF.Silu)
    modrow = A.alloc("modrow", [1, 6144], F32)
    adab = A.alloc("adab", [1, 6144], F32)
    k.dma("sync", adab, ada_b.re("(o n) -> o n", o=1))
    ones1 = A.alloc("ones1", [1, 128], F32); k.memset("vector", ones1, 1.0)
    aslots = Rot([A.alloc(f"adaw{i}", [128, 8, 512], BF16) for i in range(2)])
    prot = Rot(banks[0:4])
    adaw_v = ada_w.re("(kc p) n -> p kc n", p=128)
    for nb in range(12):
        sl = aslots.next()
        k.dma("gpsimd", sl, adaw_v[:, :, nb * 512:(nb + 1) * 512])
        pb = prot.next()
        for kc in range(8):
            k.mm(pb[0:1, :], siluc[:, kc:kc + 1], sl[:, kc, :], start=(kc == 0), stop=(kc == 7))
        k.tt("vector", modrow[0:1, nb * 512:(nb + 1) * 512], pb[0:1, :], adab[0:1, nb * 512:(nb + 1) * 512], ALU.add)
    for (dst, off) in ((g1bc, 2048), (g2bc, 5120), (sh2row, 3072), (s2row, 4096)):
        for nb in range(2):
            pb = prot.next()
            k.mm(pb, ones1[0:1, :], modrow[0:1, off + nb * 512: off + (nb + 1) * 512])
            k.copy("vector", dst[:, nb * 512:(nb + 1) * 512], pb)
    pb = prot.next()
    for i, off in enumerate((0, 1024, 3072, 4096)):
        for j in range(8):
            k.mm(pb[:, i * 8 + j: i * 8 + j + 1], modrow[0:1, off + j * 128: off + (j + 1) * 128], ones1[0:1, 0:1])
    k.copy("vector", mcols, pb[:, 0:32])
    n2bc = A.alloc("n2bc", [128, 1024], F32)
    k.dma("sync", n2bc, T(n2g.ap.partition_broadcast(128), n2g.buf))
    k.ts("vector", s2row, s2row, 1.0, None, ALU.add)
    k.tt("vector", s2row, s2row, n2bc, ALU.mult)
    gcol = A.alloc("gcol", [128, 16], F32)
    k.dma("sync", gcol[:, 0:8], n1g.re("(j p) -> p j", p=128), allow_slow_non_contiguous=True)
    k.dma("sync", gcol[:, 8:16], n2g.re("(j p) -> p j", p=128), allow_slow_non_contiguous=True)
    tmp8 = A.alloc("tmp8", [128, 8], F32)
    k.ts("vector", tmp8, mcols[:, 8:16], 1.0, None, ALU.add)
    k.tt("vector", s1col, tmp8, gcol[:, 0:8], ALU.mult)
    tmp8b = A.alloc("tmp8b", [128, 8], F32)
    k.ts("vector", tmp8b, mcols[:, 24:32], 1.0, None, ALU.add)
    k.tt("vector", s2col, tmp8b, gcol[:, 8:16], ALU.mult)
    sh1col = mcols[:, 0:8]; sh2col = mcols[:, 16:24]
    lbl_sb = A.alloc("lbl_sb", [128, 2, 2], F32)
    for l in range(2):
        k.dma("sync", lbl_sb[:, l, :], lbl[l, :].re("(c p) -> p c", p=128), allow_slow_non_contiguous=True)
    dl = A.alloc("dl", [128, 2], F32)
    k.tt("vector", dl, lbl_sb[:, 0, :], lbl_sb[:, 1, :], ALU.subtract)
    k.act(lbcol, dl, AF.Sigmoid)
    k.ts("vector", omlb, lbcol, -1.0, 1.0, ALU.mult, ALU.add)
    k.barrier()
    A.reset(persist_mark)

    def hT_steps(src, tile0, ntiles, scol, shcol, hT, xslots, tmps, prot):
        sqj, xns, tmpf = tmps
        sbc = scol.unsq(2).bc([128, 8, 128])
        shbc = shcol.unsq(2).bc([128, 8, 128])

        def norm(i):
            xt = xslots.next()
            k.dma("sync", xt, src[(tile0 + i) * 128:(tile0 + i + 1) * 128, :])
            ss = smalls.next(); rs = smalls.next(); rstd = smalls.next()
            k.act(sqj, xt, AF.Square, accum=ss)
            k.act(rs, ss, AF.Sqrt, scale=1.0 / 1024, bias=epsc)
            k.recip(rstd, rs)
            xn = xns[i % 2]
            k.act(xn, xt, AF.Identity, scale=rstd)
            return xn

        def evac(i, xn):
            pb = prot.next()
            pv = pb.bitcast(BF16).re("p (j t) -> p j t", j=8)
            for j in range(8):
                k.tr(pv[:, j, :], xn[:, j * 128:(j + 1) * 128], identb)
            k.tt("vector", tmpf, pv, sbc, ALU.mult)
            k.tt("vector", hT[:, :, i * 128:(i + 1) * 128], tmpf, shbc, ALU.add)

        state = {}
        steps = [lambda: state.__setitem__(0, norm(0))]
        for i in range(ntiles):
            def st(i=i):
                if i + 1 < ntiles:
                    state[i + 1] = norm(i + 1)
                evac(i, state[i])
            steps.append(st)
        return steps

    def make_hT(*args):
        for stp_ in hT_steps(*args):
            stp_()

    def rope_tables(slot0, n, cosg, sing, tmps):
        posi, pf, ang, kf = tmps
        ki32 = posi
        k.dma("sync", posi[:, 0:n], T(posr.ap[slot0:slot0 + n].partition_broadcast(128), posr.buf))
        k.copy("vector", pf[:, 0:n], posi[:, 0:n])
        k.ts("vector", ang[:, 0:n], pf[:, 0:n], invf, None, ALU.mult)
        k.ts("vector", kf[:, 0:n], ang[:, 0:n], 1.0 / (2 * PI), None, ALU.mult)
        k.copy("vector", ki32[:, 0:n], kf[:, 0:n])
        k.copy("vector", kf[:, 0:n], ki32[:, 0:n])
        C1 = 6.28125
        C2 = 2 * PI - C1
        k.stt(ang[:, 0:n], kf[:, 0:n], -C1, ang[:, 0:n], ALU.mult, ALU.add)
        k.stt(ang[:, 0:n], kf[:, 0:n], -C2, ang[:, 0:n], ALU.mult, ALU.add)
        k.ts("vector", ang[:, 0:n], ang[:, 0:n], -PI, PI, ALU.max, ALU.min)
        k.act(sing[:, 0:n], ang[:, 0:n], AF.Sin, scale=sgnc)
        k.ts("vector", pf[:, 0:n], ang[:, 0:n], PI / 2, None, ALU.add)
        k.ts("vector", kf[:, 0:n], pf[:, 0:n], PI, -2 * PI, ALU.is_gt, ALU.mult)
        k.tt("vector", pf[:, 0:n], pf[:, 0:n], kf[:, 0:n], ALU.add)
        k.ts("vector", pf[:, 0:n], pf[:, 0:n], -PI, PI, ALU.max, ALU.min)
        k.act(cosg[:, 0:n], pf[:, 0:n], AF.Sin)

    def proj_fm(dst_fn, wsb, wsw, nchunks, hT, n, cosg, sing, prot, rtmps, only=None):
        t1, t2 = rtmps
        for c in (range(nchunks) if only is None else [only]):
            pa = prot.next()
            for kc in range(8):
                k.mm(pa[:, 0:n], wsb[:, kc, c * 128:(c + 1) * 128], hT[:, kc, 0:n], start=(kc == 0), stop=(kc == 7))
            if wsw is None:
                k.copy("scalar", dst_fn(c), pa[:, 0:n])
                continue
            pb2 = prot.next()
            for kc in range(8):
                k.mm(pb2[:, 0:n], wsw[:, kc, c * 128:(c + 1) * 128], hT[:, kc, 0:n], start=(kc == 0), stop=(kc == 7))
            k.tt("vector", t1[:, 0:n], pa[:, 0:n], cosg[:, 0:n], ALU.mult)
            k.tt("vector", t2[:, 0:n], pb2[:, 0:n], sing[:, 0:n], ALU.mult)
            dst_fn(c, t1[:, 0:n], t2[:, 0:n])

    kaT = A.alloc("kaT", [128, 4, 4096], BF16)
    kiT = A.alloc("kiT", [128, 4096], BF16)
    va = A.alloc("va", [128, NT_ALL, 8, 65], BF16)
    k.memset("gpsimd", va[:, :, :, 64:65], 1.0)
    kside_mark = A.mark()
    wka = A.alloc("wka", [128, 8, 512], BF16); wload(wka, w_ka)
    wkas = A.alloc("wkas", [128, 8, 512], BF16); wload(wkas, w_kas)
    wki = A.alloc("wki", [128, 8, 128], BF16); wload(wki, w_ki)
    wkis = A.alloc("wkis", [128, 8, 128], BF16); wload(wkis, w_kis)
    wva = A.alloc("wva", [128, 8, 512], BF16); wload(wva, w_va)
    hT = A.alloc("hT", [128, 8, 512], BF16)
    xslots = Rot([A.alloc(f"xs{i}", [128, 1024], F32) for i in range(2)])
    ntmps = (A.alloc("sqj", [128, 1024], BF16), [A.alloc(f"xn{i_}", [128, 1024], BF16) for i_ in range(2)], A.alloc("tmpf", [128, 8, 128], F32))
    cosg = A.alloc("cosg", [128, 512], F32); sing = A.alloc("sing", [128, 512], F32)
    rtm = (A.alloc("posi", [128, 512], I32), A.alloc("pf", [128, 512], F32), A.alloc("ang", [128, 512], F32),
           A.alloc("kf", [128, 512], F32))
    rt12 = (rtm[1], rtm[3])
    prot = Rot(banks)
    hTs = [hT, A.alloc("hT2", [128, 8, 512], BF16)]

    def p1_units(g, hTg):
        def dst_ka(c, a=None, b=None):
            k.tt("gpsimd", kaT[:, c, g * 512:(g + 1) * 512], a, b, ALU.add)

        def dst_ki(c, a=None, b=None):
            k.tt("gpsimd", kiT[:, g * 512:(g + 1) * 512], a, b, ALU.add)
        units = []
        for c in range(4):
            units.append(lambda c=c: proj_fm(dst_ka, wka, wkas, 4, hTg, 512, cosg, sing, prot, rt12, only=c))
        units.append(lambda: proj_fm(dst_ki, wki, wkis, 1, hTg, 512, cosg, sing, prot, rt12, only=0))
        for i in range(4):
            def va_unit(i=i):
                pa = prot.next()
                for kc in range(8):
                    k.mm(pa, hTg[:, kc, i * 128:(i + 1) * 128], wva[:, kc, :], start=(kc == 0), stop=(kc == 7))
                k.copy("scalar", va[:, g * 4 + i, :, 0:64], pa.re("p (h d) -> p h d", h=8))
            units.append(va_unit)
        return units

    def p1_tiles(g):
        return hT_steps(xp if g < 4 else xo, (g % 4) * 4, 4, s1col, sh1col, hTs[g % 2], xslots, ntmps, prot)

    rope_tables(0, 512, cosg, sing, rtm)
    for stp_ in p1_tiles(0):
        stp_()
    for g in range(8):
        units = p1_units(g, hTs[g % 2])
        tsteps = p1_tiles(g + 1) if g + 1 < 8 else []
        ti = 0
        for ui, u in enumerate(units):
            u()
            if ui % 2 == 1 and ti < len(tsteps):
                tsteps[ti]()
                ti += 1
        while ti < len(tsteps):
            tsteps[ti]()
            ti += 1
        if g + 1 < 8:
            rope_tables((g + 1) * 512, 512, cosg, sing, rtm)
    k.barrier()
    A.reset(kside_mark)

    qaT = A.alloc("qaT", [128, 4, 2048], BF16)
    qiT = A.alloc("qiT", [128, 2, 2048], BF16)
    wis = A.alloc("wis", [128, 16, 4], F32)
    qside_mark = A.mark()
    wqa = A.alloc("wqa", [128, 8, 512], BF16); wload(wqa, w_qa)
    wqas = A.alloc("wqas", [128, 8, 512], BF16); wload(wqas, w_qas)
    wqi = A.alloc("wqi", [128, 8, 256], BF16); wload(wqi, w_qi)
    wqis = A.alloc("wqis", [128, 8, 256], BF16); wload(wqis, w_qis)
    wwi = A.alloc("wwi", [128, 8, 4], BF16); wload(wwi, w_wi)
    hT = A.alloc("hT", [128, 8, 512], BF16)
    xslots = Rot([A.alloc(f"xs{i}", [128, 1024], F32) for i in range(2)])
    ntmps = (A.alloc("sqj", [128, 1024], BF16), [A.alloc(f"xn{i_}", [128, 1024], BF16) for i_ in range(2)], A.alloc("tmpf", [128, 8, 128], F32))
    cosg = A.alloc("cosg", [128, 512], F32); sing = A.alloc("sing", [128, 512], F32)
    rtm = (A.alloc("posi", [128, 512], I32), A.alloc("pf", [128, 512], F32), A.alloc("ang", [128, 512], F32),
           A.alloc("kf", [128, 512], F32))
    rt12 = (rtm[1], rtm[3])
    for g in range(4):
        rope_tables(2048 + g * 512, 512, cosg, sing, rtm)
        make_hT(xo, g * 4, 4, s1col, sh1col, hT, xslots, ntmps, prot)

        def dst_qa(c, a=None, b=None, g=g):
            k.tt("gpsimd", qaT[:, c, g * 512:(g + 1) * 512], a, b, ALU.add)
        proj_fm(dst_qa, wqa, wqas, 4, hT, 512, cosg, sing, prot, rt12)

        def dst_qi(c, a=None, b=None, g=g):
            k.tt("gpsimd", qiT[:, c, g * 512:(g + 1) * 512], a, b, ALU.add)
        proj_fm(dst_qi, wqi, wqis, 2, hT, 512, cosg, sing, prot, rt12)
        for i in range(4):
            pa = prot.next()
            for kc in range(8):
                k.mm(pa[:, 0:4], hT[:, kc, i * 128:(i + 1) * 128], wwi[:, kc, :], start=(kc == 0), stop=(kc == 7))
            k.ts("vector", wis[:, g * 4 + i, :], pa[:, 0:4], 0.5 * 0.125, None, ALU.mult)
    k.barrier()
    A.reset(qside_mark)

    sc = A.alloc("sc", [128, 4096], F32)
    mbars = Rot([A.alloc(f"mbar{i}", [128, 4096], BF16) for i in range(2)])
    rts = Rot([A.alloc(f"rt{i}", [128, 512], F32) for i in range(4)])
    pTs = Rot([A.alloc(f"pT{i}", [128, 512], BF16) for i in range(3)])
    absw = A.alloc("absw", [128, 4], F32); sgnw = A.alloc("sgnw", [128, 4], F32)
    lo = A.alloc("lo", [128, 1], F32); mid = A.alloc("mid", [128, 1], F32)
    cnt = A.alloc("cnt", [128, 1], F32); dlt = A.alloc("dlt", [128, 1], F32)
    rmax = A.alloc("rmax", [128, 1], F32)
    sga = A.alloc("sga", [128, 1], F32); tcomb = A.alloc("tcomb", [128, 1], F32)
    rden = A.alloc("rden", [128, 8], F32)
    oa = A.alloc("oa", [128, 8, 64], BF16)
    zqs = Rot([A.alloc(f"zq{i}", [128, 4, 2, 128], BF16) for i in range(2)])
    zis = Rot([A.alloc(f"zi{i}", [128, 2, 2, 128], BF16) for i in range(2)])
    for z in zqs.items + zis.items:
        k.memset("gpsimd", z, 0.0)
    sc_rot = Rot(banks[0:2])
    s_rot = Rot(banks[2:4])
    po_rot = Rot([(banks[4], banks[5]), (banks[6], banks[7])])

    def stage_a(n):
        nkt = 16 + n + 1
        nk = nkt * 128
        qs = slice(n * 128, (n + 1) * 128)
        zq = zqs.next(); zi = zis.next(); mbar = mbars.next()
        for hp in range(2):
            ph = slice(hp * 64, (hp + 1) * 64)
            k.copy("gpsimd", zq[ph, :, hp, :], qaT[ph, :, qs])
            k.copy("gpsimd", zi[ph, :, hp, :], qiT[ph, :, qs])
        k.ts("vector", sgnw, wis[:, n, :], -1.0, None, ALU.mult)
        k.tt("vector", absw, wis[:, n, :], sgnw, ALU.max)
        k.ts("vector", sgnw, wis[:, n, :], 0.0, 2.0, ALU.is_ge, ALU.mult)
        k.ts("vector", sgnw, sgnw, -1.0, None, ALU.add)
        for kb in range((nkt + 3) // 4):
            ncols = min(512, nk - kb * 512)
            cs = slice(kb * 512, kb * 512 + ncols)
            for h in range(4):
                c, hp = h // 2, h % 2
                pa = sc_rot.next()
                k.mm(pa[:, 0:ncols], zi[:, c, hp, :], kiT[:, cs])
                rt = rts.next()
                k.act(rt[:, 0:ncols], pa[:, 0:ncols], AF.Relu, scale=absw[:, h:h + 1])
                if h == 0:
                    k.ts("vector", sc[:, cs], rt[:, 0:ncols], sgnw[:, 0:1], None, ALU.mult)
                else:
                    k.stt(sc[:, cs], rt[:, 0:ncols], sgnw[:, h:h + 1], sc[:, cs], ALU.mult, ALU.add)
        k.ts("vector", sc[:, 0:2048], sc[:, 0:2048], negb, None, ALU.add)
        ds_ = slice((nkt - 1) * 128, nkt * 128)
        k.tt("vector", sc[:, ds_], sc[:, ds_], diagm, ALU.add)
        k.reduce(rmax, sc[:, 0:nk], ALU.max)
        k.ts("vector", mid, rmax, -RNG + RNG / 2, None, ALU.add)
        for it in range(NBIS):
            wn = RNG / (2 ** (it + 2))
            k.ts("vector", mbar[:, 0:nk], sc[:, 0:nk], mid, None, ALU.is_gt, op1=ALU.add, accum=cnt)
            k.ts("vector", dlt, cnt, 255.5, 2.0 * wn, ALU.is_ge, ALU.mult)
            k.stt(mid, dlt, -wn, mid, ALU.add, ALU.add)
        k.ts("vector", lo, mid, -RNG / (2 ** (NBIS + 1)), None, ALU.add)
        k.ts("vector", mbar[:, 0:nk], sc[:, 0:nk], lo, None, ALU.is_le)
        return (n, nkt, qs, zq, mbar)

    def stage_b(ctx):
        n, nkt, qs, zq, mbar = ctx
        poA, poB = po_rot.next()
        k.mm(poA[:, 0:260], zerob[:, 0:128], zerob[:, 0:260], start=True, stop=False, skip_group_check=True)
        k.mm(poB[:, 0:260], zerob[:, 0:128], zerob[:, 0:260], start=True, stop=False, skip_group_check=True)
        groups = [(j, gq) for j in range(nkt) for gq in range(2)]

        def logits(j, gq):
            ks_ = slice(j * 128, (j + 1) * 128)
            pS = s_rot.next()
            for hh in range(4):
                h = gq * 4 + hh
                c, hp = h // 2, h % 2
                k.mm(pS[:, hh * 128:(hh + 1) * 128], kaT[:, c, ks_], zq[:, c, hp, :], start=True, stop=False)
                k.mm(pS[:, hh * 128:(hh + 1) * 128], mbar[:, ks_], negI, start=False, stop=True)
            pT = pTs.next()
            k.act(pT, pS, AF.Exp, scale=0.125)
            return pT

        def pv_mm(j, gq, pT):
            po = poA if gq == 0 else poB
            for hh in range(4):
                h = gq * 4 + hh
                k.mm(po[:, hh * 65:(hh + 1) * 65], pT[:, hh * 128:(hh + 1) * 128], va[:, j, h, :],
                     start=False, stop=(j == nkt - 1), skip_group_check=True)

        pend = None
        for (j, gq) in groups:
            pT = logits(j, gq)
            if pend is not None:
                pv_mm(*pend)
            pend = (j, gq, pT)
        pv_mm(*pend)
        for gq, po in enumerate((poA, poB)):
            pv = po[:, 0:260].re("p (h d) -> p h d", h=4)
            k.recip(rden[:, gq * 4:(gq + 1) * 4], pv[:, :, 64])
            k.tt("vector", oa[:, gq * 4:(gq + 1) * 4, :], pv[:, :, 0:64],
                 rden[:, gq * 4:(gq + 1) * 4].unsq(2).bc([128, 4, 64]), ALU.mult)
        pa = s_rot.next()
        pv = pa.bitcast(BF16).re("p (j t) -> p j t", j=8)
        oaf = oa.re("p h d -> p (h d)")
        for c in range(4):
            k.tr(pv[:, c, :], oaf[:, c * 128:(c + 1) * 128], identb)
        k.copy("scalar", mixA[:, :, qs], pv[:, 0:4, :])

    ctx = stage_a(0)
    for n in range(NT_OWN):
        nxt = stage_a(n + 1) if n + 1 < NT_OWN else None
        stage_b(ctx)
        ctx = nxt
    k.barrier()
    A.reset(persist_mark)

    mixB = A.alloc("mixB", [128, 4, 2048], BF16)
    hg_mark = A.mark()
    wqb = A.alloc("wqb", [128, 8, 256], BF16); wload(wqb, w_qb)
    wfb = A.alloc("wfb", [128, 8, 256], BF16); wload(wfb, w_fb)
    wib = A.alloc("wib", [128, 8, 512], BF16); wload(wib, w_ib)
    wgb = A.alloc("wgb", [128, 8, 512], BF16); wload(wgb, w_gb)
    hT = A.alloc("hT", [128, 8, 512], BF16)
    xslots = Rot([A.alloc(f"xs{i}", [128, 1024], F32) for i in range(2)])
    ntmps = (A.alloc("sqj", [128, 1024], BF16), [A.alloc(f"xn{i_}", [128, 1024], BF16) for i_ in range(2)], A.alloc("tmpf", [128, 8, 128], F32))
    vtok_s = [A.alloc(f"vtok{i}", [128, 4, 512], BF16) for i in range(2)]
    sgt_s = [A.alloc(f"sgt{i}", [128, 4, 512], BF16) for i in range(2)]
    sgf = A.alloc("sgf", [128, 512], F32)
    S = A.alloc("S", [128, 2, 128], F32); k.memset("vector", S, 0.0)
    Sbf = [A.alloc(f"Sbf{i}", [128, 2, 128], BF16) for i in range(8)]
    Bext = [A.alloc(f"Bext{c2}", [128, 513], F32) for c2 in range(2)]
    for c2 in range(2):
        k.memset("vector", Bext[c2][:, 0:1], 0.0)
    sig = A.alloc("sig", [128, 512], F32); fT = A.alloc("fT", [128, 512], F32)
    logf = A.alloc("logf", [128, 512], F32); omf = A.alloc("omf", [128, 512], F32)
    qbf = A.alloc("qbf", [128, 512], F32)
    D1 = A.alloc("D1", [128, 8, 64], F32); D3 = A.alloc("D3", [128, 8, 64], F32); D4 = A.alloc("D4", [128, 8, 64], F32)
    E1 = A.alloc("E1", [128, 512], F32); E2 = A.alloc("E2", [128, 512], F32)
    E3 = A.alloc("E3", [128, 512], F32); E4 = A.alloc("E4", [128, 512], F32)
    dd = A.alloc("dd", [128, 8], F32)
    gsets = []
    for si in range(2):
        dec_ = [A.alloc(f"dec{si}_{c2}", [128, 8], F32) for c2 in range(2)]
        qtZ_ = [A.alloc(f"qtZ{si}_{c2}", [128, 2, 512], BF16) for c2 in range(2)]
        qeZ_ = [A.alloc(f"qeZ{si}_{c2}", [128, 2, 512], BF16) for c2 in range(2)]
        ktT_ = [A.alloc(f"ktT{si}_{c2}", [128, 512], BF16) for c2 in range(2)]
        kdT_ = [A.alloc(f"kdT{si}_{c2}", [128, 512], BF16) for c2 in range(2)]
        for c2 in range(2):
            k.memset("gpsimd", qtZ_[c2], 0.0)
            k.memset("gpsimd", qeZ_[c2], 0.0)
        gsets.append((vtok_s[si], sgt_s[si], dec_, qtZ_, qeZ_, ktT_, kdT_))
    kdZ = [[A.alloc(f"kdZ{c2}_{i}", [128, 2, 128], BF16) for i in range(2)] for c2 in range(2)]
    for c2 in range(2):
        for i in range(2):
            k.memset("gpsimd", kdZ[c2][i], 0.0)
    attS = [A.alloc(f"attS{i}", [128, 4, 128], BF16) for i in range(2)]
    for i in range(2):
        k.memset("gpsimd", attS[i], 0.0)
    ssq = A.alloc("ssq", [128, 4], F32); rs4 = A.alloc("rs4", [128, 4], F32); rstd4 = A.alloc("rstd4", [128, 4], F32)
    sqj2 = A.alloc("sqj2", [128, 128], BF16)
    ob1 = A.alloc("ob1", [128, 4, 128], F32); ob2 = A.alloc("ob2", [128, 4, 128], F32)
    obb = A.alloc("obb", [128, 512], BF16)
    prot = Rot(banks[0:4])
    kv_rot = Rot(banks[4:6])
    at_rot = Rot([(banks[6], banks[7])])
    tri2b = tri2.unsq(1).bc([128, 2, 128])
    def hg_front(g):
        own = g >= 4
        vtok, sgt, dec, qtZ, qeZ, ktT, kdT = gsets[g % 2]
        src = xo if own else xp
        make_hT(src, (g % 4) * 4, 4, s1col, sh1col, hT, xslots, ntmps, prot)
        for i in range(4):
            pa = prot.next()
            for kc in range(8):
                k.mm(pa, hT[:, kc, i * 128:(i + 1) * 128], wib[:, kc, :], start=(kc == 0), stop=(kc == 7))
            k.copy("scalar", vtok[:, i, :], pa)
            if own:
                pa = prot.next()
                for kc in range(8):
                    k.mm(pa, hT[:, kc, i * 128:(i + 1) * 128], wgb[:, kc, :], start=(kc == 0), stop=(kc == 7))
                k.act(sgf, pa, AF.Sigmoid)
                k.tt("vector", sgt[:, i, :], sgf, pa, ALU.mult)
        for c2 in range(2):
            pa = prot.next()
            for kc in range(8):
                k.mm(pa, wfb[:, kc, c2 * 128:(c2 + 1) * 128], hT[:, kc, :], start=(kc == 0), stop=(kc == 7))
            k.act(sig, pa, AF.Sigmoid)
            k.ts("vector", fT, sig, omlb[:, c2:c2 + 1], lbcol[:, c2:c2 + 1], ALU.mult, ALU.add)
            k.act(logf, fT, AF.Ln)
            k.ts("vector", omf, fT, -1.0, 1.0, ALU.mult, ALU.add)
            Bx = Bext[c2]
            k.scan_add(Bx[:, 1:513], onesf, logf, Bx[:, 0:1])
            Bg = Bx[:, 1:513].re("p (c t) -> p c t", c=8)
            Bprev = Bx[:, 0:512].re("p (c t) -> p c t", c=8)[:, :, 0:1]
            Blast = Bg[:, :, 63:64]
            Bmid = Bg[:, :, 31:32]
            k.tt("vector", D4, Blast.bc([128, 8, 64]), Bg, ALU.subtract)
            k.act(E4, D4.re("p c t -> p (c t)"), AF.Exp)
            k.tt("vector", kdT[c2], omf, E4, ALU.mult)
            k.tt("vector", dd, Blast.re("p c o -> p (c o)"), Bprev.re("p c o -> p (c o)"), ALU.subtract)
            k.act(dec[c2], dd, AF.Exp)
            if own:
                pq = prot.next()
                for kc in range(8):
                    k.mm(pq, wqb[:, kc, c2 * 128:(c2 + 1) * 128], hT[:, kc, :], start=(kc == 0), stop=(kc == 7))
                k.copy("scalar", qbf, pq)
                k.tt("vector", D1, Bg, Bmid.bc([128, 8, 64]), ALU.subtract)
                k.act(E1, D1.re("p c t -> p (c t)"), AF.Exp)
                k.act(E2, D1.re("p c t -> p (c t)"), AF.Exp, scale=-1.0)
                k.tt("vector", D3, Bg, Bprev.bc([128, 8, 64]), ALU.subtract)
                k.act(E3, D3.re("p c t -> p (c t)"), AF.Exp)
                k.tt("vector", ktT[c2], omf, E2, ALU.mult)
                for hp in range(2):
                    ph = slice(hp * 64, (hp + 1) * 64)
                    k.tt("vector", qtZ[c2][ph, hp, :], qbf[ph, :], E1[ph, :], ALU.mult)
                    k.tt("vector", qeZ[c2][ph, hp, :], qbf[ph, :], E3[ph, :], ALU.mult)
            k.copy("vector", Bx[:, 0:1], Bx[:, 512:513])

    def hg_back(g):
        own = g >= 4
        vtok, sgt, dec, qtZ, qeZ, ktT, kdT = gsets[g % 2]
        for i in range(4):
            ts_ = slice(i * 128, (i + 1) * 128)
            kz = []
            for c2 in range(2):
                pa = prot.next()
                pv = pa.bitcast(BF16)
                k.tr(pv[:, 0:128], kdT[c2][:, ts_], identb)
                kzz = kdZ[c2][i % 2]
                for cp in range(2):
                    k.copy("scalar", kzz[cp * 64:(cp + 1) * 64, cp, :], pv[cp * 64:(cp + 1) * 64, 0:128])
                kz.append(kzz)
            if own:
                pA0, pA1 = at_rot.next()
                for cp in range(2):
                    cs_ = slice(i * 128 + cp * 64, i * 128 + (cp + 1) * 64)
                    for h in range(4):
                        c2, hp = h // 2, h % 2
                        pbk = pA0 if hp == 0 else pA1
                        k.mm(pbk[cp * 64:(cp + 1) * 64, c2 * 64:(c2 + 1) * 64], ktT[c2][:, cs_], qtZ[c2][:, hp, cs_])
                aS = attS[i % 2]
                for hp, pbk in enumerate((pA0, pA1)):
                    for c2 in range(2):
                        h = 2 * c2 + hp
                        for cp in range(2):
                            ph = slice(cp * 64, (cp + 1) * 64)
                            k.tt("vector", aS[ph, h, cp * 64:(cp + 1) * 64], pbk[ph, c2 * 64:(c2 + 1) * 64],
                                 tri2[ph, cp * 64:(cp + 1) * 64], ALU.mult)
            for cp in range(2):
                cidx = i * 2 + cp
                if own:
                    k.copy("scalar", Sbf[cidx], S)
                pkv = kv_rot.next()
                for h in range(4):
                    c2, hp = h // 2, h % 2
                    k.mm(pkv[hp * 64:(hp + 1) * 64, c2 * 128:(c2 + 1) * 128], kz[c2][:, cp, hp * 64:(hp + 1) * 64],
                         vtok[:, i, h * 128:(h + 1) * 128])
                for c2 in range(2):
                    k.stt(S[:, c2, :], S[:, c2, :], dec[c2][:, cidx:cidx + 1], pkv[:, c2 * 128:(c2 + 1) * 128], ALU.mult, ALU.add)
                if g == 3 and cidx == 7:
                    k.ts("vector", S.re("p a b -> p (a b)"), S.re("p a b -> p (a b)"), sflag, None, ALU.mult)
            if own:
                po = prot.next()
                for h in range(4):
                    c2, hp = h // 2, h % 2
                    k.mm(po[:, h * 128:(h + 1) * 128], aS[:, h, :], vtok[:, i, h * 128:(h + 1) * 128], start=True, stop=False)
                    for cp in range(2):
                        cidx = i * 2 + cp
                        cs_ = slice(i * 128 + cp * 64, i * 128 + (cp + 1) * 64)
                        k.mm(po[cp * 64:(cp + 1) * 64, h * 128:(h + 1) * 128], qeZ[c2][:, hp, cs_], Sbf[cidx][:, c2, :],
                             start=False, stop=True)
                pov = po.re("p (h e) -> p h e", h=4)
                for h in range(4):
                    k.act(sqj2, po[:, h * 128:(h + 1) * 128], AF.Square, accum=ssq[:, h:h + 1])
                k.act(rs4, ssq, AF.Sqrt, scale=1.0 / 128, bias=epsc)
                k.recip(rstd4, rs4)
                k.tt("vector", ob1, pov, rstd4.unsq(2).bc([128, 4, 128]), ALU.mult)
                k.tt("vector", ob2, ob1, hgbc.unsq(1).bc([128, 4, 128]), ALU.mult)
                k.tt("vector", obb, ob2.re("p h e -> p (h e)"), sgt[:, i, :], ALU.mult)
                pa = prot.next()
                pv = pa.bitcast(BF16).re("p (j t) -> p j t", j=8)
                for c in range(4):
                    k.tr(pv[:, c, :], obb[:, c * 128:(c + 1) * 128], identb)
                tt0 = (g - 4) * 512 + i * 128
                k.copy("scalar", mixB[:, :, tt0:tt0 + 128], pv[:, 0:4, :])

    hg_front(0)
    for g in range(8):
        if g + 1 < 8:
            hg_front(g + 1)
        hg_back(g)
    k.barrier()
    A.reset(hg_mark)

    idxall = A.alloc("idxall", [128, 16, 4], I32, top=True)
    gkall = A.alloc("gkall", [128, 16, 4], F32, top=True)
    nblk_i = A.alloc("nblk_i", [128, 32], I32, top=True)
    p5_mark = A.mark()
    wo = A.alloc("wo", [128, 8, 1024], BF16); wload(wo, w_out)
    rwf = A.alloc("rwf", [128, 8, 32], F32)
    k.dma("sync", rwf, rw.re("(kc p) e -> p kc e", p=128))
    LTb = A.alloc("LTb", [128, 128], BF16); k.dma("sync", LTb, c_ltb)
    onesb = A.alloc("onesb", [128, 128], BF16); k.memset("vector", onesb, 1.0)
    erow = A.alloc("erow", [128, 32], F32); k.dma("sync", erow, c_erow)
    carry = A.alloc("carry", [128, 32], F32); k.memset("vector", carry, 0.0)
    xslots = Rot([A.alloc(f"xs{i}", [128, 1024], F32) for i in range(2)])
    x1r = Rot([A.alloc(f"x1_{i}", [128, 1024], F32) for i in range(2)])
    h2ts = Rot([A.alloc(f"h2tok{i}", [128, 1024], BF16) for i in range(2)])
    ytmp = A.alloc("ytmp", [128, 1024], F32)
    sqj = A.alloc("sqj", [128, 1024], BF16)
    xnf = A.alloc("xnf", [128, 1024], F32)
    h2f = A.alloc("h2f", [128, 8, 128], F32); h2ft = A.alloc("h2ft", [128, 8, 128], F32)
    lg = A.alloc("lg", [128, 32], F32); m8 = A.alloc("m8", [128, 8], F32)
    msk = A.alloc("msk", [128, 32], F32); ex = A.alloc("ex", [128, 32], F32)
    mskb = A.alloc("mskb", [128, 32], BF16)
    rank = A.alloc("rank", [128, 32], F32); keyt = A.alloc("keyt", [128, 32], F32)
    ek8 = A.alloc("ek8", [128, 8], F32); oh = A.alloc("oh", [128, 32], F32); ohr = A.alloc("ohr", [128, 32], F32)
    rk = A.alloc("rk", [128, 1], F32); slf = A.alloc("slf", [128, 1], F32)
    nmax = A.alloc("nmax", [128, 1], F32); zs = A.alloc("zs", [128, 1], F32); rz = A.alloc("rz", [128, 1], F32)
    y_rot = Rot([(banks[0], banks[1]), (banks[2], banks[3])])
    t_rot = Rot([(banks[4], banks[5]), (banks[6], banks[7])])
    s2bc = s2col.unsq(2).bc([128, 8, 128]); sh2bc = sh2col.unsq(2).bc([128, 8, 128])
    def p5_front(i):
        lg = lgs.next()
        ts_ = slice(i * 128, (i + 1) * 128)
        xt = xslots.next()
        k.dma("sync", xt, xo[ts_, :])
        py = y_rot.next()
        for nb in range(2):
            for kc in range(8):
                mixsrc = mixA if kc < 4 else mixB
                k.mm(py[nb], mixsrc[:, kc % 4, ts_], wo[:, kc, nb * 512:(nb + 1) * 512], start=(kc == 0), stop=(kc == 7))
        x1 = x1r.next()
        for nb in range(2):
            ns_ = slice(nb * 512, (nb + 1) * 512)
            k.tt("vector", ytmp[:, ns_], py[nb], g1bc[:, ns_], ALU.mult)
            k.tt("vector", x1[:, ns_], ytmp[:, ns_], xt[:, ns_], ALU.add)
        k.dma("sync", x1s[ts_, :], x1, chan=x1.buf)
        if debug:
            k.dma("sync", dbg["x1"][ts_, :], x1, chan=x1.buf)
        ss = smalls.next(); rs = smalls.next(); rstd = smalls.next()
        k.act(sqj, x1, AF.Square, accum=ss)
        k.act(rs, ss, AF.Sqrt, scale=1.0 / 1024, bias=epsc)
        k.recip(rstd, rs)
        k.act(xnf, x1, AF.Identity, scale=rstd)
        h2tok = h2ts.next()
        k.tt("vector", ytmp, xnf, s2row, ALU.mult)
        k.tt("vector", h2tok, ytmp, sh2row, ALU.add)
        pt = t_rot.next()
        for j in range(8):
            k.tr(pt[j // 4][:, (j % 4) * 128:(j % 4 + 1) * 128], xnf[:, j * 128:(j + 1) * 128], identf)
        for hb in range(2):
            pv = pt[hb].re("p (j t) -> p j t", j=4)
            k.tt("vector", h2ft[:, hb * 4:(hb + 1) * 4, :], pv, s2bc[:, hb * 4:(hb + 1) * 4, :], ALU.mult)
        k.tt("vector", h2f, h2ft, sh2bc, ALU.add)
        pl = y_rot.next()[0]
        for kc in range(8):
            k.mm(pl[:, 0:32], h2f[:, kc, :], rwf[:, kc, :], start=(kc == 0), stop=(kc == 7))
        k.tt("vector", lg, pl[:, 0:32], rbbc, ALU.add)
        return (i, lg, h2tok)

    def p5_back(ctx):
        i, lg, h2tok = ctx
        k.vmax8(m8, lg)
        k.ts("vector", msk, lg, m8[:, 3:4], None, ALU.is_ge)
        k.ts("vector", nmax, m8[:, 0:1], -1.0, None, ALU.mult)
        k.act(ex, lg, AF.Exp, bias=nmax)
        k.tt("vector", ex, ex, msk, ALU.mult)
        k.reduce(zs, ex, ALU.add)
        k.recip(rz, zs)
        k.ts("vector", Gall[:, i, :], ex, rz, None, ALU.mult)
        k.copy("vector", mskb, msk)
        pr = y_rot.next()[1]
        k.mm(pr[:, 0:32], LTb, mskb)
        k.mm(pr[:, 32:64], onesb, mskb)
        k.tt("vector", rank, pr[:, 0:32], carry, ALU.add)
        k.tt("vector", carry, carry, pr[:, 32:64], ALU.add)
        k.tt("vector", keyt, msk, erow, ALU.mult)
        k.vmax8(ek8, keyt)
        for kk in range(4):
            k.ts("vector", oh, erow, ek8[:, kk:kk + 1], None, ALU.is_equal)
            k.tt("vector", ohr, oh, rank, ALU.mult)
            k.reduce(rk, ohr, ALU.add)
            k.tt("vector", ohr, oh, Gall[:, i, :], ALU.mult)
            k.reduce(gkall[:, i, kk:kk + 1], ohr, ALU.add)
            k.ts("vector", slf, ek8[:, kk:kk + 1], -1.0, 2048.0, ALU.add, ALU.mult)
            k.tt("vector", slf, slf, rk, ALU.add)
            ix = T(idxall.ap[:, i, kk:kk + 1], Buf(f"idx_{i}_{kk}"))
            idxT[(i, kk)] = ix
            k.copy("vector", ix, slf)
            k.idma(XG, h2tok, ix, scatter=True, chan=h2tok.buf)

    idxT = {}
    lgs = Rot([A.alloc(f"lg{i}", [128, 32], F32) for i in range(2)])
    ctx5 = p5_front(0)
    for i in range(NT_OWN):
        nxt5 = p5_front(i + 1) if i + 1 < NT_OWN else None
        p5_back(ctx5)
        ctx5 = nxt5
    k.ts("vector", rank, carry, 63.5, 1.0 / 128, ALU.add, ALU.mult)
    k.copy("vector", nblk_i, rank)
    if debug:
        k.dma("sync", dbg["G"], Gall.re("p a b -> p (a b)"), chan=Gall.buf)
    k.barrier()
    A.reset(small_mark)

    b1T = A.alloc("b1T", [128, 16, 32], F32)
    b1_mark = A.mark()
    b1sb = A.alloc("b1sb", [32, 2048], F32); k.dma("sync", b1sb, b1)
    pb = banks[0]
    for c in range(16):
        k.tr(pb[:, c * 32:(c + 1) * 32], b1sb[0:32, c * 128:(c + 1) * 128], identf[0:32, 0:32])
    k.copy("vector", b1T, pb.re("p (c e) -> p c e", c=16))
    k.barrier()
    A.reset(b1_mark)
    p6_mark = A.mark()
    w1b = A.alloc("w1b", [128, 8, 2048], BF16)
    w2b = A.alloc("w2b", [128, 8, 1024], BF16)
    stg1 = [A.alloc(f"stg1_{p}", [128, 2048], F32) for p in range(8)]
    stg2 = [A.alloc(f"stg2_{j}", [128, 2, 1024], F32) for j in range(4)]
    xbs = Rot([A.alloc(f"xb{i}", [128, 1024], BF16) for i in range(3)])
    xets = Rot([A.alloc(f"xet{i}", [128, 8, 128], BF16) for i in range(2)])
    actTs = Rot([A.alloc(f"actT{i}", [128, 8, 128], BF16) for i in range(2)])
    ysbs = Rot([A.alloc(f"ysb{i}", [128, 1024], F32) for i in range(2)])
    gts = Rot([A.alloc(f"gt{i}", [128, 4, 128], F32) for i in range(1)])
    lts = Rot([A.alloc(f"lt{i}", [128, 4, 128], F32) for i in range(1)])
    sts = Rot([A.alloc(f"st{i}", [128, 4, 128], F32) for i in range(1)])
    gss = Rot([A.alloc(f"gs{i}", [128, 4, 128], F32) for i in range(1)])
    ptr_bank = banks[0]
    mm1_banks = banks[1:5]
    y_banks = (banks[5], banks[6])

    def piece_dma(e, p):
        if p < 8:
            k.dma("sync", stg1[p], w1[e][p * 128:(p + 1) * 128, :])
        else:
            j = p - 8
            k.dma("sync", stg2[j], w2[e][j * 256:(j + 1) * 256, :].re("(a p) n -> p a n", p=128))

    def piece_cast(p):
        dst, src = (w1b[:, p, :], stg1[p]) if p < 8 else (w2b[:, 2 * (p - 8):2 * (p - 8) + 2, :], stg2[p - 8])
        k.copy("vector", dst, src)

    for p in range(12):
        piece_dma(0, p)
    for p in range(12):
        piece_cast(p)
        piece_dma(1, p)
    key_next = k.val_load(nblk_i[0:1, 0:1])
    xb_first = xbs.next()
    k.dma("gpsimd", xb_first, XG[0:128, :])
    for e in range(32):
        key = key_next
        xb_next = xb_first
        for blk in range(16):
            k.cond_begin(key, blk)
            r0 = e * 2048 + blk * 128
            xb = xb_next
            if blk + 1 < 16:
                xb_next = xbs.next()
                k.dma("gpsimd", xb_next, XG[r0 + 128:r0 + 256, :])
            pv = ptr_bank.bitcast(BF16).re("p (j t) -> p j t", j=8)
            for j in range(8):
                k.tr(pv[:, j, :], xb[:, j * 128:(j + 1) * 128], identb)
            xet = xets.next()
            k.copy("scalar", xet, pv)
            for gp in range(2):
                for g4 in (gp, 2 + gp):
                    pbk = mm1_banks[g4]
                    for q in range(4):
                        fc = g4 * 4 + q
                        for kc in range(8):
                            k.mm(pbk[:, q * 128:(q + 1) * 128], w1b[:, kc, fc * 128:(fc + 1) * 128], xet[:, kc, :],
                                 start=(kc == 0), stop=(kc == 7))
            actT = actTs.next()
            ysb = ysbs.next()
            for gp in range(2):
                pg = mm1_banks[gp].re("p (q c) -> p q c", q=4)
                pl_ = mm1_banks[2 + gp].re("p (q c) -> p q c", q=4)
                bg = b1T[:, gp * 4:(gp + 1) * 4, e:e + 1].bc([128, 4, 128])
                bl = b1T[:, 8 + gp * 4:8 + (gp + 1) * 4, e:e + 1].bc([128, 4, 128])
                gt_ = gts.next(); lt_ = lts.next(); st_ = sts.next(); gs_ = gss.next()
                k.tt("vector", gt_, pg, bg, ALU.add)
                k.ts("vector", gt_, gt_, 7.0, None, ALU.min)
                k.act(st_, gt_, AF.Sigmoid, scale=1.702)
                k.tt("vector", lt_, pl_, bl, ALU.add)
                k.ts("vector", lt_, lt_, 7.0, -7.0, ALU.min, ALU.max)
                k.tt("vector", gs_, gt_, st_, ALU.mult)
                k.stt(actT[:, gp * 4:(gp + 1) * 4, :], lt_, 1.0, gs_, ALU.add, ALU.mult)
                for nb in range(2):
                    for f in range(gp * 4, gp * 4 + 4):
                        k.mm(y_banks[nb], actT[:, f, :], w2b[:, f, nb * 512:(nb + 1) * 512], start=(f == 0), stop=(f == 7))
            k.copy("scalar", ysb[:, 0:512], y_banks[0])
            k.copy("vector", ysb[:, 512:1024], y_banks[1])
            k.dma("gpsimd", YG[r0:r0 + 128, :], ysb, chan=ysb.buf)
        for blk in range(16):
            k.cond_end()
        if e + 1 < 32:
            key_next = k.val_load(nblk_i[0:1, e + 1:e + 2])
            xb_first = xbs.next()
            k.dma("gpsimd", xb_first, XG[(e + 1) * 2048:(e + 1) * 2048 + 128, :])
            for p in range(12):
                piece_cast(p)
                if e + 2 < 32:
                    piece_dma(e + 2, p)
    k.barrier()
    A.reset(p6_mark)

    GT = A.alloc("GT", [32, 128], F32)
    b2sb = A.alloc("b2sb", [32, 1024], F32); k.dma("sync", b2sb, b2)
    accs = Rot([A.alloc(f"acc{i}", [128, 1024], F32) for i in range(2)])
    ygs = Rot([A.alloc(f"yg{i}", [128, 1024], F32) for i in range(8)])
    xslots = Rot([A.alloc(f"xs{i}", [128, 1024], F32) for i in range(2)])
    fo = Rot([A.alloc(f"fo{i}", [128, 1024], F32) for i in range(2)])
    sqj = A.alloc("sqj", [128, 1024], BF16)
    pg_rot = Rot(banks[0:2])
    y_rot = Rot([(banks[4], banks[5]), (banks[6], banks[7])])
    for ti in range(NT_OWN):
        ts_ = slice(ti * 128, (ti + 1) * 128)
        acc = accs.next()
        pgt = pg_rot.next()
        k.tr(pgt[0:32, 0:128], Gall[:, ti, :], identf)
        k.copy("vector", GT, pgt[0:32, 0:128])
        py = y_rot.next()
        for nb in range(2):
            k.mm(py[nb], GT[0:32, :], b2sb[0:32, nb * 512:(nb + 1) * 512])
            k.copy("scalar", acc[:, nb * 512:(nb + 1) * 512], py[nb])
        for kk in range(4):
            yg = ygs.next()
            k.idma(yg, YG, idxT[(ti, kk)], scatter=False, chan=yg.buf)
            k.stt(acc, yg, gkall[:, ti, kk:kk + 1], acc, ALU.mult, ALU.add)
        xt = xslots.next()
        k.dma("sync", xt, x1s[ts_, :])
        k.tt("vector", acc, acc, g2bc, ALU.mult)
        k.tt("vector", xt, xt, acc, ALU.add)
        ss = smalls.next(); rs = smalls.next(); rstd = smalls.next()
        k.act(sqj, xt, AF.Square, accum=ss)
        k.act(rs, ss, AF.Sqrt, scale=1.0 / 1024, bias=epsc)
        k.recip(rstd, rs)
        ot = fo.next()
        k.stt(ot, xt, rstd, fgbc, ALU.mult, ALU.mult)
        k.dma("sync", yout[ts_, :], ot, chan=ot.buf)
    k.final_wait("sync")
    k.emit()
    st.close()
    return nc, k


SPL = np.cumsum([0, 512, 512, 512, 256, 64, 4, 256, 256, 512, 512])


def _swap_perm(ncols):
    p = np.arange(ncols)
    for h0 in range(0, ncols, 64):
        p[h0:h0 + 8] = np.arange(h0 + 8, h0 + 16)
        p[h0 + 8:h0 + 16] = np.arange(h0, h0 + 8)
    return p


def _consts(half):
    identf = np.eye(128, dtype=np.float32)
    identb = identf.astype(ml_dtypes.bfloat16)
    negI = (-1024.0 * identf).astype(ml_dtypes.bfloat16)
    s = np.arange(128)[:, None]
    t = np.arange(128)[None, :]
    tri2 = (((s // 64) == (t // 64)) & ((s % 64) <= (t % 64))).astype(np.float32)
    diag = np.where((t // 64) <= (s // 64), 0.0, NEG).astype(np.float32)
    cols = np.zeros((128, 4), np.float32)
    inv_freq = (500000.0 ** (-(np.arange(0, 16, 2, dtype=np.float32) / 16))).astype(np.float32)
    for p in range(128):
        d = p % 64
        if d < 16:
            cols[p, 0] = inv_freq[d % 8]
            cols[p, 1] = -1.0 if d < 8 else 1.0
    cols[:, 2] = 0.0 if half == 1 else NEG
    cols[:, 3] = float(half)
    ltb = (s < t).astype(np.float32).astype(ml_dtypes.bfloat16)
    erow = np.broadcast_to(np.arange(1, 33, dtype=np.float32)[None, :], (128, 32)).copy()
    return {"c_ltb": ltb, "c_erow": erow, "c_identb": identb, "c_identf": identf, "c_negI": negI, "c_tri2": tri2, "c_diag": diag, "c_cols": cols}


_CACHE = {}


def kernel(x, c, positions, ada_w, ada_b, norm1_g, w_in, hg_norm_g, lb_logits, w_out, norm2_g,
           router_w, router_b, moe_w1, moe_b1, moe_w2, moe_b2, final_g, _debug=False):
    f = lambda a: np.ascontiguousarray(np.asarray(a, dtype=np.float32))
    x = f(x); c = f(c); positions = np.ascontiguousarray(np.asarray(positions, dtype=np.int32))
    w_in0 = f(w_in)[0]
    parts = [w_in0[:, SPL[i]:SPL[i + 1]] for i in range(10)]
    qa, ka, va, qi, ki, wi, qb, fb, ib, gb = parts
    ki2 = np.concatenate([ki, ki], axis=1)
    shared = {
        "ada_w": f(ada_w)[0], "ada_b": f(ada_b)[0], "norm1_g": f(norm1_g)[0], "norm2_g": f(norm2_g)[0],
        "final_g": f(final_g), "hg_norm_g": f(hg_norm_g)[0], "lb_logits": f(lb_logits),
        "w_ka": f(ka), "w_kas": f(ka[:, _swap_perm(512)]), "w_ki": f(ki2), "w_kis": f(ki2[:, _swap_perm(128)]),
        "w_va": f(va), "w_qa": f(qa), "w_qas": f(qa[:, _swap_perm(512)]),
        "w_qi": f(qi), "w_qis": f(qi[:, _swap_perm(256)]), "w_wi": f(wi),
        "w_qb": f(qb), "w_fb": f(fb), "w_ib": f(ib), "w_gb": f(gb),
        "w_out": f(w_out)[0], "router_w": f(router_w)[0], "router_b": f(router_b)[0],
        "moe_w1": f(moe_w1)[0], "moe_b1": f(moe_b1)[0], "moe_w2": f(moe_w2)[0], "moe_b2": f(moe_b2)[0],
    }
    in_maps = []
    for j in range(8):
        b, half = j // 2, j % 2
        m = dict(shared)
        m["xo"] = np.ascontiguousarray(x[b, half * 2048:(half + 1) * 2048])
        m["xp"] = np.ascontiguousarray(x[b, 0:2048])
        m["cvec"] = np.ascontiguousarray(c[b])
        m["posr"] = np.ascontiguousarray(np.concatenate([positions[b, 0:2048], positions[b, half * 2048:(half + 1) * 2048]]))
        m.update(_consts(half))
        in_maps.append(m)
    key = bool(_debug)
    if key not in _CACHE:
        _CACHE[key] = build_nc(debug=key)[0]
    nc = _CACHE[key]
    res = run_bass_kernel_spmd(nc, in_maps, core_ids=list(range(8)))
    out = np.empty((4, 4096, 1024), np.float32)
    for j in range(8):
        b, half = j // 2, j % 2
        out[b, half * 2048:(half + 1) * 2048] = res.results[j]["y"]
    if _debug:
        return out, res.results
    return out
```

```python
from contextlib import ExitStack
import numpy as np
import ml_dtypes
import concourse.bass as bass
import concourse.mybir as mybir
from concourse.bass_utils import run_bass_kernel_spmd

F32 = mybir.dt.float32
BF16 = mybir.dt.bfloat16
I32 = mybir.dt.int32
U8 = mybir.dt.uint8
AF = mybir.ActivationFunctionType
ALU = mybir.AluOpType
AX = mybir.AxisListType
DSZ = {F32: 4, BF16: 2, I32: 4, U8: 1}

ENGS = ["tensor", "vector", "scalar", "gpsimd", "sync"]
SEM_LIMIT = 30000
EPS = 1e-6
PI = float(np.pi)
NT_OWN = 16
NT_ALL = 32
RNG = 32.0
NBIS = 13
NEG = -1.0e30


class Buf:
    __slots__ = ("name", "w", "r", "dsem", "dcount")

    def __init__(self, name):
        self.name = name
        self.w = {}
        self.r = {}
        self.dsem = None
        self.dcount = 0


class T:
    __slots__ = ("ap", "buf")

    def __init__(self, ap, buf):
        self.ap = ap
        self.buf = buf

    def __getitem__(self, key):
        return T(self.ap[key], self.buf)

    def bitcast(self, dt):
        return T(self.ap.bitcast(dt), self.buf)

    def re(self, pat, **kw):
        return T(self.ap.rearrange(pat, **kw), self.buf)

    def bc(self, shape):
        return T(self.ap.to_broadcast(list(shape)), self.buf)

    def unsq(self, ax):
        return T(self.ap.unsqueeze(ax), self.buf)


def _bufs(ts):
    out = []
    for t in ts:
        if t is None:
            continue
        b = t.buf if isinstance(t, T) else t
        if b is not None and b not in out:
            out.append(b)
    return out


class Rot:
    def __init__(self, items):
        self.items = list(items)
        self.i = 0

    def next(self):
        t = self.items[self.i % len(self.items)]
        self.i += 1
        return t


class K:
    def __init__(self, nc, stack):
        self.nc = nc
        self.stack = stack
        self.streams = {e: [] for e in ENGS}
        self.esem = {}
        self.ecount = {e: 0 for e in ENGS}
        self.known = {e: {} for e in ENGS}
        self.nsem = 0
        for e in ENGS:
            self.esem[e] = self.new_sem("e_" + e)
        self.dma_bufs = []
        self.n_ops = 0
        self.rstack = []

    def new_sem(self, name):
        self.nsem += 1
        return self.stack.enter_context(self.nc.semaphore(f"{name}_{self.nsem}"))

    def _need(self, eng, tokens):
        waits = []
        kn = self.known[eng]
        for sem, val in tokens.items():
            if kn.get(sem, 0) < val:
                kn[sem] = val
                waits.append((sem, val))
        return waits

    def _deps(self, eng, rd, wr, skip_waw=None):
        tokens = {}
        mysem = self.esem[eng]

        def add(d, is_raw):
            for sem, val in d.items():
                if sem is mysem and eng == "tensor":
                    continue
                if (not is_raw) and skip_waw is not None and sem is skip_waw:
                    continue
                if tokens.get(sem, 0) < val:
                    tokens[sem] = val
        for b in rd:
            add(b.w, True)
        for b in wr:
            add(b.w, False)
            add(b.r, False)
        self._note_region(eng, tokens)
        return self._need(eng, tokens)

    def _note_region(self, eng, tokens):
        for rg in self.rstack:
            ext = rg["ext"][eng]
            for sem, val in tokens.items():
                if val <= rg["start"].get(sem, 0):
                    if ext.get(sem, 0) < val:
                        ext[sem] = val

    def _note_inc(self, eng, sem, inc):
        for rg in self.rstack:
            d = rg["incs"][eng]
            d[sem] = d.get(sem, 0) + inc

    def val_load(self, t):
        self.nvals = getattr(self, "nvals", 0) + 1
        key = self.nvals
        for e in ENGS:
            waits = self._deps(e, _bufs([t]), [])
            self.streams[e].append(("load", waits, key, t.ap))
        return key

    def cond_begin(self, key, thr):
        if not self.rstack:
            for e in ENGS:
                if self.ecount[e] >= SEM_LIMIT - 6000:
                    self.esem[e] = self.new_sem("e_" + e)
                    self.ecount[e] = 0
        start = {}
        for e in ENGS:
            start[self.esem[e]] = self.ecount[e]
        for b in self.dma_bufs:
            start[b.dsem] = b.dcount
        rg = {"key": key, "thr": thr, "start": start,
              "known0": {e: dict(self.known[e]) for e in ENGS},
              "ext": {e: {} for e in ENGS}, "incs": {e: {} for e in ENGS}}
        self.rstack.append(rg)
        for e in ENGS:
            self.streams[e].append(("begin", rg))

    def cond_end(self):
        rg = self.rstack.pop()
        for e in ENGS:
            kn = dict(rg["known0"][e])
            ew = []
            for sem, val in rg["ext"][e].items():
                if kn.get(sem, 0) < val:
                    kn[sem] = val
                    ew.append((sem, val))
            rg["ext"][e] = ew
            self.known[e] = kn
            self.streams[e].append(("end", rg))

    def op(self, eng, fn, rd=(), wr=()):
        rd = _bufs(rd)
        wr = _bufs(wr)
        waits = self._deps(eng, rd, wr)
        if self.ecount[eng] >= SEM_LIMIT and not self.rstack:
            self.esem[eng] = self.new_sem("e_" + eng)
            self.ecount[eng] = 0
        self.ecount[eng] += 1
        sem = self.esem[eng]
        val = self.ecount[eng]
        self.streams[eng].append((waits, fn, sem, 1))
        self._note_inc(eng, sem, 1)
        for b in rd:
            if b.r.get(sem, 0) < val:
                b.r[sem] = val
        for b in wr:
            b.w = {sem: val}
            b.r = {}
        self.n_ops += 1

    def dma(self, eng, out, in_, chan=None, **kw):
        rd = _bufs([in_])
        wr = _bufs([out])
        if chan is None:
            chan = out.buf
        if chan.dsem is None:
            chan.dsem = self.new_sem("d_" + chan.name)
            self.dma_bufs.append(chan)
        waits = self._deps(eng, rd, wr, skip_waw=chan.dsem)
        chan.dcount += 16
        sem, val = chan.dsem, chan.dcount
        oap, iap = out.ap, in_.ap
        self.streams[eng].append((waits, lambda e: e.dma_start(out=oap, in_=iap, **kw), sem, 16))
        self._note_inc(eng, sem, 16)
        for b in rd:
            if b.r.get(sem, 0) < val:
                b.r[sem] = val
        for b in wr:
            b.w = {sem: val}
            b.r = {}
        self.n_ops += 1

    def idma(self, out, in_, idx, scatter, chan):
        rd = _bufs([in_, idx])
        wr = _bufs([out])
        if chan.dsem is None:
            chan.dsem = self.new_sem("d_" + chan.name)
            self.dma_bufs.append(chan)
        waits = self._deps("gpsimd", rd, wr, skip_waw=chan.dsem)
        chan.dcount += 16
        sem, val = chan.dsem, chan.dcount
        oap, iap, xap = out.ap, in_.ap, idx.ap
        if scatter:
            fn = lambda e: e.indirect_dma_start(out=oap, out_offset=bass.IndirectOffsetOnAxis(xap, 0), in_=iap, in_offset=None)
        else:
            fn = lambda e: e.indirect_dma_start(out=oap, out_offset=None, in_=iap, in_offset=bass.IndirectOffsetOnAxis(xap, 0))
        self.streams["gpsimd"].append((waits, fn, sem, 16))
        self._note_inc("gpsimd", sem, 16)
        for b in rd:
            if b.r.get(sem, 0) < val:
                b.r[sem] = val
        for b in wr:
            b.w = {sem: val}
            b.r = {}
        self.n_ops += 1

    def _all_tokens(self):
        tokens = {}
        for e in ENGS:
            if self.ecount[e] > 0:
                tokens[self.esem[e]] = self.ecount[e]
        for b in self.dma_bufs:
            tokens[b.dsem] = b.dcount
        return tokens

    def barrier(self):
        tokens = self._all_tokens()
        for e in ENGS:
            waits = self._need(e, dict(tokens))
            if waits:
                self.streams[e].append((waits, None, None, 0))

    def final_wait(self, eng="sync"):
        waits = self._need(eng, self._all_tokens())
        self.streams[eng].append((waits, None, None, 0))

    def emit(self):
        nc = self.nc
        with nc.Block() as block:
            def run(name):
                def body(e):
                    items = self.streams[name]
                    vals = {}

                    def run_items(lst):
                        i = 0
                        while i < len(lst):
                            it = lst[i]
                            if it[0] == "load":
                                for s, v in it[1]:
                                    e.wait_ge(s, v)
                                if "reg" not in vals:
                                    vals["reg"] = e.alloc_register("cnd_" + name)
                                e.load(vals["reg"], it[3])
                                vals["key"] = it[2]
                                i += 1
                            elif it[0] == "begin":
                                rg = it[1]
                                assert vals["key"] == rg["key"]
                                j = i + 1
                                while not (lst[j][0] == "end" and lst[j][1] is rg):
                                    j += 1
                                bodyl = lst[i + 1:j]
                                with e.If_cmp(vals["reg"], rg["thr"], "IS_LE"):
                                    for s, v in rg["ext"][name]:
                                        e.wait_ge(s, v)
                                    for s, tot in rg["incs"][name].items():
                                        if rg["start"].get(s, 0) > 0:
                                            e.wait_ge(s, rg["start"][s])
                                        e.nop().then_inc(s, tot)
                                with e.Else():
                                    run_items(bodyl)
                                i = j + 1
                            else:
                                waits, fn, sem, inc = it
                                for s, v in waits:
                                    e.wait_ge(s, v)
                                if fn is not None:
                                    fn(e).then_inc(sem, inc)
                                i += 1
                    run_items(items)
                return body
            block.tensor(run("tensor"))
            block.vector(run("vector"))
            block.scalar(run("scalar"))
            block.gpsimd(run("gpsimd"))
            block.sync(run("sync"))

    def ps(self, name, shape, dtype):
        t = self.stack.enter_context(self.nc.psum_tensor(name, list(shape), dtype))
        return T(t[:], Buf(name))

    def dram(self, name, shape, dtype, kind):
        t = self.nc.dram_tensor(name, list(shape), dtype, kind=kind)
        return T(t.ap(), Buf(name))

    def mm(self, out, lhsT, rhs, start=True, stop=True, **kw):
        rd = [lhsT, rhs] + ([] if start else [out])
        self.op("tensor", lambda e: e.matmul(out.ap, lhsT.ap, rhs.ap, start=start, stop=stop, **kw),
                rd=rd, wr=[out])

    def tr(self, out, in_, ident):
        self.op("tensor", lambda e: e.transpose(out.ap, in_.ap, ident.ap), rd=[in_, ident], wr=[out])

    def act(self, out, in_, func, bias=None, scale=None, accum=None):
        kw = {}
        rd = [in_]
        if bias is not None:
            if isinstance(bias, T):
                kw["bias"] = bias.ap
                rd.append(bias)
            else:
                kw["bias"] = bias
        if scale is not None:
            if isinstance(scale, T):
                kw["scale"] = scale.ap
                rd.append(scale)
            else:
                kw["scale"] = scale
        wr = [out]
        if accum is not None:
            kw["accum_out"] = accum.ap
            wr.append(accum)
        self.op("scalar", lambda e: e.activation(out.ap, in_.ap, func, **kw), rd=rd, wr=wr)

    def ts(self, eng, out, in0, s1, s2, op0, op1=None, accum=None):
        rd = [in0]
        a1, a2 = s1, s2
        if isinstance(s1, T):
            rd.append(s1)
            a1 = s1.ap
        if isinstance(s2, T):
            rd.append(s2)
            a2 = s2.ap
        kw = {}
        wr = [out]
        if op1 is not None:
            kw["op1"] = op1
        if accum is not None:
            kw["accum_out"] = accum.ap
            wr.append(accum)
        self.op(eng, lambda e: e.tensor_scalar(out.ap, in0.ap, a1, a2, op0, **kw), rd=rd, wr=wr)

    def tt(self, eng, out, in0, in1, op):
        self.op(eng, lambda e: e.tensor_tensor(out.ap, in0.ap, in1.ap, op), rd=[in0, in1], wr=[out])

    def stt(self, out, in0, scalar, in1, op0, op1):
        rd = [in0, in1]
        a = scalar
        if isinstance(scalar, T):
            rd.append(scalar)
            a = scalar.ap
        self.op("vector", lambda e: e.scalar_tensor_tensor(out.ap, in0.ap, a, in1.ap, op0, op1),
                rd=rd, wr=[out])

    def copy(self, eng, out, in_):
        if eng == "scalar":
            self.op(eng, lambda e: e.copy(out.ap, in_.ap), rd=[in_], wr=[out])
        else:
            self.op(eng, lambda e: e.tensor_copy(out.ap, in_.ap), rd=[in_], wr=[out])

    def memset(self, eng, out, val):
        self.op(eng, lambda e: e.memset(out.ap, val), rd=[], wr=[out])

    def recip(self, out, in_):
        self.op("vector", lambda e: e.reciprocal(out.ap, in_.ap), rd=[in_], wr=[out])

    def reduce(self, out, in_, op):
        self.op("vector", lambda e: e.tensor_reduce(out.ap, in_.ap, AX.X, op), rd=[in_], wr=[out])

    def scan_add(self, out, ones, data, initial):
        rd = [ones, data]
        a = initial
        if isinstance(initial, T):
            rd.append(initial)
            a = initial.ap
        self.op("vector", lambda e: e.tensor_tensor_scan(out.ap, ones.ap, data.ap, a, ALU.mult, ALU.add),
                rd=rd, wr=[out])

    def vmax8(self, out, in_):
        self.op("vector", lambda e: e.max(out.ap, in_.ap), rd=[in_], wr=[out])


class Arena:
    def __init__(self, k, nbytes):
        self.k = k
        self.nbytes = nbytes
        t = k.stack.enter_context(k.nc.sbuf_tensor("arena", [128, nbytes], U8))
        self.ap = t[:]
        self.off = 0
        self.top = nbytes
        self.n = 0

    def alloc(self, name, shape, dtype, top=False):
        free = int(np.prod(shape[1:]))
        nb = free * DSZ[dtype]
        if top:
            off = (self.top - nb) // 64 * 64
            assert off >= self.off, f"arena overflow (top) at {name}"
            self.top = off
        else:
            off = (self.off + 63) // 64 * 64
            assert off + nb <= self.top, f"arena overflow at {name}: {off}+{nb} > {self.top}"
            self.off = off + nb
        ap = self.ap[0:shape[0], off:off + nb].bitcast(dtype)
        if len(shape) == 3:
            ap = ap.rearrange("p (a b) -> p a b", a=shape[1])
        elif len(shape) == 4:
            ap = ap.rearrange("p (a b c) -> p a b c", a=shape[1], b=shape[2])
        self.n += 1
        return T(ap, Buf(f"{name}_{self.n}"))

    def mark(self):
        return self.off

    def reset(self, m):
        self.off = m


def build_nc(debug=False):
    nc = bass.Bass("TRN2", target_bir_lowering=False)
    st = ExitStack()
    k = K(nc, st)
    A = Arena(k, 207 * 1024)

    def DI(name, shape, dt=F32):
        return k.dram(name, shape, dt, "ExternalInput")

    xo = DI("xo", [2048, 1024]); xp = DI("xp", [2048, 1024])
    cvec = DI("cvec", [1024]); posr = DI("posr", [4096], I32)
    ada_w = DI("ada_w", [1024, 6144]); ada_b = DI("ada_b", [6144])
    n1g = DI("norm1_g", [1024]); n2g = DI("norm2_g", [1024]); fing = DI("final_g", [1024])
    hgg = DI("hg_norm_g", [128]); lbl = DI("lb_logits", [2, 256])
    w_ka = DI("w_ka", [1024, 512]); w_kas = DI("w_kas", [1024, 512])
    w_ki = DI("w_ki", [1024, 128]); w_kis = DI("w_kis", [1024, 128])
    w_va = DI("w_va", [1024, 512])
    w_qa = DI("w_qa", [1024, 512]); w_qas = DI("w_qas", [1024, 512])
    w_qi = DI("w_qi", [1024, 256]); w_qis = DI("w_qis", [1024, 256])
    w_wi = DI("w_wi", [1024, 4])
    w_qb = DI("w_qb", [1024, 256]); w_fb = DI("w_fb", [1024, 256])
    w_ib = DI("w_ib", [1024, 512]); w_gb = DI("w_gb", [1024, 512])
    w_out = DI("w_out", [1024, 1024])
    rw = DI("router_w", [1024, 32]); rb = DI("router_b", [32])
    w1 = DI("moe_w1", [32, 1024, 2048]); b1 = DI("moe_b1", [32, 2048])
    w2 = DI("moe_w2", [32, 1024, 1024]); b2 = DI("moe_b2", [32, 1024])
    c_identb = DI("c_identb", [128, 128], BF16); c_identf = DI("c_identf", [128, 128])
    c_negI = DI("c_negI", [128, 128], BF16); c_tri2 = DI("c_tri2", [128, 128])
    c_diag = DI("c_diag", [128, 128]); c_cols = DI("c_cols", [128, 4])
    yout = k.dram("y", [2048, 1024], F32, "ExternalOutput")
    x1s = k.dram("x1s", [2048, 1024], F32, "Internal")
    XG = k.dram("XG", [32 * 2048, 1024], BF16, "Internal")
    YG = k.dram("YG", [32 * 2048, 1024], F32, "Internal")
    c_ltb = DI("c_ltb", [128, 128], BF16); c_erow = DI("c_erow", [128, 32])
    dbg = {}
    if debug:
        dbg["mixT"] = k.dram("dbg_mixT", [8, 128, 2048], F32, "ExternalOutput")
        dbg["x1"] = k.dram("dbg_x1", [2048, 1024], F32, "ExternalOutput")
        dbg["G"] = k.dram("dbg_G", [128, 16 * 32], F32, "ExternalOutput")

    banks = [k.ps(f"bank{i}", [128, 512], F32) for i in range(8)]

    def wload(dst, src, rows=8, eng="gpsimd"):
        k.dma(eng, dst, src.re("(kc p) n -> p kc n", p=128))

    identb = A.alloc("identb", [128, 128], BF16); k.dma("sync", identb, c_identb)
    identf = A.alloc("identf", [128, 128], F32); k.dma("sync", identf, c_identf)
    negI = A.alloc("negI", [128, 128], BF16); k.dma("sync", negI, c_negI)
    tri2 = A.alloc("tri2", [128, 128], F32); k.dma("sync", tri2, c_tri2)
    diagm = A.alloc("diagm", [128, 128], F32); k.dma("sync", diagm, c_diag)
    ccols = A.alloc("ccols", [128, 4], F32); k.dma("sync", ccols, c_cols)
    invf = ccols[:, 0:1]; sgnc = ccols[:, 1:2]; negb = ccols[:, 2:3]; sflag = ccols[:, 3:4]
    zerob = A.alloc("zerob", [128, 512], BF16); k.memset("vector", zerob, 0.0)
    onesf = A.alloc("onesf", [128, 512], F32); k.memset("vector", onesf, 1.0)
    g1bc = A.alloc("g1bc", [128, 1024], F32)
    g2bc = A.alloc("g2bc", [128, 1024], F32)
    fgbc = A.alloc("fgbc", [128, 1024], F32)
    s2row = A.alloc("s2row", [128, 1024], F32)
    sh2row = A.alloc("sh2row", [128, 1024], F32)
    k.dma("sync", fgbc, T(fing.ap.partition_broadcast(128), fing.buf))
    hgbc = A.alloc("hgbc", [128, 128], F32)
    k.dma("sync", hgbc, T(hgg.ap.partition_broadcast(128), hgg.buf))
    rbbc = A.alloc("rbbc", [128, 32], F32)
    k.dma("sync", rbbc, T(rb.ap.partition_broadcast(128), rb.buf))
    mcols = A.alloc("mcols", [128, 32], F32)
    s1col = A.alloc("s1col", [128, 8], F32); s2col = A.alloc("s2col", [128, 8], F32)
    lbcol = A.alloc("lbcol", [128, 2], F32); omlb = A.alloc("omlb", [128, 2], F32)
    Gall = A.alloc("Gall", [128, 16, 32], F32)
    smalls = Rot([A.alloc(f"sm{i}", [128, 1], F32) for i in range(12)])
    epsc = A.alloc("epsc", [128, 1], F32); k.memset("vector", epsc, EPS)
    small_mark = A.mark()
    mixA = A.alloc("mixA", [128, 4, 2048], BF16)
    persist_mark = A.mark()

    cT = A.alloc("cT", [128, 8], F32)
    k.dma("sync", cT, cvec.re("(j p) -> p j", p=128), allow_slow_non_contiguous=True)
    siluc = A.alloc("siluc", [128, 8], BF16)
    k.act(siluc, cT, AF.Silu)
    modrow = A.alloc("modrow", [1, 6144], F32)
    adab = A.alloc("adab", [1, 6144], F32)
    k.dma("sync", adab, ada_b.re("(o n) -> o n", o=1))
    ones1 = A.alloc("ones1", [1, 128], F32); k.memset("vector", ones1, 1.0)
    aslots = Rot([A.alloc(f"adaw{i}", [128, 8, 512], BF16) for i in range(2)])
    prot = Rot(banks[0:4])
    adaw_v = ada_w.re("(kc p) n -> p kc n", p=128)
    for nb in range(12):
        sl = aslots.next()
        k.dma("gpsimd", sl, adaw_v[:, :, nb * 512:(nb + 1) * 512])
        pb = prot.next()
        for kc in range(8):
            k.mm(pb[0:1, :], siluc[:, kc:kc + 1], sl[:, kc, :], start=(kc == 0), stop=(kc == 7))
        k.tt("vector", modrow[0:1, nb * 512:(nb + 1) * 512], pb[0:1, :], adab[0:1, nb * 512:(nb + 1) * 512], ALU.add)
    for (dst, off) in ((g1bc, 2048), (g2bc, 5120), (sh2row, 3072), (s2row, 4096)):
        for nb in range(2):
            pb = prot.next()
            k.mm(pb, ones1[0:1, :], modrow[0:1, off + nb * 512: off + (nb + 1) * 512])
            k.copy("vector", dst[:, nb * 512:(nb + 1) * 512], pb)
    pb = prot.next()
    for i, off in enumerate((0, 1024, 3072, 4096)):
        for j in range(8):
            k.mm(pb[:, i * 8 + j: i * 8 + j + 1], modrow[0:1, off + j * 128: off + (j + 1) * 128], ones1[0:1, 0:1])
    k.copy("vector", mcols, pb[:, 0:32])
    n2bc = A.alloc("n2bc", [128, 1024], F32)
    k.dma("sync", n2bc, T(n2g.ap.partition_broadcast(128), n2g.buf))
    k.ts("vector", s2row, s2row, 1.0, None, ALU.add)
    k.tt("vector", s2row, s2row, n2bc, ALU.mult)
    gcol = A.alloc("gcol", [128, 16], F32)
    k.dma("sync", gcol[:, 0:8], n1g.re("(j p) -> p j", p=128), allow_slow_non_contiguous=True)
    k.dma("sync", gcol[:, 8:16], n2g.re("(j p) -> p j", p=128), allow_slow_non_contiguous=True)
    tmp8 = A.alloc("tmp8", [128, 8], F32)
    k.ts("vector", tmp8, mcols[:, 8:16], 1.0, None, ALU.add)
    k.tt("vector", s1col, tmp8, gcol[:, 0:8], ALU.mult)
    tmp8b = A.alloc("tmp8b", [128, 8], F32)
    k.ts("vector", tmp8b, mcols[:, 24:32], 1.0, None, ALU.add)
    k.tt("vector", s2col, tmp8b, gcol[:, 8:16], ALU.mult)
    sh1col = mcols[:, 0:8]; sh2col = mcols[:, 16:24]
    lbl_sb = A.alloc("lbl_sb", [128, 2, 2], F32)
    for l in range(2):
        k.dma("sync", lbl_sb[:, l, :], lbl[l, :].re("(c p) -> p c", p=128), allow_slow_non_contiguous=True)
    dl = A.alloc("dl", [128, 2], F32)
    k.tt("vector", dl, lbl_sb[:, 0, :], lbl_sb[:, 1, :], ALU.subtract)
    k.act(lbcol, dl, AF.Sigmoid)
    k.ts("vector", omlb, lbcol, -1.0, 1.0, ALU.mult, ALU.add)
    k.barrier()
    A.reset(persist_mark)

    def hT_steps(src, tile0, ntiles, scol, shcol, hT, xslots, tmps, prot):
        sqj, xns, tmpf = tmps
        sbc = scol.unsq(2).bc([128, 8, 128])
        shbc = shcol.unsq(2).bc([128, 8, 128])

        def norm(i):
            xt = xslots.next()
            k.dma("sync", xt, src[(tile0 + i) * 128:(tile0 + i + 1) * 128, :])
            ss = smalls.next(); rs = smalls.next(); rstd = smalls.next()
            k.act(sqj, xt, AF.Square, accum=ss)
            k.act(rs, ss, AF.Sqrt, scale=1.0 / 1024, bias=epsc)
            k.recip(rstd, rs)
            xn = xns[i % 2]
            k.act(xn, xt, AF.Identity, scale=rstd)
            return xn

        def evac(i, xn):
            pb = prot.next()
            pv = pb.bitcast(BF16).re("p (j t) -> p j t", j=8)
            for j in range(8):
                k.tr(pv[:, j, :], xn[:, j * 128:(j + 1) * 128], identb)
            k.tt("vector", tmpf, pv, sbc, ALU.mult)
            k.tt("vector", hT[:, :, i * 128:(i + 1) * 128], tmpf, shbc, ALU.add)

        state = {}
        steps = [lambda: state.__setitem__(0, norm(0))]
        for i in range(ntiles):
            def st(i=i):
                if i + 1 < ntiles:
                    state[i + 1] = norm(i + 1)
                evac(i, state[i])
            steps.append(st)
        return steps

    def make_hT(*args):
        for stp_ in hT_steps(*args):
            stp_()

    def rope_tables(slot0, n, cosg, sing, tmps):
        posi, pf, ang, kf = tmps
        ki32 = posi
        k.dma("sync", posi[:, 0:n], T(posr.ap[slot0:slot0 + n].partition_broadcast(128), posr.buf))
        k.copy("vector", pf[:, 0:n], posi[:, 0:n])
        k.ts("vector", ang[:, 0:n], pf[:, 0:n], invf, None, ALU.mult)
        k.ts("vector", kf[:, 0:n], ang[:, 0:n], 1.0 / (2 * PI), None, ALU.mult)
        k.copy("vector", ki32[:, 0:n], kf[:, 0:n])
        k.copy("vector", kf[:, 0:n], ki32[:, 0:n])
        C1 = 6.28125
        C2 = 2 * PI - C1
        k.stt(ang[:, 0:n], kf[:, 0:n], -C1, ang[:, 0:n], ALU.mult, ALU.add)
        k.stt(ang[:, 0:n], kf[:, 0:n], -C2, ang[:, 0:n], ALU.mult, ALU.add)
        k.ts("vector", ang[:, 0:n], ang[:, 0:n], -PI, PI, ALU.max, ALU.min)
        k.act(sing[:, 0:n], ang[:, 0:n], AF.Sin, scale=sgnc)
        k.ts("vector", pf[:, 0:n], ang[:, 0:n], PI / 2, None, ALU.add)
        k.ts("vector", kf[:, 0:n], pf[:, 0:n], PI, -2 * PI, ALU.is_gt, ALU.mult)
        k.tt("vector", pf[:, 0:n], pf[:, 0:n], kf[:, 0:n], ALU.add)
        k.ts("vector", pf[:, 0:n], pf[:, 0:n], -PI, PI, ALU.max, ALU.min)
        k.act(cosg[:, 0:n], pf[:, 0:n], AF.Sin)

    def proj_fm(dst_fn, wsb, wsw, nchunks, hT, n, cosg, sing, prot, rtmps, only=None):
        t1, t2 = rtmps
        for c in (range(nchunks) if only is None else [only]):
            pa = prot.next()
            for kc in range(8):
                k.mm(pa[:, 0:n], wsb[:, kc, c * 128:(c + 1) * 128], hT[:, kc, 0:n], start=(kc == 0), stop=(kc == 7))
            if wsw is None:
                k.copy("scalar", dst_fn(c), pa[:, 0:n])
                continue
            pb2 = prot.next()
            for kc in range(8):
                k.mm(pb2[:, 0:n], wsw[:, kc, c * 128:(c + 1) * 128], hT[:, kc, 0:n], start=(kc == 0), stop=(kc == 7))
            k.tt("vector", t1[:, 0:n], pa[:, 0:n], cosg[:, 0:n], ALU.mult)
            k.tt("vector", t2[:, 0:n], pb2[:, 0:n], sing[:, 0:n], ALU.mult)
            dst_fn(c, t1[:, 0:n], t2[:, 0:n])

    kaT = A.alloc("kaT", [128, 4, 4096], BF16)
    kiT = A.alloc("kiT", [128, 4096], BF16)
    va = A.alloc("va", [128, NT_ALL, 8, 65], BF16)
    k.memset("gpsimd", va[:, :, :, 64:65], 1.0)
    kside_mark = A.mark()
    wka = A.alloc("wka", [128, 8, 512], BF16); wload(wka, w_ka)
    wkas = A.alloc("wkas", [128, 8, 512], BF16); wload(wkas, w_kas)
    wki = A.alloc("wki", [128, 8, 128], BF16); wload(wki, w_ki)
    wkis = A.alloc("wkis", [128, 8, 128], BF16); wload(wkis, w_kis)
    wva = A.alloc("wva", [128, 8, 512], BF16); wload(wva, w_va)
    hT = A.alloc("hT", [128, 8, 512], BF16)
    xslots = Rot([A.alloc(f"xs{i}", [128, 1024], F32) for i in range(2)])
    ntmps = (A.alloc("sqj", [128, 1024], BF16), [A.alloc(f"xn{i_}", [128, 1024], BF16) for i_ in range(2)], A.alloc("tmpf", [128, 8, 128], F32))
    cosg = A.alloc("cosg", [128, 512], F32); sing = A.alloc("sing", [128, 512], F32)
    rtm = (A.alloc("posi", [128, 512], I32), A.alloc("pf", [128, 512], F32), A.alloc("ang", [128, 512], F32),
           A.alloc("kf", [128, 512], F32))
    rt12 = (rtm[1], rtm[3])
    prot = Rot(banks)
    hTs = [hT, A.alloc("hT2", [128, 8, 512], BF16)]

    def p1_units(g, hTg):
        def dst_ka(c, a=None, b=None):
            k.tt("gpsimd", kaT[:, c, g * 512:(g + 1) * 512], a, b, ALU.add)

        def dst_ki(c, a=None, b=None):
            k.tt("gpsimd", kiT[:, g * 512:(g + 1) * 512], a, b, ALU.add)
        units = []
        for c in range(4):
            units.append(lambda c=c: proj_fm(dst_ka, wka, wkas, 4, hTg, 512, cosg, sing, prot, rt12, only=c))
        units.append(lambda: proj_fm(dst_ki, wki, wkis, 1, hTg, 512, cosg, sing, prot, rt12, only=0))
        for i in range(4):
            def va_unit(i=i):
                pa = prot.next()
                for kc in range(8):
                    k.mm(pa, hTg[:, kc, i * 128:(i + 1) * 128], wva[:, kc, :], start=(kc == 0), stop=(kc == 7))
                k.copy("scalar", va[:, g * 4 + i, :, 0:64], pa.re("p (h d) -> p h d", h=8))
            units.append(va_unit)
        return units

    def p1_tiles(g):
        return hT_steps(xp if g < 4 else xo, (g % 4) * 4, 4, s1col, sh1col, hTs[g % 2], xslots, ntmps, prot)

    rope_tables(0, 512, cosg, sing, rtm)
    for stp_ in p1_tiles(0):
        stp_()
    for g in range(8):
        units = p1_units(g, hTs[g % 2])
        tsteps = p1_tiles(g + 1) if g + 1 < 8 else []
        ti = 0
        for ui, u in enumerate(units):
            u()
            if ui % 2 == 1 and ti < len(tsteps):
                tsteps[ti]()
                ti += 1
        while ti < len(tsteps):
            tsteps[ti]()
            ti += 1
        if g + 1 < 8:
            rope_tables((g + 1) * 512, 512, cosg, sing, rtm)
    k.barrier()
    A.reset(kside_mark)

    qaT = A.alloc("qaT", [128, 4, 2048], BF16)
    qiT = A.alloc("qiT", [128, 2, 2048], BF16)
    wis = A.alloc("wis", [128, 16, 4], F32)
    qside_mark = A.mark()
    wqa = A.alloc("wqa", [128, 8, 512], BF16); wload(wqa, w_qa)
    wqas = A.alloc("wqas", [128, 8, 512], BF16); wload(wqas, w_qas)
    wqi = A.alloc("wqi", [128, 8, 256], BF16); wload(wqi, w_qi)
    wqis = A.alloc("wqis", [128, 8, 256], BF16); wload(wqis, w_qis)
    wwi = A.alloc("wwi", [128, 8, 4], BF16); wload(wwi, w_wi)
    hT = A.alloc("hT", [128, 8, 512], BF16)
    xslots = Rot([A.alloc(f"xs{i}", [128, 1024], F32) for i in range(2)])
    ntmps = (A.alloc("sqj", [128, 1024], BF16), [A.alloc(f"xn{i_}", [128, 1024], BF16) for i_ in range(2)], A.alloc("tmpf", [128, 8, 128], F32))
    cosg = A.alloc("cosg", [128, 512], F32); sing = A.alloc("sing", [128, 512], F32)
    rtm = (A.alloc("posi", [128, 512], I32), A.alloc("pf", [128, 512], F32), A.alloc("ang", [128, 512], F32),
           A.alloc("kf", [128, 512], F32))
    rt12 = (rtm[1], rtm[3])
    for g in range(4):
        rope_tables(2048 + g * 512, 512, cosg, sing, rtm)
        make_hT(xo, g * 4, 4, s1col, sh1col, hT, xslots, ntmps, prot)

        def dst_qa(c, a=None, b=None, g=g):
            k.tt("gpsimd", qaT[:, c, g * 512:(g + 1) * 512], a, b, ALU.add)
        proj_fm(dst_qa, wqa, wqas, 4, hT, 512, cosg, sing, prot, rt12)

        def dst_qi(c, a=None, b=None, g=g):
            k.tt("gpsimd", qiT[:, c, g * 512:(g + 1) * 512], a, b, ALU.add)
        proj_fm(dst_qi, wqi, wqis, 2, hT, 512, cosg, sing, prot, rt12)
        for i in range(4):
            pa = prot.next()
            for kc in range(8):
                k.mm(pa[:, 0:4], hT[:, kc, i * 128:(i + 1) * 128], wwi[:, kc, :], start=(kc == 0), stop=(kc == 7))
            k.ts("vector", wis[:, g * 4 + i, :], pa[:, 0:4], 0.5 * 0.125, None, ALU.mult)
    k.barrier()
    A.reset(qside_mark)

    sc = A.alloc("sc", [128, 4096], F32)
    mbars = Rot([A.alloc(f"mbar{i}", [128, 4096], BF16) for i in range(2)])
    rts = Rot([A.alloc(f"rt{i}", [128, 512], F32) for i in range(4)])
    pTs = Rot([A.alloc(f"pT{i}", [128, 512], BF16) for i in range(3)])
    absw = A.alloc("absw", [128, 4], F32); sgnw = A.alloc("sgnw", [128, 4], F32)
    lo = A.alloc("lo", [128, 1], F32); mid = A.alloc("mid", [128, 1], F32)
    cnt = A.alloc("cnt", [128, 1], F32); dlt = A.alloc("dlt", [128, 1], F32)
    rmax = A.alloc("rmax", [128, 1], F32)
    sga = A.alloc("sga", [128, 1], F32); tcomb = A.alloc("tcomb", [128, 1], F32)
    rden = A.alloc("rden", [128, 8], F32)
    oa = A.alloc("oa", [128, 8, 64], BF16)
    zqs = Rot([A.alloc(f"zq{i}", [128, 4, 2, 128], BF16) for i in range(2)])
    zis = Rot([A.alloc(f"zi{i}", [128, 2, 2, 128], BF16) for i in range(2)])
    for z in zqs.items + zis.items:
        k.memset("gpsimd", z, 0.0)
    sc_rot = Rot(banks[0:2])
    s_rot = Rot(banks[2:4])
    po_rot = Rot([(banks[4], banks[5]), (banks[6], banks[7])])

    def stage_a(n):
        nkt = 16 + n + 1
        nk = nkt * 128
        qs = slice(n * 128, (n + 1) * 128)
        zq = zqs.next(); zi = zis.next(); mbar = mbars.next()
        for hp in range(2):
            ph = slice(hp * 64, (hp + 1) * 64)
            k.copy("gpsimd", zq[ph, :, hp, :], qaT[ph, :, qs])
            k.copy("gpsimd", zi[ph, :, hp, :], qiT[ph, :, qs])
        k.ts("vector", sgnw, wis[:, n, :], -1.0, None, ALU.mult)
        k.tt("vector", absw, wis[:, n, :], sgnw, ALU.max)
        k.ts("vector", sgnw, wis[:, n, :], 0.0, 2.0, ALU.is_ge, ALU.mult)
        k.ts("vector", sgnw, sgnw, -1.0, None, ALU.add)
        for kb in range((nkt + 3) // 4):
            ncols = min(512, nk - kb * 512)
            cs = slice(kb * 512, kb * 512 + ncols)
            for h in range(4):
                c, hp = h // 2, h % 2
                pa = sc_rot.next()
                k.mm(pa[:, 0:ncols], zi[:, c, hp, :], kiT[:, cs])
                rt = rts.next()
                k.act(rt[:, 0:ncols], pa[:, 0:ncols], AF.Relu, scale=absw[:, h:h + 1])
                if h == 0:
                    k.ts("vector", sc[:, cs], rt[:, 0:ncols], sgnw[:, 0:1], None, ALU.mult)
                else:
                    k.stt(sc[:, cs], rt[:, 0:ncols], sgnw[:, h:h + 1], sc[:, cs], ALU.mult, ALU.add)
        k.ts("vector", sc[:, 0:2048], sc[:, 0:2048], negb, None, ALU.add)
        ds_ = slice((nkt - 1) * 128, nkt * 128)
        k.tt("vector", sc[:, ds_], sc[:, ds_], diagm, ALU.add)
        k.reduce(rmax, sc[:, 0:nk], ALU.max)
        k.ts("vector", mid, rmax, -RNG + RNG / 2, None, ALU.add)
        for it in range(NBIS):
            wn = RNG / (2 ** (it + 2))
            k.ts("vector", mbar[:, 0:nk], sc[:, 0:nk], mid, None, ALU.is_gt, op1=ALU.add, accum=cnt)
            k.ts("vector", dlt, cnt, 255.5, 2.0 * wn, ALU.is_ge, ALU.mult)
            k.stt(mid, dlt, -wn, mid, ALU.add, ALU.add)
        k.ts("vector", lo, mid, -RNG / (2 ** (NBIS + 1)), None, ALU.add)
        k.ts("vector", mbar[:, 0:nk], sc[:, 0:nk], lo, None, ALU.is_le)
        return (n, nkt, qs, zq, mbar)

    def stage_b(ctx):
        n, nkt, qs, zq, mbar = ctx
        poA, poB = po_rot.next()
        k.mm(poA[:, 0:260], zerob[:, 0:128], zerob[:, 0:260], start=True, stop=False, skip_group_check=True)
        k.mm(poB[:, 0:260], zerob[:, 0:128], zerob[:, 0:260], start=True, stop=False, skip_group_check=True)
        groups = [(j, gq) for j in range(nkt) for gq in range(2)]

        def logits(j, gq):
            ks_ = slice(j * 128, (j + 1) * 128)
            pS = s_rot.next()
            for hh in range(4):
                h = gq * 4 + hh
                c, hp = h // 2, h % 2
                k.mm(pS[:, hh * 128:(hh + 1) * 128], kaT[:, c, ks_], zq[:, c, hp, :], start=True, stop=False)
                k.mm(pS[:, hh * 128:(hh + 1) * 128], mbar[:, ks_], negI, start=False, stop=True)
            pT = pTs.next()
            k.act(pT, pS, AF.Exp, scale=0.125)
            return pT

        def pv_mm(j, gq, pT):
            po = poA if gq == 0 else poB
            for hh in range(4):
                h = gq * 4 + hh
                k.mm(po[:, hh * 65:(hh + 1) * 65], pT[:, hh * 128:(hh + 1) * 128], va[:, j, h, :],
                     start=False, stop=(j == nkt - 1), skip_group_check=True)

        pend = None
        for (j, gq) in groups:
            pT = logits(j, gq)
            if pend is not None:
                pv_mm(*pend)
            pend = (j, gq, pT)
        pv_mm(*pend)
        for gq, po in enumerate((poA, poB)):
            pv = po[:, 0:260].re("p (h d) -> p h d", h=4)
            k.recip(rden[:, gq * 4:(gq + 1) * 4], pv[:, :, 64])
            k.tt("vector", oa[:, gq * 4:(gq + 1) * 4, :], pv[:, :, 0:64],
                 rden[:, gq * 4:(gq + 1) * 4].unsq(2).bc([128, 4, 64]), ALU.mult)
        pa = s_rot.next()
        pv = pa.bitcast(BF16).re("p (j t) -> p j t", j=8)
        oaf = oa.re("p h d -> p (h d)")
        for c in range(4):
            k.tr(pv[:, c, :], oaf[:, c * 128:(c + 1) * 128], identb)
        k.copy("scalar", mixA[:, :, qs], pv[:, 0:4, :])

    ctx = stage_a(0)
    for n in range(NT_OWN):
        nxt = stage_a(n + 1) if n + 1 < NT_OWN else None
        stage_b(ctx)
        ctx = nxt
    k.barrier()
    A.reset(persist_mark)

    mixB = A.alloc("mixB", [128, 4, 2048], BF16)
    hg_mark = A.mark()
    wqb = A.alloc("wqb", [128, 8, 256], BF16); wload(wqb, w_qb)
    wfb = A.alloc("wfb", [128, 8, 256], BF16); wload(wfb, w_fb)
    wib = A.alloc("wib", [128, 8, 512], BF16); wload(wib, w_ib)
    wgb = A.alloc("wgb", [128, 8, 512], BF16); wload(wgb, w_gb)
    hT = A.alloc("hT", [128, 8, 512], BF16)
    xslots = Rot([A.alloc(f"xs{i}", [128, 1024], F32) for i in range(2)])
    ntmps = (A.alloc("sqj", [128, 1024], BF16), [A.alloc(f"xn{i_}", [128, 1024], BF16) for i_ in range(2)], A.alloc("tmpf", [128, 8, 128], F32))
    vtok_s = [A.alloc(f"vtok{i}", [128, 4, 512], BF16) for i in range(2)]
    sgt_s = [A.alloc(f"sgt{i}", [128, 4, 512], BF16) for i in range(2)]
    sgf = A.alloc("sgf", [128, 512], F32)
    S = A.alloc("S", [128, 2, 128], F32); k.memset("vector", S, 0.0)
    Sbf = [A.alloc(f"Sbf{i}", [128, 2, 128], BF16) for i in range(8)]
    Bext = [A.alloc(f"Bext{c2}", [128, 513], F32) for c2 in range(2)]
    for c2 in range(2):
        k.memset("vector", Bext[c2][:, 0:1], 0.0)
    sig = A.alloc("sig", [128, 512], F32); fT = A.alloc("fT", [128, 512], F32)
    logf = A.alloc("logf", [128, 512], F32); omf = A.alloc("omf", [128, 512], F32)
    qbf = A.alloc("qbf", [128, 512], F32)
    D1 = A.alloc("D1", [128, 8, 64], F32); D3 = A.alloc("D3", [128, 8, 64], F32); D4 = A.alloc("D4", [128, 8, 64], F32)
    E1 = A.alloc("E1", [128, 512], F32); E2 = A.alloc("E2", [128, 512], F32)
    E3 = A.alloc("E3", [128, 512], F32); E4 = A.alloc("E4", [128, 512], F32)
    dd = A.alloc("dd", [128, 8], F32)
    gsets = []
    for si in range(2):
        dec_ = [A.alloc(f"dec{si}_{c2}", [128, 8], F32) for c2 in range(2)]
        qtZ_ = [A.alloc(f"qtZ{si}_{c2}", [128, 2, 512], BF16) for c2 in range(2)]
        qeZ_ = [A.alloc(f"qeZ{si}_{c2}", [128, 2, 512], BF16) for c2 in range(2)]
        ktT_ = [A.alloc(f"ktT{si}_{c2}", [128, 512], BF16) for c2 in range(2)]
        kdT_ = [A.alloc(f"kdT{si}_{c2}", [128, 512], BF16) for c2 in range(2)]
        for c2 in range(2):
            k.memset("gpsimd", qtZ_[c2], 0.0)
            k.memset("gpsimd", qeZ_[c2], 0.0)
        gsets.append((vtok_s[si], sgt_s[si], dec_, qtZ_, qeZ_, ktT_, kdT_))
    kdZ = [[A.alloc(f"kdZ{c2}_{i}", [128, 2, 128], BF16) for i in range(2)] for c2 in range(2)]
    for c2 in range(2):
        for i in range(2):
            k.memset("gpsimd", kdZ[c2][i], 0.0)
    attS = [A.alloc(f"attS{i}", [128, 4, 128], BF16) for i in range(2)]
    for i in range(2):
        k.memset("gpsimd", attS[i], 0.0)
    ssq = A.alloc("ssq", [128, 4], F32); rs4 = A.alloc("rs4", [128, 4], F32); rstd4 = A.alloc("rstd4", [128, 4], F32)
    sqj2 = A.alloc("sqj2", [128, 128], BF16)
    ob1 = A.alloc("ob1", [128, 4, 128], F32); ob2 = A.alloc("ob2", [128, 4, 128], F32)
    obb = A.alloc("obb", [128, 512], BF16)
    prot = Rot(banks[0:4])
    kv_rot = Rot(banks[4:6])
    at_rot = Rot([(banks[6], banks[7])])
    tri2b = tri2.unsq(1).bc([128, 2, 128])
    def hg_front(g):
        own = g >= 4
        vtok, sgt, dec, qtZ, qeZ, ktT, kdT = gsets[g % 2]
        src = xo if own else xp
        steps = list(hT_steps(src, (g % 4) * 4, 4, s1col, sh1col, hT, xslots, ntmps, prot))
        for i in range(4):
            def _st(i=i):
                pa = prot.next()
                for kc in range(8):
                    k.mm(pa, hT[:, kc, i * 128:(i + 1) * 128], wib[:, kc, :], start=(kc == 0), stop=(kc == 7))
                k.copy("scalar", vtok[:, i, :], pa)
                if own:
                    pa = prot.next()
                    for kc in range(8):
                        k.mm(pa, hT[:, kc, i * 128:(i + 1) * 128], wgb[:, kc, :], start=(kc == 0), stop=(kc == 7))
                    k.act(sgf, pa, AF.Sigmoid)
                    k.tt("vector", sgt[:, i, :], sgf, pa, ALU.mult)
            steps.append(_st)
        for c2 in range(2):
            def _st(c2=c2):
                pa = prot.next()
                for kc in range(8):
                    k.mm(pa, wfb[:, kc, c2 * 128:(c2 + 1) * 128], hT[:, kc, :], start=(kc == 0), stop=(kc == 7))
                k.act(sig, pa, AF.Sigmoid)
                k.ts("vector", fT, sig, omlb[:, c2:c2 + 1], lbcol[:, c2:c2 + 1], ALU.mult, ALU.add)
                k.act(logf, fT, AF.Ln)
                k.ts("vector", omf, fT, -1.0, 1.0, ALU.mult, ALU.add)
                Bx = Bext[c2]
                k.scan_add(Bx[:, 1:513], onesf, logf, Bx[:, 0:1])
                Bg = Bx[:, 1:513].re("p (c t) -> p c t", c=8)
                Bprev = Bx[:, 0:512].re("p (c t) -> p c t", c=8)[:, :, 0:1]
                Blast = Bg[:, :, 63:64]
                Bmid = Bg[:, :, 31:32]
                k.tt("vector", D4, Blast.bc([128, 8, 64]), Bg, ALU.subtract)
                k.act(E4, D4.re("p c t -> p (c t)"), AF.Exp)
                k.tt("vector", kdT[c2], omf, E4, ALU.mult)
                k.tt("vector", dd, Blast.re("p c o -> p (c o)"), Bprev.re("p c o -> p (c o)"), ALU.subtract)
                k.act(dec[c2], dd, AF.Exp)
                if own:
                    pq = prot.next()
                    for kc in range(8):
                        k.mm(pq, wqb[:, kc, c2 * 128:(c2 + 1) * 128], hT[:, kc, :], start=(kc == 0), stop=(kc == 7))
                    k.copy("scalar", qbf, pq)
                    k.tt("vector", D1, Bg, Bmid.bc([128, 8, 64]), ALU.subtract)
                    k.act(E1, D1.re("p c t -> p (c t)"), AF.Exp)
                    k.act(E2, D1.re("p c t -> p (c t)"), AF.Exp, scale=-1.0)
                    k.tt("vector", D3, Bg, Bprev.bc([128, 8, 64]), ALU.subtract)
                    k.act(E3, D3.re("p c t -> p (c t)"), AF.Exp)
                    k.tt("vector", ktT[c2], omf, E2, ALU.mult)
                    for hp in range(2):
                        ph = slice(hp * 64, (hp + 1) * 64)
                        k.tt("vector", qtZ[c2][ph, hp, :], qbf[ph, :], E1[ph, :], ALU.mult)
                        k.tt("vector", qeZ[c2][ph, hp, :], qbf[ph, :], E3[ph, :], ALU.mult)
                k.copy("vector", Bx[:, 0:1], Bx[:, 512:513])

            steps.append(_st)
        return steps

    def hg_back(g):
        own = g >= 4
        vtok, sgt, dec, qtZ, qeZ, ktT, kdT = gsets[g % 2]
        steps = []
        for i in range(4):
            def _st(i=i):
                ts_ = slice(i * 128, (i + 1) * 128)
                kz = []
                for c2 in range(2):
                    pa = prot.next()
                    pv = pa.bitcast(BF16)
                    k.tr(pv[:, 0:128], kdT[c2][:, ts_], identb)
                    kzz = kdZ[c2][i % 2]
                    for cp in range(2):
                        k.copy("scalar", kzz[cp * 64:(cp + 1) * 64, cp, :], pv[cp * 64:(cp + 1) * 64, 0:128])
                    kz.append(kzz)
                if own:
                    pA0, pA1 = at_rot.next()
                    for cp in range(2):
                        cs_ = slice(i * 128 + cp * 64, i * 128 + (cp + 1) * 64)
                        for h in range(4):
                            c2, hp = h // 2, h % 2
                            pbk = pA0 if hp == 0 else pA1
                            k.mm(pbk[cp * 64:(cp + 1) * 64, c2 * 64:(c2 + 1) * 64], ktT[c2][:, cs_], qtZ[c2][:, hp, cs_])
                    aS = attS[i % 2]
                    for hp, pbk in enumerate((pA0, pA1)):
                        for c2 in range(2):
                            h = 2 * c2 + hp
                            for cp in range(2):
                                ph = slice(cp * 64, (cp + 1) * 64)
                                k.tt("vector", aS[ph, h, cp * 64:(cp + 1) * 64], pbk[ph, c2 * 64:(c2 + 1) * 64],
                                     tri2[ph, cp * 64:(cp + 1) * 64], ALU.mult)
                for cp in range(2):
                    cidx = i * 2 + cp
                    if own:
                        k.copy("scalar", Sbf[cidx], S)
                    pkv = kv_rot.next()
                    for h in range(4):
                        c2, hp = h // 2, h % 2
                        k.mm(pkv[hp * 64:(hp + 1) * 64, c2 * 128:(c2 + 1) * 128], kz[c2][:, cp, hp * 64:(hp + 1) * 64],
                             vtok[:, i, h * 128:(h + 1) * 128])
                    for c2 in range(2):
                        k.stt(S[:, c2, :], S[:, c2, :], dec[c2][:, cidx:cidx + 1], pkv[:, c2 * 128:(c2 + 1) * 128], ALU.mult, ALU.add)
                    if g == 3 and cidx == 7:
                        k.ts("vector", S.re("p a b -> p (a b)"), S.re("p a b -> p (a b)"), sflag, None, ALU.mult)
                if own:
                    po = prot.next()
                    for h in range(4):
                        c2, hp = h // 2, h % 2
                        k.mm(po[:, h * 128:(h + 1) * 128], aS[:, h, :], vtok[:, i, h * 128:(h + 1) * 128], start=True, stop=False)
                        for cp in range(2):
                            cidx = i * 2 + cp
                            cs_ = slice(i * 128 + cp * 64, i * 128 + (cp + 1) * 64)
                            k.mm(po[cp * 64:(cp + 1) * 64, h * 128:(h + 1) * 128], qeZ[c2][:, hp, cs_], Sbf[cidx][:, c2, :],
                                 start=False, stop=True)
                    pov = po.re("p (h e) -> p h e", h=4)
                    for h in range(4):
                        k.act(sqj2, po[:, h * 128:(h + 1) * 128], AF.Square, accum=ssq[:, h:h + 1])
                    k.act(rs4, ssq, AF.Sqrt, scale=1.0 / 128, bias=epsc)
                    k.recip(rstd4, rs4)
                    k.tt("vector", ob1, pov, rstd4.unsq(2).bc([128, 4, 128]), ALU.mult)
                    k.tt("vector", ob2, ob1, hgbc.unsq(1).bc([128, 4, 128]), ALU.mult)
                    k.tt("vector", obb, ob2.re("p h e -> p (h e)"), sgt[:, i, :], ALU.mult)
                    pa = prot.next()
                    pv = pa.bitcast(BF16).re("p (j t) -> p j t", j=8)
                    for c in range(4):
                        k.tr(pv[:, c, :], obb[:, c * 128:(c + 1) * 128], identb)
                    tt0 = (g - 4) * 512 + i * 128
                    k.copy("scalar", mixB[:, :, tt0:tt0 + 128], pv[:, 0:4, :])


            steps.append(_st)
        return steps

    for stp_ in hg_front(0):
        stp_()
    for g in range(8):
        nf = hg_front(g + 1) if g + 1 < 8 else []
        fi_ = 0
        for bstep in hg_back(g):
            for _ in range(3):
                if fi_ < len(nf):
                    nf[fi_]()
                    fi_ += 1
            bstep()
        while fi_ < len(nf):
            nf[fi_]()
            fi_ += 1
    k.barrier()
    A.reset(hg_mark)

    idxall = A.alloc("idxall", [128, 16, 4], I32, top=True)
    gkall = A.alloc("gkall", [128, 16, 4], F32, top=True)
    nblk_i = A.alloc("nblk_i", [128, 32], I32, top=True)
    p5_mark = A.mark()
    wo = A.alloc("wo", [128, 8, 1024], BF16); wload(wo, w_out)
    rwf = A.alloc("rwf", [128, 8, 32], F32)
    k.dma("sync", rwf, rw.re("(kc p) e -> p kc e", p=128))
    LTb = A.alloc("LTb", [128, 128], BF16); k.dma("sync", LTb, c_ltb)
    onesb = A.alloc("onesb", [128, 128], BF16); k.memset("vector", onesb, 1.0)
    erow = A.alloc("erow", [128, 32], F32); k.dma("sync", erow, c_erow)
    carry = A.alloc("carry", [128, 32], F32); k.memset("vector", carry, 0.0)
    xslots = Rot([A.alloc(f"xs{i}", [128, 1024], F32) for i in range(2)])
    x1r = Rot([A.alloc(f"x1_{i}", [128, 1024], F32) for i in range(2)])
    h2ts = Rot([A.alloc(f"h2tok{i}", [128, 1024], BF16) for i in range(2)])
    ytmp = A.alloc("ytmp", [128, 1024], F32)
    sqj = A.alloc("sqj", [128, 1024], BF16)
    xnf = A.alloc("xnf", [128, 1024], F32)
    h2f = A.alloc("h2f", [128, 8, 128], F32); h2ft = A.alloc("h2ft", [128, 8, 128], F32)
    lg = A.alloc("lg", [128, 32], F32); m8 = A.alloc("m8", [128, 8], F32)
    msk = A.alloc("msk", [128, 32], F32); ex = A.alloc("ex", [128, 32], F32)
    mskb = A.alloc("mskb", [128, 32], BF16)
    rank = A.alloc("rank", [128, 32], F32); keyt = A.alloc("keyt", [128, 32], F32)
    ek8 = A.alloc("ek8", [128, 8], F32); oh = A.alloc("oh", [128, 32], F32); ohr = A.alloc("ohr", [128, 32], F32)
    rk = A.alloc("rk", [128, 1], F32); slf = A.alloc("slf", [128, 1], F32)
    nmax = A.alloc("nmax", [128, 1], F32); zs = A.alloc("zs", [128, 1], F32); rz = A.alloc("rz", [128, 1], F32)
    y_rot = Rot([(banks[0], banks[1]), (banks[2], banks[3])])
    t_rot = Rot([(banks[4], banks[5]), (banks[6], banks[7])])
    s2bc = s2col.unsq(2).bc([128, 8, 128]); sh2bc = sh2col.unsq(2).bc([128, 8, 128])
    def p5_front(i):
        lg = lgs.next()
        ts_ = slice(i * 128, (i + 1) * 128)
        xt = xslots.next()
        k.dma("sync", xt, xo[ts_, :])
        py = y_rot.next()
        for nb in range(2):
            for kc in range(8):
                mixsrc = mixA if kc < 4 else mixB
                k.mm(py[nb], mixsrc[:, kc % 4, ts_], wo[:, kc, nb * 512:(nb + 1) * 512], start=(kc == 0), stop=(kc == 7))
        x1 = x1r.next()
        for nb in range(2):
            ns_ = slice(nb * 512, (nb + 1) * 512)
            k.tt("vector", ytmp[:, ns_], py[nb], g1bc[:, ns_], ALU.mult)
            k.tt("vector", x1[:, ns_], ytmp[:, ns_], xt[:, ns_], ALU.add)
        k.dma("sync", x1s[ts_, :], x1, chan=x1.buf)
        if debug:
            k.dma("sync", dbg["x1"][ts_, :], x1, chan=x1.buf)
        ss = smalls.next(); rs = smalls.next(); rstd = smalls.next()
        k.act(sqj, x1, AF.Square, accum=ss)
        k.act(rs, ss, AF.Sqrt, scale=1.0 / 1024, bias=epsc)
        k.recip(rstd, rs)
        k.act(xnf, x1, AF.Identity, scale=rstd)
        h2tok = h2ts.next()
        k.tt("vector", ytmp, xnf, s2row, ALU.mult)
        k.tt("vector", h2tok, ytmp, sh2row, ALU.add)
        pt = t_rot.next()
        for j in range(8):
            k.tr(pt[j // 4][:, (j % 4) * 128:(j % 4 + 1) * 128], xnf[:, j * 128:(j + 1) * 128], identf)
        for hb in range(2):
            pv = pt[hb].re("p (j t) -> p j t", j=4)
            k.tt("vector", h2ft[:, hb * 4:(hb + 1) * 4, :], pv, s2bc[:, hb * 4:(hb + 1) * 4, :], ALU.mult)
        k.tt("vector", h2f, h2ft, sh2bc, ALU.add)
        pl = y_rot.next()[0]
        for kc in range(8):
            k.mm(pl[:, 0:32], h2f[:, kc, :], rwf[:, kc, :], start=(kc == 0), stop=(kc == 7))
        k.tt("vector", lg, pl[:, 0:32], rbbc, ALU.add)
        return (i, lg, h2tok)

    def p5_back(ctx):
        i, lg, h2tok = ctx
        k.vmax8(m8, lg)
        k.ts("vector", msk, lg, m8[:, 3:4], None, ALU.is_ge)
        k.ts("vector", nmax, m8[:, 0:1], -1.0, None, ALU.mult)
        k.act(ex, lg, AF.Exp, bias=nmax)
        k.tt("vector", ex, ex, msk, ALU.mult)
        k.reduce(zs, ex, ALU.add)
        k.recip(rz, zs)
        k.ts("vector", Gall[:, i, :], ex, rz, None, ALU.mult)
        k.copy("vector", mskb, msk)
        pr = y_rot.next()[1]
        k.mm(pr[:, 0:32], LTb, mskb)
        k.mm(pr[:, 32:64], onesb, mskb)
        k.tt("vector", rank, pr[:, 0:32], carry, ALU.add)
        k.tt("vector", carry, carry, pr[:, 32:64], ALU.add)
        k.tt("vector", keyt, msk, erow, ALU.mult)
        k.vmax8(ek8, keyt)
        for kk in range(4):
            k.ts("vector", oh, erow, ek8[:, kk:kk + 1], None, ALU.is_equal)
            k.tt("vector", ohr, oh, rank, ALU.mult)
            k.reduce(rk, ohr, ALU.add)
            k.tt("vector", ohr, oh, Gall[:, i, :], ALU.mult)
            k.reduce(gkall[:, i, kk:kk + 1], ohr, ALU.add)
            k.ts("vector", slf, ek8[:, kk:kk + 1], -1.0, 2048.0, ALU.add, ALU.mult)
            k.tt("vector", slf, slf, rk, ALU.add)
            ix = T(idxall.ap[:, i, kk:kk + 1], Buf(f"idx_{i}_{kk}"))
            idxT[(i, kk)] = ix
            k.copy("vector", ix, slf)
            k.idma(XG, h2tok, ix, scatter=True, chan=h2tok.buf)

    idxT = {}
    lgs = Rot([A.alloc(f"lg{i}", [128, 32], F32) for i in range(2)])
    ctx5 = p5_front(0)
    for i in range(NT_OWN):
        nxt5 = p5_front(i + 1) if i + 1 < NT_OWN else None
        p5_back(ctx5)
        ctx5 = nxt5
    k.ts("vector", rank, carry, 63.5, 1.0 / 128, ALU.add, ALU.mult)
    k.copy("vector", nblk_i, rank)
    if debug:
        k.dma("sync", dbg["G"], Gall.re("p a b -> p (a b)"), chan=Gall.buf)
    k.barrier()
    A.reset(small_mark)

    b1T = A.alloc("b1T", [128, 16, 32], F32)
    b1_mark = A.mark()
    b1sb = A.alloc("b1sb", [32, 2048], F32); k.dma("sync", b1sb, b1)
    pb = banks[0]
    for c in range(16):
        k.tr(pb[:, c * 32:(c + 1) * 32], b1sb[0:32, c * 128:(c + 1) * 128], identf[0:32, 0:32])
    k.copy("vector", b1T, pb.re("p (c e) -> p c e", c=16))
    k.barrier()
    A.reset(b1_mark)
    p6_mark = A.mark()
    w1b = A.alloc("w1b", [128, 8, 2048], BF16)
    w2b = A.alloc("w2b", [128, 8, 1024], BF16)
    stg1 = [A.alloc(f"stg1_{p}", [128, 2048], F32) for p in range(8)]
    stg2 = [A.alloc(f"stg2_{j}", [128, 2, 1024], F32) for j in range(4)]
    xbs = Rot([A.alloc(f"xb{i}", [128, 1024], BF16) for i in range(3)])
    xets = Rot([A.alloc(f"xet{i}", [128, 8, 128], BF16) for i in range(2)])
    actTs = Rot([A.alloc(f"actT{i}", [128, 8, 128], BF16) for i in range(2)])
    ysbs = Rot([A.alloc(f"ysb{i}", [128, 1024], F32) for i in range(2)])
    gts = Rot([A.alloc(f"gt{i}", [128, 4, 128], F32) for i in range(1)])
    lts = Rot([A.alloc(f"lt{i}", [128, 4, 128], F32) for i in range(1)])
    sts = Rot([A.alloc(f"st{i}", [128, 4, 128], F32) for i in range(1)])
    gss = Rot([A.alloc(f"gs{i}", [128, 4, 128], F32) for i in range(1)])
    ptr_bank = banks[0]
    mm1_banks = banks[1:5]
    y_banks = (banks[5], banks[6])

    def piece_dma(e, p):
        if p < 8:
            k.dma("sync", stg1[p], w1[e][p * 128:(p + 1) * 128, :])
        else:
            j = p - 8
            k.dma("sync", stg2[j], w2[e][j * 256:(j + 1) * 256, :].re("(a p) n -> p a n", p=128))

    def piece_cast(p):
        dst, src = (w1b[:, p, :], stg1[p]) if p < 8 else (w2b[:, 2 * (p - 8):2 * (p - 8) + 2, :], stg2[p - 8])
        k.copy("vector", dst, src)

    for p in range(12):
        piece_dma(0, p)
    for p in range(12):
        piece_cast(p)
        piece_dma(1, p)
    key_next = k.val_load(nblk_i[0:1, 0:1])
    xb_first = xbs.next()
    k.dma("gpsimd", xb_first, XG[0:128, :])
    for e in range(32):
        key = key_next
        xb_next = xb_first
        for blk in range(16):
            k.cond_begin(key, blk)
            r0 = e * 2048 + blk * 128
            xb = xb_next
            if blk + 1 < 16:
                xb_next = xbs.next()
                k.dma("gpsimd", xb_next, XG[r0 + 128:r0 + 256, :])
            pv = ptr_bank.bitcast(BF16).re("p (j t) -> p j t", j=8)
            for j in range(8):
                k.tr(pv[:, j, :], xb[:, j * 128:(j + 1) * 128], identb)
            xet = xets.next()
            k.copy("scalar", xet, pv)
            for gp in range(2):
                for g4 in (gp, 2 + gp):
                    pbk = mm1_banks[g4]
                    for q in range(4):
                        fc = g4 * 4 + q
                        for kc in range(8):
                            k.mm(pbk[:, q * 128:(q + 1) * 128], w1b[:, kc, fc * 128:(fc + 1) * 128], xet[:, kc, :],
                                 start=(kc == 0), stop=(kc == 7))
            actT = actTs.next()
            ysb = ysbs.next()
            for gp in range(2):
                pg = mm1_banks[gp].re("p (q c) -> p q c", q=4)
                pl_ = mm1_banks[2 + gp].re("p (q c) -> p q c", q=4)
                bg = b1T[:, gp * 4:(gp + 1) * 4, e:e + 1].bc([128, 4, 128])
                bl = b1T[:, 8 + gp * 4:8 + (gp + 1) * 4, e:e + 1].bc([128, 4, 128])
                gt_ = gts.next(); lt_ = lts.next(); st_ = sts.next(); gs_ = gss.next()
                k.tt("vector", gt_, pg, bg, ALU.add)
                k.ts("vector", gt_, gt_, 7.0, None, ALU.min)
                k.act(st_, gt_, AF.Sigmoid, scale=1.702)
                k.tt("vector", lt_, pl_, bl, ALU.add)
                k.ts("vector", lt_, lt_, 7.0, -7.0, ALU.min, ALU.max)
                k.tt("vector", gs_, gt_, st_, ALU.mult)
                k.stt(actT[:, gp * 4:(gp + 1) * 4, :], lt_, 1.0, gs_, ALU.add, ALU.mult)
                for nb in range(2):
                    for f in range(gp * 4, gp * 4 + 4):
                        k.mm(y_banks[nb], actT[:, f, :], w2b[:, f, nb * 512:(nb + 1) * 512], start=(f == 0), stop=(f == 7))
            k.copy("scalar", ysb[:, 0:512], y_banks[0])
            k.copy("vector", ysb[:, 512:1024], y_banks[1])
            k.dma("gpsimd", YG[r0:r0 + 128, :], ysb, chan=ysb.buf)
        for blk in range(16):
            k.cond_end()
        if e + 1 < 32:
            key_next = k.val_load(nblk_i[0:1, e + 1:e + 2])
            xb_first = xbs.next()
            k.dma("gpsimd", xb_first, XG[(e + 1) * 2048:(e + 1) * 2048 + 128, :])
            for p in range(12):
                piece_cast(p)
                if e + 2 < 32:
                    piece_dma(e + 2, p)
    k.barrier()
    A.reset(p6_mark)

    GT = A.alloc("GT", [32, 128], F32)
    b2sb = A.alloc("b2sb", [32, 1024], F32); k.dma("sync", b2sb, b2)
    accs = Rot([A.alloc(f"acc{i}", [128, 1024], F32) for i in range(2)])
    ygs = Rot([A.alloc(f"yg{i}", [128, 1024], F32) for i in range(8)])
    xslots = Rot([A.alloc(f"xs{i}", [128, 1024], F32) for i in range(2)])
    fo = Rot([A.alloc(f"fo{i}", [128, 1024], F32) for i in range(2)])
    sqj = A.alloc("sqj", [128, 1024], BF16)
    pg_rot = Rot(banks[0:2])
    y_rot = Rot([(banks[4], banks[5]), (banks[6], banks[7])])
    for ti in range(NT_OWN):
        ts_ = slice(ti * 128, (ti + 1) * 128)
        acc = accs.next()
        pgt = pg_rot.next()
        k.tr(pgt[0:32, 0:128], Gall[:, ti, :], identf)
        k.copy("vector", GT, pgt[0:32, 0:128])
        py = y_rot.next()
        for nb in range(2):
            k.mm(py[nb], GT[0:32, :], b2sb[0:32, nb * 512:(nb + 1) * 512])
            k.copy("scalar", acc[:, nb * 512:(nb + 1) * 512], py[nb])
        for kk in range(4):
            yg = ygs.next()
            k.idma(yg, YG, idxT[(ti, kk)], scatter=False, chan=yg.buf)
            k.stt(acc, yg, gkall[:, ti, kk:kk + 1], acc, ALU.mult, ALU.add)
        xt = xslots.next()
        k.dma("sync", xt, x1s[ts_, :])
        k.tt("vector", acc, acc, g2bc, ALU.mult)
        k.tt("vector", xt, xt, acc, ALU.add)
        ss = smalls.next(); rs = smalls.next(); rstd = smalls.next()
        k.act(sqj, xt, AF.Square, accum=ss)
        k.act(rs, ss, AF.Sqrt, scale=1.0 / 1024, bias=epsc)
        k.recip(rstd, rs)
        ot = fo.next()
        k.stt(ot, xt, rstd, fgbc, ALU.mult, ALU.mult)
        k.dma("sync", yout[ts_, :], ot, chan=ot.buf)
    k.final_wait("sync")
    k.emit()
    st.close()
    return nc, k


SPL = np.cumsum([0, 512, 512, 512, 256, 64, 4, 256, 256, 512, 512])


def _swap_perm(ncols):
    p = np.arange(ncols)
    for h0 in range(0, ncols, 64):
        p[h0:h0 + 8] = np.arange(h0 + 8, h0 + 16)
        p[h0 + 8:h0 + 16] = np.arange(h0, h0 + 8)
    return p


def _consts(half):
    identf = np.eye(128, dtype=np.float32)
    identb = identf.astype(ml_dtypes.bfloat16)
    negI = (-1024.0 * identf).astype(ml_dtypes.bfloat16)
    s = np.arange(128)[:, None]
    t = np.arange(128)[None, :]
    tri2 = (((s // 64) == (t // 64)) & ((s % 64) <= (t % 64))).astype(np.float32)
    diag = np.where((t // 64) <= (s // 64), 0.0, NEG).astype(np.float32)
    cols = np.zeros((128, 4), np.float32)
    inv_freq = (500000.0 ** (-(np.arange(0, 16, 2, dtype=np.float32) / 16))).astype(np.float32)
    for p in range(128):
        d = p % 64
        if d < 16:
            cols[p, 0] = inv_freq[d % 8]
            cols[p, 1] = -1.0 if d < 8 else 1.0
    cols[:, 2] = 0.0 if half == 1 else NEG
    cols[:, 3] = float(half)
    ltb = (s < t).astype(np.float32).astype(ml_dtypes.bfloat16)
    erow = np.broadcast_to(np.arange(1, 33, dtype=np.float32)[None, :], (128, 32)).copy()
    return {"c_ltb": ltb, "c_erow": erow, "c_identb": identb, "c_identf": identf, "c_negI": negI, "c_tri2": tri2, "c_diag": diag, "c_cols": cols}


_CACHE = {}


def kernel(x, c, positions, ada_w, ada_b, norm1_g, w_in, hg_norm_g, lb_logits, w_out, norm2_g,
           router_w, router_b, moe_w1, moe_b1, moe_w2, moe_b2, final_g, _debug=False):
    f = lambda a: np.ascontiguousarray(np.asarray(a, dtype=np.float32))
    x = f(x); c = f(c); positions = np.ascontiguousarray(np.asarray(positions, dtype=np.int32))
    w_in0 = f(w_in)[0]
    parts = [w_in0[:, SPL[i]:SPL[i + 1]] for i in range(10)]
    qa, ka, va, qi, ki, wi, qb, fb, ib, gb = parts
    ki2 = np.concatenate([ki, ki], axis=1)
    shared = {
        "ada_w": f(ada_w)[0], "ada_b": f(ada_b)[0], "norm1_g": f(norm1_g)[0], "norm2_g": f(norm2_g)[0],
        "final_g": f(final_g), "hg_norm_g": f(hg_norm_g)[0], "lb_logits": f(lb_logits),
        "w_ka": f(ka), "w_kas": f(ka[:, _swap_perm(512)]), "w_ki": f(ki2), "w_kis": f(ki2[:, _swap_perm(128)]),
        "w_va": f(va), "w_qa": f(qa), "w_qas": f(qa[:, _swap_perm(512)]),
        "w_qi": f(qi), "w_qis": f(qi[:, _swap_perm(256)]), "w_wi": f(wi),
        "w_qb": f(qb), "w_fb": f(fb), "w_ib": f(ib), "w_gb": f(gb),
        "w_out": f(w_out)[0], "router_w": f(router_w)[0], "router_b": f(router_b)[0],
        "moe_w1": f(moe_w1)[0], "moe_b1": f(moe_b1)[0], "moe_w2": f(moe_w2)[0], "moe_b2": f(moe_b2)[0],
    }
    in_maps = []
    for j in range(8):
        b, half = j // 2, j % 2
        m = dict(shared)
        m["xo"] = np.ascontiguousarray(x[b, half * 2048:(half + 1) * 2048])
        m["xp"] = np.ascontiguousarray(x[b, 0:2048])
        m["cvec"] = np.ascontiguousarray(c[b])
        m["posr"] = np.ascontiguousarray(np.concatenate([positions[b, 0:2048], positions[b, half * 2048:(half + 1) * 2048]]))
        m.update(_consts(half))
        in_maps.append(m)
    key = bool(_debug)
    if key not in _CACHE:
        _CACHE[key] = build_nc(debug=key)[0]
    nc = _CACHE[key]
    res = run_bass_kernel_spmd(nc, in_maps, core_ids=list(range(8)))
    out = np.empty((4, 4096, 1024), np.float32)
    for j in range(8):
        b, half = j // 2, j % 2
        out[b, half * 2048:(half + 1) * 2048] = res.results[j]["y"]
    if _debug:
        return out, res.results
    return out
```

```python
from contextlib import ExitStack
import numpy as np
import ml_dtypes
import concourse.bass as bass
import concourse.mybir as mybir
from concourse.bass_utils import run_bass_kernel_spmd

F32 = mybir.dt.float32
BF16 = mybir.dt.bfloat16
I32 = mybir.dt.int32
U8 = mybir.dt.uint8
AF = mybir.ActivationFunctionType
ALU = mybir.AluOpType
AX = mybir.AxisListType
DSZ = {F32: 4, BF16: 2, I32: 4, U8: 1}

ENGS = ["tensor", "vector", "scalar", "gpsimd", "sync"]
SEM_LIMIT = 30000
EPS = 1e-6
PI = float(np.pi)
NT_OWN = 16
NT_ALL = 32
RNG = 32.0
NBIS = 13
NEG = -1.0e30


class Buf:
    __slots__ = ("name", "w", "r", "dsem", "dcount")

    def __init__(self, name):
        self.name = name
        self.w = {}
        self.r = {}
        self.dsem = None
        self.dcount = 0


class T:
    __slots__ = ("ap", "buf")

    def __init__(self, ap, buf):
        self.ap = ap
        self.buf = buf

    def __getitem__(self, key):
        return T(self.ap[key], self.buf)

    def bitcast(self, dt):
        return T(self.ap.bitcast(dt), self.buf)

    def re(self, pat, **kw):
        return T(self.ap.rearrange(pat, **kw), self.buf)

    def bc(self, shape):
        return T(self.ap.to_broadcast(list(shape)), self.buf)

    def unsq(self, ax):
        return T(self.ap.unsqueeze(ax), self.buf)


def _bufs(ts):
    out = []
    for t in ts:
        if t is None:
            continue
        b = t.buf if isinstance(t, T) else t
        if b is not None and b not in out:
            out.append(b)
    return out


class Rot:
    def __init__(self, items):
        self.items = list(items)
        self.i = 0

    def next(self):
        t = self.items[self.i % len(self.items)]
        self.i += 1
        return t


class K:
    def __init__(self, nc, stack):
        self.nc = nc
        self.stack = stack
        self.streams = {e: [] for e in ENGS}
        self.esem = {}
        self.ecount = {e: 0 for e in ENGS}
        self.known = {e: {} for e in ENGS}
        self.nsem = 0
        for e in ENGS:
            self.esem[e] = self.new_sem("e_" + e)
        self.dma_bufs = []
        self.n_ops = 0
        self.rstack = []

    def new_sem(self, name):
        self.nsem += 1
        return self.stack.enter_context(self.nc.semaphore(f"{name}_{self.nsem}"))

    def _need(self, eng, tokens):
        waits = []
        kn = self.known[eng]
        for sem, val in tokens.items():
            if kn.get(sem, 0) < val:
                kn[sem] = val
                waits.append((sem, val))
        return waits

    def _deps(self, eng, rd, wr, skip_waw=None):
        tokens = {}
        mysem = self.esem[eng]

        def add(d, is_raw):
            for sem, val in d.items():
                if sem is mysem and eng == "tensor":
                    continue
                if (not is_raw) and skip_waw is not None and sem is skip_waw:
                    continue
                if tokens.get(sem, 0) < val:
                    tokens[sem] = val
        for b in rd:
            add(b.w, True)
        for b in wr:
            add(b.w, False)
            add(b.r, False)
        self._note_region(eng, tokens)
        return self._need(eng, tokens)

    def _note_region(self, eng, tokens):
        for rg in self.rstack:
            ext = rg["ext"][eng]
            for sem, val in tokens.items():
                if val <= rg["start"].get(sem, 0):
                    if ext.get(sem, 0) < val:
                        ext[sem] = val

    def _note_inc(self, eng, sem, inc):
        for rg in self.rstack:
            d = rg["incs"][eng]
            d[sem] = d.get(sem, 0) + inc

    def val_load(self, t):
        self.nvals = getattr(self, "nvals", 0) + 1
        key = self.nvals
        for e in ENGS:
            waits = self._deps(e, _bufs([t]), [])
            self.streams[e].append(("load", waits, key, t.ap))
        return key

    def cond_begin(self, key, thr):
        if not self.rstack:
            for e in ENGS:
                if self.ecount[e] >= SEM_LIMIT - 6000:
                    self.esem[e] = self.new_sem("e_" + e)
                    self.ecount[e] = 0
        start = {}
        for e in ENGS:
            start[self.esem[e]] = self.ecount[e]
        for b in self.dma_bufs:
            start[b.dsem] = b.dcount
        rg = {"key": key, "thr": thr, "start": start,
              "known0": {e: dict(self.known[e]) for e in ENGS},
              "ext": {e: {} for e in ENGS}, "incs": {e: {} for e in ENGS}}
        self.rstack.append(rg)
        for e in ENGS:
            self.streams[e].append(("begin", rg))

    def cond_end(self):
        rg = self.rstack.pop()
        for e in ENGS:
            kn = dict(rg["known0"][e])
            ew = []
            for sem, val in rg["ext"][e].items():
                if kn.get(sem, 0) < val:
                    kn[sem] = val
                    ew.append((sem, val))
            rg["ext"][e] = ew
            self.known[e] = kn
            self.streams[e].append(("end", rg))

    def op(self, eng, fn, rd=(), wr=()):
        rd = _bufs(rd)
        wr = _bufs(wr)
        waits = self._deps(eng, rd, wr)
        if self.ecount[eng] >= SEM_LIMIT and not self.rstack:
            self.esem[eng] = self.new_sem("e_" + eng)
            self.ecount[eng] = 0
        self.ecount[eng] += 1
        sem = self.esem[eng]
        val = self.ecount[eng]
        self.streams[eng].append((waits, fn, sem, 1))
        self._note_inc(eng, sem, 1)
        for b in rd:
            if b.r.get(sem, 0) < val:
                b.r[sem] = val
        for b in wr:
            b.w = {sem: val}
            b.r = {}
        self.n_ops += 1

    def dma(self, eng, out, in_, chan=None, **kw):
        rd = _bufs([in_])
        wr = _bufs([out])
        if chan is None:
            chan = out.buf
        if chan.dsem is None:
            chan.dsem = self.new_sem("d_" + chan.name)
            self.dma_bufs.append(chan)
        waits = self._deps(eng, rd, wr, skip_waw=chan.dsem)
        chan.dcount += 16
        sem, val = chan.dsem, chan.dcount
        oap, iap = out.ap, in_.ap
        self.streams[eng].append((waits, lambda e: e.dma_start(out=oap, in_=iap, **kw), sem, 16))
        self._note_inc(eng, sem, 16)
        for b in rd:
            if b.r.get(sem, 0) < val:
                b.r[sem] = val
        for b in wr:
            b.w = {sem: val}
            b.r = {}
        self.n_ops += 1

    def idma(self, out, in_, idx, scatter, chan):
        rd = _bufs([in_, idx])
        wr = _bufs([out])
        if chan.dsem is None:
            chan.dsem = self.new_sem("d_" + chan.name)
            self.dma_bufs.append(chan)
        waits = self._deps("gpsimd", rd, wr, skip_waw=chan.dsem)
        chan.dcount += 16
        sem, val = chan.dsem, chan.dcount
        oap, iap, xap = out.ap, in_.ap, idx.ap
        if scatter:
            fn = lambda e: e.indirect_dma_start(out=oap, out_offset=bass.IndirectOffsetOnAxis(xap, 0), in_=iap, in_offset=None)
        else:
            fn = lambda e: e.indirect_dma_start(out=oap, out_offset=None, in_=iap, in_offset=bass.IndirectOffsetOnAxis(xap, 0))
        self.streams["gpsimd"].append((waits, fn, sem, 16))
        self._note_inc("gpsimd", sem, 16)
        for b in rd:
            if b.r.get(sem, 0) < val:
                b.r[sem] = val
        for b in wr:
            b.w = {sem: val}
            b.r = {}
        self.n_ops += 1

    def _all_tokens(self):
        tokens = {}
        for e in ENGS:
            if self.ecount[e] > 0:
                tokens[self.esem[e]] = self.ecount[e]
        for b in self.dma_bufs:
            tokens[b.dsem] = b.dcount
        return tokens

    def barrier(self):
        tokens = self._all_tokens()
        for e in ENGS:
            waits = self._need(e, dict(tokens))
            if waits:
                self.streams[e].append((waits, None, None, 0))

    def final_wait(self, eng="sync"):
        waits = self._need(eng, self._all_tokens())
        self.streams[eng].append((waits, None, None, 0))

    def emit(self):
        nc = self.nc
        with nc.Block() as block:
            def run(name):
                def body(e):
                    items = self.streams[name]
                    vals = {}

                    def run_items(lst):
                        i = 0
                        while i < len(lst):
                            it = lst[i]
                            if it[0] == "load":
                                for s, v in it[1]:
                                    e.wait_ge(s, v)
                                if "reg" not in vals:
                                    vals["reg"] = e.alloc_register("cnd_" + name)
                                e.load(vals["reg"], it[3])
                                vals["key"] = it[2]
                                i += 1
                            elif it[0] == "begin":
                                rg = it[1]
                                assert vals["key"] == rg["key"]
                                j = i + 1
                                while not (lst[j][0] == "end" and lst[j][1] is rg):
                                    j += 1
                                bodyl = lst[i + 1:j]
                                with e.If_cmp(vals["reg"], rg["thr"], "IS_LE"):
                                    for s, v in rg["ext"][name]:
                                        e.wait_ge(s, v)
                                    for s, tot in rg["incs"][name].items():
                                        if rg["start"].get(s, 0) > 0:
                                            e.wait_ge(s, rg["start"][s])
                                        e.nop().then_inc(s, tot)
                                with e.Else():
                                    run_items(bodyl)
                                i = j + 1
                            else:
                                waits, fn, sem, inc = it
                                for s, v in waits:
                                    e.wait_ge(s, v)
                                if fn is not None:
                                    fn(e).then_inc(sem, inc)
                                i += 1
                    run_items(items)
                return body
            block.tensor(run("tensor"))
            block.vector(run("vector"))
            block.scalar(run("scalar"))
            block.gpsimd(run("gpsimd"))
            block.sync(run("sync"))

    def ps(self, name, shape, dtype):
        t = self.stack.enter_context(self.nc.psum_tensor(name, list(shape), dtype))
        return T(t[:], Buf(name))

    def dram(self, name, shape, dtype, kind):
        t = self.nc.dram_tensor(name, list(shape), dtype, kind=kind)
        return T(t.ap(), Buf(name))

    def mm(self, out, lhsT, rhs, start=True, stop=True, **kw):
        rd = [lhsT, rhs] + ([] if start else [out])
        self.op("tensor", lambda e: e.matmul(out.ap, lhsT.ap, rhs.ap, start=start, stop=stop, **kw),
                rd=rd, wr=[out])

    def tr(self, out, in_, ident):
        self.op("tensor", lambda e: e.transpose(out.ap, in_.ap, ident.ap), rd=[in_, ident], wr=[out])

    def act(self, out, in_, func, bias=None, scale=None, accum=None):
        kw = {}
        rd = [in_]
        if bias is not None:
            if isinstance(bias, T):
                kw["bias"] = bias.ap
                rd.append(bias)
            else:
                kw["bias"] = bias
        if scale is not None:
            if isinstance(scale, T):
                kw["scale"] = scale.ap
                rd.append(scale)
            else:
                kw["scale"] = scale
        wr = [out]
        if accum is not None:
            kw["accum_out"] = accum.ap
            wr.append(accum)
        self.op("scalar", lambda e: e.activation(out.ap, in_.ap, func, **kw), rd=rd, wr=wr)

    def ts(self, eng, out, in0, s1, s2, op0, op1=None, accum=None):
        rd = [in0]
        a1, a2 = s1, s2
        if isinstance(s1, T):
            rd.append(s1)
            a1 = s1.ap
        if isinstance(s2, T):
            rd.append(s2)
            a2 = s2.ap
        kw = {}
        wr = [out]
        if op1 is not None:
            kw["op1"] = op1
        if accum is not None:
            kw["accum_out"] = accum.ap
            wr.append(accum)
        self.op(eng, lambda e: e.tensor_scalar(out.ap, in0.ap, a1, a2, op0, **kw), rd=rd, wr=wr)

    def tt(self, eng, out, in0, in1, op):
        self.op(eng, lambda e: e.tensor_tensor(out.ap, in0.ap, in1.ap, op), rd=[in0, in1], wr=[out])

    def stt(self, out, in0, scalar, in1, op0, op1):
        rd = [in0, in1]
        a = scalar
        if isinstance(scalar, T):
            rd.append(scalar)
            a = scalar.ap
        self.op("vector", lambda e: e.scalar_tensor_tensor(out.ap, in0.ap, a, in1.ap, op0, op1),
                rd=rd, wr=[out])

    def copy(self, eng, out, in_):
        if eng == "scalar":
            self.op(eng, lambda e: e.copy(out.ap, in_.ap), rd=[in_], wr=[out])
        else:
            self.op(eng, lambda e: e.tensor_copy(out.ap, in_.ap), rd=[in_], wr=[out])

    def memset(self, eng, out, val):
        self.op(eng, lambda e: e.memset(out.ap, val), rd=[], wr=[out])

    def recip(self, out, in_):
        self.op("vector", lambda e: e.reciprocal(out.ap, in_.ap), rd=[in_], wr=[out])

    def reduce(self, out, in_, op):
        self.op("vector", lambda e: e.tensor_reduce(out.ap, in_.ap, AX.X, op), rd=[in_], wr=[out])

    def scan_add(self, out, ones, data, initial):
        rd = [ones, data]
        a = initial
        if isinstance(initial, T):
            rd.append(initial)
            a = initial.ap
        self.op("vector", lambda e: e.tensor_tensor_scan(out.ap, ones.ap, data.ap, a, ALU.mult, ALU.add),
                rd=rd, wr=[out])

    def vmax8(self, out, in_):
        self.op("vector", lambda e: e.max(out.ap, in_.ap), rd=[in_], wr=[out])


class Arena:
    def __init__(self, k, nbytes):
        self.k = k
        self.nbytes = nbytes
        t = k.stack.enter_context(k.nc.sbuf_tensor("arena", [128, nbytes], U8))
        self.ap = t[:]
        self.off = 0
        self.top = nbytes
        self.n = 0

    def alloc(self, name, shape, dtype, top=False):
        free = int(np.prod(shape[1:]))
        nb = free * DSZ[dtype]
        if top:
            off = (self.top - nb) // 64 * 64
            assert off >= self.off, f"arena overflow (top) at {name}"
            self.top = off
        else:
            off = (self.off + 63) // 64 * 64
            assert off + nb <= self.top, f"arena overflow at {name}: {off}+{nb} > {self.top}"
            self.off = off + nb
        ap = self.ap[0:shape[0], off:off + nb].bitcast(dtype)
        if len(shape) == 3:
            ap = ap.rearrange("p (a b) -> p a b", a=shape[1])
        elif len(shape) == 4:
            ap = ap.rearrange("p (a b c) -> p a b c", a=shape[1], b=shape[2])
        self.n += 1
        return T(ap, Buf(f"{name}_{self.n}"))

    def mark(self):
        return self.off

    def reset(self, m):
        self.off = m


def build_nc(debug=False):
    nc = bass.Bass("TRN2", target_bir_lowering=False)
    st = ExitStack()
    k = K(nc, st)
    A = Arena(k, 207 * 1024)

    def DI(name, shape, dt=F32):
        return k.dram(name, shape, dt, "ExternalInput")

    xo = DI("xo", [2048, 1024]); xp = DI("xp", [2048, 1024])
    cvec = DI("cvec", [1024]); posr = DI("posr", [4096], I32)
    ada_w = DI("ada_w", [1024, 6144]); ada_b = DI("ada_b", [6144])
    n1g = DI("norm1_g", [1024]); n2g = DI("norm2_g", [1024]); fing = DI("final_g", [1024])
    hgg = DI("hg_norm_g", [128]); lbl = DI("lb_logits", [2, 256])
    w_ka = DI("w_ka", [1024, 512]); w_kas = DI("w_kas", [1024, 512])
    w_ki = DI("w_ki", [1024, 128]); w_kis = DI("w_kis", [1024, 128])
    w_va = DI("w_va", [1024, 512])
    w_qa = DI("w_qa", [1024, 512]); w_qas = DI("w_qas", [1024, 512])
    w_qi = DI("w_qi", [1024, 256]); w_qis = DI("w_qis", [1024, 256])
    w_wi = DI("w_wi", [1024, 4])
    w_qb = DI("w_qb", [1024, 256]); w_fb = DI("w_fb", [1024, 256])
    w_ib = DI("w_ib", [1024, 512]); w_gb = DI("w_gb", [1024, 512])
    w_out = DI("w_out", [1024, 1024])
    rw = DI("router_w", [1024, 32]); rb = DI("router_b", [32])
    w1 = DI("moe_w1", [32, 1024, 2048]); b1 = DI("moe_b1", [32, 2048])
    w2 = DI("moe_w2", [32, 1024, 1024]); b2 = DI("moe_b2", [32, 1024])
    c_identb = DI("c_identb", [128, 128], BF16); c_identf = DI("c_identf", [128, 128])
    c_negI = DI("c_negI", [128, 128], BF16); c_tri2 = DI("c_tri2", [128, 128])
    c_diag = DI("c_diag", [128, 128]); c_cols = DI("c_cols", [128, 4])
    yout = k.dram("y", [2048, 1024], F32, "ExternalOutput")
    x1s = k.dram("x1s", [2048, 1024], F32, "Internal")
    XG = k.dram("XG", [32 * 2048, 1024], BF16, "Internal")
    YG = k.dram("YG", [32 * 2048, 1024], F32, "Internal")
    c_ltb = DI("c_ltb", [128, 128], BF16); c_erow = DI("c_erow", [128, 32])
    dbg = {}
    if debug:
        dbg["mixT"] = k.dram("dbg_mixT", [8, 128, 2048], F32, "ExternalOutput")
        dbg["x1"] = k.dram("dbg_x1", [2048, 1024], F32, "ExternalOutput")
        dbg["G"] = k.dram("dbg_G", [128, 16 * 32], F32, "ExternalOutput")

    banks = [k.ps(f"bank{i}", [128, 512], F32) for i in range(8)]

    def wload(dst, src, rows=8, eng="gpsimd"):
        k.dma(eng, dst, src.re("(kc p) n -> p kc n", p=128))

    identb = A.alloc("identb", [128, 128], BF16); k.dma("sync", identb, c_identb)
    identf = A.alloc("identf", [128, 128], F32); k.dma("sync", identf, c_identf)
    negI = A.alloc("negI", [128, 128], BF16); k.dma("sync", negI, c_negI)
    tri2 = A.alloc("tri2", [128, 128], F32); k.dma("sync", tri2, c_tri2)
    diagm = A.alloc("diagm", [128, 128], F32); k.dma("sync", diagm, c_diag)
    ccols = A.alloc("ccols", [128, 4], F32); k.dma("sync", ccols, c_cols)
    invf = ccols[:, 0:1]; sgnc = ccols[:, 1:2]; negb = ccols[:, 2:3]; sflag = ccols[:, 3:4]
    zerob = A.alloc("zerob", [128, 512], BF16); k.memset("vector", zerob, 0.0)
    onesf = A.alloc("onesf", [128, 512], F32); k.memset("vector", onesf, 1.0)
    g1bc = A.alloc("g1bc", [128, 1024], F32)
    g2bc = A.alloc("g2bc", [128, 1024], F32)
    fgbc = A.alloc("fgbc", [128, 1024], F32)
    s2row = A.alloc("s2row", [128, 1024], F32)
    sh2row = A.alloc("sh2row", [128, 1024], F32)
    k.dma("sync", fgbc, T(fing.ap.partition_broadcast(128), fing.buf))
    hgbc = A.alloc("hgbc", [128, 128], F32)
    k.dma("sync", hgbc, T(hgg.ap.partition_broadcast(128), hgg.buf))
    rbbc = A.alloc("rbbc", [128, 32], F32)
    k.dma("sync", rbbc, T(rb.ap.partition_broadcast(128), rb.buf))
    mcols = A.alloc("mcols", [128, 32], F32)
    s1col = A.alloc("s1col", [128, 8], F32); s2col = A.alloc("s2col", [128, 8], F32)
    lbcol = A.alloc("lbcol", [128, 2], F32); omlb = A.alloc("omlb", [128, 2], F32)
    Gall = A.alloc("Gall", [128, 16, 32], F32)
    smalls = Rot([A.alloc(f"sm{i}", [128, 1], F32) for i in range(12)])
    epsc = A.alloc("epsc", [128, 1], F32); k.memset("vector", epsc, EPS)
    small_mark = A.mark()
    mixA = A.alloc("mixA", [128, 4, 2048], BF16)
    persist_mark = A.mark()

    cT = A.alloc("cT", [128, 8], F32)
    k.dma("sync", cT, cvec.re("(j p) -> p j", p=128), allow_slow_non_contiguous=True)
    siluc = A.alloc("siluc", [128, 8], BF16)
    k.act(siluc, cT, AF.Silu)
    modrow = A.alloc("modrow", [1, 6144], F32)
    adab = A.alloc("adab", [1, 6144], F32)
    k.dma("sync", adab, ada_b.re("(o n) -> o n", o=1))
    ones1 = A.alloc("ones1", [1, 128], F32); k.memset("vector", ones1, 1.0)
    aslots = Rot([A.alloc(f"adaw{i}", [128, 8, 512], BF16) for i in range(2)])
    prot = Rot(banks[0:4])
    adaw_v = ada_w.re("(kc p) n -> p kc n", p=128)
    for nb in range(12):
        sl = aslots.next()
        k.dma("gpsimd", sl, adaw_v[:, :, nb * 512:(nb + 1) * 512])
        pb = prot.next()
        for kc in range(8):
            k.mm(pb[0:1, :], siluc[:, kc:kc + 1], sl[:, kc, :], start=(kc == 0), stop=(kc == 7))
        k.tt("vector", modrow[0:1, nb * 512:(nb + 1) * 512], pb[0:1, :], adab[0:1, nb * 512:(nb + 1) * 512], ALU.add)
    for (dst, off) in ((g1bc, 2048), (g2bc, 5120), (sh2row, 3072), (s2row, 4096)):
        for nb in range(2):
            pb = prot.next()
            k.mm(pb, ones1[0:1, :], modrow[0:1, off + nb * 512: off + (nb + 1) * 512])
            k.copy("vector", dst[:, nb * 512:(nb + 1) * 512], pb)
    pb = prot.next()
    for i, off in enumerate((0, 1024, 3072, 4096)):
        for j in range(8):
            k.mm(pb[:, i * 8 + j: i * 8 + j + 1], modrow[0:1, off + j * 128: off + (j + 1) * 128], ones1[0:1, 0:1])
    k.copy("vector", mcols, pb[:, 0:32])
    n2bc = A.alloc("n2bc", [128, 1024], F32)
    k.dma("sync", n2bc, T(n2g.ap.partition_broadcast(128), n2g.buf))
    k.ts("vector", s2row, s2row, 1.0, None, ALU.add)
    k.tt("vector", s2row, s2row, n2bc, ALU.mult)
    gcol = A.alloc("gcol", [128, 16], F32)
    k.dma("sync", gcol[:, 0:8], n1g.re("(j p) -> p j", p=128), allow_slow_non_contiguous=True)
    k.dma("sync", gcol[:, 8:16], n2g.re("(j p) -> p j", p=128), allow_slow_non_contiguous=True)
    tmp8 = A.alloc("tmp8", [128, 8], F32)
    k.ts("vector", tmp8, mcols[:, 8:16], 1.0, None, ALU.add)
    k.tt("vector", s1col, tmp8, gcol[:, 0:8], ALU.mult)
    tmp8b = A.alloc("tmp8b", [128, 8], F32)
    k.ts("vector", tmp8b, mcols[:, 24:32], 1.0, None, ALU.add)
    k.tt("vector", s2col, tmp8b, gcol[:, 8:16], ALU.mult)
    sh1col = mcols[:, 0:8]; sh2col = mcols[:, 16:24]
    lbl_sb = A.alloc("lbl_sb", [128, 2, 2], F32)
    for l in range(2):
        k.dma("sync", lbl_sb[:, l, :], lbl[l, :].re("(c p) -> p c", p=128), allow_slow_non_contiguous=True)
    dl = A.alloc("dl", [128, 2], F32)
    k.tt("vector", dl, lbl_sb[:, 0, :], lbl_sb[:, 1, :], ALU.subtract)
    k.act(lbcol, dl, AF.Sigmoid)
    k.ts("vector", omlb, lbcol, -1.0, 1.0, ALU.mult, ALU.add)
    k.barrier()
    A.reset(persist_mark)

    def hT_steps(src, tile0, ntiles, scol, shcol, hT, xslots, tmps, prot):
        sqj, xns, tmpf = tmps
        sbc = scol.unsq(2).bc([128, 8, 128])
        shbc = shcol.unsq(2).bc([128, 8, 128])

        def norm(i):
            xt = xslots.next()
            k.dma("sync", xt, src[(tile0 + i) * 128:(tile0 + i + 1) * 128, :])
            ss = smalls.next(); rs = smalls.next(); rstd = smalls.next()
            k.act(sqj, xt, AF.Square, accum=ss)
            k.act(rs, ss, AF.Sqrt, scale=1.0 / 1024, bias=epsc)
            k.recip(rstd, rs)
            xn = xns[i % 2]
            k.act(xn, xt, AF.Identity, scale=rstd)
            return xn

        def evac(i, xn):
            pb = prot.next()
            pv = pb.bitcast(BF16).re("p (j t) -> p j t", j=8)
            for j in range(8):
                k.tr(pv[:, j, :], xn[:, j * 128:(j + 1) * 128], identb)
            k.tt("vector", tmpf, pv, sbc, ALU.mult)
            k.tt("vector", hT[:, :, i * 128:(i + 1) * 128], tmpf, shbc, ALU.add)

        state = {}
        steps = [lambda: state.__setitem__(0, norm(0))]
        for i in range(ntiles):
            def st(i=i):
                if i + 1 < ntiles:
                    state[i + 1] = norm(i + 1)
                evac(i, state[i])
            steps.append(st)
        return steps

    def make_hT(*args):
        for stp_ in hT_steps(*args):
            stp_()

    def rope_tables(slot0, n, cosg, sing, tmps):
        posi, pf, ang, kf = tmps
        ki32 = posi
        k.dma("sync", posi[:, 0:n], T(posr.ap[slot0:slot0 + n].partition_broadcast(128), posr.buf))
        k.copy("vector", pf[:, 0:n], posi[:, 0:n])
        k.ts("vector", ang[:, 0:n], pf[:, 0:n], invf, None, ALU.mult)
        k.ts("vector", kf[:, 0:n], ang[:, 0:n], 1.0 / (2 * PI), None, ALU.mult)
        k.copy("vector", ki32[:, 0:n], kf[:, 0:n])
        k.copy("vector", kf[:, 0:n], ki32[:, 0:n])
        C1 = 6.28125
        C2 = 2 * PI - C1
        k.stt(ang[:, 0:n], kf[:, 0:n], -C1, ang[:, 0:n], ALU.mult, ALU.add)
        k.stt(ang[:, 0:n], kf[:, 0:n], -C2, ang[:, 0:n], ALU.mult, ALU.add)
        k.ts("vector", ang[:, 0:n], ang[:, 0:n], -PI, PI, ALU.max, ALU.min)
        k.act(sing[:, 0:n], ang[:, 0:n], AF.Sin, scale=sgnc)
        k.ts("vector", pf[:, 0:n], ang[:, 0:n], PI / 2, None, ALU.add)
        k.ts("vector", kf[:, 0:n], pf[:, 0:n], PI, -2 * PI, ALU.is_gt, ALU.mult)
        k.tt("vector", pf[:, 0:n], pf[:, 0:n], kf[:, 0:n], ALU.add)
        k.ts("vector", pf[:, 0:n], pf[:, 0:n], -PI, PI, ALU.max, ALU.min)
        k.act(cosg[:, 0:n], pf[:, 0:n], AF.Sin)

    def proj_fm(dst_fn, wsb, wsw, nchunks, hT, n, cosg, sing, prot, rtmps, only=None):
        t1, t2 = rtmps
        for c in (range(nchunks) if only is None else [only]):
            pa = prot.next()
            for kc in range(8):
                k.mm(pa[:, 0:n], wsb[:, kc, c * 128:(c + 1) * 128], hT[:, kc, 0:n], start=(kc == 0), stop=(kc == 7))
            if wsw is None:
                k.copy("scalar", dst_fn(c), pa[:, 0:n])
                continue
            pb2 = prot.next()
            for kc in range(8):
                k.mm(pb2[:, 0:n], wsw[:, kc, c * 128:(c + 1) * 128], hT[:, kc, 0:n], start=(kc == 0), stop=(kc == 7))
            k.tt("vector", t1[:, 0:n], pa[:, 0:n], cosg[:, 0:n], ALU.mult)
            k.tt("vector", t2[:, 0:n], pb2[:, 0:n], sing[:, 0:n], ALU.mult)
            dst_fn(c, t1[:, 0:n], t2[:, 0:n])

    kaT = A.alloc("kaT", [128, 4, 4096], BF16)
    kiT = A.alloc("kiT", [128, 4096], BF16)
    va = A.alloc("va", [128, NT_ALL, 8, 65], BF16)
    k.memset("gpsimd", va[:, :, :, 64:65], 1.0)
    kside_mark = A.mark()
    wka = A.alloc("wka", [128, 8, 512], BF16); wload(wka, w_ka)
    wkas = A.alloc("wkas", [128, 8, 512], BF16); wload(wkas, w_kas)
    wki = A.alloc("wki", [128, 8, 128], BF16); wload(wki, w_ki)
    wkis = A.alloc("wkis", [128, 8, 128], BF16); wload(wkis, w_kis)
    wva = A.alloc("wva", [128, 8, 512], BF16); wload(wva, w_va)
    hT = A.alloc("hT", [128, 8, 512], BF16)
    xslots = Rot([A.alloc(f"xs{i}", [128, 1024], F32) for i in range(2)])
    ntmps = (A.alloc("sqj", [128, 1024], BF16), [A.alloc(f"xn{i_}", [128, 1024], BF16) for i_ in range(2)], A.alloc("tmpf", [128, 8, 128], F32))
    cosg = A.alloc("cosg", [128, 512], F32); sing = A.alloc("sing", [128, 512], F32)
    rtm = (A.alloc("posi", [128, 512], I32), A.alloc("pf", [128, 512], F32), A.alloc("ang", [128, 512], F32),
           A.alloc("kf", [128, 512], F32))
    rt12 = (rtm[1], rtm[3])
    prot = Rot(banks)
    hTs = [hT, A.alloc("hT2", [128, 8, 512], BF16)]

    def p1_units(g, hTg):
        def dst_ka(c, a=None, b=None):
            k.tt("gpsimd", kaT[:, c, g * 512:(g + 1) * 512], a, b, ALU.add)

        def dst_ki(c, a=None, b=None):
            k.tt("gpsimd", kiT[:, g * 512:(g + 1) * 512], a, b, ALU.add)
        units = []
        for c in range(4):
            units.append(lambda c=c: proj_fm(dst_ka, wka, wkas, 4, hTg, 512, cosg, sing, prot, rt12, only=c))
        units.append(lambda: proj_fm(dst_ki, wki, wkis, 1, hTg, 512, cosg, sing, prot, rt12, only=0))
        for i in range(4):
            def va_unit(i=i):
                pa = prot.next()
                for kc in range(8):
                    k.mm(pa, hTg[:, kc, i * 128:(i + 1) * 128], wva[:, kc, :], start=(kc == 0), stop=(kc == 7))
                k.copy("scalar", va[:, g * 4 + i, :, 0:64], pa.re("p (h d) -> p h d", h=8))
            units.append(va_unit)
        return units

    def p1_tiles(g):
        return hT_steps(xp if g < 4 else xo, (g % 4) * 4, 4, s1col, sh1col, hTs[g % 2], xslots, ntmps, prot)

    rope_tables(0, 512, cosg, sing, rtm)
    for stp_ in p1_tiles(0):
        stp_()
    for g in range(8):
        units = p1_units(g, hTs[g % 2])
        tsteps = p1_tiles(g + 1) if g + 1 < 8 else []
        ti = 0
        for ui, u in enumerate(units):
            u()
            if ui % 2 == 1 and ti < len(tsteps):
                tsteps[ti]()
                ti += 1
        while ti < len(tsteps):
            tsteps[ti]()
            ti += 1
        if g + 1 < 8:
            rope_tables((g + 1) * 512, 512, cosg, sing, rtm)
    k.barrier()
    A.reset(kside_mark)

    qaT = A.alloc("qaT", [128, 4, 2048], BF16)
    qiT = A.alloc("qiT", [128, 2, 2048], BF16)
    wis = A.alloc("wis", [128, 16, 4], F32)
    qside_mark = A.mark()
    wqa = A.alloc("wqa", [128, 8, 512], BF16); wload(wqa, w_qa)
    wqas = A.alloc("wqas", [128, 8, 512], BF16); wload(wqas, w_qas)
    wqi = A.alloc("wqi", [128, 8, 256], BF16); wload(wqi, w_qi)
    wqis = A.alloc("wqis", [128, 8, 256], BF16); wload(wqis, w_qis)
    wwi = A.alloc("wwi", [128, 8, 4], BF16); wload(wwi, w_wi)
    hT = A.alloc("hT", [128, 8, 512], BF16)
    xslots = Rot([A.alloc(f"xs{i}", [128, 1024], F32) for i in range(2)])
    ntmps = (A.alloc("sqj", [128, 1024], BF16), [A.alloc(f"xn{i_}", [128, 1024], BF16) for i_ in range(2)], A.alloc("tmpf", [128, 8, 128], F32))
    cosg = A.alloc("cosg", [128, 512], F32); sing = A.alloc("sing", [128, 512], F32)
    rtm = (A.alloc("posi", [128, 512], I32), A.alloc("pf", [128, 512], F32), A.alloc("ang", [128, 512], F32),
           A.alloc("kf", [128, 512], F32))
    rt12 = (rtm[1], rtm[3])
    for g in range(4):
        rope_tables(2048 + g * 512, 512, cosg, sing, rtm)
        make_hT(xo, g * 4, 4, s1col, sh1col, hT, xslots, ntmps, prot)

        def dst_qa(c, a=None, b=None, g=g):
            k.tt("gpsimd", qaT[:, c, g * 512:(g + 1) * 512], a, b, ALU.add)
        proj_fm(dst_qa, wqa, wqas, 4, hT, 512, cosg, sing, prot, rt12)

        def dst_qi(c, a=None, b=None, g=g):
            k.tt("gpsimd", qiT[:, c, g * 512:(g + 1) * 512], a, b, ALU.add)
        proj_fm(dst_qi, wqi, wqis, 2, hT, 512, cosg, sing, prot, rt12)
        for i in range(4):
            pa = prot.next()
            for kc in range(8):
                k.mm(pa[:, 0:4], hT[:, kc, i * 128:(i + 1) * 128], wwi[:, kc, :], start=(kc == 0), stop=(kc == 7))
            k.ts("vector", wis[:, g * 4 + i, :], pa[:, 0:4], 0.5 * 0.125, None, ALU.mult)
    k.barrier()
    A.reset(qside_mark)

    sc = A.alloc("sc", [128, 4096], F32)
    mbars = Rot([A.alloc(f"mbar{i}", [128, 4096], BF16) for i in range(2)])
    rts = Rot([A.alloc(f"rt{i}", [128, 512], F32) for i in range(4)])
    pTs = Rot([A.alloc(f"pT{i}", [128, 512], BF16) for i in range(3)])
    absw = A.alloc("absw", [128, 4], F32); sgnw = A.alloc("sgnw", [128, 4], F32)
    lo = A.alloc("lo", [128, 1], F32); mid = A.alloc("mid", [128, 1], F32)
    cnt = A.alloc("cnt", [128, 1], F32); dlt = A.alloc("dlt", [128, 1], F32)
    rmax = A.alloc("rmax", [128, 1], F32)
    sga = A.alloc("sga", [128, 1], F32); tcomb = A.alloc("tcomb", [128, 1], F32)
    rden = A.alloc("rden", [128, 8], F32)
    oa = A.alloc("oa", [128, 8, 64], BF16)
    zqs = Rot([A.alloc(f"zq{i}", [128, 4, 2, 128], BF16) for i in range(2)])
    zis = Rot([A.alloc(f"zi{i}", [128, 2, 2, 128], BF16) for i in range(2)])
    for z in zqs.items + zis.items:
        k.memset("gpsimd", z, 0.0)
    sc_rot = Rot(banks[0:2])
    s_rot = Rot(banks[2:4])
    po_rot = Rot([(banks[4], banks[5]), (banks[6], banks[7])])

    def stage_a(n):
        nkt = 16 + n + 1
        nk = nkt * 128
        qs = slice(n * 128, (n + 1) * 128)
        zq = zqs.next(); zi = zis.next(); mbar = mbars.next()
        for hp in range(2):
            ph = slice(hp * 64, (hp + 1) * 64)
            k.copy("gpsimd", zq[ph, :, hp, :], qaT[ph, :, qs])
            k.copy("gpsimd", zi[ph, :, hp, :], qiT[ph, :, qs])
        k.ts("vector", sgnw, wis[:, n, :], -1.0, None, ALU.mult)
        k.tt("vector", absw, wis[:, n, :], sgnw, ALU.max)
        k.ts("vector", sgnw, wis[:, n, :], 0.0, 2.0, ALU.is_ge, ALU.mult)
        k.ts("vector", sgnw, sgnw, -1.0, None, ALU.add)
        for kb in range((nkt + 3) // 4):
            ncols = min(512, nk - kb * 512)
            cs = slice(kb * 512, kb * 512 + ncols)
            for h in range(4):
                c, hp = h // 2, h % 2
                pa = sc_rot.next()
                k.mm(pa[:, 0:ncols], zi[:, c, hp, :], kiT[:, cs])
                rt = rts.next()
                k.act(rt[:, 0:ncols], pa[:, 0:ncols], AF.Relu, scale=absw[:, h:h + 1])
                if h == 0:
                    k.ts("vector", sc[:, cs], rt[:, 0:ncols], sgnw[:, 0:1], None, ALU.mult)
                else:
                    k.stt(sc[:, cs], rt[:, 0:ncols], sgnw[:, h:h + 1], sc[:, cs], ALU.mult, ALU.add)
        k.ts("vector", sc[:, 0:2048], sc[:, 0:2048], negb, None, ALU.add)
        ds_ = slice((nkt - 1) * 128, nkt * 128)
        k.tt("vector", sc[:, ds_], sc[:, ds_], diagm, ALU.add)
        k.reduce(rmax, sc[:, 0:nk], ALU.max)
        k.ts("vector", mid, rmax, -RNG + RNG / 2, None, ALU.add)
        for it in range(NBIS):
            wn = RNG / (2 ** (it + 2))
            k.ts("vector", mbar[:, 0:nk], sc[:, 0:nk], mid, None, ALU.is_gt, op1=ALU.add, accum=cnt)
            k.ts("vector", dlt, cnt, 255.5, 2.0 * wn, ALU.is_ge, ALU.mult)
            k.stt(mid, dlt, -wn, mid, ALU.add, ALU.add)
        k.ts("vector", lo, mid, -RNG / (2 ** (NBIS + 1)), None, ALU.add)
        k.ts("vector", mbar[:, 0:nk], sc[:, 0:nk], lo, None, ALU.is_le)
        return (n, nkt, qs, zq, mbar)

    def stage_b(ctx):
        n, nkt, qs, zq, mbar = ctx
        poA, poB = po_rot.next()
        k.mm(poA[:, 0:260], zerob[:, 0:128], zerob[:, 0:260], start=True, stop=False, skip_group_check=True)
        k.mm(poB[:, 0:260], zerob[:, 0:128], zerob[:, 0:260], start=True, stop=False, skip_group_check=True)
        groups = [(j, gq) for j in range(nkt) for gq in range(2)]

        def logits(j, gq):
            ks_ = slice(j * 128, (j + 1) * 128)
            pS = s_rot.next()
            for hh in range(4):
                h = gq * 4 + hh
                c, hp = h // 2, h % 2
                k.mm(pS[:, hh * 128:(hh + 1) * 128], kaT[:, c, ks_], zq[:, c, hp, :], start=True, stop=False)
                k.mm(pS[:, hh * 128:(hh + 1) * 128], mbar[:, ks_], negI, start=False, stop=True)
            pT = pTs.next()
            k.act(pT, pS, AF.Exp, scale=0.125)
            return pT

        def pv_mm(j, gq, pT):
            po = poA if gq == 0 else poB
            for hh in range(4):
                h = gq * 4 + hh
                k.mm(po[:, hh * 65:(hh + 1) * 65], pT[:, hh * 128:(hh + 1) * 128], va[:, j, h, :],
                     start=False, stop=(j == nkt - 1), skip_group_check=True)

        pend = None
        for (j, gq) in groups:
            pT = logits(j, gq)
            if pend is not None:
                pv_mm(*pend)
            pend = (j, gq, pT)
        pv_mm(*pend)
        for gq, po in enumerate((poA, poB)):
            pv = po[:, 0:260].re("p (h d) -> p h d", h=4)
            k.recip(rden[:, gq * 4:(gq + 1) * 4], pv[:, :, 64])
            k.tt("vector", oa[:, gq * 4:(gq + 1) * 4, :], pv[:, :, 0:64],
                 rden[:, gq * 4:(gq + 1) * 4].unsq(2).bc([128, 4, 64]), ALU.mult)
        pa = s_rot.next()
        pv = pa.bitcast(BF16).re("p (j t) -> p j t", j=8)
        oaf = oa.re("p h d -> p (h d)")
        for c in range(4):
            k.tr(pv[:, c, :], oaf[:, c * 128:(c + 1) * 128], identb)
        k.copy("scalar", mixA[:, :, qs], pv[:, 0:4, :])

    ctx = stage_a(0)
    for n in range(NT_OWN):
        nxt = stage_a(n + 1) if n + 1 < NT_OWN else None
        stage_b(ctx)
        ctx = nxt
    k.barrier()
    A.reset(persist_mark)

    mixB = A.alloc("mixB", [128, 4, 2048], BF16)
    hg_mark = A.mark()
    wqb = A.alloc("wqb", [128, 8, 256], BF16); wload(wqb, w_qb)
    wfb = A.alloc("wfb", [128, 8, 256], BF16); wload(wfb, w_fb)
    wib = A.alloc("wib", [128, 8, 512], BF16); wload(wib, w_ib)
    wgb = A.alloc("wgb", [128, 8, 512], BF16); wload(wgb, w_gb)
    hT = A.alloc("hT", [128, 8, 512], BF16)
    xslots = Rot([A.alloc(f"xs{i}", [128, 1024], F32) for i in range(2)])
    ntmps = (A.alloc("sqj", [128, 1024], BF16), [A.alloc(f"xn{i_}", [128, 1024], BF16) for i_ in range(2)], A.alloc("tmpf", [128, 8, 128], F32))
    vtok_s = [A.alloc(f"vtok{i}", [128, 4, 512], BF16) for i in range(2)]
    sgt_s = [A.alloc(f"sgt{i}", [128, 4, 512], BF16) for i in range(2)]
    sgf = A.alloc("sgf", [128, 512], F32)
    S = A.alloc("S", [128, 2, 128], F32); k.memset("vector", S, 0.0)
    Sbf = [A.alloc(f"Sbf{i}", [128, 2, 128], BF16) for i in range(8)]
    Bext = [A.alloc(f"Bext{c2}", [128, 513], F32) for c2 in range(2)]
    for c2 in range(2):
        k.memset("vector", Bext[c2][:, 0:1], 0.0)
    sig = A.alloc("sig", [128, 512], F32); fT = A.alloc("fT", [128, 512], F32)
    logf = A.alloc("logf", [128, 512], F32); omf = A.alloc("omf", [128, 512], F32)
    qbf = A.alloc("qbf", [128, 512], F32)
    D1 = A.alloc("D1", [128, 8, 64], F32); D3 = A.alloc("D3", [128, 8, 64], F32); D4 = A.alloc("D4", [128, 8, 64], F32)
    E1 = A.alloc("E1", [128, 512], F32); E2 = A.alloc("E2", [128, 512], F32)
    E3 = A.alloc("E3", [128, 512], F32); E4 = A.alloc("E4", [128, 512], F32)
    dd = A.alloc("dd", [128, 8], F32)
    gsets = []
    for si in range(2):
        dec_ = [A.alloc(f"dec{si}_{c2}", [128, 8], F32) for c2 in range(2)]
        qtZ_ = [A.alloc(f"qtZ{si}_{c2}", [128, 2, 512], BF16) for c2 in range(2)]
        qeZ_ = [A.alloc(f"qeZ{si}_{c2}", [128, 2, 512], BF16) for c2 in range(2)]
        ktT_ = [A.alloc(f"ktT{si}_{c2}", [128, 512], BF16) for c2 in range(2)]
        kdT_ = [A.alloc(f"kdT{si}_{c2}", [128, 512], BF16) for c2 in range(2)]
        for c2 in range(2):
            k.memset("gpsimd", qtZ_[c2], 0.0)
            k.memset("gpsimd", qeZ_[c2], 0.0)
        gsets.append((vtok_s[si], sgt_s[si], dec_, qtZ_, qeZ_, ktT_, kdT_))
    kdZ = [[A.alloc(f"kdZ{c2}_{i}", [128, 2, 128], BF16) for i in range(2)] for c2 in range(2)]
    for c2 in range(2):
        for i in range(2):
            k.memset("gpsimd", kdZ[c2][i], 0.0)
    attS = [A.alloc(f"attS{i}", [128, 4, 128], BF16) for i in range(2)]
    for i in range(2):
        k.memset("gpsimd", attS[i], 0.0)
    ssq = A.alloc("ssq", [128, 4], F32); rs4 = A.alloc("rs4", [128, 4], F32); rstd4 = A.alloc("rstd4", [128, 4], F32)
    sqj2 = A.alloc("sqj2", [128, 128], BF16)
    ob1 = A.alloc("ob1", [128, 4, 128], F32); ob2 = A.alloc("ob2", [128, 4, 128], F32)
    obb = A.alloc("obb", [128, 512], BF16)
    prot = Rot(banks[0:4])
    kv_rot = Rot(banks[4:6])
    at_rot = Rot([(banks[6], banks[7])])
    tri2b = tri2.unsq(1).bc([128, 2, 128])
    def hg_front(g):
        own = g >= 4
        vtok, sgt, dec, qtZ, qeZ, ktT, kdT = gsets[g % 2]
        src = xo if own else xp
        make_hT(src, (g % 4) * 4, 4, s1col, sh1col, hT, xslots, ntmps, prot)
        for i in range(4):
            pa = prot.next()
            for kc in range(8):
                k.mm(pa, hT[:, kc, i * 128:(i + 1) * 128], wib[:, kc, :], start=(kc == 0), stop=(kc == 7))
            k.copy("scalar", vtok[:, i, :], pa)
            if own:
                pa = prot.next()
                for kc in range(8):
                    k.mm(pa, hT[:, kc, i * 128:(i + 1) * 128], wgb[:, kc, :], start=(kc == 0), stop=(kc == 7))
                k.act(sgf, pa, AF.Sigmoid)
                k.tt("vector", sgt[:, i, :], sgf, pa, ALU.mult)
        for c2 in range(2):
            pa = prot.next()
            for kc in range(8):
                k.mm(pa, wfb[:, kc, c2 * 128:(c2 + 1) * 128], hT[:, kc, :], start=(kc == 0), stop=(kc == 7))
            k.act(sig, pa, AF.Sigmoid)
            k.ts("vector", fT, sig, omlb[:, c2:c2 + 1], lbcol[:, c2:c2 + 1], ALU.mult, ALU.add)
            k.act(logf, fT, AF.Ln)
            k.ts("vector", omf, fT, -1.0, 1.0, ALU.mult, ALU.add)
            Bx = Bext[c2]
            k.scan_add(Bx[:, 1:513], onesf, logf, Bx[:, 0:1])
            Bg = Bx[:, 1:513].re("p (c t) -> p c t", c=8)
            Bprev = Bx[:, 0:512].re("p (c t) -> p c t", c=8)[:, :, 0:1]
            Blast = Bg[:, :, 63:64]
            Bmid = Bg[:, :, 31:32]
            k.tt("vector", D4, Blast.bc([128, 8, 64]), Bg, ALU.subtract)
            k.act(E4, D4.re("p c t -> p (c t)"), AF.Exp)
            k.tt("vector", kdT[c2], omf, E4, ALU.mult)
            k.tt("vector", dd, Blast.re("p c o -> p (c o)"), Bprev.re("p c o -> p (c o)"), ALU.subtract)
            k.act(dec[c2], dd, AF.Exp)
            if own:
                pq = prot.next()
                for kc in range(8):
                    k.mm(pq, wqb[:, kc, c2 * 128:(c2 + 1) * 128], hT[:, kc, :], start=(kc == 0), stop=(kc == 7))
                k.copy("scalar", qbf, pq)
                k.tt("vector", D1, Bg, Bmid.bc([128, 8, 64]), ALU.subtract)
                k.act(E1, D1.re("p c t -> p (c t)"), AF.Exp)
                k.act(E2, D1.re("p c t -> p (c t)"), AF.Exp, scale=-1.0)
                k.tt("vector", D3, Bg, Bprev.bc([128, 8, 64]), ALU.subtract)
                k.act(E3, D3.re("p c t -> p (c t)"), AF.Exp)
                k.tt("vector", ktT[c2], omf, E2, ALU.mult)
                for hp in range(2):
                    ph = slice(hp * 64, (hp + 1) * 64)
                    k.tt("vector", qtZ[c2][ph, hp, :], qbf[ph, :], E1[ph, :], ALU.mult)
                    k.tt("vector", qeZ[c2][ph, hp, :], qbf[ph, :], E3[ph, :], ALU.mult)
            k.copy("vector", Bx[:, 0:1], Bx[:, 512:513])

    def hg_back(g):
        own = g >= 4
        vtok, sgt, dec, qtZ, qeZ, ktT, kdT = gsets[g % 2]
        for i in range(4):
            ts_ = slice(i * 128, (i + 1) * 128)
            kz = []
            for c2 in range(2):
                pa = prot.next()
                pv = pa.bitcast(BF16)
                k.tr(pv[:, 0:128], kdT[c2][:, ts_], identb)
                kzz = kdZ[c2][i % 2]
                for cp in range(2):
                    k.copy("scalar", kzz[cp * 64:(cp + 1) * 64, cp, :], pv[cp * 64:(cp + 1) * 64, 0:128])
                kz.append(kzz)
            if own:
                pA0, pA1 = at_rot.next()
                for cp in range(2):
                    cs_ = slice(i * 128 + cp * 64, i * 128 + (cp + 1) * 64)
                    for h in range(4):
                        c2, hp = h // 2, h % 2
                        pbk = pA0 if hp == 0 else pA1
                        k.mm(pbk[cp * 64:(cp + 1) * 64, c2 * 64:(c2 + 1) * 64], ktT[c2][:, cs_], qtZ[c2][:, hp, cs_])
                aS = attS[i % 2]
                for hp, pbk in enumerate((pA0, pA1)):
                    for c2 in range(2):
                        h = 2 * c2 + hp
                        for cp in range(2):
                            ph = slice(cp * 64, (cp + 1) * 64)
                            k.tt("vector", aS[ph, h, cp * 64:(cp + 1) * 64], pbk[ph, c2 * 64:(c2 + 1) * 64],
                                 tri2[ph, cp * 64:(cp + 1) * 64], ALU.mult)
            for cp in range(2):
                cidx = i * 2 + cp
                if own:
                    k.copy("scalar", Sbf[cidx], S)
                pkv = kv_rot.next()
                for h in range(4):
                    c2, hp = h // 2, h % 2
                    k.mm(pkv[hp * 64:(hp + 1) * 64, c2 * 128:(c2 + 1) * 128], kz[c2][:, cp, hp * 64:(hp + 1) * 64],
                         vtok[:, i, h * 128:(h + 1) * 128])
                for c2 in range(2):
                    k.stt(S[:, c2, :], S[:, c2, :], dec[c2][:, cidx:cidx + 1], pkv[:, c2 * 128:(c2 + 1) * 128], ALU.mult, ALU.add)
                if g == 3 and cidx == 7:
                    k.ts("vector", S.re("p a b -> p (a b)"), S.re("p a b -> p (a b)"), sflag, None, ALU.mult)
            if own:
                po = prot.next()
                for h in range(4):
                    c2, hp = h // 2, h % 2
                    k.mm(po[:, h * 128:(h + 1) * 128], aS[:, h, :], vtok[:, i, h * 128:(h + 1) * 128], start=True, stop=False)
                    for cp in range(2):
                        cidx = i * 2 + cp
                        cs_ = slice(i * 128 + cp * 64, i * 128 + (cp + 1) * 64)
                        k.mm(po[cp * 64:(cp + 1) * 64, h * 128:(h + 1) * 128], qeZ[c2][:, hp, cs_], Sbf[cidx][:, c2, :],
                             start=False, stop=True)
                pov = po.re("p (h e) -> p h e", h=4)
                for h in range(4):
                    k.act(sqj2, po[:, h * 128:(h + 1) * 128], AF.Square, accum=ssq[:, h:h + 1])
                k.act(rs4, ssq, AF.Sqrt, scale=1.0 / 128, bias=epsc)
                k.recip(rstd4, rs4)
                k.tt("vector", ob1, pov, rstd4.unsq(2).bc([128, 4, 128]), ALU.mult)
                k.tt("vector", ob2, ob1, hgbc.unsq(1).bc([128, 4, 128]), ALU.mult)
                k.tt("vector", obb, ob2.re("p h e -> p (h e)"), sgt[:, i, :], ALU.mult)
                pa = prot.next()
                pv = pa.bitcast(BF16).re("p (j t) -> p j t", j=8)
                for c in range(4):
                    k.tr(pv[:, c, :], obb[:, c * 128:(c + 1) * 128], identb)
                tt0 = (g - 4) * 512 + i * 128
                k.copy("scalar", mixB[:, :, tt0:tt0 + 128], pv[:, 0:4, :])

    hg_front(0)
    for g in range(8):
        if g + 1 < 8:
            hg_front(g + 1)
        hg_back(g)
    k.barrier()
    A.reset(hg_mark)

    idxall = A.alloc("idxall", [128, 16, 4], I32, top=True)
    gkall = A.alloc("gkall", [128, 16, 4], F32, top=True)
    nblk_i = A.alloc("nblk_i", [128, 32], I32, top=True)
    p5_mark = A.mark()
    wo = A.alloc("wo", [128, 8, 1024], BF16); wload(wo, w_out)
    rwf = A.alloc("rwf", [128, 8, 32], F32)
    k.dma("sync", rwf, rw.re("(kc p) e -> p kc e", p=128))
    LTb = A.alloc("LTb", [128, 128], BF16); k.dma("sync", LTb, c_ltb)
    onesb = A.alloc("onesb", [128, 128], BF16); k.memset("vector", onesb, 1.0)
    erow = A.alloc("erow", [128, 32], F32); k.dma("sync", erow, c_erow)
    carry = A.alloc("carry", [128, 32], F32); k.memset("vector", carry, 0.0)
    xslots = Rot([A.alloc(f"xs{i}", [128, 1024], F32) for i in range(2)])
    x1r = Rot([A.alloc(f"x1_{i}", [128, 1024], F32) for i in range(2)])
    h2ts = Rot([A.alloc(f"h2tok{i}", [128, 1024], BF16) for i in range(2)])
    ytmp = A.alloc("ytmp", [128, 1024], F32)
    sqj = A.alloc("sqj", [128, 1024], BF16)
    xnf = A.alloc("xnf", [128, 1024], F32)
    h2f = A.alloc("h2f", [128, 8, 128], F32); h2ft = A.alloc("h2ft", [128, 8, 128], F32)
    lg = A.alloc("lg", [128, 32], F32); m8 = A.alloc("m8", [128, 8], F32)
    msk = A.alloc("msk", [128, 32], F32); ex = A.alloc("ex", [128, 32], F32)
    mskb = A.alloc("mskb", [128, 32], BF16)
    rank = A.alloc("rank", [128, 32], F32); keyt = A.alloc("keyt", [128, 32], F32)
    ek8 = A.alloc("ek8", [128, 8], F32); oh = A.alloc("oh", [128, 32], F32); ohr = A.alloc("ohr", [128, 32], F32)
    rk = A.alloc("rk", [128, 1], F32); slf = A.alloc("slf", [128, 1], F32)
    nmax = A.alloc("nmax", [128, 1], F32); zs = A.alloc("zs", [128, 1], F32); rz = A.alloc("rz", [128, 1], F32)
    y_rot = Rot([(banks[0], banks[1]), (banks[2], banks[3])])
    t_rot = Rot([(banks[4], banks[5]), (banks[6], banks[7])])
    s2bc = s2col.unsq(2).bc([128, 8, 128]); sh2bc = sh2col.unsq(2).bc([128, 8, 128])
    def p5_front(i):
        lg = lgs.next()
        ts_ = slice(i * 128, (i + 1) * 128)
        xt = xslots.next()
        k.dma("sync", xt, xo[ts_, :])
        py = y_rot.next()
        for nb in range(2):
            for kc in range(8):
                mixsrc = mixA if kc < 4 else mixB
                k.mm(py[nb], mixsrc[:, kc % 4, ts_], wo[:, kc, nb * 512:(nb + 1) * 512], start=(kc == 0), stop=(kc == 7))
        x1 = x1r.next()
        for nb in range(2):
            ns_ = slice(nb * 512, (nb + 1) * 512)
            k.tt("vector", ytmp[:, ns_], py[nb], g1bc[:, ns_], ALU.mult)
            k.tt("vector", x1[:, ns_], ytmp[:, ns_], xt[:, ns_], ALU.add)
        k.dma("sync", x1s[ts_, :], x1, chan=x1.buf)
        if debug:
            k.dma("sync", dbg["x1"][ts_, :], x1, chan=x1.buf)
        ss = smalls.next(); rs = smalls.next(); rstd = smalls.next()
        k.act(sqj, x1, AF.Square, accum=ss)
        k.act(rs, ss, AF.Sqrt, scale=1.0 / 1024, bias=epsc)
        k.recip(rstd, rs)
        k.act(xnf, x1, AF.Identity, scale=rstd)
        h2tok = h2ts.next()
        k.tt("vector", ytmp, xnf, s2row, ALU.mult)
        k.tt("vector", h2tok, ytmp, sh2row, ALU.add)
        pt = t_rot.next()
        for j in range(8):
            k.tr(pt[j // 4][:, (j % 4) * 128:(j % 4 + 1) * 128], xnf[:, j * 128:(j + 1) * 128], identf)
        for hb in range(2):
            pv = pt[hb].re("p (j t) -> p j t", j=4)
            k.tt("vector", h2ft[:, hb * 4:(hb + 1) * 4, :], pv, s2bc[:, hb * 4:(hb + 1) * 4, :], ALU.mult)
        k.tt("vector", h2f, h2ft, sh2bc, ALU.add)
        pl = y_rot.next()[0]
        for kc in range(8):
            k.mm(pl[:, 0:32], h2f[:, kc, :], rwf[:, kc, :], start=(kc == 0), stop=(kc == 7))
        k.tt("vector", lg, pl[:, 0:32], rbbc, ALU.add)
        return (i, lg, h2tok)

    def p5_back(ctx):
        i, lg, h2tok = ctx
        k.vmax8(m8, lg)
        k.ts("vector", msk, lg, m8[:, 3:4], None, ALU.is_ge)
        k.ts("vector", nmax, m8[:, 0:1], -1.0, None, ALU.mult)
        k.act(ex, lg, AF.Exp, bias=nmax)
        k.tt("vector", ex, ex, msk, ALU.mult)
        k.reduce(zs, ex, ALU.add)
        k.recip(rz, zs)
        k.ts("vector", Gall[:, i, :], ex, rz, None, ALU.mult)
        k.copy("vector", mskb, msk)
        pr = y_rot.next()[1]
        k.mm(pr[:, 0:32], LTb, mskb)
        k.mm(pr[:, 32:64], onesb, mskb)
        k.tt("vector", rank, pr[:, 0:32], carry, ALU.add)
        k.tt("vector", carry, carry, pr[:, 32:64], ALU.add)
        k.tt("vector", keyt, msk, erow, ALU.mult)
        k.vmax8(ek8, keyt)
        for kk in range(4):
            k.ts("vector", oh, erow, ek8[:, kk:kk + 1], None, ALU.is_equal)
            k.tt("vector", ohr, oh, rank, ALU.mult)
            k.reduce(rk, ohr, ALU.add)
            k.tt("vector", ohr, oh, Gall[:, i, :], ALU.mult)
            k.reduce(gkall[:, i, kk:kk + 1], ohr, ALU.add)
            k.ts("vector", slf, ek8[:, kk:kk + 1], -1.0, 2048.0, ALU.add, ALU.mult)
            k.tt("vector", slf, slf, rk, ALU.add)
            ix = T(idxall.ap[:, i, kk:kk + 1], Buf(f"idx_{i}_{kk}"))
            idxT[(i, kk)] = ix
            k.copy("vector", ix, slf)
            k.idma(XG, h2tok, ix, scatter=True, chan=h2tok.buf)

    idxT = {}
    lgs = Rot([A.alloc(f"lg{i}", [128, 32], F32) for i in range(2)])
    ctx5 = p5_front(0)
    for i in range(NT_OWN):
        nxt5 = p5_front(i + 1) if i + 1 < NT_OWN else None
        p5_back(ctx5)
        ctx5 = nxt5
    k.ts("vector", rank, carry, 63.5, 1.0 / 128, ALU.add, ALU.mult)
    k.copy("vector", nblk_i, rank)
    if debug:
        k.dma("sync", dbg["G"], Gall.re("p a b -> p (a b)"), chan=Gall.buf)
    k.barrier()
    A.reset(small_mark)

    b1T = A.alloc("b1T", [128, 16, 32], F32)
    b1_mark = A.mark()
    b1sb = A.alloc("b1sb", [32, 2048], F32); k.dma("sync", b1sb, b1)
    pb = banks[0]
    for c in range(16):
        k.tr(pb[:, c * 32:(c + 1) * 32], b1sb[0:32, c * 128:(c + 1) * 128], identf[0:32, 0:32])
    k.copy("vector", b1T, pb.re("p (c e) -> p c e", c=16))
    k.barrier()
    A.reset(b1_mark)
    p6_mark = A.mark()
    w1b = A.alloc("w1b", [128, 8, 2048], BF16)
    w2b = A.alloc("w2b", [128, 8, 1024], BF16)
    stg1 = [A.alloc(f"stg1_{p}", [128, 2048], F32) for p in range(8)]
    stg2 = [A.alloc(f"stg2_{j}", [128, 2, 1024], F32) for j in range(4)]
    xbs = Rot([A.alloc(f"xb{i}", [128, 1024], BF16) for i in range(3)])
    xets = Rot([A.alloc(f"xet{i}", [128, 8, 128], BF16) for i in range(2)])
    actTs = Rot([A.alloc(f"actT{i}", [128, 8, 128], BF16) for i in range(2)])
    ysbs = Rot([A.alloc(f"ysb{i}", [128, 1024], F32) for i in range(2)])
    gts = Rot([A.alloc(f"gt{i}", [128, 4, 128], F32) for i in range(1)])
    lts = Rot([A.alloc(f"lt{i}", [128, 4, 128], F32) for i in range(1)])
    sts = Rot([A.alloc(f"st{i}", [128, 4, 128], F32) for i in range(1)])
    gss = Rot([A.alloc(f"gs{i}", [128, 4, 128], F32) for i in range(1)])
    ptr_bank = banks[0]
    mm1_banks = banks[1:5]
    y_banks = (banks[5], banks[6])

    def piece_dma(e, p):
        if p < 8:
            k.dma("sync", stg1[p], w1[e][p * 128:(p + 1) * 128, :])
        else:
            j = p - 8
            k.dma("sync", stg2[j], w2[e][j * 256:(j + 1) * 256, :].re("(a p) n -> p a n", p=128))

    def piece_cast(p):
        dst, src = (w1b[:, p, :], stg1[p]) if p < 8 else (w2b[:, 2 * (p - 8):2 * (p - 8) + 2, :], stg2[p - 8])
        k.copy("vector", dst, src)

    for p in range(12):
        piece_dma(0, p)
    for p in range(12):
        piece_cast(p)
        piece_dma(1, p)
    key_next = k.val_load(nblk_i[0:1, 0:1])
    xb_first = xbs.next()
    k.dma("gpsimd", xb_first, XG[0:128, :])
    for e in range(32):
        key = key_next
        xb_next = xb_first
        for blk in range(16):
            k.cond_begin(key, blk)
            r0 = e * 2048 + blk * 128
            xb = xb_next
            if blk + 1 < 16:
                xb_next = xbs.next()
                k.dma("gpsimd", xb_next, XG[r0 + 128:r0 + 256, :])
            pv = ptr_bank.bitcast(BF16).re("p (j t) -> p j t", j=8)
            for j in range(8):
                k.tr(pv[:, j, :], xb[:, j * 128:(j + 1) * 128], identb)
            xet = xets.next()
            k.copy("scalar", xet, pv)
            for gp in range(2):
                for g4 in (gp, 2 + gp):
                    pbk = mm1_banks[g4]
                    for q in range(4):
                        fc = g4 * 4 + q
                        for kc in range(8):
                            k.mm(pbk[:, q * 128:(q + 1) * 128], w1b[:, kc, fc * 128:(fc + 1) * 128], xet[:, kc, :],
                                 start=(kc == 0), stop=(kc == 7))
            actT = actTs.next()
            ysb = ysbs.next()
            for gp in range(2):
                pg = mm1_banks[gp].re("p (q c) -> p q c", q=4)
                pl_ = mm1_banks[2 + gp].re("p (q c) -> p q c", q=4)
                bg = b1T[:, gp * 4:(gp + 1) * 4, e:e + 1].bc([128, 4, 128])
                bl = b1T[:, 8 + gp * 4:8 + (gp + 1) * 4, e:e + 1].bc([128, 4, 128])
                gt_ = gts.next(); lt_ = lts.next(); st_ = sts.next(); gs_ = gss.next()
                k.tt("vector", gt_, pg, bg, ALU.add)
                k.ts("vector", gt_, gt_, 7.0, None, ALU.min)
                k.act(st_, gt_, AF.Sigmoid, scale=1.702)
                k.tt("vector", lt_, pl_, bl, ALU.add)
                k.ts("vector", lt_, lt_, 7.0, -7.0, ALU.min, ALU.max)
                k.tt("vector", gs_, gt_, st_, ALU.mult)
                k.stt(actT[:, gp * 4:(gp + 1) * 4, :], lt_, 1.0, gs_, ALU.add, ALU.mult)
                for nb in range(2):
                    for f in range(gp * 4, gp * 4 + 4):
                        k.mm(y_banks[nb], actT[:, f, :], w2b[:, f, nb * 512:(nb + 1) * 512], start=(f == 0), stop=(f == 7))
            k.copy("scalar", ysb[:, 0:512], y_banks[0])
            k.copy("vector", ysb[:, 512:1024], y_banks[1])
            k.dma("gpsimd", YG[r0:r0 + 128, :], ysb, chan=ysb.buf)
        for blk in range(16):
            k.cond_end()
        if e + 1 < 32:
            key_next = k.val_load(nblk_i[0:1, e + 1:e + 2])
            xb_first = xbs.next()
            k.dma("gpsimd", xb_first, XG[(e + 1) * 2048:(e + 1) * 2048 + 128, :])
            for p in range(12):
                piece_cast(p)
                if e + 2 < 32:
                    piece_dma(e + 2, p)
    k.barrier()
    A.reset(p6_mark)

    GTs = Rot([A.alloc(f"GT{i}", [32, 128], F32) for i in range(2)])
    b2sb = A.alloc("b2sb", [32, 1024], F32); k.dma("sync", b2sb, b2)
    accs = Rot([A.alloc(f"acc{i}", [128, 1024], F32) for i in range(2)])
    ygs = Rot([A.alloc(f"yg{i}", [128, 1024], F32) for i in range(8)])
    xslots = Rot([A.alloc(f"xs{i}", [128, 1024], F32) for i in range(3)])
    fo = Rot([A.alloc(f"fo{i}", [128, 1024], F32) for i in range(2)])
    sqj = A.alloc("sqj", [128, 1024], BF16)
    pg_rot = Rot(banks[0:2])
    y_rot = Rot([(banks[4], banks[5]), (banks[6], banks[7])])

    def p7_init(ti):
        ts_ = slice(ti * 128, (ti + 1) * 128)
        acc = accs.next()
        GT = GTs.next()
        pgt = pg_rot.next()
        k.tr(pgt[0:32, 0:128], Gall[:, ti, :], identf)
        k.copy("vector", GT, pgt[0:32, 0:128])
        py = y_rot.next()
        for nb in range(2):
            k.mm(py[nb], GT[0:32, :], b2sb[0:32, nb * 512:(nb + 1) * 512])
            k.copy("scalar", acc[:, nb * 512:(nb + 1) * 512], py[nb])
        ygl = []
        for kk in range(4):
            yg = ygs.next()
            k.idma(yg, YG, idxT[(ti, kk)], scatter=False, chan=yg.buf)
            ygl.append(yg)
        xt = xslots.next()
        k.dma("sync", xt, x1s[ts_, :])
        return dict(ti=ti, ts=ts_, acc=acc, ygl=ygl, xt=xt)

    def p7_main(cx):
        ti, acc, xt = cx["ti"], cx["acc"], cx["xt"]
        for kk in range(4):
            k.stt(acc, cx["ygl"][kk], gkall[:, ti, kk:kk + 1], acc, ALU.mult, ALU.add)
        k.tt("vector", acc, acc, g2bc, ALU.mult)
        k.tt("vector", xt, xt, acc, ALU.add)
        ss = smalls.next(); rs = smalls.next()
        k.act(sqj, xt, AF.Square, accum=ss)
        k.act(rs, ss, AF.Sqrt, scale=1.0 / 1024, bias=epsc)
        cx["rs"] = rs

    def p7_tail(cx):
        rstd = smalls.next()
        k.recip(rstd, cx["rs"])
        ot = fo.next()
        k.stt(ot, cx["xt"], rstd, fgbc, ALU.mult, ALU.mult)
        k.dma("sync", yout[cx["ts"], :], ot, chan=ot.buf)

    cur = p7_init(0)
    prev = None
    for ti in range(NT_OWN):
        nxt = p7_init(ti + 1) if ti + 1 < NT_OWN else None
        p7_main(cur)
        if prev is not None:
            p7_tail(prev)
        prev = cur
        cur = nxt
    p7_tail(prev)
    k.final_wait("sync")
    k.emit()
    st.close()
    return nc, k


SPL = np.cumsum([0, 512, 512, 512, 256, 64, 4, 256, 256, 512, 512])


def _swap_perm(ncols):
    p = np.arange(ncols)
    for h0 in range(0, ncols, 64):
        p[h0:h0 + 8] = np.arange(h0 + 8, h0 + 16)
        p[h0 + 8:h0 + 16] = np.arange(h0, h0 + 8)
    return p


def _consts(half):
    identf = np.eye(128, dtype=np.float32)
    identb = identf.astype(ml_dtypes.bfloat16)
    negI = (-1024.0 * identf).astype(ml_dtypes.bfloat16)
    s = np.arange(128)[:, None]
    t = np.arange(128)[None, :]
    tri2 = (((s // 64) == (t // 64)) & ((s % 64) <= (t % 64))).astype(np.float32)
    diag = np.where((t // 64) <= (s // 64), 0.0, NEG).astype(np.float32)
    cols = np.zeros((128, 4), np.float32)
    inv_freq = (500000.0 ** (-(np.arange(0, 16, 2, dtype=np.float32) / 16))).astype(np.float32)
    for p in range(128):
        d = p % 64
        if d < 16:
            cols[p, 0] = inv_freq[d % 8]
            cols[p, 1] = -1.0 if d < 8 else 1.0
    cols[:, 2] = 0.0 if half == 1 else NEG
    cols[:, 3] = float(half)
    ltb = (s < t).astype(np.float32).astype(ml_dtypes.bfloat16)
    erow = np.broadcast_to(np.arange(1, 33, dtype=np.float32)[None, :], (128, 32)).copy()
    return {"c_ltb": ltb, "c_erow": erow, "c_identb": identb, "c_identf": identf, "c_negI": negI, "c_tri2": tri2, "c_diag": diag, "c_cols": cols}


_CACHE = {}


def kernel(x, c, positions, ada_w, ada_b, norm1_g, w_in, hg_norm_g, lb_logits, w_out, norm2_g,
           router_w, router_b, moe_w1, moe_b1, moe_w2, moe_b2, final_g, _debug=False):
    f = lambda a: np.ascontiguousarray(np.asarray(a, dtype=np.float32))
    x = f(x); c = f(c); positions = np.ascontiguousarray(np.asarray(positions, dtype=np.int32))
    w_in0 = f(w_in)[0]
    parts = [w_in0[:, SPL[i]:SPL[i + 1]] for i in range(10)]
    qa, ka, va, qi, ki, wi, qb, fb, ib, gb = parts
    ki2 = np.concatenate([ki, ki], axis=1)
    shared = {
        "ada_w": f(ada_w)[0], "ada_b": f(ada_b)[0], "norm1_g": f(norm1_g)[0], "norm2_g": f(norm2_g)[0],
        "final_g": f(final_g), "hg_norm_g": f(hg_norm_g)[0], "lb_logits": f(lb_logits),
        "w_ka": f(ka), "w_kas": f(ka[:, _swap_perm(512)]), "w_ki": f(ki2), "w_kis": f(ki2[:, _swap_perm(128)]),
        "w_va": f(va), "w_qa": f(qa), "w_qas": f(qa[:, _swap_perm(512)]),
        "w_qi": f(qi), "w_qis": f(qi[:, _swap_perm(256)]), "w_wi": f(wi),
        "w_qb": f(qb), "w_fb": f(fb), "w_ib": f(ib), "w_gb": f(gb),
        "w_out": f(w_out)[0], "router_w": f(router_w)[0], "router_b": f(router_b)[0],
        "moe_w1": f(moe_w1)[0], "moe_b1": f(moe_b1)[0], "moe_w2": f(moe_w2)[0], "moe_b2": f(moe_b2)[0],
    }
    in_maps = []
    for j in range(8):
        b, half = j // 2, j % 2
        m = dict(shared)
        m["xo"] = np.ascontiguousarray(x[b, half * 2048:(half + 1) * 2048])
        m["xp"] = np.ascontiguousarray(x[b, 0:2048])
        m["cvec"] = np.ascontiguousarray(c[b])
        m["posr"] = np.ascontiguousarray(np.concatenate([positions[b, 0:2048], positions[b, half * 2048:(half + 1) * 2048]]))
        m.update(_consts(half))
        in_maps.append(m)
    key = bool(_debug)
    if key not in _CACHE:
        _CACHE[key] = build_nc(debug=key)[0]
    nc = _CACHE[key]
    res = run_bass_kernel_spmd(nc, in_maps, core_ids=list(range(8)))
    out = np.empty((4, 4096, 1024), np.float32)
    for j in range(8):
        b, half = j // 2, j % 2
        out[b, half * 2048:(half + 1) * 2048] = res.results[j]["y"]
    if _debug:
        return out, res.results
    return out
```

```python
from contextlib import ExitStack
import numpy as np
import ml_dtypes
import concourse.bass as bass
import concourse.mybir as mybir
from concourse.bass_utils import run_bass_kernel_spmd

F32 = mybir.dt.float32
BF16 = mybir.dt.bfloat16
I32 = mybir.dt.int32
U8 = mybir.dt.uint8
AF = mybir.ActivationFunctionType
ALU = mybir.AluOpType
AX = mybir.AxisListType
DSZ = {F32: 4, BF16: 2, I32: 4, U8: 1}

ENGS = ["tensor", "vector", "scalar", "gpsimd", "sync"]
SEM_LIMIT = 30000
EPS = 1e-6
PI = float(np.pi)
NT_OWN = 16
NT_ALL = 32
RNG = 32.0
NBIS = 13
NEG = -1.0e30


class Buf:
    __slots__ = ("name", "w", "r", "dsem", "dcount")

    def __init__(self, name):
        self.name = name
        self.w = {}
        self.r = {}
        self.dsem = None
        self.dcount = 0


class T:
    __slots__ = ("ap", "buf")

    def __init__(self, ap, buf):
        self.ap = ap
        self.buf = buf

    def __getitem__(self, key):
        return T(self.ap[key], self.buf)

    def bitcast(self, dt):
        return T(self.ap.bitcast(dt), self.buf)

    def re(self, pat, **kw):
        return T(self.ap.rearrange(pat, **kw), self.buf)

    def bc(self, shape):
        return T(self.ap.to_broadcast(list(shape)), self.buf)

    def unsq(self, ax):
        return T(self.ap.unsqueeze(ax), self.buf)


def _bufs(ts):
    out = []
    for t in ts:
        if t is None:
            continue
        b = t.buf if isinstance(t, T) else t
        if b is not None and b not in out:
            out.append(b)
    return out


class Rot:
    def __init__(self, items):
        self.items = list(items)
        self.i = 0

    def next(self):
        t = self.items[self.i % len(self.items)]
        self.i += 1
        return t


class K:
    def __init__(self, nc, stack):
        self.nc = nc
        self.stack = stack
        self.streams = {e: [] for e in ENGS}
        self.esem = {}
        self.ecount = {e: 0 for e in ENGS}
        self.known = {e: {} for e in ENGS}
        self.nsem = 0
        for e in ENGS:
            self.esem[e] = self.new_sem("e_" + e)
        self.dma_bufs = []
        self.n_ops = 0
        self.rstack = []

    def new_sem(self, name):
        self.nsem += 1
        return self.stack.enter_context(self.nc.semaphore(f"{name}_{self.nsem}"))

    def _need(self, eng, tokens):
        waits = []
        kn = self.known[eng]
        for sem, val in tokens.items():
            if kn.get(sem, 0) < val:
                kn[sem] = val
                waits.append((sem, val))
        return waits

    def _deps(self, eng, rd, wr, skip_waw=None):
        tokens = {}
        mysem = self.esem[eng]

        def add(d, is_raw):
            for sem, val in d.items():
                if sem is mysem and eng == "tensor":
                    continue
                if (not is_raw) and skip_waw is not None and sem is skip_waw:
                    continue
                if tokens.get(sem, 0) < val:
                    tokens[sem] = val
        for b in rd:
            add(b.w, True)
        for b in wr:
            add(b.w, False)
            add(b.r, False)
        self._note_region(eng, tokens)
        return self._need(eng, tokens)

    def _note_region(self, eng, tokens):
        for rg in self.rstack:
            ext = rg["ext"][eng]
            for sem, val in tokens.items():
                if val <= rg["start"].get(sem, 0):
                    if ext.get(sem, 0) < val:
                        ext[sem] = val

    def _note_inc(self, eng, sem, inc):
        for rg in self.rstack:
            d = rg["incs"][eng]
            d[sem] = d.get(sem, 0) + inc

    def val_load(self, t):
        self.nvals = getattr(self, "nvals", 0) + 1
        key = self.nvals
        for e in ENGS:
            waits = self._deps(e, _bufs([t]), [])
            self.streams[e].append(("load", waits, key, t.ap))
        return key

    def cond_begin(self, key, thr):
        if not self.rstack:
            for e in ENGS:
                if self.ecount[e] >= SEM_LIMIT - 6000:
                    self.esem[e] = self.new_sem("e_" + e)
                    self.ecount[e] = 0
        start = {}
        for e in ENGS:
            start[self.esem[e]] = self.ecount[e]
        for b in self.dma_bufs:
            start[b.dsem] = b.dcount
        rg = {"key": key, "thr": thr, "start": start,
              "known0": {e: dict(self.known[e]) for e in ENGS},
              "ext": {e: {} for e in ENGS}, "incs": {e: {} for e in ENGS}}
        self.rstack.append(rg)
        for e in ENGS:
            self.streams[e].append(("begin", rg))

    def cond_end(self):
        rg = self.rstack.pop()
        for e in ENGS:
            kn = dict(rg["known0"][e])
            ew = []
            for sem, val in rg["ext"][e].items():
                if kn.get(sem, 0) < val:
                    kn[sem] = val
                    ew.append((sem, val))
            rg["ext"][e] = ew
            self.known[e] = kn
            self.streams[e].append(("end", rg))

    def op(self, eng, fn, rd=(), wr=()):
        rd = _bufs(rd)
        wr = _bufs(wr)
        waits = self._deps(eng, rd, wr)
        if self.ecount[eng] >= SEM_LIMIT and not self.rstack:
            self.esem[eng] = self.new_sem("e_" + eng)
            self.ecount[eng] = 0
        self.ecount[eng] += 1
        sem = self.esem[eng]
        val = self.ecount[eng]
        self.streams[eng].append((waits, fn, sem, 1))
        self._note_inc(eng, sem, 1)
        for b in rd:
            if b.r.get(sem, 0) < val:
                b.r[sem] = val
        for b in wr:
            b.w = {sem: val}
            b.r = {}
        self.n_ops += 1

    def dma(self, eng, out, in_, chan=None, **kw):
        rd = _bufs([in_])
        wr = _bufs([out])
        if chan is None:
            chan = out.buf
        if chan.dsem is None:
            chan.dsem = self.new_sem("d_" + chan.name)
            self.dma_bufs.append(chan)
        waits = self._deps(eng, rd, wr, skip_waw=chan.dsem)
        chan.dcount += 16
        sem, val = chan.dsem, chan.dcount
        oap, iap = out.ap, in_.ap
        self.streams[eng].append((waits, lambda e: e.dma_start(out=oap, in_=iap, **kw), sem, 16))
        self._note_inc(eng, sem, 16)
        for b in rd:
            if b.r.get(sem, 0) < val:
                b.r[sem] = val
        for b in wr:
            b.w = {sem: val}
            b.r = {}
        self.n_ops += 1

    def idma(self, out, in_, idx, scatter, chan):
        rd = _bufs([in_, idx])
        wr = _bufs([out])
        if chan.dsem is None:
            chan.dsem = self.new_sem("d_" + chan.name)
            self.dma_bufs.append(chan)
        waits = self._deps("gpsimd", rd, wr, skip_waw=chan.dsem)
        chan.dcount += 16
        sem, val = chan.dsem, chan.dcount
        oap, iap, xap = out.ap, in_.ap, idx.ap
        if scatter:
            fn = lambda e: e.indirect_dma_start(out=oap, out_offset=bass.IndirectOffsetOnAxis(xap, 0), in_=iap, in_offset=None)
        else:
            fn = lambda e: e.indirect_dma_start(out=oap, out_offset=None, in_=iap, in_offset=bass.IndirectOffsetOnAxis(xap, 0))
        self.streams["gpsimd"].append((waits, fn, sem, 16))
        self._note_inc("gpsimd", sem, 16)
        for b in rd:
            if b.r.get(sem, 0) < val:
                b.r[sem] = val
        for b in wr:
            b.w = {sem: val}
            b.r = {}
        self.n_ops += 1

    def _all_tokens(self):
        tokens = {}
        for e in ENGS:
            if self.ecount[e] > 0:
                tokens[self.esem[e]] = self.ecount[e]
        for b in self.dma_bufs:
            tokens[b.dsem] = b.dcount
        return tokens

    def barrier(self):
        tokens = self._all_tokens()
        for e in ENGS:
            waits = self._need(e, dict(tokens))
            if waits:
                self.streams[e].append((waits, None, None, 0))

    def final_wait(self, eng="sync"):
        waits = self._need(eng, self._all_tokens())
        self.streams[eng].append((waits, None, None, 0))

    def emit(self):
        nc = self.nc
        with nc.Block() as block:
            def run(name):
                def body(e):
                    items = self.streams[name]
                    vals = {}

                    def run_items(lst):
                        i = 0
                        while i < len(lst):
                            it = lst[i]
                            if it[0] == "load":
                                for s, v in it[1]:
                                    e.wait_ge(s, v)
                                if "reg" not in vals:
                                    vals["reg"] = e.alloc_register("cnd_" + name)
                                e.load(vals["reg"], it[3])
                                vals["key"] = it[2]
                                i += 1
                            elif it[0] == "begin":
                                rg = it[1]
                                assert vals["key"] == rg["key"]
                                j = i + 1
                                while not (lst[j][0] == "end" and lst[j][1] is rg):
                                    j += 1
                                bodyl = lst[i + 1:j]
                                with e.If_cmp(vals["reg"], rg["thr"], "IS_LE"):
                                    for s, v in rg["ext"][name]:
                                        e.wait_ge(s, v)
                                    for s, tot in rg["incs"][name].items():
                                        if rg["start"].get(s, 0) > 0:
                                            e.wait_ge(s, rg["start"][s])
                                        e.nop().then_inc(s, tot)
                                with e.Else():
                                    run_items(bodyl)
                                i = j + 1
                            else:
                                waits, fn, sem, inc = it
                                for s, v in waits:
                                    e.wait_ge(s, v)
                                if fn is not None:
                                    fn(e).then_inc(sem, inc)
                                i += 1
                    run_items(items)
                return body
            block.tensor(run("tensor"))
            block.vector(run("vector"))
            block.scalar(run("scalar"))
            block.gpsimd(run("gpsimd"))
            block.sync(run("sync"))

    def ps(self, name, shape, dtype):
        t = self.stack.enter_context(self.nc.psum_tensor(name, list(shape), dtype))
        return T(t[:], Buf(name))

    def dram(self, name, shape, dtype, kind):
        t = self.nc.dram_tensor(name, list(shape), dtype, kind=kind)
        return T(t.ap(), Buf(name))

    def mm(self, out, lhsT, rhs, start=True, stop=True, **kw):
        rd = [lhsT, rhs] + ([] if start else [out])
        self.op("tensor", lambda e: e.matmul(out.ap, lhsT.ap, rhs.ap, start=start, stop=stop, **kw),
                rd=rd, wr=[out])

    def tr(self, out, in_, ident):
        self.op("tensor", lambda e: e.transpose(out.ap, in_.ap, ident.ap), rd=[in_, ident], wr=[out])

    def act(self, out, in_, func, bias=None, scale=None, accum=None):
        kw = {}
        rd = [in_]
        if bias is not None:
            if isinstance(bias, T):
                kw["bias"] = bias.ap
                rd.append(bias)
            else:
                kw["bias"] = bias
        if scale is not None:
            if isinstance(scale, T):
                kw["scale"] = scale.ap
                rd.append(scale)
            else:
                kw["scale"] = scale
        wr = [out]
        if accum is not None:
            kw["accum_out"] = accum.ap
            wr.append(accum)
        self.op("scalar", lambda e: e.activation(out.ap, in_.ap, func, **kw), rd=rd, wr=wr)

    def ts(self, eng, out, in0, s1, s2, op0, op1=None, accum=None):
        rd = [in0]
        a1, a2 = s1, s2
        if isinstance(s1, T):
            rd.append(s1)
            a1 = s1.ap
        if isinstance(s2, T):
            rd.append(s2)
            a2 = s2.ap
        kw = {}
        wr = [out]
        if op1 is not None:
            kw["op1"] = op1
        if accum is not None:
            kw["accum_out"] = accum.ap
            wr.append(accum)
        self.op(eng, lambda e: e.tensor_scalar(out.ap, in0.ap, a1, a2, op0, **kw), rd=rd, wr=wr)

    def tt(self, eng, out, in0, in1, op):
        self.op(eng, lambda e: e.tensor_tensor(out.ap, in0.ap, in1.ap, op), rd=[in0, in1], wr=[out])

    def stt(self, out, in0, scalar, in1, op0, op1):
        rd = [in0, in1]
        a = scalar
        if isinstance(scalar, T):
            rd.append(scalar)
            a = scalar.ap
        self.op("vector", lambda e: e.scalar_tensor_tensor(out.ap, in0.ap, a, in1.ap, op0, op1),
                rd=rd, wr=[out])

    def copy(self, eng, out, in_):
        if eng == "scalar":
            self.op(eng, lambda e: e.copy(out.ap, in_.ap), rd=[in_], wr=[out])
        else:
            self.op(eng, lambda e: e.tensor_copy(out.ap, in_.ap), rd=[in_], wr=[out])

    def memset(self, eng, out, val):
        self.op(eng, lambda e: e.memset(out.ap, val), rd=[], wr=[out])

    def recip(self, out, in_):
        self.op("vector", lambda e: e.reciprocal(out.ap, in_.ap), rd=[in_], wr=[out])

    def reduce(self, out, in_, op):
        self.op("vector", lambda e: e.tensor_reduce(out.ap, in_.ap, AX.X, op), rd=[in_], wr=[out])

    def scan_add(self, out, ones, data, initial):
        rd = [ones, data]
        a = initial
        if isinstance(initial, T):
            rd.append(initial)
            a = initial.ap
        self.op("vector", lambda e: e.tensor_tensor_scan(out.ap, ones.ap, data.ap, a, ALU.mult, ALU.add),
                rd=rd, wr=[out])

    def vmax8(self, out, in_):
        self.op("vector", lambda e: e.max(out.ap, in_.ap), rd=[in_], wr=[out])


class Arena:
    def __init__(self, k, nbytes):
        self.k = k
        self.nbytes = nbytes
        t = k.stack.enter_context(k.nc.sbuf_tensor("arena", [128, nbytes], U8))
        self.ap = t[:]
        self.off = 0
        self.top = nbytes
        self.n = 0

    def alloc(self, name, shape, dtype, top=False):
        free = int(np.prod(shape[1:]))
        nb = free * DSZ[dtype]
        if top:
            off = (self.top - nb) // 64 * 64
            assert off >= self.off, f"arena overflow (top) at {name}"
            self.top = off
        else:
            off = (self.off + 63) // 64 * 64
            assert off + nb <= self.top, f"arena overflow at {name}: {off}+{nb} > {self.top}"
            self.off = off + nb
        ap = self.ap[0:shape[0], off:off + nb].bitcast(dtype)
        if len(shape) == 3:
            ap = ap.rearrange("p (a b) -> p a b", a=shape[1])
        elif len(shape) == 4:
            ap = ap.rearrange("p (a b c) -> p a b c", a=shape[1], b=shape[2])
        self.n += 1
        return T(ap, Buf(f"{name}_{self.n}"))

    def mark(self):
        return self.off

    def reset(self, m):
        self.off = m


def build_nc(debug=False):
    nc = bass.Bass("TRN2", target_bir_lowering=False)
    st = ExitStack()
    k = K(nc, st)
    A = Arena(k, 207 * 1024)

    def DI(name, shape, dt=F32):
        return k.dram(name, shape, dt, "ExternalInput")

    xo = DI("xo", [2048, 1024]); xp = DI("xp", [2048, 1024])
    cvec = DI("cvec", [1024]); posr = DI("posr", [4096], I32)
    ada_w = DI("ada_w", [1024, 6144]); ada_b = DI("ada_b", [6144])
    n1g = DI("norm1_g", [1024]); n2g = DI("norm2_g", [1024]); fing = DI("final_g", [1024])
    hgg = DI("hg_norm_g", [128]); lbl = DI("lb_logits", [2, 256])
    w_ka = DI("w_ka", [1024, 512]); w_kas = DI("w_kas", [1024, 512])
    w_ki = DI("w_ki", [1024, 128]); w_kis = DI("w_kis", [1024, 128])
    w_va = DI("w_va", [1024, 512])
    w_qa = DI("w_qa", [1024, 512]); w_qas = DI("w_qas", [1024, 512])
    w_qi = DI("w_qi", [1024, 256]); w_qis = DI("w_qis", [1024, 256])
    w_wi = DI("w_wi", [1024, 4])
    w_qb = DI("w_qb", [1024, 256]); w_fb = DI("w_fb", [1024, 256])
    w_ib = DI("w_ib", [1024, 512]); w_gb = DI("w_gb", [1024, 512])
    w_out = DI("w_out", [1024, 1024])
    rw = DI("router_w", [1024, 32]); rb = DI("router_b", [32])
    w1 = DI("moe_w1", [32, 1024, 2048]); b1 = DI("moe_b1", [32, 2048])
    w2 = DI("moe_w2", [32, 1024, 1024]); b2 = DI("moe_b2", [32, 1024])
    c_identb = DI("c_identb", [128, 128], BF16); c_identf = DI("c_identf", [128, 128])
    c_negI = DI("c_negI", [128, 128], BF16); c_tri2 = DI("c_tri2", [128, 128])
    c_diag = DI("c_diag", [128, 128]); c_cols = DI("c_cols", [128, 4])
    yout = k.dram("y", [2048, 1024], F32, "ExternalOutput")
    x1s = k.dram("x1s", [2048, 1024], F32, "Internal")
    XG = k.dram("XG", [32 * 2048, 1024], BF16, "Internal")
    YG = k.dram("YG", [32 * 2048, 1024], F32, "Internal")
    c_ltb = DI("c_ltb", [128, 128], BF16); c_erow = DI("c_erow", [128, 32])
    dbg = {}
    if debug:
        dbg["mixT"] = k.dram("dbg_mixT", [8, 128, 2048], F32, "ExternalOutput")
        dbg["x1"] = k.dram("dbg_x1", [2048, 1024], F32, "ExternalOutput")
        dbg["G"] = k.dram("dbg_G", [128, 16 * 32], F32, "ExternalOutput")

    banks = [k.ps(f"bank{i}", [128, 512], F32) for i in range(8)]

    def wload(dst, src, rows=8, eng="gpsimd"):
        k.dma(eng, dst, src.re("(kc p) n -> p kc n", p=128))

    identb = A.alloc("identb", [128, 128], BF16); k.dma("sync", identb, c_identb)
    identf = A.alloc("identf", [128, 128], F32); k.dma("sync", identf, c_identf)
    negI = A.alloc("negI", [128, 128], BF16); k.dma("sync", negI, c_negI)
    tri2 = A.alloc("tri2", [128, 128], F32); k.dma("sync", tri2, c_tri2)
    diagm = A.alloc("diagm", [128, 128], F32); k.dma("sync", diagm, c_diag)
    ccols = A.alloc("ccols", [128, 4], F32); k.dma("sync", ccols, c_cols)
    invf = ccols[:, 0:1]; sgnc = ccols[:, 1:2]; negb = ccols[:, 2:3]; sflag = ccols[:, 3:4]
    zerob = A.alloc("zerob", [128, 512], BF16); k.memset("vector", zerob, 0.0)
    onesf = A.alloc("onesf", [128, 512], F32); k.memset("vector", onesf, 1.0)
    g1bc = A.alloc("g1bc", [128, 1024], F32)
    g2bc = A.alloc("g2bc", [128, 1024], F32)
    fgbc = A.alloc("fgbc", [128, 1024], F32)
    s2row = A.alloc("s2row", [128, 1024], F32)
    sh2row = A.alloc("sh2row", [128, 1024], F32)
    k.dma("sync", fgbc, T(fing.ap.partition_broadcast(128), fing.buf))
    hgbc = A.alloc("hgbc", [128, 128], F32)
    k.dma("sync", hgbc, T(hgg.ap.partition_broadcast(128), hgg.buf))
    rbbc = A.alloc("rbbc", [128, 32], F32)
    k.dma("sync", rbbc, T(rb.ap.partition_broadcast(128), rb.buf))
    mcols = A.alloc("mcols", [128, 32], F32)
    s1col = A.alloc("s1col", [128, 8], F32); s2col = A.alloc("s2col", [128, 8], F32)
    lbcol = A.alloc("lbcol", [128, 2], F32); omlb = A.alloc("omlb", [128, 2], F32)
    Gall = A.alloc("Gall", [128, 16, 32], F32)
    smalls = Rot([A.alloc(f"sm{i}", [128, 1], F32) for i in range(12)])
    epsc = A.alloc("epsc", [128, 1], F32); k.memset("vector", epsc, EPS)
    small_mark = A.mark()
    mixA = A.alloc("mixA", [128, 4, 2048], BF16)
    persist_mark = A.mark()

    cT = A.alloc("cT", [128, 8], F32)
    k.dma("sync", cT, cvec.re("(j p) -> p j", p=128), allow_slow_non_contiguous=True)
    siluc = A.alloc("siluc", [128, 8], BF16)
    k.act(siluc, cT, AF.Silu)
    modrow = A.alloc("modrow", [1, 6144], F32)
    adab = A.alloc("adab", [1, 6144], F32)
    k.dma("sync", adab, ada_b.re("(o n) -> o n", o=1))
    ones1 = A.alloc("ones1", [1, 128], F32); k.memset("vector", ones1, 1.0)
    aslots = Rot([A.alloc(f"adaw{i}", [128, 8, 512], BF16) for i in range(2)])
    prot = Rot(banks[0:4])
    adaw_v = ada_w.re("(kc p) n -> p kc n", p=128)
    for nb in range(12):
        sl = aslots.next()
        k.dma("gpsimd", sl, adaw_v[:, :, nb * 512:(nb + 1) * 512])
        pb = prot.next()
        for kc in range(8):
            k.mm(pb[0:1, :], siluc[:, kc:kc + 1], sl[:, kc, :], start=(kc == 0), stop=(kc == 7))
        k.tt("vector", modrow[0:1, nb * 512:(nb + 1) * 512], pb[0:1, :], adab[0:1, nb * 512:(nb + 1) * 512], ALU.add)
    for (dst, off) in ((g1bc, 2048), (g2bc, 5120), (sh2row, 3072), (s2row, 4096)):
        for nb in range(2):
            pb = prot.next()
            k.mm(pb, ones1[0:1, :], modrow[0:1, off + nb * 512: off + (nb + 1) * 512])
            k.copy("vector", dst[:, nb * 512:(nb + 1) * 512], pb)
    pb = prot.next()
    for i, off in enumerate((0, 1024, 3072, 4096)):
        for j in range(8):
            k.mm(pb[:, i * 8 + j: i * 8 + j + 1], modrow[0:1, off + j * 128: off + (j + 1) * 128], ones1[0:1, 0:1])
    k.copy("vector", mcols, pb[:, 0:32])
    n2bc = A.alloc("n2bc", [128, 1024], F32)
    k.dma("sync", n2bc, T(n2g.ap.partition_broadcast(128), n2g.buf))
    k.ts("vector", s2row, s2row, 1.0, None, ALU.add)
    k.tt("vector", s2row, s2row, n2bc, ALU.mult)
    gcol = A.alloc("gcol", [128, 16], F32)
    k.dma("sync", gcol[:, 0:8], n1g.re("(j p) -> p j", p=128), allow_slow_non_contiguous=True)
    k.dma("sync", gcol[:, 8:16], n2g.re("(j p) -> p j", p=128), allow_slow_non_contiguous=True)
    tmp8 = A.alloc("tmp8", [128, 8], F32)
    k.ts("vector", tmp8, mcols[:, 8:16], 1.0, None, ALU.add)
    k.tt("vector", s1col, tmp8, gcol[:, 0:8], ALU.mult)
    tmp8b = A.alloc("tmp8b", [128, 8], F32)
    k.ts("vector", tmp8b, mcols[:, 24:32], 1.0, None, ALU.add)
    k.tt("vector", s2col, tmp8b, gcol[:, 8:16], ALU.mult)
    sh1col = mcols[:, 0:8]; sh2col = mcols[:, 16:24]
    lbl_sb = A.alloc("lbl_sb", [128, 2, 2], F32)
    for l in range(2):
        k.dma("sync", lbl_sb[:, l, :], lbl[l, :].re("(c p) -> p c", p=128), allow_slow_non_contiguous=True)
    dl = A.alloc("dl", [128, 2], F32)
    k.tt("vector", dl, lbl_sb[:, 0, :], lbl_sb[:, 1, :], ALU.subtract)
    k.act(lbcol, dl, AF.Sigmoid)
    k.ts("vector", omlb, lbcol, -1.0, 1.0, ALU.mult, ALU.add)
    k.barrier()
    A.reset(persist_mark)

    def hT_steps(src, tile0, ntiles, scol, shcol, hT, xslots, tmps, prot):
        sqj, xns, tmpf = tmps
        sbc = scol.unsq(2).bc([128, 8, 128])
        shbc = shcol.unsq(2).bc([128, 8, 128])

        def norm(i):
            xt = xslots.next()
            k.dma("sync", xt, src[(tile0 + i) * 128:(tile0 + i + 1) * 128, :])
            ss = smalls.next(); rs = smalls.next(); rstd = smalls.next()
            k.act(sqj, xt, AF.Square, accum=ss)
            k.act(rs, ss, AF.Sqrt, scale=1.0 / 1024, bias=epsc)
            k.recip(rstd, rs)
            xn = xns[i % 2]
            k.act(xn, xt, AF.Identity, scale=rstd)
            return xn

        def evac(i, xn):
            pb = prot.next()
            pv = pb.bitcast(BF16).re("p (j t) -> p j t", j=8)
            for j in range(8):
                k.tr(pv[:, j, :], xn[:, j * 128:(j + 1) * 128], identb)
            k.tt("vector", tmpf, pv, sbc, ALU.mult)
            k.tt("vector", hT[:, :, i * 128:(i + 1) * 128], tmpf, shbc, ALU.add)

        state = {}
        steps = [lambda: state.__setitem__(0, norm(0))]
        for i in range(ntiles):
            def st(i=i):
                if i + 1 < ntiles:
                    state[i + 1] = norm(i + 1)
                evac(i, state[i])
            steps.append(st)
        return steps

    def make_hT(*args):
        for stp_ in hT_steps(*args):
            stp_()

    def rope_tables(slot0, n, cosg, sing, tmps):
        posi, pf, ang, kf = tmps
        ki32 = posi
        k.dma("sync", posi[:, 0:n], T(posr.ap[slot0:slot0 + n].partition_broadcast(128), posr.buf))
        k.copy("vector", pf[:, 0:n], posi[:, 0:n])
        k.ts("vector", ang[:, 0:n], pf[:, 0:n], invf, None, ALU.mult)
        k.ts("vector", kf[:, 0:n], ang[:, 0:n], 1.0 / (2 * PI), None, ALU.mult)
        k.copy("vector", ki32[:, 0:n], kf[:, 0:n])
        k.copy("vector", kf[:, 0:n], ki32[:, 0:n])
        C1 = 6.28125
        C2 = 2 * PI - C1
        k.stt(ang[:, 0:n], kf[:, 0:n], -C1, ang[:, 0:n], ALU.mult, ALU.add)
        k.stt(ang[:, 0:n], kf[:, 0:n], -C2, ang[:, 0:n], ALU.mult, ALU.add)
        k.ts("vector", ang[:, 0:n], ang[:, 0:n], -PI, PI, ALU.max, ALU.min)
        k.act(sing[:, 0:n], ang[:, 0:n], AF.Sin, scale=sgnc)
        k.ts("vector", pf[:, 0:n], ang[:, 0:n], PI / 2, None, ALU.add)
        k.ts("vector", kf[:, 0:n], pf[:, 0:n], PI, -2 * PI, ALU.is_gt, ALU.mult)
        k.tt("vector", pf[:, 0:n], pf[:, 0:n], kf[:, 0:n], ALU.add)
        k.ts("vector", pf[:, 0:n], pf[:, 0:n], -PI, PI, ALU.max, ALU.min)
        k.act(cosg[:, 0:n], pf[:, 0:n], AF.Sin)

    def proj_fm(dst_fn, wsb, wsw, nchunks, hT, n, cosg, sing, prot, rtmps, only=None):
        t1, t2 = rtmps
        for c in (range(nchunks) if only is None else [only]):
            pa = prot.next()
            for kc in range(8):
                k.mm(pa[:, 0:n], wsb[:, kc, c * 128:(c + 1) * 128], hT[:, kc, 0:n], start=(kc == 0), stop=(kc == 7))
            if wsw is None:
                k.copy("scalar", dst_fn(c), pa[:, 0:n])
                continue
            pb2 = prot.next()
            for kc in range(8):
                k.mm(pb2[:, 0:n], wsw[:, kc, c * 128:(c + 1) * 128], hT[:, kc, 0:n], start=(kc == 0), stop=(kc == 7))
            k.tt("vector", t1[:, 0:n], pa[:, 0:n], cosg[:, 0:n], ALU.mult)
            k.tt("vector", t2[:, 0:n], pb2[:, 0:n], sing[:, 0:n], ALU.mult)
            dst_fn(c, t1[:, 0:n], t2[:, 0:n])

    kaT = A.alloc("kaT", [128, 4, 4096], BF16)
    kiT = A.alloc("kiT", [128, 4096], BF16)
    va = A.alloc("va", [128, NT_ALL, 8, 65], BF16)
    k.memset("gpsimd", va[:, :, :, 64:65], 1.0)
    kside_mark = A.mark()
    wka = A.alloc("wka", [128, 8, 512], BF16); wload(wka, w_ka)
    wkas = A.alloc("wkas", [128, 8, 512], BF16); wload(wkas, w_kas)
    wki = A.alloc("wki", [128, 8, 128], BF16); wload(wki, w_ki)
    wkis = A.alloc("wkis", [128, 8, 128], BF16); wload(wkis, w_kis)
    wva = A.alloc("wva", [128, 8, 512], BF16); wload(wva, w_va)
    hT = A.alloc("hT", [128, 8, 512], BF16)
    xslots = Rot([A.alloc(f"xs{i}", [128, 1024], F32) for i in range(2)])
    ntmps = (A.alloc("sqj", [128, 1024], BF16), [A.alloc(f"xn{i_}", [128, 1024], BF16) for i_ in range(2)], A.alloc("tmpf", [128, 8, 128], F32))
    cosg = A.alloc("cosg", [128, 512], F32); sing = A.alloc("sing", [128, 512], F32)
    rtm = (A.alloc("posi", [128, 512], I32), A.alloc("pf", [128, 512], F32), A.alloc("ang", [128, 512], F32),
           A.alloc("kf", [128, 512], F32))
    rt12 = (rtm[1], rtm[3])
    prot = Rot(banks)
    hTs = [hT, A.alloc("hT2", [128, 8, 512], BF16)]

    def p1_units(g, hTg):
        def dst_ka(c, a=None, b=None):
            k.tt("gpsimd", kaT[:, c, g * 512:(g + 1) * 512], a, b, ALU.add)

        def dst_ki(c, a=None, b=None):
            k.tt("gpsimd", kiT[:, g * 512:(g + 1) * 512], a, b, ALU.add)
        units = []
        for c in range(4):
            units.append(lambda c=c: proj_fm(dst_ka, wka, wkas, 4, hTg, 512, cosg, sing, prot, rt12, only=c))
        units.append(lambda: proj_fm(dst_ki, wki, wkis, 1, hTg, 512, cosg, sing, prot, rt12, only=0))
        for i in range(4):
            def va_unit(i=i):
                pa = prot.next()
                for kc in range(8):
                    k.mm(pa, hTg[:, kc, i * 128:(i + 1) * 128], wva[:, kc, :], start=(kc == 0), stop=(kc == 7))
                k.copy("scalar", va[:, g * 4 + i, :, 0:64], pa.re("p (h d) -> p h d", h=8))
            units.append(va_unit)
        return units

    def p1_tiles(g):
        return hT_steps(xp if g < 4 else xo, (g % 4) * 4, 4, s1col, sh1col, hTs[g % 2], xslots, ntmps, prot)

    rope_tables(0, 512, cosg, sing, rtm)
    for stp_ in p1_tiles(0):
        stp_()
    for g in range(8):
        units = p1_units(g, hTs[g % 2])
        tsteps = p1_tiles(g + 1) if g + 1 < 8 else []
        ti = 0
        for ui, u in enumerate(units):
            u()
            if ui % 2 == 1 and ti < len(tsteps):
                tsteps[ti]()
                ti += 1
        while ti < len(tsteps):
            tsteps[ti]()
            ti += 1
        if g + 1 < 8:
            rope_tables((g + 1) * 512, 512, cosg, sing, rtm)
    k.barrier()
    A.reset(kside_mark)

    qaT = A.alloc("qaT", [128, 4, 2048], BF16)
    qiT = A.alloc("qiT", [128, 2, 2048], BF16)
    wis = A.alloc("wis", [128, 16, 4], F32)
    qside_mark = A.mark()
    wqa = A.alloc("wqa", [128, 8, 512], BF16); wload(wqa, w_qa)
    wqas = A.alloc("wqas", [128, 8, 512], BF16); wload(wqas, w_qas)
    wqi = A.alloc("wqi", [128, 8, 256], BF16); wload(wqi, w_qi)
    wqis = A.alloc("wqis", [128, 8, 256], BF16); wload(wqis, w_qis)
    wwi = A.alloc("wwi", [128, 8, 4], BF16); wload(wwi, w_wi)
    hT = A.alloc("hT", [128, 8, 512], BF16)
    xslots = Rot([A.alloc(f"xs{i}", [128, 1024], F32) for i in range(2)])
    ntmps = (A.alloc("sqj", [128, 1024], BF16), [A.alloc(f"xn{i_}", [128, 1024], BF16) for i_ in range(2)], A.alloc("tmpf", [128, 8, 128], F32))
    cosg = A.alloc("cosg", [128, 512], F32); sing = A.alloc("sing", [128, 512], F32)
    rtm = (A.alloc("posi", [128, 512], I32), A.alloc("pf", [128, 512], F32), A.alloc("ang", [128, 512], F32),
           A.alloc("kf", [128, 512], F32))
    rt12 = (rtm[1], rtm[3])
    for g in range(4):
        rope_tables(2048 + g * 512, 512, cosg, sing, rtm)
        make_hT(xo, g * 4, 4, s1col, sh1col, hT, xslots, ntmps, prot)

        def dst_qa(c, a=None, b=None, g=g):
            k.tt("gpsimd", qaT[:, c, g * 512:(g + 1) * 512], a, b, ALU.add)
        proj_fm(dst_qa, wqa, wqas, 4, hT, 512, cosg, sing, prot, rt12)

        def dst_qi(c, a=None, b=None, g=g):
            k.tt("gpsimd", qiT[:, c, g * 512:(g + 1) * 512], a, b, ALU.add)
        proj_fm(dst_qi, wqi, wqis, 2, hT, 512, cosg, sing, prot, rt12)
        for i in range(4):
            pa = prot.next()
            for kc in range(8):
                k.mm(pa[:, 0:4], hT[:, kc, i * 128:(i + 1) * 128], wwi[:, kc, :], start=(kc == 0), stop=(kc == 7))
            k.ts("vector", wis[:, g * 4 + i, :], pa[:, 0:4], 0.5 * 0.125, None, ALU.mult)
    k.barrier()
    A.reset(qside_mark)

    sc = A.alloc("sc", [128, 4096], F32)
    mbars = Rot([A.alloc(f"mbar{i}", [128, 4096], BF16) for i in range(2)])
    rts = Rot([A.alloc(f"rt{i}", [128, 512], F32) for i in range(4)])
    pTs = Rot([A.alloc(f"pT{i}", [128, 512], BF16) for i in range(3)])
    absw = A.alloc("absw", [128, 4], F32); sgnw = A.alloc("sgnw", [128, 4], F32)
    lo = A.alloc("lo", [128, 1], F32); mid = A.alloc("mid", [128, 1], F32)
    cnt = A.alloc("cnt", [128, 1], F32); dlt = A.alloc("dlt", [128, 1], F32)
    rmax = A.alloc("rmax", [128, 1], F32)
    sga = A.alloc("sga", [128, 1], F32); tcomb = A.alloc("tcomb", [128, 1], F32)
    rden = A.alloc("rden", [128, 8], F32)
    oa = A.alloc("oa", [128, 8, 64], BF16)
    zqs = Rot([A.alloc(f"zq{i}", [128, 4, 2, 128], BF16) for i in range(2)])
    zis = Rot([A.alloc(f"zi{i}", [128, 2, 2, 128], BF16) for i in range(2)])
    for z in zqs.items + zis.items:
        k.memset("gpsimd", z, 0.0)
    sc_rot = Rot(banks[0:2])
    s_rot = Rot(banks[2:4])
    po_rot = Rot([(banks[4], banks[5]), (banks[6], banks[7])])

    def stage_a(n):
        nkt = 16 + n + 1
        nk = nkt * 128
        qs = slice(n * 128, (n + 1) * 128)
        zq = zqs.next(); zi = zis.next(); mbar = mbars.next()
        for hp in range(2):
            ph = slice(hp * 64, (hp + 1) * 64)
            k.copy("gpsimd", zq[ph, :, hp, :], qaT[ph, :, qs])
            k.copy("gpsimd", zi[ph, :, hp, :], qiT[ph, :, qs])
        k.ts("vector", sgnw, wis[:, n, :], -1.0, None, ALU.mult)
        k.tt("vector", absw, wis[:, n, :], sgnw, ALU.max)
        k.ts("vector", sgnw, wis[:, n, :], 0.0, 2.0, ALU.is_ge, ALU.mult)
        k.ts("vector", sgnw, sgnw, -1.0, None, ALU.add)
        for kb in range((nkt + 3) // 4):
            ncols = min(512, nk - kb * 512)
            cs = slice(kb * 512, kb * 512 + ncols)
            for h in range(4):
                c, hp = h // 2, h % 2
                pa = sc_rot.next()
                k.mm(pa[:, 0:ncols], zi[:, c, hp, :], kiT[:, cs])
                rt = rts.next()
                k.act(rt[:, 0:ncols], pa[:, 0:ncols], AF.Relu, scale=absw[:, h:h + 1])
                if h == 0:
                    k.ts("vector", sc[:, cs], rt[:, 0:ncols], sgnw[:, 0:1], None, ALU.mult)
                else:
                    k.stt(sc[:, cs], rt[:, 0:ncols], sgnw[:, h:h + 1], sc[:, cs], ALU.mult, ALU.add)
        k.ts("vector", sc[:, 0:2048], sc[:, 0:2048], negb, None, ALU.add)
        ds_ = slice((nkt - 1) * 128, nkt * 128)
        k.tt("vector", sc[:, ds_], sc[:, ds_], diagm, ALU.add)
        k.reduce(rmax, sc[:, 0:nk], ALU.max)
        k.ts("vector", mid, rmax, -RNG + RNG / 2, None, ALU.add)
        for it in range(NBIS):
            wn = RNG / (2 ** (it + 2))
            k.ts("vector", mbar[:, 0:nk], sc[:, 0:nk], mid, None, ALU.is_gt, op1=ALU.add, accum=cnt)
            k.ts("vector", dlt, cnt, 255.5, 2.0 * wn, ALU.is_ge, ALU.mult)
            k.stt(mid, dlt, -wn, mid, ALU.add, ALU.add)
        k.ts("vector", lo, mid, -RNG / (2 ** (NBIS + 1)), None, ALU.add)
        k.ts("vector", mbar[:, 0:nk], sc[:, 0:nk], lo, None, ALU.is_le)
        return (n, nkt, qs, zq, mbar)

    def stage_b(ctx):
        n, nkt, qs, zq, mbar = ctx
        poA, poB = po_rot.next()
        k.mm(poA[:, 0:260], zerob[:, 0:128], zerob[:, 0:260], start=True, stop=False, skip_group_check=True)
        k.mm(poB[:, 0:260], zerob[:, 0:128], zerob[:, 0:260], start=True, stop=False, skip_group_check=True)
        groups = [(j, gq) for j in range(nkt) for gq in range(2)]

        def logits(j, gq):
            ks_ = slice(j * 128, (j + 1) * 128)
            pS = s_rot.next()
            for hh in range(4):
                h = gq * 4 + hh
                c, hp = h // 2, h % 2
                k.mm(pS[:, hh * 128:(hh + 1) * 128], kaT[:, c, ks_], zq[:, c, hp, :], start=True, stop=False)
                k.mm(pS[:, hh * 128:(hh + 1) * 128], mbar[:, ks_], negI, start=False, stop=True)
            pT = pTs.next()
            k.act(pT, pS, AF.Exp, scale=0.125)
            return pT

        def pv_mm(j, gq, pT):
            po = poA if gq == 0 else poB
            for hh in range(4):
                h = gq * 4 + hh
                k.mm(po[:, hh * 65:(hh + 1) * 65], pT[:, hh * 128:(hh + 1) * 128], va[:, j, h, :],
                     start=False, stop=(j == nkt - 1), skip_group_check=True)

        pend = None
        for (j, gq) in groups:
            pT = logits(j, gq)
            if pend is not None:
                pv_mm(*pend)
            pend = (j, gq, pT)
        pv_mm(*pend)
        for gq, po in enumerate((poA, poB)):
            pv = po[:, 0:260].re("p (h d) -> p h d", h=4)
            k.recip(rden[:, gq * 4:(gq + 1) * 4], pv[:, :, 64])
            k.tt("vector", oa[:, gq * 4:(gq + 1) * 4, :], pv[:, :, 0:64],
                 rden[:, gq * 4:(gq + 1) * 4].unsq(2).bc([128, 4, 64]), ALU.mult)
        pa = s_rot.next()
        pv = pa.bitcast(BF16).re("p (j t) -> p j t", j=8)
        oaf = oa.re("p h d -> p (h d)")
        for c in range(4):
            k.tr(pv[:, c, :], oaf[:, c * 128:(c + 1) * 128], identb)
        k.copy("scalar", mixA[:, :, qs], pv[:, 0:4, :])

    ctx = stage_a(0)
    for n in range(NT_OWN):
        nxt = stage_a(n + 1) if n + 1 < NT_OWN else None
        stage_b(ctx)
        ctx = nxt
    k.barrier()
    A.reset(persist_mark)

    mixB = A.alloc("mixB", [128, 4, 2048], BF16)
    hg_mark = A.mark()
    wqb = A.alloc("wqb", [128, 8, 256], BF16); wload(wqb, w_qb)
    wfb = A.alloc("wfb", [128, 8, 256], BF16); wload(wfb, w_fb)
    wib = A.alloc("wib", [128, 8, 512], BF16); wload(wib, w_ib)
    wgb = A.alloc("wgb", [128, 8, 512], BF16); wload(wgb, w_gb)
    hT = A.alloc("hT", [128, 8, 512], BF16)
    xslots = Rot([A.alloc(f"xs{i}", [128, 1024], F32) for i in range(2)])
    ntmps = (A.alloc("sqj", [128, 1024], BF16), [A.alloc(f"xn{i_}", [128, 1024], BF16) for i_ in range(2)], A.alloc("tmpf", [128, 8, 128], F32))
    vtok_s = [A.alloc(f"vtok{i}", [128, 4, 512], BF16) for i in range(2)]
    sgt_s = [A.alloc(f"sgt{i}", [128, 4, 512], BF16) for i in range(2)]
    sgf = A.alloc("sgf", [128, 512], F32)
    S = A.alloc("S", [128, 2, 128], F32); k.memset("vector", S, 0.0)
    Sbf = [A.alloc(f"Sbf{i}", [128, 2, 128], BF16) for i in range(8)]
    Bext = [A.alloc(f"Bext{c2}", [128, 513], F32) for c2 in range(2)]
    for c2 in range(2):
        k.memset("vector", Bext[c2][:, 0:1], 0.0)
    sig = A.alloc("sig", [128, 512], F32); fT = A.alloc("fT", [128, 512], F32)
    logf = A.alloc("logf", [128, 512], F32); omf = A.alloc("omf", [128, 512], F32)
    qbf = A.alloc("qbf", [128, 512], F32)
    D1 = A.alloc("D1", [128, 8, 64], F32); D3 = A.alloc("D3", [128, 8, 64], F32); D4 = A.alloc("D4", [128, 8, 64], F32)
    E1 = A.alloc("E1", [128, 512], F32); E2 = A.alloc("E2", [128, 512], F32)
    E3 = A.alloc("E3", [128, 512], F32); E4 = A.alloc("E4", [128, 512], F32)
    dd = A.alloc("dd", [128, 8], F32)
    gsets = []
    for si in range(2):
        dec_ = [A.alloc(f"dec{si}_{c2}", [128, 8], F32) for c2 in range(2)]
        qtZ_ = [A.alloc(f"qtZ{si}_{c2}", [128, 2, 512], BF16) for c2 in range(2)]
        qeZ_ = [A.alloc(f"qeZ{si}_{c2}", [128, 2, 512], BF16) for c2 in range(2)]
        ktT_ = [A.alloc(f"ktT{si}_{c2}", [128, 512], BF16) for c2 in range(2)]
        kdT_ = [A.alloc(f"kdT{si}_{c2}", [128, 512], BF16) for c2 in range(2)]
        for c2 in range(2):
            k.memset("gpsimd", qtZ_[c2], 0.0)
            k.memset("gpsimd", qeZ_[c2], 0.0)
        gsets.append((vtok_s[si], sgt_s[si], dec_, qtZ_, qeZ_, ktT_, kdT_))
    kdZ = [[A.alloc(f"kdZ{c2}_{i}", [128, 2, 128], BF16) for i in range(2)] for c2 in range(2)]
    for c2 in range(2):
        for i in range(2):
            k.memset("gpsimd", kdZ[c2][i], 0.0)
    attS = [A.alloc(f"attS{i}", [128, 4, 128], BF16) for i in range(2)]
    for i in range(2):
        k.memset("gpsimd", attS[i], 0.0)
    ssq = A.alloc("ssq", [128, 4], F32); rs4 = A.alloc("rs4", [128, 4], F32); rstd4 = A.alloc("rstd4", [128, 4], F32)
    sqj2 = A.alloc("sqj2", [128, 128], BF16)
    ob1 = A.alloc("ob1", [128, 4, 128], F32); ob2 = A.alloc("ob2", [128, 4, 128], F32)
    obb = A.alloc("obb", [128, 512], BF16)
    prot = Rot(banks[0:4])
    kv_rot = Rot(banks[4:6])
    at_rot = Rot([(banks[6], banks[7])])
    tri2b = tri2.unsq(1).bc([128, 2, 128])
    def hg_front(g):
        own = g >= 4
        vtok, sgt, dec, qtZ, qeZ, ktT, kdT = gsets[g % 2]
        src = xo if own else xp
        make_hT(src, (g % 4) * 4, 4, s1col, sh1col, hT, xslots, ntmps, prot)
        for i in range(4):
            pa = prot.next()
            for kc in range(8):
                k.mm(pa, hT[:, kc, i * 128:(i + 1) * 128], wib[:, kc, :], start=(kc == 0), stop=(kc == 7))
            k.copy("scalar", vtok[:, i, :], pa)
            if own:
                pa = prot.next()
                for kc in range(8):
                    k.mm(pa, hT[:, kc, i * 128:(i + 1) * 128], wgb[:, kc, :], start=(kc == 0), stop=(kc == 7))
                k.act(sgf, pa, AF.Sigmoid)
                k.tt("vector", sgt[:, i, :], sgf, pa, ALU.mult)
        for c2 in range(2):
            pa = prot.next()
            for kc in range(8):
                k.mm(pa, wfb[:, kc, c2 * 128:(c2 + 1) * 128], hT[:, kc, :], start=(kc == 0), stop=(kc == 7))
            k.act(sig, pa, AF.Sigmoid)
            k.ts("vector", fT, sig, omlb[:, c2:c2 + 1], lbcol[:, c2:c2 + 1], ALU.mult, ALU.add)
            k.act(logf, fT, AF.Ln)
            k.ts("vector", omf, fT, -1.0, 1.0, ALU.mult, ALU.add)
            Bx = Bext[c2]
            k.scan_add(Bx[:, 1:513], onesf, logf, Bx[:, 0:1])
            Bg = Bx[:, 1:513].re("p (c t) -> p c t", c=8)
            Bprev = Bx[:, 0:512].re("p (c t) -> p c t", c=8)[:, :, 0:1]
            Blast = Bg[:, :, 63:64]
            Bmid = Bg[:, :, 31:32]
            k.tt("vector", D4, Blast.bc([128, 8, 64]), Bg, ALU.subtract)
            k.act(E4, D4.re("p c t -> p (c t)"), AF.Exp)
            k.tt("vector", kdT[c2], omf, E4, ALU.mult)
            k.tt("vector", dd, Blast.re("p c o -> p (c o)"), Bprev.re("p c o -> p (c o)"), ALU.subtract)
            k.act(dec[c2], dd, AF.Exp)
            if own:
                pq = prot.next()
                for kc in range(8):
                    k.mm(pq, wqb[:, kc, c2 * 128:(c2 + 1) * 128], hT[:, kc, :], start=(kc == 0), stop=(kc == 7))
                k.copy("scalar", qbf, pq)
                k.tt("vector", D1, Bg, Bmid.bc([128, 8, 64]), ALU.subtract)
                k.act(E1, D1.re("p c t -> p (c t)"), AF.Exp)
                k.act(E2, D1.re("p c t -> p (c t)"), AF.Exp, scale=-1.0)
                k.tt("vector", D3, Bg, Bprev.bc([128, 8, 64]), ALU.subtract)
                k.act(E3, D3.re("p c t -> p (c t)"), AF.Exp)
                k.tt("vector", ktT[c2], omf, E2, ALU.mult)
                for hp in range(2):
                    ph = slice(hp * 64, (hp + 1) * 64)
                    k.tt("vector", qtZ[c2][ph, hp, :], qbf[ph, :], E1[ph, :], ALU.mult)
                    k.tt("vector", qeZ[c2][ph, hp, :], qbf[ph, :], E3[ph, :], ALU.mult)
            k.copy("vector", Bx[:, 0:1], Bx[:, 512:513])

    def hg_back(g):
        own = g >= 4
        vtok, sgt, dec, qtZ, qeZ, ktT, kdT = gsets[g % 2]
        for i in range(4):
            ts_ = slice(i * 128, (i + 1) * 128)
            kz = []
            for c2 in range(2):
                pa = prot.next()
                pv = pa.bitcast(BF16)
                k.tr(pv[:, 0:128], kdT[c2][:, ts_], identb)
                kzz = kdZ[c2][i % 2]
                for cp in range(2):
                    k.copy("scalar", kzz[cp * 64:(cp + 1) * 64, cp, :], pv[cp * 64:(cp + 1) * 64, 0:128])
                kz.append(kzz)
            if own:
                pA0, pA1 = at_rot.next()
                for cp in range(2):
                    cs_ = slice(i * 128 + cp * 64, i * 128 + (cp + 1) * 64)
                    for h in range(4):
                        c2, hp = h // 2, h % 2
                        pbk = pA0 if hp == 0 else pA1
                        k.mm(pbk[cp * 64:(cp + 1) * 64, c2 * 64:(c2 + 1) * 64], ktT[c2][:, cs_], qtZ[c2][:, hp, cs_])
                aS = attS[i % 2]
                for hp, pbk in enumerate((pA0, pA1)):
                    for c2 in range(2):
                        h = 2 * c2 + hp
                        for cp in range(2):
                            ph = slice(cp * 64, (cp + 1) * 64)
                            k.tt("vector", aS[ph, h, cp * 64:(cp + 1) * 64], pbk[ph, c2 * 64:(c2 + 1) * 64],
                                 tri2[ph, cp * 64:(cp + 1) * 64], ALU.mult)
            for cp in range(2):
                cidx = i * 2 + cp
                if own:
                    k.copy("scalar", Sbf[cidx], S)
                pkv = kv_rot.next()
                for h in range(4):
                    c2, hp = h // 2, h % 2
                    k.mm(pkv[hp * 64:(hp + 1) * 64, c2 * 128:(c2 + 1) * 128], kz[c2][:, cp, hp * 64:(hp + 1) * 64],
                         vtok[:, i, h * 128:(h + 1) * 128])
                for c2 in range(2):
                    k.stt(S[:, c2, :], S[:, c2, :], dec[c2][:, cidx:cidx + 1], pkv[:, c2 * 128:(c2 + 1) * 128], ALU.mult, ALU.add)
                if g == 3 and cidx == 7:
                    k.ts("vector", S.re("p a b -> p (a b)"), S.re("p a b -> p (a b)"), sflag, None, ALU.mult)
            if own:
                po = prot.next()
                for h in range(4):
                    c2, hp = h // 2, h % 2
                    k.mm(po[:, h * 128:(h + 1) * 128], aS[:, h, :], vtok[:, i, h * 128:(h + 1) * 128], start=True, stop=False)
                    for cp in range(2):
                        cidx = i * 2 + cp
                        cs_ = slice(i * 128 + cp * 64, i * 128 + (cp + 1) * 64)
                        k.mm(po[cp * 64:(cp + 1) * 64, h * 128:(h + 1) * 128], qeZ[c2][:, hp, cs_], Sbf[cidx][:, c2, :],
                             start=False, stop=True)
                pov = po.re("p (h e) -> p h e", h=4)
                for h in range(4):
                    k.act(sqj2, po[:, h * 128:(h + 1) * 128], AF.Square, accum=ssq[:, h:h + 1])
                k.act(rs4, ssq, AF.Sqrt, scale=1.0 / 128, bias=epsc)
                k.recip(rstd4, rs4)
                k.tt("vector", ob1, pov, rstd4.unsq(2).bc([128, 4, 128]), ALU.mult)
                k.tt("vector", ob2, ob1, hgbc.unsq(1).bc([128, 4, 128]), ALU.mult)
                k.tt("vector", obb, ob2.re("p h e -> p (h e)"), sgt[:, i, :], ALU.mult)
                pa = prot.next()
                pv = pa.bitcast(BF16).re("p (j t) -> p j t", j=8)
                for c in range(4):
                    k.tr(pv[:, c, :], obb[:, c * 128:(c + 1) * 128], identb)
                tt0 = (g - 4) * 512 + i * 128
                k.copy("scalar", mixB[:, :, tt0:tt0 + 128], pv[:, 0:4, :])

    hg_front(0)
    for g in range(8):
        if g + 1 < 8:
            hg_front(g + 1)
        hg_back(g)
    k.barrier()
    A.reset(hg_mark)

    idxall = A.alloc("idxall", [128, 16, 4], I32, top=True)
    gkall = A.alloc("gkall", [128, 16, 4], F32, top=True)
    nblk_i = A.alloc("nblk_i", [128, 32], I32, top=True)
    p5_mark = A.mark()
    wo = A.alloc("wo", [128, 8, 1024], BF16); wload(wo, w_out)
    rwf = A.alloc("rwf", [128, 8, 32], F32)
    k.dma("sync", rwf, rw.re("(kc p) e -> p kc e", p=128))
    LTb = A.alloc("LTb", [128, 128], BF16); k.dma("sync", LTb, c_ltb)
    onesb = A.alloc("onesb", [128, 128], BF16); k.memset("vector", onesb, 1.0)
    erow = A.alloc("erow", [128, 32], F32); k.dma("sync", erow, c_erow)
    carry = A.alloc("carry", [128, 32], F32); k.memset("vector", carry, 0.0)
    xslots = Rot([A.alloc(f"xs{i}", [128, 1024], F32) for i in range(2)])
    x1r = Rot([A.alloc(f"x1_{i}", [128, 1024], F32) for i in range(2)])
    h2ts = Rot([A.alloc(f"h2tok{i}", [128, 1024], BF16) for i in range(2)])
    ytmp = A.alloc("ytmp", [128, 1024], F32)
    sqj = A.alloc("sqj", [128, 1024], BF16)
    xnf = A.alloc("xnf", [128, 1024], F32)
    h2f = A.alloc("h2f", [128, 8, 128], F32); h2ft = A.alloc("h2ft", [128, 8, 128], F32)
    lg = A.alloc("lg", [128, 32], F32); m8 = A.alloc("m8", [128, 8], F32)
    msk = A.alloc("msk", [128, 32], F32); ex = A.alloc("ex", [128, 32], F32)
    mskb = A.alloc("mskb", [128, 32], BF16)
    rank = A.alloc("rank", [128, 32], F32); keyt = A.alloc("keyt", [128, 32], F32)
    ek8 = A.alloc("ek8", [128, 8], F32); oh = A.alloc("oh", [128, 32], F32); ohr = A.alloc("ohr", [128, 32], F32)
    rk = A.alloc("rk", [128, 1], F32); slf = A.alloc("slf", [128, 1], F32)
    nmax = A.alloc("nmax", [128, 1], F32); zs = A.alloc("zs", [128, 1], F32); rz = A.alloc("rz", [128, 1], F32)
    y_rot = Rot([(banks[0], banks[1]), (banks[2], banks[3])])
    t_rot = Rot([(banks[4], banks[5]), (banks[6], banks[7])])
    s2bc = s2col.unsq(2).bc([128, 8, 128]); sh2bc = sh2col.unsq(2).bc([128, 8, 128])
    def p5_front(i):
        ts_ = slice(i * 128, (i + 1) * 128)
        xt = xslots.next()
        k.dma("sync", xt, xo[ts_, :])
        py = y_rot.next()
        for nb in range(2):
            for kc in range(8):
                mixsrc = mixA if kc < 4 else mixB
                k.mm(py[nb], mixsrc[:, kc % 4, ts_], wo[:, kc, nb * 512:(nb + 1) * 512], start=(kc == 0), stop=(kc == 7))
        x1 = x1r.next()
        for nb in range(2):
            ns_ = slice(nb * 512, (nb + 1) * 512)
            k.tt("vector", ytmp[:, ns_], py[nb], g1bc[:, ns_], ALU.mult)
            k.tt("vector", x1[:, ns_], ytmp[:, ns_], xt[:, ns_], ALU.add)
        k.dma("sync", x1s[ts_, :], x1, chan=x1.buf)
        if debug:
            k.dma("sync", dbg["x1"][ts_, :], x1, chan=x1.buf)
        ss = smalls.next(); rs = smalls.next(); rstd = smalls.next()
        k.act(sqj, x1, AF.Square, accum=ss)
        k.act(rs, ss, AF.Sqrt, scale=1.0 / 1024, bias=epsc)
        return (i, x1, rs, rstd)

    def p5_mid(c1):
        i, x1, rs, rstd = c1
        lg = lgs.next()
        k.recip(rstd, rs)
        k.act(xnf, x1, AF.Identity, scale=rstd)
        h2tok = h2ts.next()
        k.tt("vector", ytmp2, xnf, s2row, ALU.mult)
        k.tt("vector", h2tok, ytmp2, sh2row, ALU.add)
        pt = t_rot.next()
        for j in range(8):
            k.tr(pt[j // 4][:, (j % 4) * 128:(j % 4 + 1) * 128], xnf[:, j * 128:(j + 1) * 128], identf)
        for hb in range(2):
            pv = pt[hb].re("p (j t) -> p j t", j=4)
            k.tt("vector", h2ft[:, hb * 4:(hb + 1) * 4, :], pv, s2bc[:, hb * 4:(hb + 1) * 4, :], ALU.mult)
        k.tt("vector", h2f, h2ft, sh2bc, ALU.add)
        pl = y_rot.next()[0]
        for kc in range(8):
            k.mm(pl[:, 0:32], h2f[:, kc, :], rwf[:, kc, :], start=(kc == 0), stop=(kc == 7))
        k.tt("vector", lg, pl[:, 0:32], rbbc, ALU.add)
        return (i, lg, h2tok)

    def p5_back(ctx):
        i, lg, h2tok = ctx
        k.vmax8(m8, lg)
        k.ts("vector", msk, lg, m8[:, 3:4], None, ALU.is_ge)
        k.ts("vector", nmax, m8[:, 0:1], -1.0, None, ALU.mult)
        k.act(ex, lg, AF.Exp, bias=nmax)
        k.tt("vector", ex, ex, msk, ALU.mult)
        k.reduce(zs, ex, ALU.add)
        k.recip(rz, zs)
        k.ts("vector", Gall[:, i, :], ex, rz, None, ALU.mult)
        k.copy("vector", mskb, msk)
        pr = y_rot.next()[1]
        k.mm(pr[:, 0:32], LTb, mskb)
        k.mm(pr[:, 32:64], onesb, mskb)
        k.tt("vector", rank, pr[:, 0:32], carry, ALU.add)
        k.tt("vector", carry, carry, pr[:, 32:64], ALU.add)
        k.tt("vector", keyt, msk, erow, ALU.mult)
        k.vmax8(ek8, keyt)
        for kk in range(4):
            k.ts("vector", oh, erow, ek8[:, kk:kk + 1], None, ALU.is_equal)
            k.tt("vector", ohr, oh, rank, ALU.mult)
            k.reduce(rk, ohr, ALU.add)
            k.tt("vector", ohr, oh, Gall[:, i, :], ALU.mult)
            k.reduce(gkall[:, i, kk:kk + 1], ohr, ALU.add)
            k.ts("vector", slf, ek8[:, kk:kk + 1], -1.0, 2048.0, ALU.add, ALU.mult)
            k.tt("vector", slf, slf, rk, ALU.add)
            ix = T(idxall.ap[:, i, kk:kk + 1], Buf(f"idx_{i}_{kk}"))
            idxT[(i, kk)] = ix
            k.copy("vector", ix, slf)
            k.idma(XG, h2tok, ix, scatter=True, chan=h2tok.buf)

    idxT = {}
    lgs = Rot([A.alloc(f"lg{i}", [128, 32], F32) for i in range(2)])
    ytmp2 = A.alloc("ytmp2", [128, 1024], F32)
    x1r.items.append(A.alloc("x1_2", [128, 1024], F32))
    c1s = {0: p5_front(0)}
    if NT_OWN > 1:
        c1s[1] = p5_front(1)
    c2s = {0: p5_mid(c1s.pop(0))}
    for i in range(NT_OWN):
        if i + 2 < NT_OWN:
            c1s[i + 2] = p5_front(i + 2)
        if i + 1 < NT_OWN:
            c2s[i + 1] = p5_mid(c1s.pop(i + 1))
        p5_back(c2s.pop(i))
    k.ts("vector", rank, carry, 63.5, 1.0 / 128, ALU.add, ALU.mult)
    k.copy("vector", nblk_i, rank)
    if debug:
        k.dma("sync", dbg["G"], Gall.re("p a b -> p (a b)"), chan=Gall.buf)
    k.barrier()
    A.reset(small_mark)

    b1T = A.alloc("b1T", [128, 16, 32], F32)
    b1_mark = A.mark()
    b1sb = A.alloc("b1sb", [32, 2048], F32); k.dma("sync", b1sb, b1)
    pb = banks[0]
    for c in range(16):
        k.tr(pb[:, c * 32:(c + 1) * 32], b1sb[0:32, c * 128:(c + 1) * 128], identf[0:32, 0:32])
    k.copy("vector", b1T, pb.re("p (c e) -> p c e", c=16))
    k.barrier()
    A.reset(b1_mark)
    p6_mark = A.mark()
    w1b = A.alloc("w1b", [128, 8, 2048], BF16)
    w2b = A.alloc("w2b", [128, 8, 1024], BF16)
    stg1 = [A.alloc(f"stg1_{p}", [128, 2048], F32) for p in range(8)]
    stg2 = [A.alloc(f"stg2_{j}", [128, 2, 1024], F32) for j in range(4)]
    xbs = Rot([A.alloc(f"xb{i}", [128, 1024], BF16) for i in range(3)])
    xets = Rot([A.alloc(f"xet{i}", [128, 8, 128], BF16) for i in range(2)])
    actTs = Rot([A.alloc(f"actT{i}", [128, 8, 128], BF16) for i in range(2)])
    ysbs = Rot([A.alloc(f"ysb{i}", [128, 1024], F32) for i in range(2)])
    gts = Rot([A.alloc(f"gt{i}", [128, 4, 128], F32) for i in range(1)])
    lts = Rot([A.alloc(f"lt{i}", [128, 4, 128], F32) for i in range(1)])
    sts = Rot([A.alloc(f"st{i}", [128, 4, 128], F32) for i in range(1)])
    gss = Rot([A.alloc(f"gs{i}", [128, 4, 128], F32) for i in range(1)])
    ptr_bank = banks[0]
    mm1_banks = banks[1:5]
    y_banks = (banks[5], banks[6])

    def piece_dma(e, p):
        if p < 8:
            k.dma("sync", stg1[p], w1[e][p * 128:(p + 1) * 128, :])
        else:
            j = p - 8
            k.dma("sync", stg2[j], w2[e][j * 256:(j + 1) * 256, :].re("(a p) n -> p a n", p=128))

    def piece_cast(p):
        dst, src = (w1b[:, p, :], stg1[p]) if p < 8 else (w2b[:, 2 * (p - 8):2 * (p - 8) + 2, :], stg2[p - 8])
        k.copy("vector", dst, src)

    for p in range(12):
        piece_dma(0, p)
    for p in range(12):
        piece_cast(p)
        piece_dma(1, p)
    key_next = k.val_load(nblk_i[0:1, 0:1])
    xb_first = xbs.next()
    k.dma("gpsimd", xb_first, XG[0:128, :])
    for e in range(32):
        key = key_next
        xb_next = xb_first
        for blk in range(16):
            k.cond_begin(key, blk)
            r0 = e * 2048 + blk * 128
            xb = xb_next
            if blk + 1 < 16:
                xb_next = xbs.next()
                k.dma("gpsimd", xb_next, XG[r0 + 128:r0 + 256, :])
            pv = ptr_bank.bitcast(BF16).re("p (j t) -> p j t", j=8)
            for j in range(8):
                k.tr(pv[:, j, :], xb[:, j * 128:(j + 1) * 128], identb)
            xet = xets.next()
            k.copy("scalar", xet, pv)
            for gp in range(2):
                for g4 in (gp, 2 + gp):
                    pbk = mm1_banks[g4]
                    for q in range(4):
                        fc = g4 * 4 + q
                        for kc in range(8):
                            k.mm(pbk[:, q * 128:(q + 1) * 128], w1b[:, kc, fc * 128:(fc + 1) * 128], xet[:, kc, :],
                                 start=(kc == 0), stop=(kc == 7))
            actT = actTs.next()
            ysb = ysbs.next()
            for gp in range(2):
                pg = mm1_banks[gp].re("p (q c) -> p q c", q=4)
                pl_ = mm1_banks[2 + gp].re("p (q c) -> p q c", q=4)
                bg = b1T[:, gp * 4:(gp + 1) * 4, e:e + 1].bc([128, 4, 128])
                bl = b1T[:, 8 + gp * 4:8 + (gp + 1) * 4, e:e + 1].bc([128, 4, 128])
                gt_ = gts.next(); lt_ = lts.next(); st_ = sts.next(); gs_ = gss.next()
                k.tt("vector", gt_, pg, bg, ALU.add)
                k.ts("vector", gt_, gt_, 7.0, None, ALU.min)
                k.act(st_, gt_, AF.Sigmoid, scale=1.702)
                k.tt("vector", lt_, pl_, bl, ALU.add)
                k.ts("vector", lt_, lt_, 7.0, -7.0, ALU.min, ALU.max)
                k.tt("vector", gs_, gt_, st_, ALU.mult)
                k.stt(actT[:, gp * 4:(gp + 1) * 4, :], lt_, 1.0, gs_, ALU.add, ALU.mult)
                for nb in range(2):
                    for f in range(gp * 4, gp * 4 + 4):
                        k.mm(y_banks[nb], actT[:, f, :], w2b[:, f, nb * 512:(nb + 1) * 512], start=(f == 0), stop=(f == 7))
            k.copy("scalar", ysb[:, 0:512], y_banks[0])
            k.copy("vector", ysb[:, 512:1024], y_banks[1])
            k.dma("gpsimd", YG[r0:r0 + 128, :], ysb, chan=ysb.buf)
        for blk in range(16):
            k.cond_end()
        if e + 1 < 32:
            key_next = k.val_load(nblk_i[0:1, e + 1:e + 2])
            xb_first = xbs.next()
            k.dma("gpsimd", xb_first, XG[(e + 1) * 2048:(e + 1) * 2048 + 128, :])
            for p in range(12):
                piece_cast(p)
                if e + 2 < 32:
                    piece_dma(e + 2, p)
    k.barrier()
    A.reset(p6_mark)

    GTs = Rot([A.alloc(f"GT{i}", [32, 128], F32) for i in range(2)])
    b2sb = A.alloc("b2sb", [32, 1024], F32); k.dma("sync", b2sb, b2)
    accs = Rot([A.alloc(f"acc{i}", [128, 1024], F32) for i in range(2)])
    ygs = Rot([A.alloc(f"yg{i}", [128, 1024], F32) for i in range(8)])
    xslots = Rot([A.alloc(f"xs{i}", [128, 1024], F32) for i in range(3)])
    fo = Rot([A.alloc(f"fo{i}", [128, 1024], F32) for i in range(2)])
    sqj = A.alloc("sqj", [128, 1024], BF16)
    pg_rot = Rot(banks[0:2])
    y_rot = Rot([(banks[4], banks[5]), (banks[6], banks[7])])

    def p7_init(ti):
        ts_ = slice(ti * 128, (ti + 1) * 128)
        acc = accs.next()
        GT = GTs.next()
        pgt = pg_rot.next()
        k.tr(pgt[0:32, 0:128], Gall[:, ti, :], identf)
        k.copy("vector", GT, pgt[0:32, 0:128])
        py = y_rot.next()
        for nb in range(2):
            k.mm(py[nb], GT[0:32, :], b2sb[0:32, nb * 512:(nb + 1) * 512])
            k.copy("scalar", acc[:, nb * 512:(nb + 1) * 512], py[nb])
        ygl = []
        for kk in range(4):
            yg = ygs.next()
            k.idma(yg, YG, idxT[(ti, kk)], scatter=False, chan=yg.buf)
            ygl.append(yg)
        xt = xslots.next()
        k.dma("sync", xt, x1s[ts_, :])
        return dict(ti=ti, ts=ts_, acc=acc, ygl=ygl, xt=xt)

    def p7_main(cx):
        ti, acc, xt = cx["ti"], cx["acc"], cx["xt"]
        for kk in range(4):
            k.stt(acc, cx["ygl"][kk], gkall[:, ti, kk:kk + 1], acc, ALU.mult, ALU.add)
        k.tt("vector", acc, acc, g2bc, ALU.mult)
        k.tt("vector", xt, xt, acc, ALU.add)
        ss = smalls.next(); rs = smalls.next()
        k.act(sqj, xt, AF.Square, accum=ss)
        k.act(rs, ss, AF.Sqrt, scale=1.0 / 1024, bias=epsc)
        cx["rs"] = rs

    def p7_tail(cx):
        rstd = smalls.next()
        k.recip(rstd, cx["rs"])
        ot = fo.next()
        k.stt(ot, cx["xt"], rstd, fgbc, ALU.mult, ALU.mult)
        k.dma("sync", yout[cx["ts"], :], ot, chan=ot.buf)

    cur = p7_init(0)
    prev = None
    for ti in range(NT_OWN):
        nxt = p7_init(ti + 1) if ti + 1 < NT_OWN else None
        p7_main(cur)
        if prev is not None:
            p7_tail(prev)
        prev = cur
        cur = nxt
    p7_tail(prev)
    k.final_wait("sync")
    k.emit()
    st.close()
    return nc, k


SPL = np.cumsum([0, 512, 512, 512, 256, 64, 4, 256, 256, 512, 512])


def _swap_perm(ncols):
    p = np.arange(ncols)
    for h0 in range(0, ncols, 64):
        p[h0:h0 + 8] = np.arange(h0 + 8, h0 + 16)
        p[h0 + 8:h0 + 16] = np.arange(h0, h0 + 8)
    return p


def _consts(half):
    identf = np.eye(128, dtype=np.float32)
    identb = identf.astype(ml_dtypes.bfloat16)
    negI = (-1024.0 * identf).astype(ml_dtypes.bfloat16)
    s = np.arange(128)[:, None]
    t = np.arange(128)[None, :]
    tri2 = (((s // 64) == (t // 64)) & ((s % 64) <= (t % 64))).astype(np.float32)
    diag = np.where((t // 64) <= (s // 64), 0.0, NEG).astype(np.float32)
    cols = np.zeros((128, 4), np.float32)
    inv_freq = (500000.0 ** (-(np.arange(0, 16, 2, dtype=np.float32) / 16))).astype(np.float32)
    for p in range(128):
        d = p % 64
        if d < 16:
            cols[p, 0] = inv_freq[d % 8]
            cols[p, 1] = -1.0 if d < 8 else 1.0
    cols[:, 2] = 0.0 if half == 1 else NEG
    cols[:, 3] = float(half)
    ltb = (s < t).astype(np.float32).astype(ml_dtypes.bfloat16)
    erow = np.broadcast_to(np.arange(1, 33, dtype=np.float32)[None, :], (128, 32)).copy()
    return {"c_ltb": ltb, "c_erow": erow, "c_identb": identb, "c_identf": identf, "c_negI": negI, "c_tri2": tri2, "c_diag": diag, "c_cols": cols}


_CACHE = {}


def kernel(x, c, positions, ada_w, ada_b, norm1_g, w_in, hg_norm_g, lb_logits, w_out, norm2_g,
           router_w, router_b, moe_w1, moe_b1, moe_w2, moe_b2, final_g, _debug=False):
    f = lambda a: np.ascontiguousarray(np.asarray(a, dtype=np.float32))
    x = f(x); c = f(c); positions = np.ascontiguousarray(np.asarray(positions, dtype=np.int32))
    w_in0 = f(w_in)[0]
    parts = [w_in0[:, SPL[i]:SPL[i + 1]] for i in range(10)]
    qa, ka, va, qi, ki, wi, qb, fb, ib, gb = parts
    ki2 = np.concatenate([ki, ki], axis=1)
    shared = {
        "ada_w": f(ada_w)[0], "ada_b": f(ada_b)[0], "norm1_g": f(norm1_g)[0], "norm2_g": f(norm2_g)[0],
        "final_g": f(final_g), "hg_norm_g": f(hg_norm_g)[0], "lb_logits": f(lb_logits),
        "w_ka": f(ka), "w_kas": f(ka[:, _swap_perm(512)]), "w_ki": f(ki2), "w_kis": f(ki2[:, _swap_perm(128)]),
        "w_va": f(va), "w_qa": f(qa), "w_qas": f(qa[:, _swap_perm(512)]),
        "w_qi": f(qi), "w_qis": f(qi[:, _swap_perm(256)]), "w_wi": f(wi),
        "w_qb": f(qb), "w_fb": f(fb), "w_ib": f(ib), "w_gb": f(gb),
        "w_out": f(w_out)[0], "router_w": f(router_w)[0], "router_b": f(router_b)[0],
        "moe_w1": f(moe_w1)[0], "moe_b1": f(moe_b1)[0], "moe_w2": f(moe_w2)[0], "moe_b2": f(moe_b2)[0],
    }
    in_maps = []
    for j in range(8):
        b, half = j // 2, j % 2
        m = dict(shared)
        m["xo"] = np.ascontiguousarray(x[b, half * 2048:(half + 1) * 2048])
        m["xp"] = np.ascontiguousarray(x[b, 0:2048])
        m["cvec"] = np.ascontiguousarray(c[b])
        m["posr"] = np.ascontiguousarray(np.concatenate([positions[b, 0:2048], positions[b, half * 2048:(half + 1) * 2048]]))
        m.update(_consts(half))
        in_maps.append(m)
    key = bool(_debug)
    if key not in _CACHE:
        _CACHE[key] = build_nc(debug=key)[0]
    nc = _CACHE[key]
    res = run_bass_kernel_spmd(nc, in_maps, core_ids=list(range(8)))
    out = np.empty((4, 4096, 1024), np.float32)
    for j in range(8):
        b, half = j // 2, j % 2
        out[b, half * 2048:(half + 1) * 2048] = res.results[j]["y"]
    if _debug:
        return out, res.results
    return out
```
